# Optimizing a Trainium2 kernel written in Bass

```python
import jax
import jax.numpy as jnp
from jax import lax
import numpy as np

D_MODEL = 1024
BATCH = 8
SEQ = 4096
DEPTH = 4

CTX_LEN = 256
GRID_W = 64

FOURIER_GROUPS = 4
FOURIER_GROUP_DIM = D_MODEL // 8
FOURIER_WIDTH = FOURIER_GROUPS * FOURIER_GROUP_DIM
DN_HEADS = 8
DN_HEAD_DIM = D_MODEL // 8
DN_WIDTH = DN_HEADS * DN_HEAD_DIM
CHUNK = 64
SHORT_CONV = 3
N_DIRS = 2
N_BRANCHES = 2
N_GROUPS = 4
EXPERTS_PER_GROUP = 8
N_EXPERTS = N_GROUPS * EXPERTS_PER_GROUP
TOP_K = 2
EXPERT_DIM = D_MODEL // 2
MOE_BLOCK = 256
N_MOD = 6
EPS = 1e-6

OFF_QKV = 0
OFF_A = OFF_QKV + 3 * DN_WIDTH
OFF_B = OFF_A + N_DIRS * DN_HEADS
OFF_Z = OFF_B + N_DIRS * DN_HEADS
OFF_F = OFF_Z + DN_WIDTH
OFF_G = OFF_F + FOURIER_WIDTH
IN_COLS = OFF_G + N_BRANCHES * D_MODEL

kernel_name = 'hybrid_fourier_gdn_hmoe_diffusion'


def rmsnorm(x, w):
    xf = x.astype(jnp.float32)
    y = xf * lax.rsqrt(jnp.mean(xf * xf, axis=-1, keepdims=True) + EPS)
    return (y * w.astype(jnp.float32)).astype(x.dtype)


def l2norm(x):
    return x * lax.rsqrt(jnp.sum(x * x, axis=-1, keepdims=True) + EPS)


def modulate(h, shift, scale):
    return h * (1 + scale) + shift


def centred_dwconv(x, w):
    taps = w.shape[0]
    half = taps // 2
    t = x.shape[-2]
    xp = jnp.pad(x, [(0, 0)] * (x.ndim - 2) + [(half, half), (0, 0)])
    return sum(w[j] * xp[..., j:j + t, :] for j in range(taps))


def fourier_mix(u):
    b, t, _ = u.shape
    ug = u.astype(jnp.float32).reshape(b, t, FOURIER_GROUPS, FOURIER_GROUP_DIM)
    y = jnp.fft.fftn(ug, axes=(1, 3), norm='ortho').real
    return y.reshape(b, t, FOURIER_WIDTH).astype(u.dtype)


def delta_inputs(p, conv_w, a_log, dt_bias, rows):
    b, t, _ = p.shape
    qkv = p[..., OFF_QKV:OFF_A]
    if rows is None:
        qkv = centred_dwconv(qkv, conv_w)
    else:
        qkv = centred_dwconv(qkv.reshape(b, rows, GRID_W, 3 * DN_WIDTH), conv_w).reshape(b, t, 3 * DN_WIDTH)
    qkv = jax.nn.silu(qkv).astype(jnp.float32)
    q, k, v = (qkv[..., j * DN_WIDTH:(j + 1) * DN_WIDTH].reshape(b, t, DN_HEADS, DN_HEAD_DIM) for j in range(3))
    q = jnp.swapaxes(l2norm(q), 1, 2) * DN_HEAD_DIM ** -0.5
    k = jnp.swapaxes(l2norm(k), 1, 2)
    v = jnp.swapaxes(v, 1, 2)
    a = p[..., OFF_A:OFF_B].astype(jnp.float32).reshape(b, t, N_DIRS, DN_HEADS)
    bg = p[..., OFF_B:OFF_Z].astype(jnp.float32).reshape(b, t, N_DIRS, DN_HEADS)
    g = -jnp.exp(a_log.astype(jnp.float32)) * jax.nn.softplus(a + dt_bias.astype(jnp.float32))
    beta = jax.nn.sigmoid(bg)
    g = jnp.transpose(g, (2, 0, 3, 1))
    beta = jnp.transpose(beta, (2, 0, 3, 1))
    return q, k, v, g, beta


def gated_delta_chunked(q, k, v, g, beta, state0, need_out):
    b, h, t, dk = q.shape
    dv = v.shape[-1]
    n = t // CHUNK
    q, k, v = (u.reshape(b, h, n, CHUNK, u.shape[-1]) for u in (q, k, v))
    beta = beta.reshape(b, h, n, CHUNK)
    gcum = jnp.cumsum(g.reshape(b, h, n, CHUNK), axis=-1)
    tri = jnp.tril(jnp.ones((CHUNK, CHUNK), bool))
    strict = jnp.tril(jnp.ones((CHUNK, CHUNK), bool), -1)
    diff = gcum[..., :, None] - gcum[..., None, :]
    decay = jnp.where(tri, jnp.exp(jnp.where(tri, diff, 0.0)), 0.0)
    kk = jnp.einsum('bhnid,bhnjd->bhnij', k, k)
    lower = jnp.where(strict, beta[..., :, None] * kk * decay, 0.0)
    rhs = jnp.concatenate([v * beta[..., None], k * (beta * jnp.exp(gcum))[..., None]], axis=-1)
    sol = lax.linalg.triangular_solve(lower + jnp.eye(CHUNK, dtype=jnp.float32), rhs,
                                      left_side=True, lower=True, unit_diagonal=True)
    u, w = sol[..., :dv], sol[..., dv:]
    g_last = gcum[..., -1]
    k_tail = k * jnp.exp(g_last[..., None] - gcum)[..., None]
    seq_in = (k_tail, u, w, g_last)
    if need_out:
        q_dec = q * jnp.exp(gcum)[..., None]
        qk = jnp.einsum('bhnid,bhnjd->bhnij', q, k) * decay
        seq_in = seq_in + (q_dec, qk)
    seq_in = tuple(jnp.moveaxis(s, 2, 0) for s in seq_in)

    def step(state, xs):
        k_c, u_c, w_c, gl_c = xs[:4]
        v_new = u_c - jnp.einsum('bhck,bhkv->bhcv', w_c, state)
        new_state = state * jnp.exp(gl_c)[..., None, None] + jnp.einsum('bhck,bhcv->bhkv', k_c, v_new)
        if not need_out:
            return new_state, None
        q_c, qk_c = xs[4:]
        o = jnp.einsum('bhck,bhkv->bhcv', q_c, state) + jnp.einsum('bhcs,bhsv->bhcv', qk_c, v_new)
        return new_state, o

    final, o = lax.scan(step, state0, seq_in)
    if not need_out:
        return None, final
    return jnp.moveaxis(o, 0, 2).reshape(b, h, t, dv), final


def bidir_gated_delta(ctx_in, lat_in, need_ctx_out):
    qc, kc, vc, gc, bc = ctx_in
    ql, kl, vl, gl, bl = lat_in
    zero = jnp.zeros((ql.shape[0], DN_HEADS, DN_HEAD_DIM, DN_HEAD_DIM), jnp.float32)
    rev = lambda s: jnp.flip(s, axis=2)
    oc_f, s_f = gated_delta_chunked(qc, kc, vc, gc[0], bc[0], zero, need_ctx_out)
    ol_f, _ = gated_delta_chunked(ql, kl, vl, gl[0], bl[0], s_f, True)
    oc_b, s_b = gated_delta_chunked(rev(qc), rev(kc), rev(vc), rev(gc[1]), rev(bc[1]), zero, need_ctx_out)
    ol_b, _ = gated_delta_chunked(rev(ql), rev(kl), rev(vl), rev(gl[1]), rev(bl[1]), s_b, True)
    o_ctx = oc_f + rev(oc_b) if need_ctx_out else None
    return o_ctx, ol_f + rev(ol_b)


def branch_merge(p, o, w_fourier, w_delta, w_out, out_norm):
    b, t, _ = p.shape
    z = p[..., OFF_Z:OFF_F].reshape(b, t, DN_HEADS, DN_HEAD_DIM).astype(jnp.float32)
    o = jnp.swapaxes(o, 1, 2)
    od = (rmsnorm(o, out_norm) * jax.nn.silu(z)).astype(p.dtype).reshape(b, t, DN_WIDTH)
    pa = fourier_mix(p[..., OFF_F:OFF_G]) @ w_fourier
    pb = od @ w_delta
    gates = jax.nn.sigmoid(p[..., OFF_G:])
    return (gates[..., :D_MODEL] * pa + gates[..., D_MODEL:] * pb) @ w_out


def hier_moe(h, w_rg, b_rg, w_re, b_re, w_gate, w_up, w_down):
    n, d = h.shape
    lg = (h @ w_rg).astype(jnp.float32) + b_rg.astype(jnp.float32)
    grp = jnp.argmax(lg, axis=-1).astype(jnp.int32)
    p_grp = jnp.take_along_axis(jax.nn.softmax(lg, axis=-1), grp[:, None], axis=-1)
    le = ((h @ w_re).astype(jnp.float32) + b_re.astype(jnp.float32)).reshape(n, N_GROUPS, EXPERTS_PER_GROUP)
    le = jnp.take_along_axis(le, grp[:, None, None], axis=1)[:, 0]
    top_v, top_i = lax.top_k(le, TOP_K)
    comb = (p_grp * jax.nn.softmax(top_v, axis=-1)).astype(h.dtype)
    eid = (grp[:, None] * EXPERTS_PER_GROUP + top_i).reshape(-1).astype(jnp.int32)
    n_assign = n * TOP_K
    tok = jnp.arange(n_assign, dtype=jnp.int32) // TOP_K
    order = jnp.argsort(eid)
    e_sorted = eid[order]
    counts = jnp.bincount(eid, length=N_EXPERTS).astype(jnp.int32)
    padded = (counts + MOE_BLOCK - 1) // MOE_BLOCK * MOE_BLOCK
    pad_end = jnp.cumsum(padded)
    pad_start = pad_end - padded
    start = jnp.cumsum(counts) - counts
    slot = pad_start[e_sorted] + jnp.arange(n_assign, dtype=jnp.int32) - start[e_sorted]
    n_slots = -(-n_assign // MOE_BLOCK) * MOE_BLOCK + N_EXPERTS * MOE_BLOCK
    n_blocks = n_slots // MOE_BLOCK
    slot_tok = jnp.full((n_slots,), n, jnp.int32).at[slot].set(tok[order])
    h_pad = jnp.concatenate([h, jnp.zeros((1, d), h.dtype)], axis=0)
    xs = h_pad[slot_tok].reshape(n_blocks, MOE_BLOCK, d)
    blk_start = jnp.arange(n_blocks, dtype=jnp.int32) * MOE_BLOCK
    blk_e = jnp.minimum(jnp.searchsorted(pad_end, blk_start, side='right'), N_EXPERTS - 1).astype(jnp.int32)

    def expert_block(args):
        xb, e = args
        return (jax.nn.silu(xb @ w_gate[e]) * (xb @ w_up[e])) @ w_down[e]

    ys = lax.map(expert_block, (xs, blk_e)).reshape(n_slots, d)
    slot_of = jnp.zeros((n_assign,), jnp.int32).at[order].set(slot)
    y = ys[slot_of].reshape(n, TOP_K, d)
    return jnp.einsum('nkd,nk->nd', y, comb)


def setup_inputs(seed: int = 0) -> dict:
    key = jax.random.key(seed)
    ks = jax.random.split(key, 26)
    f32 = jnp.float32
    nrm = lambda k, shape, fan_in: jax.random.normal(k, shape, f32) * fan_in ** -0.5
    dt = jnp.exp(jax.random.uniform(ks[9], (DEPTH, N_DIRS, DN_HEADS), f32, np.log(1e-3), np.log(1e-1)))
    return {
        'x': jax.random.normal(ks[0], (BATCH, SEQ, D_MODEL), f32),
        'c': jax.random.normal(ks[1], (BATCH, D_MODEL), f32),
        'ctx': jax.random.normal(ks[2], (BATCH, CTX_LEN, D_MODEL), f32),
        'c_ctx': jax.random.normal(ks[3], (D_MODEL,), f32),
        'w_mod': 0.02 * jax.random.normal(ks[4], (DEPTH, D_MODEL, N_MOD * D_MODEL), f32),
        'b_mod': 0.02 * jax.random.normal(ks[5], (DEPTH, N_MOD * D_MODEL), f32),
        'norm_mix': 1.0 + 0.05 * jax.random.normal(ks[6], (DEPTH, D_MODEL), f32),
        'norm_ffn': 1.0 + 0.05 * jax.random.normal(ks[7], (DEPTH, D_MODEL), f32),
        'w_in': nrm(ks[8], (DEPTH, D_MODEL, IN_COLS), D_MODEL),
        'conv_w': nrm(ks[10], (DEPTH, SHORT_CONV, 3 * DN_WIDTH), SHORT_CONV),
        'a_log': jnp.log(jax.random.uniform(ks[11], (DEPTH, N_DIRS, DN_HEADS), f32, 1.0, 16.0)),
        'dt_bias': dt + jnp.log(-jnp.expm1(-dt)),
        'out_norm': 1.0 + 0.05 * jax.random.normal(ks[12], (DEPTH, DN_HEAD_DIM), f32),
        'w_fourier': nrm(ks[13], (DEPTH, FOURIER_WIDTH, D_MODEL), FOURIER_WIDTH),
        'w_delta': nrm(ks[14], (DEPTH, DN_WIDTH, D_MODEL), DN_WIDTH),
        'w_out': nrm(ks[15], (DEPTH, D_MODEL, D_MODEL), D_MODEL),
        'w_route_group': nrm(ks[16], (DEPTH, D_MODEL, N_GROUPS), D_MODEL),
        'b_route_group': 0.01 * jax.random.normal(ks[17], (DEPTH, N_GROUPS), f32),
        'w_route_expert': nrm(ks[18], (DEPTH, D_MODEL, N_EXPERTS), D_MODEL),
        'b_route_expert': 0.01 * jax.random.normal(ks[19], (DEPTH, N_EXPERTS), f32),
        'w_gate': nrm(ks[20], (DEPTH, N_EXPERTS, D_MODEL, EXPERT_DIM), D_MODEL),
        'w_up': nrm(ks[21], (DEPTH, N_EXPERTS, D_MODEL, EXPERT_DIM), D_MODEL),
        'w_down': nrm(ks[22], (DEPTH, N_EXPERTS, EXPERT_DIM, D_MODEL), EXPERT_DIM),
        'final_norm': 1.0 + 0.05 * jax.random.normal(ks[23], (D_MODEL,), f32),
    }


def reference(x, c, ctx, c_ctx, w_mod, b_mod, norm_mix, norm_ffn, w_in, conv_w, a_log, dt_bias, out_norm,
              w_fourier, w_delta, w_out, w_route_group, b_route_group, w_route_expert, b_route_expert,
              w_gate, w_up, w_down, final_norm):
    bsz, seq, d = x.shape
    rows = seq // GRID_W
    xl, xc = x, ctx
    s_lat = jax.nn.silu(c)
    s_ctx = jax.nn.silu(c_ctx)
    for i in range(DEPTH):
        need_ctx = i < DEPTH - 1
        sh1_l, sc1_l, g1_l, sh2_l, sc2_l, g2_l = jnp.split(s_lat @ w_mod[i] + b_mod[i], N_MOD, axis=-1)
        sh1_c, sc1_c, g1_c, sh2_c, sc2_c, g2_c = jnp.split(s_ctx @ w_mod[i] + b_mod[i], N_MOD, axis=-1)

        hl = modulate(rmsnorm(xl, norm_mix[i]), sh1_l[:, None], sc1_l[:, None])
        hc = modulate(rmsnorm(xc, norm_mix[i]), sh1_c, sc1_c)
        pl = hl @ w_in[i]
        pc = hc @ (w_in[i] if need_ctx else w_in[i][:, :OFF_Z])
        lat_in = delta_inputs(pl, conv_w[i], a_log[i], dt_bias[i], rows)
        ctx_in = delta_inputs(pc, conv_w[i], a_log[i], dt_bias[i], None)
        o_ctx, o_lat = bidir_gated_delta(ctx_in, lat_in, need_ctx)
        xl = xl + g1_l[:, None] * branch_merge(pl, o_lat, w_fourier[i], w_delta[i], w_out[i], out_norm[i])
        if need_ctx:
            xc = xc + g1_c * branch_merge(pc, o_ctx, w_fourier[i], w_delta[i], w_out[i], out_norm[i])

        hl2 = modulate(rmsnorm(xl, norm_ffn[i]), sh2_l[:, None], sc2_l[:, None]).reshape(-1, d)
        moe_args = (w_route_group[i], b_route_group[i], w_route_expert[i], b_route_expert[i],
                    w_gate[i], w_up[i], w_down[i])
        if need_ctx:
            hc2 = modulate(rmsnorm(xc, norm_ffn[i]), sh2_c, sc2_c).reshape(-1, d)
            n_ctx = hc2.shape[0]
            y = hier_moe(jnp.concatenate([hc2, hl2], axis=0), *moe_args)
            xc = xc + g2_c * y[:n_ctx].reshape(xc.shape)
            yl = y[n_ctx:]
        else:
            yl = hier_moe(hl2, *moe_args)
        xl = xl + g2_l[:, None] * yl.reshape(xl.shape)
    return rmsnorm(xl, final_norm)
```

```python
import numpy as np
import os
import ml_dtypes
import concourse.bass as bass
import concourse.mybir as mybir
from concourse.bass_utils import run_bass_kernel_spmd

F32 = mybir.dt.float32
BF16 = mybir.dt.bfloat16
AF = mybir.ActivationFunctionType
ALU = mybir.AluOpType

D = 1024
SEQ = 4096
CTX = 256
T = CTX + SEQ
NB = T // 128
L = 4
NE = 32
OFF_A = 3072
OFF_B = 3088
OFF_Z = 3104
OFF_F = 4128
OFF_G = 4640
IN_COLS = 6688
EPS = 1e-6
TILES = [(0, 256, 0)] + [(256 + 512 * i, 512, 1) for i in range(8)]


class PsT:
    def __init__(self, t, k):
        self.t = t
        self.k = k


class Prog:
    ENG = ('pe', 'act', 'dve', 'pool', 'sp')

    def __init__(self, nc, n_lanes=48):
        self.nc = nc
        self.h = {'pe': nc.tensor, 'act': nc.scalar, 'dve': nc.vector, 'pool': nc.gpsimd, 'sp': nc.sync}
        self.sem = {}
        self.val = {}
        for e in self.ENG:
            self.sem[e] = nc.semaphore("s_" + e).__enter__()
            self.val[e] = 0
        self.n_lanes = n_lanes
        for l in range(n_lanes):
            k = ('L', l)
            self.sem[k] = nc.semaphore("s_l%d" % l).__enter__()
            self.val[k] = 0
        self.lanes = {}
        self.seen = {e: {} for e in self.ENG}
        self.clock = {}
        self.lastw = {}
        self.readers = {}
        self.ninst = 0
        self.nwait = 0
        self.stack = [[]]
        self.uid = 0
        self.psum_pool = []
        self.psum_i = 0
        self.cut = int(os.environ['KCUT']) if os.environ.get('KCUT') else None

    def scope(self):
        self.stack.append([])

    def end_scope(self):
        for cm in reversed(self.stack.pop()):
            cm.__exit__(None, None, None)

    def sb(self, name, shape, dt):
        self.uid += 1
        cm = self.nc.sbuf_tensor("%s_%d" % (name, self.uid), list(shape), dt)
        t = cm.__enter__()
        self.stack[-1].append(cm)
        return t

    def init_psum(self):
        for i in range(7):
            t = self.nc.psum_tensor("psb%d" % i, [128, 512], F32).__enter__()
            self.psum_pool.append(PsT(t, ('ps', i)))
        self.pst = self.nc.psum_tensor("pst", [128, 1024], BF16).__enter__()
        self.pst_i = 0

    def psum(self):
        p = self.psum_pool[self.psum_i % 7]
        self.psum_i += 1
        return p

    def psum_t(self):
        i = self.pst_i % 2
        self.pst_i += 1
        return PsT(self.pst[:, i * 512:(i + 1) * 512], ('pst', i))

    def lane(self, name):
        if name not in self.lanes:
            assert len(self.lanes) < self.n_lanes, "out of lanes"
            self.lanes[name] = len(self.lanes)
        return self.lanes[name]

    def _deps(self, eng, reads, writes):
        deps = {}

        def add(d, kind):
            if d is None:
                return
            sk, v = d
            if sk == eng:
                if eng == 'pe' or kind != 'raw':
                    return
            if v > deps.get(sk, 0):
                deps[sk] = v
        for k in reads:
            add(self.lastw.get(k), 'raw')
        for k in writes:
            add(self.lastw.get(k), 'waw')
            for r in self.readers.get(k, ()):
                add(r, 'war')
        seen = self.seen[eng]
        return [(sk, v) for sk, v in deps.items() if seen.get(sk, 0) < v]

    def _absorb(self, eng, need):
        seen = self.seen[eng]
        for sk, v in need:
            c = self.clock.get((sk, v))
            if c:
                for k2, v2 in c.items():
                    if seen.get(k2, 0) < v2:
                        seen[k2] = v2
            if seen.get(sk, 0) < v:
                seen[sk] = v

    def _emit_waits(self, eng, need):
        h = self.h[eng]
        for sk, v in need[:-1]:
            h.wait_ge(self.sem[sk], v)
            self.nwait += 1
        return need[-1] if need else None

    def _record(self, tag, reads, writes):
        for k in writes:
            self.lastw[k] = tag
            self.readers[k] = []
        for k in reads:
            self.readers.setdefault(k, []).append(tag)

    def op(self, eng, fn, r=(), w=()):
        if self.cut is not None and self.ninst >= self.cut:
            return None
        need = self._deps(eng, r, w)
        last = self._emit_waits(eng, need)
        ins = fn(self.h[eng])
        if last is not None:
            ins._wait_ge(self.sem[last[0]], last[1])
        self._absorb(eng, need)
        self.val[eng] += 1
        v = self.val[eng]
        ins.then_inc(self.sem[eng], 1)
        snap = dict(self.seen[eng])
        snap[eng] = v
        self.clock[(eng, v)] = snap
        self._record((eng, v), r, w)
        self.ninst += 1
        return ins

    def dma(self, q, pairs, lane, r=(), w=()):
        if self.cut is not None and self.ninst >= self.cut:
            return None
        lk = ('L', self.lane(lane))
        need = self._deps(q, r, w)
        pv = self.val[lk]
        if pv > 0 and self.seen[q].get(lk, 0) < pv:
            need = [(sk, v) for sk, v in need if sk != lk] + [(lk, pv)]
        last = self._emit_waits(q, need)
        h = self.h[q]
        first = True
        for (o, i) in pairs:
            ins = h.dma_start(out=o, in_=i)
            if first and last is not None:
                ins._wait_ge(self.sem[last[0]], last[1])
            first = False
            ins.then_inc(self.sem[lk], 16)
            self.val[lk] += 16
            self.ninst += 1
        self._absorb(q, need)
        v = self.val[lk]
        snap = dict(self.seen[q])
        snap[lk] = v
        self.clock[(lk, v)] = snap
        self._record((lk, v), r, w)

    def wait_all(self, eng, keys):
        need = self._deps(eng, keys, ())
        for sk, v in need:
            self.h[eng].wait_ge(self.sem[sk], v)
        self._absorb(eng, need)

    def barrier(self):
        for e in self.ENG:
            need = [(sk, v) for sk, v in self.val.items()
                    if v > 0 and sk != e and self.seen[e].get(sk, 0) < v]
            for sk, v in need:
                self.h[e].wait_ge(self.sem[sk], v)
                self.nwait += 1
            for sk, v in need:
                self.seen[e][sk] = v


def build(nl=L, stop=None, dbg=(), moe_w=True):
    nc = bass.Bass("TRN2", target_bir_lowering=False)
    P = Prog(nc)
    P.init_psum()

    def din(name, shape, dt=F32):
        return nc.dram_tensor(name, list(shape), dt, kind="ExternalInput").ap()

    def dscr(name, shape, dt):
        kind = "ExternalOutput" if name in dbg else "Internal"
        return nc.dram_tensor(name, list(shape), dt, kind=kind).ap()

    xT_in = din("xT", [D, T])
    cvec_d = din("cvec", [128, 8, 2])
    bmod_d = din("bmod", [128, L, 48])
    nmix_d = din("nmix", [128, L, 8])
    nffn_d = din("nffn", [128, L, 8])
    fnorm_d = din("fnorm", [128, 8])
    convw_d = din("convw", [128, L, 3, 24])
    alog_d = din("alog", [128, L, 16])
    dtb_d = din("dtb", [128, L, 16])
    onorm_d = din("onorm", [128, L])
    brt_d = din("brt", [128, L, 36])
    w_mod = din("w_mod", [nl, D, 6 * D])
    w_in = din("w_in", [nl, D, IN_COLS])
    w_fourier = din("w_fourier", [nl, 512, D])
    w_delta = din("w_delta", [nl, D, D])
    w_out = din("w_out", [nl, D, D])
    w_rt = din("w_rt", [nl, D, 36])
    NEd = NE if moe_w else 1
    w_gate = din("w_gate", [nl, NEd, D, 512])
    w_up = din("w_up", [nl, NEd, D, 512])
    w_down = din("w_down", [nl, NEd, 512, D])
    dftc = din("dftc", [SEQ, SEQ], BF16)
    dfts = din("dfts", [SEQ, SEQ], BF16)
    dftc_c = din("dftc_c", [CTX, CTX], BF16)
    dfts_c = din("dfts_c", [CTX, CTX], BF16)
    chc_d = din("chc", [128, 128], BF16)
    chs_d = din("chs", [128, 128], BF16)
    bmask_d = din("bmask", [128, 4, 128], BF16)
    outT = nc.dram_tensor("outT", [D, SEQ], F32, kind="ExternalOutput").ap()

    xs = dscr("xs", [D, T], F32)
    qT = dscr("qT", [D, T], BF16)
    kT = dscr("kT", [D, T], BF16)
    k_tok = dscr("k_tok", [8, 128, T], BF16)
    v_tok = dscr("v_tok", [8, 128, T], BF16)
    ab_tok = dscr("ab_tok", [128, NB, 32], F32)
    zT = dscr("zT", [D, T], BF16)
    u_tok = dscr("u_tok", [128, NB, 512], BF16)
    gT = dscr("gT", [2 * D, T], BF16)
    odT = dscr("odT", [D, T], BF16)
    h2T = dscr("h2T", [D, T], BF16)
    wtT = dscr("wtT", [NE, T], F32)

    ones_bf = P.sb("ones_bf", [128, 128], BF16)
    ones_f = P.sb("ones_f", [128, 128], F32)
    zeros_f = P.sb("zeros_f", [128, 128], F32)
    ident_bf = P.sb("ident_bf", [128, 128], BF16)
    ident_f = P.sb("ident_f", [128, 128], F32)
    triF = P.sb("triF", [128, 128], F32)
    triB = P.sb("triB", [128, 128], F32)
    negF = P.sb("negF", [128, 128], F32)
    negB = P.sb("negB", [128, 128], F32)
    strF = P.sb("strF", [128, 128], F32)
    strB = P.sb("strB", [128, 128], F32)
    epsT = P.sb("epsT", [128, 1], F32)
    CK = 'const'

    def pool(fn, r=(), w=(CK,)):
        P.op('pool', fn, r=r, w=w)
    pool(lambda e: e.memset(ones_f[:], 1.0))
    pool(lambda e: e.memset(zeros_f[:], 0.0))
    pool(lambda e: e.memset(epsT[:], EPS))
    P.op('dve', lambda e: e.tensor_copy(out=ones_bf[:], in_=ones_f[:]), r=[CK], w=[CK])

    def asel(out, in_, pat, cm, op, fill):
        pool(lambda e: e.affine_select(out=out, in_=in_, pattern=pat, compare_op=op, fill=fill,
                                       base=0, channel_multiplier=cm), r=[CK])
    asel(ident_f[:], ones_f[:], [[1, 128]], -1, ALU.is_equal, 0.0)
    asel(triF[:], ones_f[:], [[1, 128]], -1, ALU.is_ge, 0.0)
    asel(triB[:], ones_f[:], [[-1, 128]], 1, ALU.is_ge, 0.0)
    asel(negF[:], zeros_f[:], [[1, 128]], -1, ALU.is_ge, -1.0e5)
    asel(negB[:], zeros_f[:], [[-1, 128]], 1, ALU.is_ge, -1.0e5)
    asel(strF[:], ones_f[:], [[1, 128]], -1, ALU.is_gt, 0.0)
    asel(strB[:], ones_f[:], [[-1, 128]], 1, ALU.is_gt, 0.0)
    P.op('dve', lambda e: e.tensor_copy(out=ident_bf[:], in_=ident_f[:]), r=[CK], w=[CK])

    bmod = P.sb("bmod", [128, L, 48], F32)
    nmix = P.sb("nmix", [128, L, 8], F32)
    nffn = P.sb("nffn", [128, L, 8], F32)
    fnorm = P.sb("fnorm", [128, 8], F32)
    convw = P.sb("convw", [128, L, 3, 24], F32)
    alog = P.sb("alog", [128, L, 16], F32)
    dtb = P.sb("dtb", [128, L, 16], F32)
    onorm = P.sb("onorm", [128, L], F32)
    brt = P.sb("brt", [128, L, 36], F32)
    chc = P.sb("chc", [128, 128], BF16)
    chs = P.sb("chs", [128, 128], BF16)
    cv = P.sb("cv", [128, 8, 2], F32)
    bmask = P.sb("bmask", [128, 4, 128], BF16)
    P.dma('sp', [(bmod[:], bmod_d), (nmix[:], nmix_d), (nffn[:], nffn_d), (fnorm[:], fnorm_d),
                 (convw[:], convw_d), (alog[:], alog_d), (dtb[:], dtb_d), (onorm[:], onorm_d),
                 (brt[:], brt_d), (chc[:], chc_d), (chs[:], chs_d), (cv[:], cvec_d), (bmask[:], bmask_d)],
          lane='par', w=['par'])
    modv = P.sb("modv", [128, L, 48, 2], F32)
    A1 = P.sb("A1", [128, L, 8, 2], F32)
    A2 = P.sb("A2", [128, L, 8, 2], F32)
    nea = P.sb("nea", [128, L, 16], F32)
    P.op('act', lambda e: e.activation(out=nea[:], in_=alog[:], func=AF.Exp), r=['par'], w=['nea'])
    P.op('dve', lambda e: e.tensor_scalar(out=nea[:], in0=nea[:], scalar1=-1.0, scalar2=None, op0=ALU.mult),
         r=['nea'], w=['nea'])

    def phase_mod():
        P.scope()
        sv = P.sb("sv", [128, 8, 2], F32)
        P.op('act', lambda e: e.activation(out=sv[:], in_=cv[:], func=AF.Silu), r=['par'], w=['sv'])
        wts = [P.sb("wmt%d" % i, [128, 8, 512], F32) for i in range(2)]
        it = 0
        for l in range(nl):
            wv = w_mod[l].rearrange("(kc p) n -> p kc n", p=128)
            for cb in range(12):
                b = it % 2
                it += 1
                wt = wts[b]
                P.dma('sp', [(wt[:], wv[:, :, cb * 512:(cb + 1) * 512])], lane='wm%d' % b, w=[('wm', b)])
                ps = P.psum()
                for j in range(4):
                    for kc in range(8):
                        P.op('pe', lambda e: e.matmul(ps.t[:, j * 2:j * 2 + 2], wt[:, kc, j * 128:(j + 1) * 128],
                                                      sv[:, kc, :], start=(kc == 0), stop=(kc == 7)),
                             r=[('wm', b), 'sv'], w=[ps.k])
                P.op('dve', lambda e: e.tensor_tensor(
                    out=modv[:, l, cb * 4:cb * 4 + 4, :],
                    in0=ps.t[:, 0:8].rearrange("p (a b) -> p a b", b=2),
                    in1=bmod[:, l, cb * 4:cb * 4 + 4].unsqueeze(2).to_broadcast([128, 4, 2]),
                    op=ALU.add), r=[ps.k, 'par'], w=['mod'])
            for (A, nw, c0) in ((A1, nmix, 8), (A2, nffn, 32)):
                P.op('dve', lambda e: e.tensor_scalar(out=A[:, l], in0=modv[:, l, c0:c0 + 8, :], scalar1=1.0,
                                                      scalar2=None, op0=ALU.add), r=['mod'], w=['mod'])
                P.op('dve', lambda e: e.tensor_tensor(out=A[:, l], in0=A[:, l],
                                                      in1=nw[:, l, :].unsqueeze(2).to_broadcast([128, 8, 2]),
                                                      op=ALU.mult), r=['mod', 'par'], w=['mod'])
        P.barrier()
        P.end_scope()

    def norm_mod(xt, xkey, W, Aap, Bap, outs, tl):
        ss = P.psum()
        for kc in range(8):
            sq = tl['sq'][kc % 2]
            P.op('act', lambda e: e.activation(out=sq[:, :W], in_=xt[:, kc, :W], func=AF.Square),
                 r=[xkey], w=[('sq', kc % 2)])
            P.op('pe', lambda e: e.matmul(ss.t[:, :W], ones_bf[:], sq[:, :W], start=(kc == 0), stop=(kc == 7)),
                 r=[('sq', kc % 2), CK], w=[ss.k])
        rstd = tl['rstd']
        P.op('act', lambda e: e.activation(out=rstd[:, :W], in_=ss.t[:, :W], func=AF.Sqrt, scale=1.0 / D,
                                           bias=epsT[:, 0:1]), r=[ss.k, CK], w=['rstd'])
        P.op('dve', lambda e: e.reciprocal(out=rstd[:, :W], in_=rstd[:, :W]), r=['rstd'], w=['rstd'])
        for kc in range(8):
            tmp = tl['tmp'][kc % 2]
            P.op('dve', lambda e: e.tensor_tensor(out=tmp[:, :W], in0=xt[:, kc, :W], in1=rstd[:, :W], op=ALU.mult),
                 r=[xkey, 'rstd'], w=[('tmp', kc % 2)])
            for (fn, ok) in outs:
                P.op('act', lambda e: e.activation(out=fn(kc), in_=tmp[:, :W], func=AF.Identity,
                                                   scale=Aap[:, kc:kc + 1], bias=Bap[:, kc:kc + 1]),
                     r=[('tmp', kc % 2), 'mod'], w=[ok])

    def phase_proj(l, xsrc):
        P.scope()
        last = (l == L - 1)
        hT = P.sb("hT", [128, 8, T], BF16)
        xt = P.sb("xt", [128, 8, 512], F32)
        tl = {'sq': [P.sb("sq%d" % i, [128, 512], BF16) for i in range(2)],
              'tmp': [P.sb("tmp%d" % i, [128, 512], F32) for i in range(2)],
              'rstd': P.sb("rstd", [128, 512], F32)}
        xv = xsrc.rearrange("(kc p) t -> p kc t", p=128)
        for ti, (t0, W, lat) in enumerate(TILES):
            ws = 0 if lat else 1
            P.dma('sp', [(xt[:, :, :W], xv[:, :, t0:t0 + W])], lane='xt', w=['xt'])
            norm_mod(xt, 'xt', W, A1[:, l, :, ws], modv[:, l, 0:8, ws],
                     [(lambda kc: hT[:, kc, t0:t0 + W], ('hT', ti))], tl)
        wps = [P.sb("wp%d" % i, [128, 8, 1024], BF16) for i in range(2)]
        st8 = [P.sb("st8_%d" % i, [128, 8, 512], BF16) for i in range(2)]
        tokst = [P.sb("tokst%d" % i, [128, 8, 4, 128], BF16) for i in range(2)]
        ust = [P.sb("ust%d" % i, [128, 4, 512], BF16) for i in range(2)]
        abst = P.sb("abst", [128, 4, 32], F32)
        acc = P.sb("acc", [128, 512], F32)
        actf = P.sb("actf", [128, 512], F32)
        actb = P.sb("actb", [128, 512], BF16)
        sqb = P.sb("sqb", [128, 512], BF16)
        rn = P.sb("rn", [128, 512], F32)
        wv = w_in[l].rearrange("(kc p) n -> p kc n", p=128)
        pieces = [('q', 0, 1024), ('k', 1024, 1024), ('v', 2048, 1024), ('ab', OFF_A, 32), ('z', OFF_Z, 1024),
                  ('F', OFF_F, 512), ('Ga', OFF_G, 1024), ('Gb', OFF_G + 1024, 1024)]
        cnt = {'st8': 0, 'tok': 0, 'u': 0}
        for pi, (pn, c0, ncol) in enumerate(pieces):
            b = pi % 2
            wp = wps[b]
            wk = ('wp', b)
            P.dma('pool', [(wp[:, :, :ncol], wv[:, :, c0:c0 + ncol])], lane='wp%d' % b, w=[wk])
            for ti, (t0, W, lat) in enumerate(TILES):
                hk = ('hT', ti)
                nbk = W // 128
                if (not lat) and last and pn in ('z', 'F', 'Ga', 'Gb'):
                    continue
                if pn in ('q', 'k', 'v'):
                    si = cnt['st8'] % 2
                    st = st8[si]
                    sk = ('st8', si)
                    if pn in ('k', 'v'):
                        tki = cnt['tok'] % 2
                        cnt['tok'] += 1
                        tk = tokst[tki]
                        tkk = ('tok', tki)
                    for cc in range(8):
                        gcc = (c0 // 128) + cc
                        ps = P.psum()
                        for kc in range(8):
                            P.op('pe', lambda e: e.matmul(ps.t[:, :W], wp[:, kc, cc * 128:(cc + 1) * 128],
                                                          hT[:, kc, t0:t0 + W], start=(kc == 0), stop=(kc == 7)),
                                 r=[wk, hk], w=[ps.k])
                        rl = 64 if lat else 256
                        pv = ps.t[:, :W].rearrange("p (r c) -> p r c", c=rl)
                        av = acc[:, :W].rearrange("p (r c) -> p r c", c=rl)
                        P.op('dve', lambda e: e.tensor_scalar(out=acc[:, :W], in0=ps.t[:, :W],
                                                              scalar1=convw[:, l, 1, gcc:gcc + 1], scalar2=None,
                                                              op0=ALU.mult), r=[ps.k, 'par'], w=['acc'])
                        P.op('dve', lambda e: e.scalar_tensor_tensor(
                            out=av[:, :, 1:rl], in0=pv[:, :, 0:rl - 1], scalar=convw[:, l, 0, gcc:gcc + 1],
                            in1=av[:, :, 1:rl], op0=ALU.mult, op1=ALU.add), r=[ps.k, 'par', 'acc'], w=['acc'])
                        P.op('dve', lambda e: e.scalar_tensor_tensor(
                            out=av[:, :, 0:rl - 1], in0=pv[:, :, 1:rl], scalar=convw[:, l, 2, gcc:gcc + 1],
                            in1=av[:, :, 0:rl - 1], op0=ALU.mult, op1=ALU.add), r=[ps.k, 'par', 'acc'], w=['acc'])
                        if pn == 'v':
                            P.op('act', lambda e: e.activation(out=actb[:, :W], in_=acc[:, :W], func=AF.Silu),
                                 r=['acc'], w=['actb'])
                            src, srk = actb, 'actb'
                        else:
                            P.op('act', lambda e: e.activation(out=actf[:, :W], in_=acc[:, :W], func=AF.Silu),
                                 r=['acc'], w=['actf'])
                            P.op('act', lambda e: e.activation(out=sqb[:, :W], in_=actf[:, :W], func=AF.Square),
                                 r=['actf'], w=['sqb'])
                            p2 = P.psum()
                            P.op('pe', lambda e: e.matmul(p2.t[:, :W], ones_bf[:], sqb[:, :W], start=True, stop=True),
                                 r=['sqb', CK], w=[p2.k])
                            P.op('act', lambda e: e.activation(out=rn[:, :W], in_=p2.t[:, :W], func=AF.Sqrt,
                                                               bias=epsT[:, 0:1]), r=[p2.k, CK], w=['rn'])
                            P.op('dve', lambda e: e.reciprocal(out=rn[:, :W], in_=rn[:, :W]), r=['rn'], w=['rn'])
                            sc = (128.0 ** -0.5) if pn == 'q' else 1.0
                            P.op('dve', lambda e: e.scalar_tensor_tensor(
                                out=st[:, cc, :W], in0=actf[:, :W], scalar=sc, in1=rn[:, :W],
                                op0=ALU.mult, op1=ALU.mult), r=['actf', 'rn'], w=[sk])
                            src, srk = st[:, cc], sk
                        if pn in ('k', 'v'):
                            pt = P.psum_t()
                            for tb in range(nbk):
                                P.op('pe', lambda e: e.transpose(pt.t[:, tb * 128:(tb + 1) * 128],
                                                                 src[:, tb * 128:(tb + 1) * 128], ident_bf[:]),
                                     r=[srk, CK], w=[pt.k])
                            P.op('act', lambda e: e.copy(
                                out=tk[:, cc, 0:nbk, :],
                                in_=pt.t[:, :nbk * 128].rearrange("p (a b) -> p a b", b=128)),
                                r=[pt.k], w=[tkk])
                    if pn in ('q', 'k'):
                        dst = qT if pn == 'q' else kT
                        P.dma('sp', [(dst.rearrange("(c p) t -> p c t", p=128)[:, :, t0:t0 + W], st[:, :, :W])],
                              lane='st8_%d' % si, r=[sk], w=[pn + 'T'])
                        cnt['st8'] += 1
                    if pn in ('k', 'v'):
                        dst = k_tok if pn == 'k' else v_tok
                        P.dma('sp', [(dst[:, :, t0:t0 + W].rearrange("c p t -> p c t"),
                                      tk[:, :, :nbk, :].rearrange("p c a b -> p c (a b)"))],
                              lane='tok%d' % tki, r=[tkk], w=[pn + '_tok'])
                elif pn == 'ab':
                    ps = P.psum()
                    for tb in range(nbk):
                        for kc in range(8):
                            P.op('pe', lambda e: e.matmul(ps.t[:, tb * 32:(tb + 1) * 32],
                                                          hT[:, kc, t0 + tb * 128:t0 + (tb + 1) * 128],
                                                          wp[:, kc, 0:32], start=(kc == 0), stop=(kc == 7)),
                                 r=[wk, hk], w=[ps.k])
                    P.op('dve', lambda e: e.tensor_copy(out=abst[:, :nbk, :],
                                                        in_=ps.t[:, :nbk * 32].rearrange("p (a b) -> p a b", b=32)),
                         r=[ps.k], w=['abst'])
                    P.dma('sp', [(ab_tok[:, t0 // 128:t0 // 128 + nbk, :], abst[:, :nbk, :])],
                          lane='abst', r=['abst'], w=['ab_tok'])
                elif pn in ('z', 'Ga', 'Gb'):
                    si = cnt['st8'] % 2
                    cnt['st8'] += 1
                    st = st8[si]
                    sk = ('st8', si)
                    fnc = AF.Silu if pn == 'z' else AF.Sigmoid
                    for cc in range(8):
                        ps = P.psum()
                        for kc in range(8):
                            P.op('pe', lambda e: e.matmul(ps.t[:, :W], wp[:, kc, cc * 128:(cc + 1) * 128],
                                                          hT[:, kc, t0:t0 + W], start=(kc == 0), stop=(kc == 7)),
                                 r=[wk, hk], w=[ps.k])
                        P.op('act', lambda e: e.activation(out=st[:, cc, :W], in_=ps.t[:, :W], func=fnc),
                             r=[ps.k], w=[sk])
                    if pn == 'z':
                        dv = zT.rearrange("(c p) t -> p c t", p=128)
                    else:
                        o8 = 0 if pn == 'Ga' else 8
                        dv = gT.rearrange("(c p) t -> p c t", p=128)[:, o8:o8 + 8, :]
                    P.dma('sp', [(dv[:, :, t0:t0 + W], st[:, :, :W])], lane='st8_%d' % si, r=[sk], w=[pn])
                elif pn == 'F':
                    ui = cnt['u'] % 2
                    cnt['u'] += 1
                    us = ust[ui]
                    uk = ('ust', ui)
                    for tb in range(nbk):
                        ps = P.psum()
                        for kc in range(8):
                            P.op('pe', lambda e: e.matmul(ps.t[:, :512], hT[:, kc, t0 + tb * 128:t0 + (tb + 1) * 128],
                                                          wp[:, kc, 0:512], start=(kc == 0), stop=(kc == 7)),
                                 r=[wk, hk], w=[ps.k])
                        P.op('act', lambda e: e.copy(out=us[:, tb, :], in_=ps.t[:, :512]), r=[ps.k], w=[uk])
                    P.dma('sp', [(u_tok[:, t0 // 128:t0 // 128 + nbk, :], us[:, :nbk, :])],
                          lane='ust%d' % ui, r=[uk], w=['u_tok'])
        P.barrier()
        P.end_scope()

    def phase_delta(l):
        P.scope()
        last = (l == L - 1)
        ab = P.sb("ab", [128, NB, 32], F32)
        P.dma('sp', [(ab[:], ab_tok)], lane='ab', w=['ab'])
        g = P.sb("g", [128, NB, 16], F32)
        beta = P.sb("beta", [128, NB, 16], F32)
        nbeta = P.sb("nbeta", [128, NB, 16], F32)
        gcum = P.sb("gcum", [128, NB, 16], F32)
        ngam = P.sb("ngam", [128, NB, 16], F32)
        tail = P.sb("tail", [128, NB, 16], F32)
        egl = P.sb("egl", [128, NB, 16], F32)
        GK = 'gates'
        P.op('dve', lambda e: e.tensor_tensor(out=g[:], in0=ab[:, :, 0:16],
                                              in1=dtb[:, l, :].unsqueeze(1).to_broadcast([128, NB, 16]), op=ALU.add),
             r=['ab', 'par'], w=[GK])
        P.op('act', lambda e: e.activation(out=g[:], in_=g[:], func=AF.Exp), r=[GK], w=[GK])
        P.op('act', lambda e: e.activation(out=g[:], in_=g[:], func=AF.Ln, bias=ones_f[:, 0:1]), r=[GK, CK], w=[GK])
        P.op('dve', lambda e: e.tensor_tensor(out=g[:], in0=g[:],
                                              in1=nea[:, l, :].unsqueeze(1).to_broadcast([128, NB, 16]), op=ALU.mult),
             r=[GK, 'nea'], w=[GK])
        P.op('act', lambda e: e.activation(out=beta[:], in_=ab[:, :, 16:32], func=AF.Sigmoid), r=['ab'], w=[GK])
        P.op('dve', lambda e: e.tensor_scalar(out=nbeta[:], in0=beta[:], scalar1=-1.0, scalar2=None, op0=ALU.mult),
             r=[GK], w=[GK])
        for d in range(2):
            tri = triF if d == 0 else triB
            ps = P.psum()
            P.op('pe', lambda e: e.matmul(ps.t[:, :NB * 8].rearrange("p (a b) -> p a b", b=8), tri[:],
                                          g[:, :, d * 8:(d + 1) * 8], start=True, stop=True), r=[GK, CK], w=[ps.k])
            P.op('dve', lambda e: e.tensor_copy(out=gcum[:, :, d * 8:(d + 1) * 8],
                                                in_=ps.t[:, :NB * 8].rearrange("p (a b) -> p a b", b=8)),
                 r=[ps.k], w=[GK])
            ps2 = P.psum()
            P.op('pe', lambda e: e.matmul(ps2.t[:, :NB * 8].rearrange("p (a b) -> p a b", b=8), ones_f[:],
                                          g[:, :, d * 8:(d + 1) * 8], start=True, stop=True), r=[GK, CK], w=[ps2.k])
            P.op('act', lambda e: e.activation(out=egl[:, :, d * 8:(d + 1) * 8],
                                               in_=ps2.t[:, :NB * 8].rearrange("p (a b) -> p a b", b=8), func=AF.Exp),
                 r=[ps2.k], w=[GK])
            P.op('dve', lambda e: e.tensor_tensor(out=tail[:, :, d * 8:(d + 1) * 8],
                                                  in0=ps2.t[:, :NB * 8].rearrange("p (a b) -> p a b", b=8),
                                                  in1=gcum[:, :, d * 8:(d + 1) * 8], op=ALU.subtract),
                 r=[ps2.k, GK], w=[GK])
        P.op('act', lambda e: e.activation(out=tail[:], in_=tail[:], func=AF.Exp), r=[GK], w=[GK])
        P.op('act', lambda e: e.activation(out=ngam[:], in_=gcum[:], func=AF.Exp), r=[GK], w=[GK])
        P.op('dve', lambda e: e.tensor_scalar(out=ngam[:], in0=ngam[:], scalar1=-1.0, scalar2=None, op0=ALU.mult),
             r=[GK], w=[GK])

        DSTOP = os.environ.get('KDSTOP', '')
        if DSTOP == 'gates':
            P.barrier()
            P.end_scope()
            return
        qTh = P.sb("qTh", [128, T], BF16)
        kTh = P.sb("kTh", [128, T], BF16)
        zTh = P.sb("zTh", [128, T], BF16)
        ktk = P.sb("ktk", [128, NB, 128], BF16)
        vtk = P.sb("vtk", [128, NB, 128], BF16)
        oacc = P.sb("oacc", [128, T], F32)
        Tt = P.sb("Tt", [128, NB, 2, 128], BF16)
        AT = P.sb("AT", [128, NB, 2, 128], BF16)
        qdec = P.sb("qdec", [128, NB, 2, 128], BF16)
        ktail = P.sb("ktail", [128, NB, 2, 128], BF16)
        Ef = [P.sb("Ef%d" % i, [128, 2, 128], F32) for i in range(2)]
        Es = [P.sb("Es%d" % i, [128, 2, 128], F32) for i in range(2)]
        Ge = [P.sb("Ge%d" % i, [128, 2, 128], F32) for i in range(2)]
        t1 = [P.sb("t1_%d" % i, [128, 2, 128], F32) for i in range(2)]
        ABt = [P.sb("ABt%d" % i, [128, 2, 2, 128], BF16) for i in range(2)]
        Ttw = [P.sb("Ttw%d" % i, [128, 2, 128], BF16) for i in range(2)]
        Xf = P.sb("Xf", [128, 2, 128], BF16)
        XfT = P.sb("XfT", [128, 2, 128], BF16)
        UT = P.sb("UT", [128, 3, 2, 128], BF16)
        Hh = P.sb("Hh", [128, 2, 128], BF16)
        Wn = P.sb("Wn", [128, 2, 128], BF16)
        Sf = [P.sb("Sf%d" % i, [128, 128], F32) for i in range(2)]
        Sb = [P.sb("Sb%d" % i, [128, 128], BF16) for i in range(2)]
        Rt = [P.sb("Rt%d" % i, [128, 128], BF16) for i in range(2)]
        vn = [P.sb("vn%d" % i, [128, 128], BF16) for i in range(2)]
        sqo = [P.sb("sqo%d" % i, [128, 512], BF16) for i in range(2)]
        rno = P.sb("rno", [128, 512], F32)
        odf = P.sb("odf", [128, 512], F32)
        odst = [P.sb("odst%d" % i, [128, 512], BF16) for i in range(2)]
        tris = (triF, triB)
        negs = (negF, negB)
        strs = (strF, strB)
        uidx = 0
        for h in range(8):
            hs = slice(h * 128, (h + 1) * 128)
            HK = ('hd', 0)
            P.dma('sp', [(qTh[:], qT[hs, :]), (kTh[:], kT[hs, :]),
                         (ktk[:].rearrange("p b f -> p (b f)"), k_tok[h]),
                         (vtk[:].rearrange("p b f -> p (b f)"), v_tok[h]),
                         (zTh[:], zT[hs, :])], lane='hd', w=[HK])
            P.op('pool', lambda e: e.memset(oacc[:], 0.0), w=['oacc'])
            for n in range(NB if DSTOP != 'pre1' else 1):
                bs = slice(n * 128, (n + 1) * 128)
                u2 = uidx % 2
                uidx += 1
                pk = P.psum()
                P.op('pe', lambda e: e.matmul(pk.t[:, 0:128], kTh[:, bs], kTh[:, bs], start=True, stop=True),
                     r=[HK], w=[pk.k])
                P.op('pe', lambda e: e.matmul(pk.t[:, 128:256], kTh[:, bs], qTh[:, bs], start=True, stop=True),
                     r=[HK], w=[pk.k])
                for d in range(2):
                    col = d * 8 + h
                    P.op('pe', lambda e: e.matmul(pk.t[:, 256 + d * 128:384 + d * 128],
                                                  g[:, n, col:col + 1].to_broadcast([128, 128]), tris[d][:],
                                                  start=True, stop=True), r=[GK, CK], w=[pk.k])
                for d in range(2):
                    col = d * 8 + h
                    P.op('dve', lambda e: e.scalar_tensor_tensor(
                        out=t1[u2][:, d, :], in0=pk.t[:, 256 + d * 128:384 + d * 128], scalar=gcum[:, n, col:col + 1],
                        in1=negs[d][:], op0=ALU.subtract, op1=ALU.add), r=[pk.k, GK, CK], w=[('t1', u2)])
                P.op('act', lambda e: e.activation(out=Ef[u2][:], in_=t1[u2][:], func=AF.Exp),
                     r=[('t1', u2)], w=[('Ef', u2)])
                P.op('act', lambda e: e.activation(out=Ge[u2][:],
                                                   in_=pk.t[:, 256:512].rearrange("p (a b) -> p a b", b=128),
                                                   func=AF.Exp), r=[pk.k], w=[('Ge', u2)])
                for d in range(2):
                    P.op('dve', lambda e: e.tensor_tensor(out=Es[u2][:, d, :], in0=Ef[u2][:, d, :], in1=strs[d][:],
                                                           op=ALU.mult), r=[('Ef', u2), CK], w=[('Es', u2)])
                for d in range(2):
                    col = d * 8 + h
                    P.op('dve', lambda e: e.tensor_tensor(out=AT[:, n, d, :], in0=pk.t[:, 128:256], in1=Ef[u2][:, d, :],
                                                          op=ALU.mult), r=[pk.k, ('Ef', u2)], w=[('pre', n)])
                    P.op('dve', lambda e: e.scalar_tensor_tensor(
                        out=Xf[:, d, :], in0=pk.t[:, 0:128], scalar=nbeta[:, n, col:col + 1],
                        in1=Es[u2][:, d, :], op0=ALU.mult, op1=ALU.mult), r=[pk.k, GK, ('Es', u2)], w=['Xf'])
                    P.op('dve', lambda e: e.tensor_tensor(out=qdec[:, n, d, :], in0=qTh[:, bs], in1=Ge[u2][:, d, :],
                                                          op=ALU.mult), r=[HK, ('Ge', u2)], w=[('pre', n)])
                    P.op('act', lambda e: e.activation(out=ktail[:, n, d, :], in_=ktk[:, n, :], func=AF.Copy,
                                                       scale=tail[:, n, col:col + 1]), r=[HK, GK], w=[('pre', n)])
                P.op('dve', lambda e: e.tensor_tensor(out=ABt[0][:, :, 0, :], in0=Xf[:],
                                                      in1=bmask[:, 0:1, :].to_broadcast([128, 2, 128]), op=ALU.mult),
                     r=['Xf', 'par'], w=[('AB', 0)])
                pt = P.psum_t()
                for d in range(2):
                    P.op('pe', lambda e: e.transpose(pt.t[:, d * 128:(d + 1) * 128], ABt[0][:, d, 0, :], ident_bf[:]),
                         r=[('AB', 0), CK], w=[pt.k])
                    P.op('pe', lambda e: e.transpose(pt.t[:, 256 + d * 128:256 + (d + 1) * 128], Xf[:, d, :],
                                                     ident_bf[:]), r=['Xf', CK], w=[pt.k])
                P.op('act', lambda e: e.copy(out=ABt[0][:, :, 1, :],
                                             in_=pt.t[:, 0:256].rearrange("p (a b) -> p a b", b=128)),
                     r=[pt.k], w=[('AB', 0)])
                P.op('act', lambda e: e.copy(out=XfT[:], in_=pt.t[:, 256:512].rearrange("p (a b) -> p a b", b=128)),
                     r=[pt.k], w=['XfT'])
                P.op('dve', lambda e: e.tensor_tensor(out=Ttw[0][:], in0=ABt[0][:, :, 0, :],
                                                      in1=ident_bf[:].unsqueeze(1).to_broadcast([128, 2, 128]),
                                                      op=ALU.add), r=[('AB', 0), CK], w=[('Ttw', 0)])
                for b3 in range(3):
                    P.op('dve', lambda e: e.tensor_tensor(out=UT[:, b3], in0=XfT[:],
                                                          in1=bmask[:, 1 + b3:2 + b3, :].to_broadcast([128, 2, 128]),
                                                          op=ALU.mult), r=['XfT', 'par'], w=['UT'])
                gi_ = 0
                for lev in range(1, 4):
                    cur = lev % 2
                    prv = 1 - cur
                    pab = P.psum()
                    for d in range(2):
                        if lev < 3:
                            P.op('pe', lambda e: e.matmul(pab.t[:, d * 256:d * 256 + 128], ABt[prv][:, d, 1, :],
                                                          ABt[prv][:, d, 0, :], start=True, stop=True),
                                 r=[('AB', prv)], w=[pab.k])
                        P.op('pe', lambda e: e.matmul(pab.t[:, d * 256 + 128:d * 256 + 256], ABt[prv][:, d, 0, :],
                                                      ABt[prv][:, d, 1, :], start=True, stop=True),
                             r=[('AB', prv)], w=[pab.k])
                    if lev < 3:
                        P.op('act', lambda e: e.copy(out=ABt[cur][:],
                                                     in_=pab.t[:, :].rearrange("p (a b c) -> p a b c", a=2, b=2)),
                             r=[pab.k], w=[('AB', cur)])
                    else:
                        P.op('act', lambda e: e.copy(
                            out=ABt[cur][:, :, 1, :],
                            in_=pab.t[:, :].rearrange("p (a b c) -> p a b c", a=2, b=2)[:, :, 1, :]),
                            r=[pab.k], w=[('AB', cur)])
                    ptt = P.psum()
                    for d in range(2):
                        P.op('pe', lambda e: e.matmul(ptt.t[:, d * 128:(d + 1) * 128], ABt[cur][:, d, 1, :],
                                                      Ttw[gi_][:, d, :], start=True, stop=True),
                             r=[('AB', cur), ('Ttw', gi_)], w=[ptt.k])
                    P.op('dve', lambda e: e.tensor_tensor(
                        out=Ttw[1 - gi_][:], in0=ptt.t[:, 0:256].rearrange("p (a b) -> p a b", b=128),
                        in1=Ttw[gi_][:], op=ALU.add), r=[ptt.k, ('Ttw', gi_)], w=[('Ttw', 1 - gi_)])
                    gi_ = 1 - gi_
                for b3 in range(3):
                    G = Ttw[gi_]
                    pt2 = P.psum_t()
                    for d in range(2):
                        P.op('pe', lambda e: e.transpose(pt2.t[:, d * 128:(d + 1) * 128], G[:, d, :], ident_bf[:]),
                             r=[('Ttw', gi_), CK], w=[pt2.k])
                    P.op('act', lambda e: e.copy(out=Hh[:], in_=pt2.t[:, 0:256].rearrange("p (a b) -> p a b", b=128)),
                         r=[pt2.k], w=['Hh'])
                    pw2 = P.psum()
                    for d in range(2):
                        P.op('pe', lambda e: e.matmul(pw2.t[:, d * 128:(d + 1) * 128], UT[:, b3, d, :], G[:, d, :],
                                                      start=True, stop=True), r=['UT', ('Ttw', gi_)], w=[pw2.k])
                    P.op('act', lambda e: e.copy(out=Wn[:], in_=pw2.t[:, 0:256].rearrange("p (a b) -> p a b", b=128)),
                         r=[pw2.k], w=['Wn'])
                    pg2 = P.psum()
                    for d in range(2):
                        P.op('pe', lambda e: e.matmul(pg2.t[:, d * 128:(d + 1) * 128], Hh[:, d, :], Wn[:, d, :],
                                                      start=True, stop=True), r=['Hh', 'Wn'], w=[pg2.k])
                    if b3 < 2:
                        P.op('dve', lambda e: e.tensor_tensor(
                            out=Ttw[1 - gi_][:], in0=pg2.t[:, 0:256].rearrange("p (a b) -> p a b", b=128),
                            in1=G[:], op=ALU.add), r=[pg2.k, ('Ttw', gi_)], w=[('Ttw', 1 - gi_)])
                    else:
                        P.op('dve', lambda e: e.tensor_tensor(
                            out=Tt[:, n, :, :], in0=pg2.t[:, 0:256].rearrange("p (a b) -> p a b", b=128),
                            in1=G[:], op=ALU.add), r=[pg2.k, ('Ttw', gi_)], w=[('pre', n)])
                    gi_ = 1 - gi_
            if DSTOP.startswith('pre'):
                P.barrier()
                P.end_scope()
                return
            orders = [list(range(NB)), [1, 0] + list(range(NB - 1, 1, -1))]
            for d in range(2):
                P.op('pool', lambda e: e.memset(Sf[d][:], 0.0), w=[('S', d)])
                P.op('pool', lambda e: e.memset(Sb[d][:], 0.0), w=[('Sb', d)])
            for step in range(NB):
                for d in range(2):
                    n = orders[d][step]
                    col = d * 8 + h
                    bs = slice(n * 128, (n + 1) * 128)
                    need_o = not (last and n < 2)
                    pc = P.psum()
                    P.op('pe', lambda e: e.matmul(pc.t[:, 0:128], kTh[:, bs], Sb[d][:], start=True, stop=True),
                         r=[HK, ('Sb', d)], w=[pc.k])
                    P.op('dve', lambda e: e.scalar_tensor_tensor(
                        out=Rt[d][:], in0=pc.t[:, 0:128], scalar=ngam[:, n, col:col + 1], in1=vtk[:, n, :],
                        op0=ALU.mult, op1=ALU.add), r=[pc.k, GK, HK], w=[('R', d)])
                    P.op('pe', lambda e: e.matmul(pc.t[:, 128:256], Tt[:, n, d, :], Rt[d][:], start=True, stop=True),
                         r=[('pre', n), ('R', d)], w=[pc.k])
                    P.op('act', lambda e: e.activation(out=vn[d][:], in_=pc.t[:, 128:256], func=AF.Copy,
                                                       scale=beta[:, n, col:col + 1]), r=[pc.k, GK], w=[('vn', d)])
                    if need_o:
                        P.op('pe', lambda e: e.matmul(pc.t[:, 256:384], Sb[d][:], qdec[:, n, d, :],
                                                      start=True, stop=False), r=[('Sb', d), ('pre', n)], w=[pc.k])
                        P.op('pe', lambda e: e.matmul(pc.t[:, 256:384], vn[d][:], AT[:, n, d, :],
                                                      start=False, stop=True), r=[('vn', d), ('pre', n)], w=[pc.k])
                        P.op('dve', lambda e: e.tensor_tensor(out=oacc[:, bs], in0=pc.t[:, 256:384], in1=oacc[:, bs],
                                                              op=ALU.add), r=[pc.k, 'oacc'], w=['oacc'])
                    P.op('pe', lambda e: e.matmul(pc.t[:, 384:512], ktail[:, n, d, :], vn[d][:], start=True, stop=True),
                         r=[('pre', n), ('vn', d)], w=[pc.k])
                    P.op('dve', lambda e: e.scalar_tensor_tensor(
                        out=Sf[d][:], in0=Sf[d][:], scalar=egl[:, n, col:col + 1], in1=pc.t[:, 384:512],
                        op0=ALU.mult, op1=ALU.add), r=[pc.k, GK, ('S', d)], w=[('S', d)])
                    P.op('act', lambda e: e.copy(out=Sb[d][:], in_=Sf[d][:]), r=[('S', d)], w=[('Sb', d)])
            if DSTOP == 'chain':
                P.barrier()
                P.end_scope()
                return
            for ti, (t0, W, lat) in enumerate(TILES):
                if last and not lat:
                    continue
                i2 = ti % 2
                P.op('act', lambda e: e.activation(out=sqo[i2][:, :W], in_=oacc[:, t0:t0 + W], func=AF.Square),
                     r=['oacc'], w=[('sqo', i2)])
                ps = P.psum()
                P.op('pe', lambda e: e.matmul(ps.t[:, :W], ones_bf[:], sqo[i2][:, :W], start=True, stop=True),
                     r=[('sqo', i2), CK], w=[ps.k])
                P.op('act', lambda e: e.activation(out=rno[:, :W], in_=ps.t[:, :W], func=AF.Sqrt, scale=1.0 / 128,
                                                   bias=epsT[:, 0:1]), r=[ps.k, CK], w=['rno'])
                P.op('dve', lambda e: e.reciprocal(out=rno[:, :W], in_=rno[:, :W]), r=['rno'], w=['rno'])
                P.op('dve', lambda e: e.scalar_tensor_tensor(
                    out=odf[:, :W], in0=oacc[:, t0:t0 + W], scalar=onorm[:, l:l + 1], in1=rno[:, :W],
                    op0=ALU.mult, op1=ALU.mult), r=['oacc', 'rno', 'par'], w=['odf'])
                P.op('dve', lambda e: e.tensor_tensor(out=odst[i2][:, :W], in0=odf[:, :W], in1=zTh[:, t0:t0 + W],
                                                      op=ALU.mult), r=['odf', HK], w=[('odst', i2)])
                P.dma('sp', [(odT[hs, t0:t0 + W], odst[i2][:, :W])], lane='odst%d' % i2, r=[('odst', i2)], w=['odT'])
        P.barrier()
        P.end_scope()

    def phase_merge(l, xsrc):
        P.scope()
        last = (l == L - 1)
        wfc = P.sb("wfc", [128, 8, D], BF16)
        wdl = P.sb("wdl", [128, 8, D], BF16)
        wo = P.sb("wo", [128, 8, D], BF16)
        wr = P.sb("wr", [128, 8, 36], F32)
        P.scope()
        wfr = P.sb("wfr", [128, 4, D], BF16)
        P.dma('pool', [(wfr[:], w_fourier[l].rearrange("(g p) n -> p g n", p=128)),
                       (wdl[:], w_delta[l].rearrange("(kc p) n -> p kc n", p=128)),
                       (wo[:], w_out[l].rearrange("(kc p) n -> p kc n", p=128))], lane='mw', w=['mw'])
        P.dma('sp', [(wr[:], w_rt[l].rearrange("(kc p) n -> p kc n", p=128))], lane='wr', w=['wr'])
        for gi in range(4):
            for (j, mat) in ((0, chc), (1, chs)):
                for hf in range(2):
                    ps = P.psum()
                    P.op('pe', lambda e: e.matmul(ps.t[:, :512], mat[:], wfr[:, gi, hf * 512:(hf + 1) * 512],
                                                  start=True, stop=True), r=['mw', 'par'], w=[ps.k])
                    P.op('act', lambda e: e.copy(out=wfc[:, gi * 2 + j, hf * 512:(hf + 1) * 512], in_=ps.t[:, :512]),
                         r=[ps.k], w=['wfc'])
        P.barrier()
        P.end_scope()
        u_l = P.sb("u_l", [128, 32, 512], BF16)
        u_c = P.sb("u_c", [128, 2, 512], BF16)
        P.dma('sp', [(u_l[:], u_tok[:, 2:NB, :])], lane='ul', w=['u_l'])
        if not last:
            P.dma('sp', [(u_c[:], u_tok[:, 0:2, :])], lane='uc', w=['u_c'])
        dbuf = {'c': [P.sb("dcb%d" % i, [128, 4, 512], BF16) for i in range(4)],
                's': [P.sb("dsb%d" % i, [128, 4, 512], BF16) for i in range(4)]}
        dctr = 0
        PQ = P.sb("PQ", [128, 8, 512], BF16)
        odt = P.sb("odt", [128, 8, 512], BF16)
        gtt = P.sb("gtt", [128, 16, 512], BF16)
        mg = P.sb("mg", [128, 8, 512], BF16)
        m1 = P.sb("m1", [128, 512], F32)
        m2 = P.sb("m2", [128, 512], F32)
        xt = P.sb("xt", [128, 8, 512], F32)
        h2f = xt
        h2b = P.sb("h2b", [128, 8, 512], BF16)
        tl = {'sq': [P.sb("sq%d" % i, [128, 512], BF16) for i in range(2)],
              'tmp': [P.sb("tmp%d" % i, [128, 512], F32) for i in range(2)],
              'rstd': P.sb("rstd", [128, 512], F32)}
        lg = P.sb("lg", [128, 36], F32)
        rt = {k: P.sb("rt_" + k, [128, 32], F32) for k in ('lem', 'oh1', 'oh2', 'wt', 'tmp')}
        rs = {k: P.sb("rs_" + k, [128, 4], F32) for k in ('mx', 'ohg', 'ex', 'pg', 'm1', 'm2', 'w1', 'w2', 'ohm')}
        wtst = P.sb("wtst", [32, 512], F32)
        xv = xsrc.rearrange("(kc p) t -> p kc t", p=128)
        xsv = xs.rearrange("(kc p) t -> p kc t", p=128)
        for ti, (t0, W, lat) in enumerate(TILES):
            if last and not lat:
                continue
            ws = 0 if lat else 1
            ntc = 32 if lat else 2
            uu = u_l if lat else u_c
            ukey = 'u_l' if lat else 'u_c'
            f0 = t0 - CTX if lat else 0
            mats = {'c': dftc if lat else dftc_c, 's': dfts if lat else dfts_c}
            sw = 4 if lat else 2
            nsub = ntc // sw
            P.dma('sp', [(odt[:, :, :W], odT.rearrange("(c p) t -> p c t", p=128)[:, :, t0:t0 + W]),
                         (gtt[:, :, :W], gT.rearrange("(c p) t -> p c t", p=128)[:, :, t0:t0 + W]),
                         (xt[:, :, :W], xv[:, :, t0:t0 + W])], lane='mt', w=['mt'])
            for gh in range(2):
                accs = [[P.psum(), P.psum()] for _ in range(2)]
                for sb_ in range(nsub):
                    bi = dctr % 4
                    dctr += 1
                    for cs in ('c', 's'):
                        P.dma('sp', [(dbuf[cs][bi][:, :sw, :W],
                                      mats[cs][sb_ * sw * 128:(sb_ + 1) * sw * 128, f0:f0 + W].rearrange(
                                          "(b p) f -> p b f", p=128))],
                              lane='d%sb%d' % (cs, bi), w=[('d' + cs, bi)])
                    for t4 in range(sw):
                        tc = sb_ * sw + t4
                        for gl in range(2):
                            gi = gh * 2 + gl
                            for j, cs in enumerate(('c', 's')):
                                P.op('pe', lambda e: e.matmul(accs[gl][j].t[:, :W], uu[:, tc, gi * 128:(gi + 1) * 128],
                                                              dbuf[cs][bi][:, t4, :W], start=(tc == 0),
                                                              stop=(tc == ntc - 1)),
                                     r=[ukey, ('d' + cs, bi)], w=[accs[gl][j].k])
                for gl in range(2):
                    gi = gh * 2 + gl
                    for j in range(2):
                        P.op('act', lambda e: e.copy(out=PQ[:, gi * 2 + j, :W], in_=accs[gl][j].t[:, :W]),
                             r=[accs[gl][j].k], w=['PQ'])
            for dc in range(8):
                pa = P.psum()
                for k8 in range(8):
                    P.op('pe', lambda e: e.matmul(pa.t[:, :W], wfc[:, k8, dc * 128:(dc + 1) * 128], PQ[:, k8, :W],
                                                  start=(k8 == 0), stop=(k8 == 7)), r=['wfc', 'PQ'], w=[pa.k])
                pb = P.psum()
                for kc in range(8):
                    P.op('pe', lambda e: e.matmul(pb.t[:, :W], wdl[:, kc, dc * 128:(dc + 1) * 128], odt[:, kc, :W],
                                                  start=(kc == 0), stop=(kc == 7)), r=['mw', 'mt'], w=[pb.k])
                P.op('dve', lambda e: e.tensor_tensor(out=m1[:, :W], in0=pa.t[:, :W], in1=gtt[:, dc, :W], op=ALU.mult),
                     r=[pa.k, 'mt'], w=['m1'])
                P.op('dve', lambda e: e.tensor_tensor(out=m2[:, :W], in0=pb.t[:, :W], in1=gtt[:, 8 + dc, :W],
                                                      op=ALU.mult), r=[pb.k, 'mt'], w=['m2'])
                P.op('dve', lambda e: e.tensor_tensor(out=mg[:, dc, :W], in0=m1[:, :W], in1=m2[:, :W], op=ALU.add),
                     r=['m1', 'm2'], w=['mg'])
            for dc in range(8):
                po = P.psum()
                for kc in range(8):
                    P.op('pe', lambda e: e.matmul(po.t[:, :W], wo[:, kc, dc * 128:(dc + 1) * 128], mg[:, kc, :W],
                                                  start=(kc == 0), stop=(kc == 7)), r=['mw', 'mg'], w=[po.k])
                P.op('dve', lambda e: e.scalar_tensor_tensor(
                    out=xt[:, dc, :W], in0=po.t[:, :W], scalar=modv[:, l, 16 + dc, ws:ws + 1], in1=xt[:, dc, :W],
                    op0=ALU.mult, op1=ALU.add), r=[po.k, 'mod', 'mt'], w=['mt'])
            P.dma('sp', [(xsv[:, :, t0:t0 + W], xt[:, :, :W])], lane='xo', r=['mt'], w=['xs'])
            norm_mod(xt, 'mt', W, A2[:, l, :, ws], modv[:, l, 24:32, ws],
                     [(lambda kc: xt[:, kc, :W], 'mt'), (lambda kc: h2b[:, kc, :W], 'h2b')], tl)
            P.dma('sp', [(h2T.rearrange("(c p) t -> p c t", p=128)[:, :, t0:t0 + W], h2b[:, :, :W])], lane='h2o',
                  r=['h2b'], w=['h2T'])
            for tb in range(W // 128):
                ps = P.psum()
                for kc in range(8):
                    P.op('pe', lambda e: e.matmul(ps.t[:, :36], h2f[:, kc, tb * 128:(tb + 1) * 128], wr[:, kc, :],
                                                  start=(kc == 0), stop=(kc == 7)), r=['mt', 'wr'], w=[ps.k])
                RK = 'rt'

                def dv(fn, r=(), w=(RK,)):
                    P.op('dve', fn, r=list(r) + [RK], w=w)
                P.op('dve', lambda e: e.tensor_tensor(out=lg[:], in0=ps.t[:, :36], in1=brt[:, l, :], op=ALU.add),
                     r=[ps.k, 'par'], w=[RK])
                dv(lambda e: e.reduce_max(out=rs['mx'][:, 0:1], in_=lg[:, 0:4], axis=mybir.AxisListType.X))
                dv(lambda e: e.tensor_scalar(out=rs['ohg'][:], in0=lg[:, 0:4], scalar1=rs['mx'][:, 0:1], scalar2=None,
                                             op0=ALU.is_ge))
                dv(lambda e: e.tensor_scalar(out=rs['ex'][:], in0=lg[:, 0:4], scalar1=rs['mx'][:, 0:1], scalar2=None,
                                             op0=ALU.subtract))
                P.op('act', lambda e: e.activation(out=rs['ex'][:], in_=rs['ex'][:], func=AF.Exp), r=[RK], w=[RK])
                dv(lambda e: e.reduce_sum(out=rs['pg'][:, 0:1], in_=rs['ex'][:], axis=mybir.AxisListType.X))
                dv(lambda e: e.reciprocal(out=rs['pg'][:, 0:1], in_=rs['pg'][:, 0:1]))
                dv(lambda e: e.tensor_scalar(out=rs['ohm'][:], in0=rs['ohg'][:], scalar1=-1.0, scalar2=1.0e9,
                                             op0=ALU.add, op1=ALU.mult))
                dv(lambda e: e.tensor_tensor(out=rt['lem'][:].rearrange("p (g e) -> p g e", e=8),
                                             in0=lg[:, 4:36].rearrange("p (g e) -> p g e", e=8),
                                             in1=rs['ohm'][:].unsqueeze(2).to_broadcast([128, 4, 8]), op=ALU.add))
                dv(lambda e: e.reduce_max(out=rs['m1'][:, 0:1], in_=rt['lem'][:], axis=mybir.AxisListType.X))
                dv(lambda e: e.tensor_scalar(out=rt['oh1'][:], in0=rt['lem'][:], scalar1=rs['m1'][:, 0:1], scalar2=None,
                                             op0=ALU.is_ge))
                dv(lambda e: e.scalar_tensor_tensor(out=rt['tmp'][:], in0=rt['oh1'][:], scalar=-1.0e9, in1=rt['lem'][:],
                                                    op0=ALU.mult, op1=ALU.add))
                dv(lambda e: e.reduce_max(out=rs['m2'][:, 0:1], in_=rt['tmp'][:], axis=mybir.AxisListType.X))
                dv(lambda e: e.tensor_scalar(out=rt['oh2'][:], in0=rt['tmp'][:], scalar1=rs['m2'][:, 0:1], scalar2=None,
                                             op0=ALU.is_ge))
                dv(lambda e: e.tensor_tensor(out=rs['w1'][:, 0:1], in0=rs['m1'][:, 0:1], in1=rs['m2'][:, 0:1],
                                             op=ALU.subtract))
                P.op('act', lambda e: e.activation(out=rs['w1'][:, 0:1], in_=rs['w1'][:, 0:1], func=AF.Sigmoid),
                     r=[RK], w=[RK])
                dv(lambda e: e.tensor_scalar(out=rs['w2'][:, 0:1], in0=rs['w1'][:, 0:1], scalar1=-1.0, scalar2=1.0,
                                             op0=ALU.mult, op1=ALU.add))
                dv(lambda e: e.tensor_tensor(out=rs['w1'][:, 0:1], in0=rs['w1'][:, 0:1], in1=rs['pg'][:, 0:1],
                                             op=ALU.mult))
                dv(lambda e: e.tensor_tensor(out=rs['w2'][:, 0:1], in0=rs['w2'][:, 0:1], in1=rs['pg'][:, 0:1],
                                             op=ALU.mult))
                dv(lambda e: e.tensor_scalar(out=rt['wt'][:], in0=rt['oh1'][:], scalar1=rs['w1'][:, 0:1], scalar2=None,
                                             op0=ALU.mult))
                dv(lambda e: e.scalar_tensor_tensor(out=rt['wt'][:], in0=rt['oh2'][:], scalar=rs['w2'][:, 0:1],
                                                    in1=rt['wt'][:], op0=ALU.mult, op1=ALU.add))
                pT = P.psum()
                P.op('pe', lambda e: e.matmul(pT.t[0:32, 0:128], rt['wt'][:], ident_f[:], start=True, stop=True),
                     r=[RK, CK], w=[pT.k])
                P.op('act', lambda e: e.copy(out=wtst[:, tb * 128:(tb + 1) * 128], in_=pT.t[0:32, 0:128]),
                     r=[pT.k], w=['wtst'])
            P.dma('sp', [(wtT[:, t0:t0 + W], wtst[:, :W])], lane='wto', r=['wtst'], w=['wtT'])
        P.barrier()
        P.end_scope()

    def phase_moe(l):
        last = (l == L - 1)
        tiles = [t for t in TILES if not (last and not t[2])]
        passes = [tiles[0:3], tiles[3:6], tiles[6:]]
        xsv = xs.rearrange("(kc p) t -> p kc t", p=128)
        for ptiles in passes:
            P.scope()
            pt0 = ptiles[0][0]
            pw = sum(t[1] for t in ptiles)
            hb = P.sb("hb", [128, 8, pw], BF16)
            yacc = P.sb("yacc", [128, 8, pw], F32)
            wtt = P.sb("wtt", [32, pw], F32)
            P.dma('sp', [(hb[:], h2T.rearrange("(c p) t -> p c t", p=128)[:, :, pt0:pt0 + pw]),
                         (wtt[:], wtT[:, pt0:pt0 + pw])], lane='hb', w=['hb'])
            wg = [P.sb("wg%d" % i, [128, 8, 512], BF16) for i in range(2)]
            wu = [P.sb("wu%d" % i, [128, 8, 512], BF16) for i in range(2)]
            wd = [P.sb("wd%d" % i, [128, 4, D], BF16) for i in range(2)]
            sg = [P.sb("sg%d" % i, [128, 512], F32) for i in range(2)]
            tt = [P.sb("tt%d" % i, [128, 512], F32) for i in range(2)]
            h1 = [P.sb("h1_%d" % i, [128, 4, 512], BF16) for i in range(2)]
            wrow = [P.sb("wrow%d" % i, [128, 512], F32) for i in range(2)]
            xt = P.sb("xt", [128, 8, 512], F32)
            it = 0
            for ex in range(NE):
                b = ex % 2
                WK = ('ew', b)
                P.dma('pool', [(wg[b][:], w_gate[l, ex].rearrange("(kc p) n -> p kc n", p=128)),
                               (wu[b][:], w_up[l, ex].rearrange("(kc p) n -> p kc n", p=128)),
                               (wd[b][:], w_down[l, ex].rearrange("(kc p) n -> p kc n", p=128))],
                      lane='ew%d' % b, w=[WK])
                for (t0, W, lat) in ptiles:
                    o0 = t0 - pt0
                    i2 = it % 2
                    it += 1
                    pw_ = P.psum()
                    P.op('pe', lambda e: e.matmul(pw_.t[:, :W], ident_f[0:32, ex:ex + 1].to_broadcast([32, 128]), wtt[:, o0:o0 + W], start=True, stop=True),
                         r=['hb', CK], w=[pw_.k])
                    P.op('act', lambda e: e.copy(out=wrow[i2][:, :W], in_=pw_.t[:, :W]), r=[pw_.k], w=[('wrow', i2)])
                    for fc in range(4):
                        pg = P.psum()
                        pu = P.psum()
                        for kc in range(8):
                            P.op('pe', lambda e: e.matmul(pg.t[:, :W], wg[b][:, kc, fc * 128:(fc + 1) * 128],
                                                          hb[:, kc, o0:o0 + W], start=(kc == 0), stop=(kc == 7)),
                                 r=[WK, 'hb'], w=[pg.k])
                        for kc in range(8):
                            P.op('pe', lambda e: e.matmul(pu.t[:, :W], wu[b][:, kc, fc * 128:(fc + 1) * 128],
                                                          hb[:, kc, o0:o0 + W], start=(kc == 0), stop=(kc == 7)),
                                 r=[WK, 'hb'], w=[pu.k])
                        f2 = fc % 2
                        P.op('act', lambda e: e.activation(out=sg[f2][:, :W], in_=pg.t[:, :W], func=AF.Silu),
                             r=[pg.k], w=[('sg', f2)])
                        P.op('dve', lambda e: e.tensor_tensor(out=tt[f2][:, :W], in0=sg[f2][:, :W], in1=pu.t[:, :W],
                                                              op=ALU.mult), r=[('sg', f2), pu.k], w=[('tt', f2)])
                        P.op('dve', lambda e: e.tensor_tensor(out=h1[i2][:, fc, :W], in0=tt[f2][:, :W], in1=wrow[i2][:, :W],
                                                              op=ALU.mult), r=[('tt', f2), ('wrow', i2)], w=[('h1', i2)])
                    for dc in range(8):
                        py = P.psum()
                        for fc in range(4):
                            P.op('pe', lambda e: e.matmul(py.t[:, :W], wd[b][:, fc, dc * 128:(dc + 1) * 128],
                                                          h1[i2][:, fc, :W], start=(fc == 0), stop=(fc == 3)),
                                 r=[WK, ('h1', i2)], w=[py.k])
                        if ex == 0:
                            P.op('act', lambda e: e.copy(out=yacc[:, dc, o0:o0 + W], in_=py.t[:, :W]),
                                 r=[py.k], w=[('y', dc)])
                        else:
                            P.op('dve', lambda e: e.tensor_tensor(out=yacc[:, dc, o0:o0 + W], in0=py.t[:, :W],
                                                                  in1=yacc[:, dc, o0:o0 + W], op=ALU.add),
                                 r=[py.k, ('y', dc)], w=[('y', dc)])
            tl = None
            if last:
                tl = {'sq': [P.sb("sq%d" % i, [128, 512], BF16) for i in range(2)],
                      'tmp': [P.sb("tmp%d" % i, [128, 512], F32) for i in range(2)],
                      'rstd': P.sb("rstd", [128, 512], F32)}
                ot = P.sb("ot", [128, 8, 512], F32)
            for (t0, W, lat) in ptiles:
                o0 = t0 - pt0
                ws = 0 if lat else 1
                P.dma('sp', [(xt[:, :, :W], xsv[:, :, t0:t0 + W])], lane='xm', r=['xs'], w=['xm'])
                for dc in range(8):
                    P.op('dve', lambda e: e.scalar_tensor_tensor(
                        out=xt[:, dc, :W], in0=yacc[:, dc, o0:o0 + W], scalar=modv[:, l, 40 + dc, ws:ws + 1],
                        in1=xt[:, dc, :W], op0=ALU.mult, op1=ALU.add), r=[('y', dc), 'mod', 'xm'], w=['xm'])
                if not last:
                    P.dma('sp', [(xsv[:, :, t0:t0 + W], xt[:, :, :W])], lane='xmo', r=['xm'], w=['xs'])
                else:
                    ss = P.psum()
                    for kc in range(8):
                        sq = tl['sq'][kc % 2]
                        P.op('act', lambda e: e.activation(out=sq[:, :W], in_=xt[:, kc, :W], func=AF.Square),
                             r=['xm'], w=[('sq', kc % 2)])
                        P.op('pe', lambda e: e.matmul(ss.t[:, :W], ones_bf[:], sq[:, :W], start=(kc == 0),
                                                      stop=(kc == 7)), r=[('sq', kc % 2), CK], w=[ss.k])
                    rstd = tl['rstd']
                    P.op('act', lambda e: e.activation(out=rstd[:, :W], in_=ss.t[:, :W], func=AF.Sqrt, scale=1.0 / D,
                                                       bias=epsT[:, 0:1]), r=[ss.k, CK], w=['rstd'])
                    P.op('dve', lambda e: e.reciprocal(out=rstd[:, :W], in_=rstd[:, :W]), r=['rstd'], w=['rstd'])
                    for kc in range(8):
                        P.op('dve', lambda e: e.scalar_tensor_tensor(
                            out=ot[:, kc, :W], in0=xt[:, kc, :W], scalar=fnorm[:, kc:kc + 1], in1=rstd[:, :W],
                            op0=ALU.mult, op1=ALU.mult), r=['xm', 'rstd', 'par'], w=['ot'])
                    P.dma('sp', [(outT.rearrange("(c p) t -> p c t", p=128)[:, :, t0 - CTX:t0 - CTX + W], ot[:, :, :W])],
                          lane='oo', r=['ot'], w=['outT'])
            P.barrier()
            P.end_scope()

    phase_mod()
    done = False
    for l in range(nl):
        xsrc = xT_in if l == 0 else xs
        for (nm, fn) in (('proj', lambda: phase_proj(l, xsrc)), ('delta', lambda: phase_delta(l)),
                         ('merge', lambda: phase_merge(l, xsrc)), ('moe', lambda: phase_moe(l))):
            fn()
            if stop == (l, nm):
                done = True
                break
        if done:
            break
    P.barrier()
    print("bass program: ninst=%d nwait=%d lanes=%d" % (P.ninst, P.nwait, len(P.lanes)))
    return nc


def _fm(v):
    return np.ascontiguousarray(np.asarray(v).reshape(-1, 128).T)


def prep_shared(inp):
    f32 = np.float32
    sh = {}
    b_mod = np.asarray(inp['b_mod'], f32)
    sh['bmod'] = np.ascontiguousarray(b_mod.reshape(L, 48, 128).transpose(2, 0, 1))
    sh['nmix'] = np.ascontiguousarray(np.asarray(inp['norm_mix'], f32).reshape(L, 8, 128).transpose(2, 0, 1))
    sh['nffn'] = np.ascontiguousarray(np.asarray(inp['norm_ffn'], f32).reshape(L, 8, 128).transpose(2, 0, 1))
    sh['fnorm'] = _fm(np.asarray(inp['final_norm'], f32))
    sh['convw'] = np.ascontiguousarray(np.asarray(inp['conv_w'], f32).reshape(L, 3, 24, 128).transpose(3, 0, 1, 2))
    sh['alog'] = np.ascontiguousarray(np.broadcast_to(np.asarray(inp['a_log'], f32).reshape(1, L, 16), (128, L, 16)))
    sh['dtb'] = np.ascontiguousarray(np.broadcast_to(np.asarray(inp['dt_bias'], f32).reshape(1, L, 16), (128, L, 16)))
    sh['onorm'] = np.ascontiguousarray(np.asarray(inp['out_norm'], f32).T)
    brt = np.concatenate([np.asarray(inp['b_route_group'], f32), np.asarray(inp['b_route_expert'], f32)], axis=1)
    sh['brt'] = np.ascontiguousarray(np.broadcast_to(brt.reshape(1, L, 36), (128, L, 36)))
    sh['w_rt'] = np.ascontiguousarray(np.concatenate([np.asarray(inp['w_route_group'], f32),
                                                      np.asarray(inp['w_route_expert'], f32)], axis=2))
    for k in ('w_mod', 'w_in', 'w_fourier', 'w_delta', 'w_out', 'w_gate', 'w_up', 'w_down'):
        sh[k] = np.ascontiguousarray(np.asarray(inp[k], f32))
    bf = ml_dtypes.bfloat16

    def tab(n, scale):
        k = np.arange(n, dtype=np.int64)
        ang = 2.0 * np.pi * ((k[:, None] * k[None, :]) % n).astype(np.float64) / n
        return (np.cos(ang) * scale).astype(bf), (np.sin(ang) * scale).astype(bf)
    sh['dftc'], sh['dfts'] = tab(SEQ, (SEQ * 128.0) ** -0.5)
    sh['dftc_c'], sh['dfts_c'] = tab(CTX, (CTX * 128.0) ** -0.5)
    c, s = tab(128, 1.0)
    sh['chc'] = c
    sh['chs'] = (-s.astype(np.float32)).astype(bf)
    idx = np.arange(128)
    def bd(b):
        return (idx[:, None] // b == idx[None, :] // b).astype(np.float32)
    bm = np.stack([bd(16), bd(32) - bd(16), bd(64) - bd(32), bd(128) - bd(64)], axis=1)
    sh['bmask'] = np.ascontiguousarray(bm).astype(bf)
    return sh


def prep_core(inp, b):
    f32 = np.float32
    x = np.asarray(inp['x'][b], f32)
    ctx = np.asarray(inp['ctx'][b], f32)
    xT = np.ascontiguousarray(np.concatenate([ctx, x], axis=0).T)
    cvec = np.stack([_fm(np.asarray(inp['c'][b], f32)), _fm(np.asarray(inp['c_ctx'], f32))], axis=2)
    return {'xT': xT, 'cvec': np.ascontiguousarray(cvec)}


_NC_CACHE = {}


def kernel(**inputs):
    if 'nc' not in _NC_CACHE:
        _NC_CACHE['nc'] = build()
    nc = _NC_CACHE['nc']
    sh = prep_shared(inputs)
    in_maps = []
    for b in range(8):
        m = dict(sh)
        m.update(prep_core(inputs, b))
        in_maps.append(m)
    res = run_bass_kernel_spmd(nc, in_maps, core_ids=list(range(8)))
    out = np.stack([np.ascontiguousarray(r["outT"].T) for r in res.results], axis=0)
    return out.astype(np.float32)
```

```python
import numpy as np
import os
import ml_dtypes
import concourse.bass as bass
import concourse.mybir as mybir
from concourse.bass_utils import run_bass_kernel_spmd

F32 = mybir.dt.float32
BF16 = mybir.dt.bfloat16
AF = mybir.ActivationFunctionType
ALU = mybir.AluOpType

D = 1024
SEQ = 4096
CTX = 256
T = CTX + SEQ
NB = T // 128
L = 4
NE = 32
OFF_A = 3072
OFF_B = 3088
OFF_Z = 3104
OFF_F = 4128
OFF_G = 4640
IN_COLS = 6688
EPS = 1e-6
TILES = [(0, 256, 0)] + [(256 + 512 * i, 512, 1) for i in range(8)]


class PsT:
    def __init__(self, t, k):
        self.t = t
        self.k = k


class Prog:
    ENG = ('pe', 'act', 'dve', 'pool', 'sp')

    def __init__(self, nc, n_lanes=64):
        self.nc = nc
        self.h = {'pe': nc.tensor, 'act': nc.scalar, 'dve': nc.vector, 'pool': nc.gpsimd, 'sp': nc.sync}
        self.sem = {}
        self.val = {}
        for e in self.ENG:
            self.sem[e] = nc.semaphore("s_" + e).__enter__()
            self.val[e] = 0
        self.n_lanes = n_lanes
        for l in range(n_lanes):
            k = ('L', l)
            self.sem[k] = nc.semaphore("s_l%d" % l).__enter__()
            self.val[k] = 0
        self.lanes = {}
        self.seen = {e: {} for e in self.ENG}
        self.clock = {}
        self.lastw = {}
        self.readers = {}
        self.ninst = 0
        self.nwait = 0
        self.stack = [[]]
        self.uid = 0
        self.psum_pool = []
        self.psum_i = 0
        self.cut = int(os.environ['KCUT']) if os.environ.get('KCUT') else None

    def scope(self):
        self.stack.append([])

    def end_scope(self):
        for cm in reversed(self.stack.pop()):
            cm.__exit__(None, None, None)

    def sb(self, name, shape, dt):
        self.uid += 1
        cm = self.nc.sbuf_tensor("%s_%d" % (name, self.uid), list(shape), dt)
        t = cm.__enter__()
        self.stack[-1].append(cm)
        return t

    def init_psum(self):
        self.NPS = 6
        for i in range(self.NPS):
            t = self.nc.psum_tensor("psb%d" % i, [128, 512], F32).__enter__()
            self.psum_pool.append(PsT(t, ('ps', i)))
        self.pst = [self.nc.psum_tensor("pst%d" % i, [128, 1024], BF16).__enter__() for i in range(2)]
        self.pst_i = 0

    def psum(self):
        p = self.psum_pool[self.psum_i % self.NPS]
        self.psum_i += 1
        return p

    def psum_t(self):
        i = self.pst_i % 2
        self.pst_i += 1
        return PsT(self.pst[i][:, 0:512], ('pst', i))

    def lane(self, name):
        if name not in self.lanes:
            assert len(self.lanes) < self.n_lanes, "out of lanes"
            self.lanes[name] = len(self.lanes)
        return self.lanes[name]

    def _deps(self, eng, reads, writes):
        deps = {}

        def add(d, kind):
            if d is None:
                return
            sk, v = d
            if sk == eng:
                if eng == 'pe' or kind != 'raw':
                    return
            if v > deps.get(sk, 0):
                deps[sk] = v
        for k in reads:
            add(self.lastw.get(k), 'raw')
        for k in writes:
            add(self.lastw.get(k), 'waw')
            for r in self.readers.get(k, ()):
                add(r, 'war')
        seen = self.seen[eng]
        return [(sk, v) for sk, v in deps.items() if seen.get(sk, 0) < v]

    def _absorb(self, eng, need):
        seen = self.seen[eng]
        for sk, v in need:
            c = self.clock.get((sk, v))
            if c:
                for k2, v2 in c.items():
                    if seen.get(k2, 0) < v2:
                        seen[k2] = v2
            if seen.get(sk, 0) < v:
                seen[sk] = v

    def _emit_waits(self, eng, need):
        h = self.h[eng]
        for sk, v in need[:-1]:
            h.wait_ge(self.sem[sk], v)
            self.nwait += 1
        return need[-1] if need else None

    def _record(self, tag, reads, writes):
        for k in writes:
            self.lastw[k] = tag
            self.readers[k] = []
        for k in reads:
            self.readers.setdefault(k, []).append(tag)

    def op(self, eng, fn, r=(), w=()):
        if self.cut is not None and self.ninst >= self.cut:
            return None
        need = self._deps(eng, r, w)
        last = self._emit_waits(eng, need)
        ins = fn(self.h[eng])
        if last is not None:
            ins._wait_ge(self.sem[last[0]], last[1])
        self._absorb(eng, need)
        self.val[eng] += 1
        v = self.val[eng]
        ins.then_inc(self.sem[eng], 1)
        snap = dict(self.seen[eng])
        snap[eng] = v
        self.clock[(eng, v)] = snap
        self._record((eng, v), r, w)
        self.ninst += 1
        return ins

    def dma(self, q, pairs, lane, r=(), w=()):
        if self.cut is not None and self.ninst >= self.cut:
            return None
        lk = ('L', self.lane(lane))
        need = self._deps(q, r, w)
        pv = self.val[lk]
        if pv > 0 and self.seen[q].get(lk, 0) < pv:
            need = [(sk, v) for sk, v in need if sk != lk] + [(lk, pv)]
        last = self._emit_waits(q, need)
        h = self.h[q]
        first = True
        for pr in pairs:
            if callable(pr):
                ins = pr(h)
            else:
                ins = h.dma_start(out=pr[0], in_=pr[1])
            if first and last is not None:
                ins._wait_ge(self.sem[last[0]], last[1])
            first = False
            ins.then_inc(self.sem[lk], 16)
            self.val[lk] += 16
            self.ninst += 1
        self._absorb(q, need)
        v = self.val[lk]
        snap = dict(self.seen[q])
        snap[lk] = v
        self.clock[(lk, v)] = snap
        self._record((lk, v), r, w)

    def wait_all(self, eng, keys):
        need = self._deps(eng, keys, ())
        for sk, v in need:
            self.h[eng].wait_ge(self.sem[sk], v)
        self._absorb(eng, need)

    def barrier(self):
        for e in self.ENG:
            need = [(sk, v) for sk, v in self.val.items()
                    if v > 0 and sk != e and self.seen[e].get(sk, 0) < v]
            for sk, v in need:
                self.h[e].wait_ge(self.sem[sk], v)
                self.nwait += 1
            for sk, v in need:
                self.seen[e][sk] = v


def build(nl=L, stop=None, dbg=(), moe_w=True):
    nc = bass.Bass("TRN2", target_bir_lowering=False)
    P = Prog(nc)
    P.init_psum()

    def din(name, shape, dt=F32):
        return nc.dram_tensor(name, list(shape), dt, kind="ExternalInput").ap()

    def dscr(name, shape, dt):
        kind = "ExternalOutput" if name in dbg else "Internal"
        return nc.dram_tensor(name, list(shape), dt, kind=kind).ap()

    xT_in = din("xT", [D, T])
    cvec_d = din("cvec", [128, 8, 2])
    bmod_d = din("bmod", [128, L, 48])
    nmix_d = din("nmix", [128, L, 8])
    nffn_d = din("nffn", [128, L, 8])
    fnorm_d = din("fnorm", [128, 8])
    convw_d = din("convw", [128, L, 3, 24])
    alog_d = din("alog", [128, L, 16])
    dtb_d = din("dtb", [128, L, 16])
    onorm_d = din("onorm", [128, L])
    brt_d = din("brt", [128, L, 36])
    w_mod = din("w_mod", [nl, D, 6 * D])
    w_in = din("w_in", [nl, D, IN_COLS])
    w_fourier = din("w_fourier", [nl, 512, D])
    w_delta = din("w_delta", [nl, D, D])
    w_out = din("w_out", [nl, D, D])
    w_rt = din("w_rt", [nl, D, 36])
    NEd = NE if moe_w else 1
    w_gate = din("w_gate", [nl, NEd, D, 512])
    w_up = din("w_up", [nl, NEd, D, 512])
    w_down = din("w_down", [nl, NEd, 512, D])
    dftc = din("dftc", [SEQ, SEQ], BF16)
    dfts = din("dfts", [SEQ, SEQ], BF16)
    dftc_c = din("dftc_c", [CTX, CTX], BF16)
    dfts_c = din("dfts_c", [CTX, CTX], BF16)
    chc_d = din("chc", [128, 128], BF16)
    chs_d = din("chs", [128, 128], BF16)
    bmask_d = din("bmask", [128, 4, 128], BF16)
    iota_d = din("iota", [128, 64], F32)
    wbase_d = din("wbase", [128, 8], F32)
    outT = nc.dram_tensor("outT", [D, SEQ], F32, kind="ExternalOutput").ap()

    xs = dscr("xs", [D, T], F32)
    qT = dscr("qT", [D, T], BF16)
    kT = dscr("kT", [D, T], BF16)
    k_tok = dscr("k_tok", [8, 128, T], BF16)
    v_tok = dscr("v_tok", [8, 128, T], BF16)
    ab_tok = dscr("ab_tok", [128, NB, 32], F32)
    zT = dscr("zT", [D, T], BF16)
    u_tok = dscr("u_tok", [128, NB, 512], BF16)
    gT = dscr("gT", [2 * D, T], BF16)
    odT = dscr("odT", [D, T], BF16)
    h2tok_d = dscr("h2tok", [128, NB, D], BF16)
    rt_d = dscr("rt", [128, NB, 66], F32)
    NBLK = 49
    xs_d = dscr("xs_slots", [NBLK * 512, D], BF16)
    ys_d = dscr("ys_slots", [NBLK * 512, D], BF16)

    ones_bf = P.sb("ones_bf", [128, 128], BF16)
    ones_f = P.sb("ones_f", [128, 128], F32)
    zeros_f = P.sb("zeros_f", [128, 128], F32)
    ident_bf = P.sb("ident_bf", [128, 128], BF16)
    ident_f = P.sb("ident_f", [128, 128], F32)
    triF = P.sb("triF", [128, 128], F32)
    triB = P.sb("triB", [128, 128], F32)
    negF = P.sb("negF", [128, 128], F32)
    negB = P.sb("negB", [128, 128], F32)
    strF = P.sb("strF", [128, 128], F32)
    strB = P.sb("strB", [128, 128], F32)
    epsT = P.sb("epsT", [128, 1], F32)
    CK = 'const'

    def pool(fn, r=(), w=(CK,)):
        P.op('pool', fn, r=r, w=w)
    pool(lambda e: e.memset(ones_f[:], 1.0))
    pool(lambda e: e.memset(zeros_f[:], 0.0))
    pool(lambda e: e.memset(epsT[:], EPS))
    P.op('dve', lambda e: e.tensor_copy(out=ones_bf[:], in_=ones_f[:]), r=[CK], w=[CK])

    def asel(out, in_, pat, cm, op, fill):
        pool(lambda e: e.affine_select(out=out, in_=in_, pattern=pat, compare_op=op, fill=fill,
                                       base=0, channel_multiplier=cm), r=[CK])
    asel(ident_f[:], ones_f[:], [[1, 128]], -1, ALU.is_equal, 0.0)
    asel(triF[:], ones_f[:], [[1, 128]], -1, ALU.is_ge, 0.0)
    asel(triB[:], ones_f[:], [[-1, 128]], 1, ALU.is_ge, 0.0)
    asel(negF[:], zeros_f[:], [[1, 128]], -1, ALU.is_ge, -1.0e5)
    asel(negB[:], zeros_f[:], [[-1, 128]], 1, ALU.is_ge, -1.0e5)
    asel(strF[:], ones_f[:], [[1, 128]], -1, ALU.is_gt, 0.0)
    asel(strB[:], ones_f[:], [[-1, 128]], 1, ALU.is_gt, 0.0)
    P.op('dve', lambda e: e.tensor_copy(out=ident_bf[:], in_=ident_f[:]), r=[CK], w=[CK])

    bmod = P.sb("bmod", [128, L, 48], F32)
    nmix = P.sb("nmix", [128, L, 8], F32)
    nffn = P.sb("nffn", [128, L, 8], F32)
    fnorm = P.sb("fnorm", [128, 8], F32)
    convw = P.sb("convw", [128, L, 3, 24], F32)
    alog = P.sb("alog", [128, L, 16], F32)
    dtb = P.sb("dtb", [128, L, 16], F32)
    onorm = P.sb("onorm", [128, L], F32)
    brt = P.sb("brt", [128, L, 36], F32)
    chc = P.sb("chc", [128, 128], BF16)
    chs = P.sb("chs", [128, 128], BF16)
    cv = P.sb("cv", [128, 8, 2], F32)
    bmask = P.sb("bmask", [128, 4, 128], BF16)
    iota = P.sb("iota", [128, 64], F32)
    wbase = P.sb("wbase", [128, 8], F32)
    P.dma('sp', [(bmod[:], bmod_d), (nmix[:], nmix_d), (nffn[:], nffn_d), (fnorm[:], fnorm_d),
                 (convw[:], convw_d), (alog[:], alog_d), (dtb[:], dtb_d), (onorm[:], onorm_d),
                 (brt[:], brt_d), (chc[:], chc_d), (chs[:], chs_d), (cv[:], cvec_d), (bmask[:], bmask_d), (iota[:], iota_d), (wbase[:], wbase_d)],
          lane='par', w=['par'])
    modv = P.sb("modv", [128, L, 48, 2], F32)
    A1 = P.sb("A1", [128, L, 8, 2], F32)
    A2 = P.sb("A2", [128, L, 8, 2], F32)
    nea = P.sb("nea", [128, L, 16], F32)
    P.op('act', lambda e: e.activation(out=nea[:], in_=alog[:], func=AF.Exp), r=['par'], w=['nea'])
    P.op('dve', lambda e: e.tensor_scalar(out=nea[:], in0=nea[:], scalar1=-1.0, scalar2=None, op0=ALU.mult),
         r=['nea'], w=['nea'])

    def phase_mod():
        P.scope()
        sv = P.sb("sv", [128, 8, 2], F32)
        P.op('act', lambda e: e.activation(out=sv[:], in_=cv[:], func=AF.Silu), r=['par'], w=['sv'])
        wts = [P.sb("wmt%d" % i, [128, 8, 512], F32) for i in range(2)]
        it = 0
        for l in range(nl):
            wv = w_mod[l].rearrange("(kc p) n -> p kc n", p=128)
            for cb in range(12):
                b = it % 2
                it += 1
                wt = wts[b]
                P.dma('sp', [(wt[:], wv[:, :, cb * 512:(cb + 1) * 512])], lane='wm%d' % b, w=[('wm', b)])
                ps = P.psum()
                for j in range(4):
                    for kc in range(8):
                        P.op('pe', lambda e: e.matmul(ps.t[:, j * 2:j * 2 + 2], wt[:, kc, j * 128:(j + 1) * 128],
                                                      sv[:, kc, :], start=(kc == 0), stop=(kc == 7)),
                             r=[('wm', b), 'sv'], w=[ps.k])
                P.op('dve', lambda e: e.tensor_tensor(
                    out=modv[:, l, cb * 4:cb * 4 + 4, :],
                    in0=ps.t[:, 0:8].rearrange("p (a b) -> p a b", b=2),
                    in1=bmod[:, l, cb * 4:cb * 4 + 4].unsqueeze(2).to_broadcast([128, 4, 2]),
                    op=ALU.add), r=[ps.k, 'par'], w=['mod'])
            for (A, nw, c0) in ((A1, nmix, 8), (A2, nffn, 32)):
                P.op('dve', lambda e: e.tensor_scalar(out=A[:, l], in0=modv[:, l, c0:c0 + 8, :], scalar1=1.0,
                                                      scalar2=None, op0=ALU.add), r=['mod'], w=['mod'])
                P.op('dve', lambda e: e.tensor_tensor(out=A[:, l], in0=A[:, l],
                                                      in1=nw[:, l, :].unsqueeze(2).to_broadcast([128, 8, 2]),
                                                      op=ALU.mult), r=['mod', 'par'], w=['mod'])
        P.barrier()
        P.end_scope()

    def norm_mod(xt, xkey, W, Aap, Bap, outs, tl):
        ss = P.psum()
        for kc in range(8):
            sq = tl['sq'][kc % 2]
            P.op('act', lambda e: e.activation(out=sq[:, :W], in_=xt[:, kc, :W], func=AF.Square),
                 r=[xkey], w=[('sq', kc % 2)])
            P.op('pe', lambda e: e.matmul(ss.t[:, :W], ones_bf[:], sq[:, :W], start=(kc == 0), stop=(kc == 7)),
                 r=[('sq', kc % 2), CK], w=[ss.k])
        rstd = tl['rstd']
        P.op('act', lambda e: e.activation(out=rstd[:, :W], in_=ss.t[:, :W], func=AF.Sqrt, scale=1.0 / D,
                                           bias=epsT[:, 0:1]), r=[ss.k, CK], w=['rstd'])
        P.op('dve', lambda e: e.reciprocal(out=rstd[:, :W], in_=rstd[:, :W]), r=['rstd'], w=['rstd'])
        for kc in range(8):
            tmp = tl['tmp'][kc % 2]
            P.op('dve', lambda e: e.tensor_tensor(out=tmp[:, :W], in0=xt[:, kc, :W], in1=rstd[:, :W], op=ALU.mult),
                 r=[xkey, 'rstd'], w=[('tmp', kc % 2)])
            for (fn, ok) in outs:
                P.op('act', lambda e: e.activation(out=fn(kc), in_=tmp[:, :W], func=AF.Identity,
                                                   scale=Aap[:, kc:kc + 1], bias=Bap[:, kc:kc + 1]),
                     r=[('tmp', kc % 2), 'mod'], w=[ok])

    def phase_proj(l, xsrc):
        P.scope()
        last = (l == L - 1)
        hT = P.sb("hT", [128, 8, T], BF16)
        xt = P.sb("xt", [128, 8, 512], F32)
        tl = {'sq': [P.sb("sq%d" % i, [128, 512], BF16) for i in range(2)],
              'tmp': [P.sb("tmp%d" % i, [128, 512], F32) for i in range(2)],
              'rstd': P.sb("rstd", [128, 512], F32)}
        xv = xsrc.rearrange("(kc p) t -> p kc t", p=128)
        for ti, (t0, W, lat) in enumerate(TILES):
            ws = 0 if lat else 1
            P.dma('sp', [(xt[:, :, :W], xv[:, :, t0:t0 + W])], lane='xt', w=['xt'])
            norm_mod(xt, 'xt', W, A1[:, l, :, ws], modv[:, l, 0:8, ws],
                     [(lambda kc: hT[:, kc, t0:t0 + W], ('hT', ti))], tl)
        wps = [P.sb("wp%d" % i, [128, 8, 1024], BF16) for i in range(2)]
        st8 = [P.sb("st8_%d" % i, [128, 8, 512], BF16) for i in range(2)]
        tokst = [P.sb("tokst%d" % i, [128, 8, 4, 128], BF16) for i in range(2)]
        ust = [P.sb("ust%d" % i, [128, 4, 512], BF16) for i in range(2)]
        abst = P.sb("abst", [128, 4, 32], F32)
        acc = P.sb("acc", [128, 512], F32)
        actf = P.sb("actf", [128, 512], F32)
        actb = P.sb("actb", [128, 512], BF16)
        sqb = P.sb("sqb", [128, 512], BF16)
        rn = P.sb("rn", [128, 512], F32)
        wv = w_in[l].rearrange("(kc p) n -> p kc n", p=128)
        pieces = [('q', 0, 1024), ('k', 1024, 1024), ('v', 2048, 1024), ('ab', OFF_A, 32), ('z', OFF_Z, 1024),
                  ('F', OFF_F, 512), ('Ga', OFF_G, 1024), ('Gb', OFF_G + 1024, 1024)]
        cnt = {'st8': 0, 'tok': 0, 'u': 0}
        for pi, (pn, c0, ncol) in enumerate(pieces):
            b = pi % 2
            wp = wps[b]
            wk = ('wp', b)
            P.dma('pool', [(wp[:, :, :ncol], wv[:, :, c0:c0 + ncol])], lane='wp%d' % b, w=[wk])
            for ti, (t0, W, lat) in enumerate(TILES):
                hk = ('hT', ti)
                nbk = W // 128
                if (not lat) and last and pn in ('z', 'F', 'Ga', 'Gb'):
                    continue
                if pn in ('q', 'k', 'v'):
                    si = cnt['st8'] % 2
                    st = st8[si]
                    sk = ('st8', si)
                    if pn in ('k', 'v'):
                        tki = cnt['tok'] % 2
                        cnt['tok'] += 1
                        tk = tokst[tki]
                        tkk = ('tok', tki)
                    for cc in range(8):
                        gcc = (c0 // 128) + cc
                        ps = P.psum()
                        for kc in range(8):
                            P.op('pe', lambda e: e.matmul(ps.t[:, :W], wp[:, kc, cc * 128:(cc + 1) * 128],
                                                          hT[:, kc, t0:t0 + W], start=(kc == 0), stop=(kc == 7)),
                                 r=[wk, hk], w=[ps.k])
                        rl = 64 if lat else 256
                        pv = ps.t[:, :W].rearrange("p (r c) -> p r c", c=rl)
                        av = acc[:, :W].rearrange("p (r c) -> p r c", c=rl)
                        P.op('dve', lambda e: e.tensor_scalar(out=acc[:, :W], in0=ps.t[:, :W],
                                                              scalar1=convw[:, l, 1, gcc:gcc + 1], scalar2=None,
                                                              op0=ALU.mult), r=[ps.k, 'par'], w=['acc'])
                        P.op('dve', lambda e: e.scalar_tensor_tensor(
                            out=av[:, :, 1:rl], in0=pv[:, :, 0:rl - 1], scalar=convw[:, l, 0, gcc:gcc + 1],
                            in1=av[:, :, 1:rl], op0=ALU.mult, op1=ALU.add), r=[ps.k, 'par', 'acc'], w=['acc'])
                        P.op('dve', lambda e: e.scalar_tensor_tensor(
                            out=av[:, :, 0:rl - 1], in0=pv[:, :, 1:rl], scalar=convw[:, l, 2, gcc:gcc + 1],
                            in1=av[:, :, 0:rl - 1], op0=ALU.mult, op1=ALU.add), r=[ps.k, 'par', 'acc'], w=['acc'])
                        if pn == 'v':
                            P.op('act', lambda e: e.activation(out=actb[:, :W], in_=acc[:, :W], func=AF.Silu),
                                 r=['acc'], w=['actb'])
                            src, srk = actb, 'actb'
                        else:
                            P.op('act', lambda e: e.activation(out=actf[:, :W], in_=acc[:, :W], func=AF.Silu),
                                 r=['acc'], w=['actf'])
                            P.op('act', lambda e: e.activation(out=sqb[:, :W], in_=actf[:, :W], func=AF.Square),
                                 r=['actf'], w=['sqb'])
                            p2 = P.psum()
                            P.op('pe', lambda e: e.matmul(p2.t[:, :W], ones_bf[:], sqb[:, :W], start=True, stop=True),
                                 r=['sqb', CK], w=[p2.k])
                            P.op('act', lambda e: e.activation(out=rn[:, :W], in_=p2.t[:, :W], func=AF.Sqrt,
                                                               bias=epsT[:, 0:1]), r=[p2.k, CK], w=['rn'])
                            P.op('dve', lambda e: e.reciprocal(out=rn[:, :W], in_=rn[:, :W]), r=['rn'], w=['rn'])
                            sc = (128.0 ** -0.5) if pn == 'q' else 1.0
                            P.op('dve', lambda e: e.scalar_tensor_tensor(
                                out=st[:, cc, :W], in0=actf[:, :W], scalar=sc, in1=rn[:, :W],
                                op0=ALU.mult, op1=ALU.mult), r=['actf', 'rn'], w=[sk])
                            src, srk = st[:, cc], sk
                        if pn in ('k', 'v'):
                            pt = P.psum_t()
                            for tb in range(nbk):
                                P.op('pe', lambda e: e.transpose(pt.t[:, tb * 128:(tb + 1) * 128],
                                                                 src[:, tb * 128:(tb + 1) * 128], ident_bf[:]),
                                     r=[srk, CK], w=[pt.k])
                            P.op('act', lambda e: e.copy(
                                out=tk[:, cc, 0:nbk, :],
                                in_=pt.t[:, :nbk * 128].rearrange("p (a b) -> p a b", b=128)),
                                r=[pt.k], w=[tkk])
                    if pn in ('q', 'k'):
                        dst = qT if pn == 'q' else kT
                        P.dma('sp', [(dst.rearrange("(c p) t -> p c t", p=128)[:, :, t0:t0 + W], st[:, :, :W])],
                              lane='st8_%d' % si, r=[sk], w=[pn + 'T'])
                        cnt['st8'] += 1
                    if pn in ('k', 'v'):
                        dst = k_tok if pn == 'k' else v_tok
                        P.dma('sp', [(dst[:, :, t0:t0 + W].rearrange("c p t -> p c t"),
                                      tk[:, :, :nbk, :].rearrange("p c a b -> p c (a b)"))],
                              lane='tok%d' % tki, r=[tkk], w=[pn + '_tok'])
                elif pn == 'ab':
                    ps = P.psum()
                    for tb in range(nbk):
                        for kc in range(8):
                            P.op('pe', lambda e: e.matmul(ps.t[:, tb * 32:(tb + 1) * 32],
                                                          hT[:, kc, t0 + tb * 128:t0 + (tb + 1) * 128],
                                                          wp[:, kc, 0:32], start=(kc == 0), stop=(kc == 7)),
                                 r=[wk, hk], w=[ps.k])
                    P.op('dve', lambda e: e.tensor_copy(out=abst[:, :nbk, :],
                                                        in_=ps.t[:, :nbk * 32].rearrange("p (a b) -> p a b", b=32)),
                         r=[ps.k], w=['abst'])
                    P.dma('sp', [(ab_tok[:, t0 // 128:t0 // 128 + nbk, :], abst[:, :nbk, :])],
                          lane='abst', r=['abst'], w=['ab_tok'])
                elif pn in ('z', 'Ga', 'Gb'):
                    si = cnt['st8'] % 2
                    cnt['st8'] += 1
                    st = st8[si]
                    sk = ('st8', si)
                    fnc = AF.Silu if pn == 'z' else AF.Sigmoid
                    for cc in range(8):
                        ps = P.psum()
                        for kc in range(8):
                            P.op('pe', lambda e: e.matmul(ps.t[:, :W], wp[:, kc, cc * 128:(cc + 1) * 128],
                                                          hT[:, kc, t0:t0 + W], start=(kc == 0), stop=(kc == 7)),
                                 r=[wk, hk], w=[ps.k])
                        P.op('act', lambda e: e.activation(out=st[:, cc, :W], in_=ps.t[:, :W], func=fnc),
                             r=[ps.k], w=[sk])
                    if pn == 'z':
                        dv = zT.rearrange("(c p) t -> p c t", p=128)
                    else:
                        o8 = 0 if pn == 'Ga' else 8
                        dv = gT.rearrange("(c p) t -> p c t", p=128)[:, o8:o8 + 8, :]
                    P.dma('sp', [(dv[:, :, t0:t0 + W], st[:, :, :W])], lane='st8_%d' % si, r=[sk], w=[pn])
                elif pn == 'F':
                    ui = cnt['u'] % 2
                    cnt['u'] += 1
                    us = ust[ui]
                    uk = ('ust', ui)
                    for tb in range(nbk):
                        ps = P.psum()
                        for kc in range(8):
                            P.op('pe', lambda e: e.matmul(ps.t[:, :512], hT[:, kc, t0 + tb * 128:t0 + (tb + 1) * 128],
                                                          wp[:, kc, 0:512], start=(kc == 0), stop=(kc == 7)),
                                 r=[wk, hk], w=[ps.k])
                        P.op('act', lambda e: e.copy(out=us[:, tb, :], in_=ps.t[:, :512]), r=[ps.k], w=[uk])
                    P.dma('sp', [(u_tok[:, t0 // 128:t0 // 128 + nbk, :], us[:, :nbk, :])],
                          lane='ust%d' % ui, r=[uk], w=['u_tok'])
        P.barrier()
        P.end_scope()

    def phase_delta(l):
        P.scope()
        last = (l == L - 1)
        ab = P.sb("ab", [128, NB, 32], F32)
        P.dma('sp', [(ab[:], ab_tok)], lane='ab', w=['ab'])
        g = P.sb("g", [128, NB, 16], F32)
        beta = P.sb("beta", [128, NB, 16], F32)
        nbeta = P.sb("nbeta", [128, NB, 16], F32)
        gcum = P.sb("gcum", [128, NB, 16], F32)
        ngam = P.sb("ngam", [128, NB, 16], F32)
        tail = P.sb("tail", [128, NB, 16], F32)
        egl = P.sb("egl", [128, NB, 16], F32)
        GK = 'gates'
        P.op('dve', lambda e: e.tensor_tensor(out=g[:], in0=ab[:, :, 0:16],
                                              in1=dtb[:, l, :].unsqueeze(1).to_broadcast([128, NB, 16]), op=ALU.add),
             r=['ab', 'par'], w=[GK])
        P.op('act', lambda e: e.activation(out=g[:], in_=g[:], func=AF.Exp), r=[GK], w=[GK])
        P.op('act', lambda e: e.activation(out=g[:], in_=g[:], func=AF.Ln, bias=ones_f[:, 0:1]), r=[GK, CK], w=[GK])
        P.op('dve', lambda e: e.tensor_tensor(out=g[:], in0=g[:],
                                              in1=nea[:, l, :].unsqueeze(1).to_broadcast([128, NB, 16]), op=ALU.mult),
             r=[GK, 'nea'], w=[GK])
        P.op('act', lambda e: e.activation(out=beta[:], in_=ab[:, :, 16:32], func=AF.Sigmoid), r=['ab'], w=[GK])
        P.op('dve', lambda e: e.tensor_scalar(out=nbeta[:], in0=beta[:], scalar1=-1.0, scalar2=None, op0=ALU.mult),
             r=[GK], w=[GK])
        for d in range(2):
            tri = triF if d == 0 else triB
            ps = P.psum()
            P.op('pe', lambda e: e.matmul(ps.t[:, :NB * 8].rearrange("p (a b) -> p a b", b=8), tri[:],
                                          g[:, :, d * 8:(d + 1) * 8], start=True, stop=True), r=[GK, CK], w=[ps.k])
            P.op('dve', lambda e: e.tensor_copy(out=gcum[:, :, d * 8:(d + 1) * 8],
                                                in_=ps.t[:, :NB * 8].rearrange("p (a b) -> p a b", b=8)),
                 r=[ps.k], w=[GK])
            ps2 = P.psum()
            P.op('pe', lambda e: e.matmul(ps2.t[:, :NB * 8].rearrange("p (a b) -> p a b", b=8), ones_f[:],
                                          g[:, :, d * 8:(d + 1) * 8], start=True, stop=True), r=[GK, CK], w=[ps2.k])
            P.op('act', lambda e: e.activation(out=egl[:, :, d * 8:(d + 1) * 8],
                                               in_=ps2.t[:, :NB * 8].rearrange("p (a b) -> p a b", b=8), func=AF.Exp),
                 r=[ps2.k], w=[GK])
            P.op('dve', lambda e: e.tensor_tensor(out=tail[:, :, d * 8:(d + 1) * 8],
                                                  in0=ps2.t[:, :NB * 8].rearrange("p (a b) -> p a b", b=8),
                                                  in1=gcum[:, :, d * 8:(d + 1) * 8], op=ALU.subtract),
                 r=[ps2.k, GK], w=[GK])
        P.op('act', lambda e: e.activation(out=tail[:], in_=tail[:], func=AF.Exp), r=[GK], w=[GK])
        P.op('act', lambda e: e.activation(out=ngam[:], in_=gcum[:], func=AF.Exp), r=[GK], w=[GK])
        P.op('dve', lambda e: e.tensor_scalar(out=ngam[:], in0=ngam[:], scalar1=-1.0, scalar2=None, op0=ALU.mult),
             r=[GK], w=[GK])

        DSTOP = os.environ.get('KDSTOP', '')
        if DSTOP == 'gates':
            P.barrier()
            P.end_scope()
            return
        qTh = P.sb("qTh", [128, T], BF16)
        kTh = P.sb("kTh", [128, T], BF16)
        zTh = P.sb("zTh", [128, T], BF16)
        ktk = P.sb("ktk", [128, NB, 128], BF16)
        vtk = P.sb("vtk", [128, NB, 128], BF16)
        oacc = P.sb("oacc", [128, T], F32)
        Tt = P.sb("Tt", [128, NB, 2, 128], BF16)
        AT = P.sb("AT", [128, NB, 2, 128], BF16)
        qdec = P.sb("qdec", [128, NB, 2, 128], BF16)
        ktail = P.sb("ktail", [128, NB, 2, 128], BF16)
        Ef = [P.sb("Ef%d" % i, [128, 2, 128], F32) for i in range(2)]
        Es = [P.sb("Es%d" % i, [128, 2, 128], F32) for i in range(2)]
        Ge = [P.sb("Ge%d" % i, [128, 2, 128], F32) for i in range(2)]
        t1 = [P.sb("t1_%d" % i, [128, 2, 128], F32) for i in range(2)]
        ABt = [P.sb("ABt%d" % i, [128, 2, 2, 128], BF16) for i in range(2)]
        Ttw = [P.sb("Ttw%d" % i, [128, 2, 128], BF16) for i in range(2)]
        Xf = P.sb("Xf", [128, 2, 128], BF16)
        XfT = P.sb("XfT", [128, 2, 128], BF16)
        UT = P.sb("UT", [128, 3, 2, 128], BF16)
        Hh = P.sb("Hh", [128, 2, 128], BF16)
        Wn = P.sb("Wn", [128, 2, 128], BF16)
        Sf = [P.sb("Sf%d" % i, [128, 128], F32) for i in range(2)]
        Sb = [P.sb("Sb%d" % i, [128, 128], BF16) for i in range(2)]
        Rt = [P.sb("Rt%d" % i, [128, 128], BF16) for i in range(2)]
        vn = [P.sb("vn%d" % i, [128, 128], BF16) for i in range(2)]
        sqo = [P.sb("sqo%d" % i, [128, 512], BF16) for i in range(2)]
        rno = P.sb("rno", [128, 512], F32)
        odf = P.sb("odf", [128, 512], F32)
        odst = [P.sb("odst%d" % i, [128, 512], BF16) for i in range(2)]
        tris = (triF, triB)
        negs = (negF, negB)
        strs = (strF, strB)
        uidx = 0
        for h in range(8):
            hs = slice(h * 128, (h + 1) * 128)
            HK = ('hd', 0)
            P.dma('sp', [(qTh[:], qT[hs, :]), (kTh[:], kT[hs, :]),
                         (ktk[:].rearrange("p b f -> p (b f)"), k_tok[h]),
                         (vtk[:].rearrange("p b f -> p (b f)"), v_tok[h]),
                         (zTh[:], zT[hs, :])], lane='hd', w=[HK])
            P.op('pool', lambda e: e.memset(oacc[:], 0.0), w=['oacc'])
            for n in range(NB if DSTOP != 'pre1' else 1):
                bs = slice(n * 128, (n + 1) * 128)
                u2 = uidx % 2
                uidx += 1
                pk = P.psum()
                P.op('pe', lambda e: e.matmul(pk.t[:, 0:128], kTh[:, bs], kTh[:, bs], start=True, stop=True),
                     r=[HK], w=[pk.k])
                P.op('pe', lambda e: e.matmul(pk.t[:, 128:256], kTh[:, bs], qTh[:, bs], start=True, stop=True),
                     r=[HK], w=[pk.k])
                for d in range(2):
                    col = d * 8 + h
                    P.op('pe', lambda e: e.matmul(pk.t[:, 256 + d * 128:384 + d * 128],
                                                  g[:, n, col:col + 1].to_broadcast([128, 128]), tris[d][:],
                                                  start=True, stop=True), r=[GK, CK], w=[pk.k])
                for d in range(2):
                    col = d * 8 + h
                    P.op('dve', lambda e: e.scalar_tensor_tensor(
                        out=t1[u2][:, d, :], in0=pk.t[:, 256 + d * 128:384 + d * 128], scalar=gcum[:, n, col:col + 1],
                        in1=negs[d][:], op0=ALU.subtract, op1=ALU.add), r=[pk.k, GK, CK], w=[('t1', u2)])
                P.op('act', lambda e: e.activation(out=Ef[u2][:], in_=t1[u2][:], func=AF.Exp),
                     r=[('t1', u2)], w=[('Ef', u2)])
                P.op('act', lambda e: e.activation(out=Ge[u2][:],
                                                   in_=pk.t[:, 256:512].rearrange("p (a b) -> p a b", b=128),
                                                   func=AF.Exp), r=[pk.k], w=[('Ge', u2)])
                for d in range(2):
                    P.op('dve', lambda e: e.tensor_tensor(out=Es[u2][:, d, :], in0=Ef[u2][:, d, :], in1=strs[d][:],
                                                           op=ALU.mult), r=[('Ef', u2), CK], w=[('Es', u2)])
                for d in range(2):
                    col = d * 8 + h
                    P.op('dve', lambda e: e.tensor_tensor(out=AT[:, n, d, :], in0=pk.t[:, 128:256], in1=Ef[u2][:, d, :],
                                                          op=ALU.mult), r=[pk.k, ('Ef', u2)], w=[('pre', n)])
                    P.op('dve', lambda e: e.scalar_tensor_tensor(
                        out=Xf[:, d, :], in0=pk.t[:, 0:128], scalar=nbeta[:, n, col:col + 1],
                        in1=Es[u2][:, d, :], op0=ALU.mult, op1=ALU.mult), r=[pk.k, GK, ('Es', u2)], w=['Xf'])
                    P.op('dve', lambda e: e.tensor_tensor(out=qdec[:, n, d, :], in0=qTh[:, bs], in1=Ge[u2][:, d, :],
                                                          op=ALU.mult), r=[HK, ('Ge', u2)], w=[('pre', n)])
                    P.op('act', lambda e: e.activation(out=ktail[:, n, d, :], in_=ktk[:, n, :], func=AF.Copy,
                                                       scale=tail[:, n, col:col + 1]), r=[HK, GK], w=[('pre', n)])
                P.op('dve', lambda e: e.tensor_tensor(out=ABt[0][:, :, 0, :], in0=Xf[:],
                                                      in1=bmask[:, 0:1, :].to_broadcast([128, 2, 128]), op=ALU.mult),
                     r=['Xf', 'par'], w=[('AB', 0)])
                pt = P.psum_t()
                for d in range(2):
                    P.op('pe', lambda e: e.transpose(pt.t[:, d * 128:(d + 1) * 128], ABt[0][:, d, 0, :], ident_bf[:]),
                         r=[('AB', 0), CK], w=[pt.k])
                    P.op('pe', lambda e: e.transpose(pt.t[:, 256 + d * 128:256 + (d + 1) * 128], Xf[:, d, :],
                                                     ident_bf[:]), r=['Xf', CK], w=[pt.k])
                P.op('act', lambda e: e.copy(out=ABt[0][:, :, 1, :],
                                             in_=pt.t[:, 0:256].rearrange("p (a b) -> p a b", b=128)),
                     r=[pt.k], w=[('AB', 0)])
                P.op('act', lambda e: e.copy(out=XfT[:], in_=pt.t[:, 256:512].rearrange("p (a b) -> p a b", b=128)),
                     r=[pt.k], w=['XfT'])
                P.op('dve', lambda e: e.tensor_tensor(out=Ttw[0][:], in0=ABt[0][:, :, 0, :],
                                                      in1=ident_bf[:].unsqueeze(1).to_broadcast([128, 2, 128]),
                                                      op=ALU.add), r=[('AB', 0), CK], w=[('Ttw', 0)])
                for b3 in range(3):
                    P.op('dve', lambda e: e.tensor_tensor(out=UT[:, b3], in0=XfT[:],
                                                          in1=bmask[:, 1 + b3:2 + b3, :].to_broadcast([128, 2, 128]),
                                                          op=ALU.mult), r=['XfT', 'par'], w=['UT'])
                gi_ = 0
                for lev in range(1, 4):
                    cur = lev % 2
                    prv = 1 - cur
                    pab = P.psum()
                    for d in range(2):
                        if lev < 3:
                            P.op('pe', lambda e: e.matmul(pab.t[:, d * 256:d * 256 + 128], ABt[prv][:, d, 1, :],
                                                          ABt[prv][:, d, 0, :], start=True, stop=True),
                                 r=[('AB', prv)], w=[pab.k])
                        P.op('pe', lambda e: e.matmul(pab.t[:, d * 256 + 128:d * 256 + 256], ABt[prv][:, d, 0, :],
                                                      ABt[prv][:, d, 1, :], start=True, stop=True),
                             r=[('AB', prv)], w=[pab.k])
                    if lev < 3:
                        P.op('act', lambda e: e.copy(out=ABt[cur][:],
                                                     in_=pab.t[:, :].rearrange("p (a b c) -> p a b c", a=2, b=2)),
                             r=[pab.k], w=[('AB', cur)])
                    else:
                        P.op('act', lambda e: e.copy(
                            out=ABt[cur][:, :, 1, :],
                            in_=pab.t[:, :].rearrange("p (a b c) -> p a b c", a=2, b=2)[:, :, 1, :]),
                            r=[pab.k], w=[('AB', cur)])
                    ptt = P.psum()
                    for d in range(2):
                        P.op('pe', lambda e: e.matmul(ptt.t[:, d * 128:(d + 1) * 128], ABt[cur][:, d, 1, :],
                                                      Ttw[gi_][:, d, :], start=True, stop=True),
                             r=[('AB', cur), ('Ttw', gi_)], w=[ptt.k])
                    P.op('dve', lambda e: e.tensor_tensor(
                        out=Ttw[1 - gi_][:], in0=ptt.t[:, 0:256].rearrange("p (a b) -> p a b", b=128),
                        in1=Ttw[gi_][:], op=ALU.add), r=[ptt.k, ('Ttw', gi_)], w=[('Ttw', 1 - gi_)])
                    gi_ = 1 - gi_
                for b3 in range(3):
                    G = Ttw[gi_]
                    pt2 = P.psum_t()
                    for d in range(2):
                        P.op('pe', lambda e: e.transpose(pt2.t[:, d * 128:(d + 1) * 128], G[:, d, :], ident_bf[:]),
                             r=[('Ttw', gi_), CK], w=[pt2.k])
                    P.op('act', lambda e: e.copy(out=Hh[:], in_=pt2.t[:, 0:256].rearrange("p (a b) -> p a b", b=128)),
                         r=[pt2.k], w=['Hh'])
                    pw2 = P.psum()
                    for d in range(2):
                        P.op('pe', lambda e: e.matmul(pw2.t[:, d * 128:(d + 1) * 128], UT[:, b3, d, :], G[:, d, :],
                                                      start=True, stop=True), r=['UT', ('Ttw', gi_)], w=[pw2.k])
                    P.op('act', lambda e: e.copy(out=Wn[:], in_=pw2.t[:, 0:256].rearrange("p (a b) -> p a b", b=128)),
                         r=[pw2.k], w=['Wn'])
                    pg2 = P.psum()
                    for d in range(2):
                        P.op('pe', lambda e: e.matmul(pg2.t[:, d * 128:(d + 1) * 128], Hh[:, d, :], Wn[:, d, :],
                                                      start=True, stop=True), r=['Hh', 'Wn'], w=[pg2.k])
                    if b3 < 2:
                        P.op('dve', lambda e: e.tensor_tensor(
                            out=Ttw[1 - gi_][:], in0=pg2.t[:, 0:256].rearrange("p (a b) -> p a b", b=128),
                            in1=G[:], op=ALU.add), r=[pg2.k, ('Ttw', gi_)], w=[('Ttw', 1 - gi_)])
                    else:
                        P.op('dve', lambda e: e.tensor_tensor(
                            out=Tt[:, n, :, :], in0=pg2.t[:, 0:256].rearrange("p (a b) -> p a b", b=128),
                            in1=G[:], op=ALU.add), r=[pg2.k, ('Ttw', gi_)], w=[('pre', n)])
                    gi_ = 1 - gi_
            if DSTOP.startswith('pre'):
                P.barrier()
                P.end_scope()
                return
            orders = [list(range(NB)), [1, 0] + list(range(NB - 1, 1, -1))]
            for d in range(2):
                P.op('pool', lambda e: e.memset(Sf[d][:], 0.0), w=[('S', d)])
                P.op('pool', lambda e: e.memset(Sb[d][:], 0.0), w=[('Sb', d)])
            for step in range(NB):
                for d in range(2):
                    n = orders[d][step]
                    col = d * 8 + h
                    bs = slice(n * 128, (n + 1) * 128)
                    need_o = not (last and n < 2)
                    pc = P.psum()
                    P.op('pe', lambda e: e.matmul(pc.t[:, 0:128], kTh[:, bs], Sb[d][:], start=True, stop=True),
                         r=[HK, ('Sb', d)], w=[pc.k])
                    P.op('dve', lambda e: e.scalar_tensor_tensor(
                        out=Rt[d][:], in0=pc.t[:, 0:128], scalar=ngam[:, n, col:col + 1], in1=vtk[:, n, :],
                        op0=ALU.mult, op1=ALU.add), r=[pc.k, GK, HK], w=[('R', d)])
                    P.op('pe', lambda e: e.matmul(pc.t[:, 128:256], Tt[:, n, d, :], Rt[d][:], start=True, stop=True),
                         r=[('pre', n), ('R', d)], w=[pc.k])
                    P.op('act', lambda e: e.activation(out=vn[d][:], in_=pc.t[:, 128:256], func=AF.Copy,
                                                       scale=beta[:, n, col:col + 1]), r=[pc.k, GK], w=[('vn', d)])
                    if need_o:
                        P.op('pe', lambda e: e.matmul(pc.t[:, 256:384], Sb[d][:], qdec[:, n, d, :],
                                                      start=True, stop=False), r=[('Sb', d), ('pre', n)], w=[pc.k])
                        P.op('pe', lambda e: e.matmul(pc.t[:, 256:384], vn[d][:], AT[:, n, d, :],
                                                      start=False, stop=True), r=[('vn', d), ('pre', n)], w=[pc.k])
                        P.op('dve', lambda e: e.tensor_tensor(out=oacc[:, bs], in0=pc.t[:, 256:384], in1=oacc[:, bs],
                                                              op=ALU.add), r=[pc.k, 'oacc'], w=['oacc'])
                    P.op('pe', lambda e: e.matmul(pc.t[:, 384:512], ktail[:, n, d, :], vn[d][:], start=True, stop=True),
                         r=[('pre', n), ('vn', d)], w=[pc.k])
                    P.op('dve', lambda e: e.scalar_tensor_tensor(
                        out=Sf[d][:], in0=Sf[d][:], scalar=egl[:, n, col:col + 1], in1=pc.t[:, 384:512],
                        op0=ALU.mult, op1=ALU.add), r=[pc.k, GK, ('S', d)], w=[('S', d)])
                    P.op('act', lambda e: e.copy(out=Sb[d][:], in_=Sf[d][:]), r=[('S', d)], w=[('Sb', d)])
            if DSTOP == 'chain':
                P.barrier()
                P.end_scope()
                return
            for ti, (t0, W, lat) in enumerate(TILES):
                if last and not lat:
                    continue
                i2 = ti % 2
                P.op('act', lambda e: e.activation(out=sqo[i2][:, :W], in_=oacc[:, t0:t0 + W], func=AF.Square),
                     r=['oacc'], w=[('sqo', i2)])
                ps = P.psum()
                P.op('pe', lambda e: e.matmul(ps.t[:, :W], ones_bf[:], sqo[i2][:, :W], start=True, stop=True),
                     r=[('sqo', i2), CK], w=[ps.k])
                P.op('act', lambda e: e.activation(out=rno[:, :W], in_=ps.t[:, :W], func=AF.Sqrt, scale=1.0 / 128,
                                                   bias=epsT[:, 0:1]), r=[ps.k, CK], w=['rno'])
                P.op('dve', lambda e: e.reciprocal(out=rno[:, :W], in_=rno[:, :W]), r=['rno'], w=['rno'])
                P.op('dve', lambda e: e.scalar_tensor_tensor(
                    out=odf[:, :W], in0=oacc[:, t0:t0 + W], scalar=onorm[:, l:l + 1], in1=rno[:, :W],
                    op0=ALU.mult, op1=ALU.mult), r=['oacc', 'rno', 'par'], w=['odf'])
                P.op('dve', lambda e: e.tensor_tensor(out=odst[i2][:, :W], in0=odf[:, :W], in1=zTh[:, t0:t0 + W],
                                                      op=ALU.mult), r=['odf', HK], w=[('odst', i2)])
                P.dma('sp', [(odT[hs, t0:t0 + W], odst[i2][:, :W])], lane='odst%d' % i2, r=[('odst', i2)], w=['odT'])
        P.barrier()
        P.end_scope()

    def phase_merge(l, xsrc):
        P.scope()
        last = (l == L - 1)
        wfc = P.sb("wfc", [128, 8, D], BF16)
        wdl = P.sb("wdl", [128, 8, D], BF16)
        wo = P.sb("wo", [128, 8, D], BF16)
        wr = P.sb("wr", [128, 8, 36], F32)
        P.scope()
        wfr = P.sb("wfr", [128, 4, D], BF16)
        P.dma('pool', [(wfr[:], w_fourier[l].rearrange("(g p) n -> p g n", p=128)),
                       (wdl[:], w_delta[l].rearrange("(kc p) n -> p kc n", p=128)),
                       (wo[:], w_out[l].rearrange("(kc p) n -> p kc n", p=128))], lane='mw', w=['mw'])
        P.dma('sp', [(wr[:], w_rt[l].rearrange("(kc p) n -> p kc n", p=128))], lane='wr', w=['wr'])
        for gi in range(4):
            for (j, mat) in ((0, chc), (1, chs)):
                for hf in range(2):
                    ps = P.psum()
                    P.op('pe', lambda e: e.matmul(ps.t[:, :512], mat[:], wfr[:, gi, hf * 512:(hf + 1) * 512],
                                                  start=True, stop=True), r=['mw', 'par'], w=[ps.k])
                    P.op('act', lambda e: e.copy(out=wfc[:, gi * 2 + j, hf * 512:(hf + 1) * 512], in_=ps.t[:, :512]),
                         r=[ps.k], w=['wfc'])
        P.barrier()
        P.end_scope()
        u_l = P.sb("u_l", [128, 32, 512], BF16)
        u_c = P.sb("u_c", [128, 2, 512], BF16)
        P.dma('sp', [(u_l[:], u_tok[:, 2:NB, :])], lane='ul', w=['u_l'])
        if not last:
            P.dma('sp', [(u_c[:], u_tok[:, 0:2, :])], lane='uc', w=['u_c'])
        NDB = 3
        dbuf = {'c': [P.sb("dcb%d" % i, [128, 4, 512], BF16) for i in range(NDB)],
                's': [P.sb("dsb%d" % i, [128, 4, 512], BF16) for i in range(NDB)]}
        dctr = 0
        PQ = P.sb("PQ", [128, 8, 512], BF16)
        odt = P.sb("odt", [128, 8, 512], BF16)
        gtt = P.sb("gtt", [128, 16, 512], BF16)
        mg = P.sb("mg", [128, 8, 512], BF16)
        m1 = P.sb("m1", [128, 512], F32)
        m2 = P.sb("m2", [128, 512], F32)
        xt = P.sb("xt", [128, 8, 512], F32)
        h2f = xt
        h2b = P.sb("h2b", [128, 8, 512], BF16)
        tl = {'sq': [P.sb("sq%d" % i, [128, 512], BF16) for i in range(2)],
              'tmp': [P.sb("tmp%d" % i, [128, 512], F32) for i in range(2)],
              'rstd': P.sb("rstd", [128, 512], F32)}
        lg = P.sb("lg", [128, 36], F32)
        rt = {k: P.sb("rt_" + k, [128, 32], F32) for k in ('lem', 'tmp')}
        rs = {k: P.sb("rs_" + k, [128, 4], F32) for k in ('mx', 'ohg', 'ex', 'pg', 'm1', 'm2', 'w1', 'w2', 'ohm')}
        rst = P.sb("rst", [128, 4, 66], F32)
        h2tk = P.sb("h2tk", [128, 4, D], BF16)
        xv = xsrc.rearrange("(kc p) t -> p kc t", p=128)
        xsv = xs.rearrange("(kc p) t -> p kc t", p=128)
        for ti, (t0, W, lat) in enumerate(TILES):
            if last and not lat:
                continue
            ws = 0 if lat else 1
            ntc = 32 if lat else 2
            uu = u_l if lat else u_c
            ukey = 'u_l' if lat else 'u_c'
            f0 = t0 - CTX if lat else 0
            mats = {'c': dftc if lat else dftc_c, 's': dfts if lat else dfts_c}
            sw = 4 if lat else 2
            nsub = ntc // sw
            P.dma('sp', [(odt[:, :, :W], odT.rearrange("(c p) t -> p c t", p=128)[:, :, t0:t0 + W]),
                         (gtt[:, :, :W], gT.rearrange("(c p) t -> p c t", p=128)[:, :, t0:t0 + W]),
                         (xt[:, :, :W], xv[:, :, t0:t0 + W])], lane='mt', w=['mt'])
            for gh in range(2):
                accs = [[P.psum(), P.psum()] for _ in range(2)]
                for sb_ in range(nsub):
                    bi = dctr % NDB
                    dctr += 1
                    for cs in ('c', 's'):
                        P.dma('sp', [(dbuf[cs][bi][:, :sw, :W],
                                      mats[cs][sb_ * sw * 128:(sb_ + 1) * sw * 128, f0:f0 + W].rearrange(
                                          "(b p) f -> p b f", p=128))],
                              lane='d%sb%d' % (cs, bi), w=[('d' + cs, bi)])
                    for t4 in range(sw):
                        tc = sb_ * sw + t4
                        for gl in range(2):
                            gi = gh * 2 + gl
                            for j, cs in enumerate(('c', 's')):
                                P.op('pe', lambda e: e.matmul(accs[gl][j].t[:, :W], uu[:, tc, gi * 128:(gi + 1) * 128],
                                                              dbuf[cs][bi][:, t4, :W], start=(tc == 0),
                                                              stop=(tc == ntc - 1)),
                                     r=[ukey, ('d' + cs, bi)], w=[accs[gl][j].k])
                for gl in range(2):
                    gi = gh * 2 + gl
                    for j in range(2):
                        P.op('act', lambda e: e.copy(out=PQ[:, gi * 2 + j, :W], in_=accs[gl][j].t[:, :W]),
                             r=[accs[gl][j].k], w=['PQ'])
            for dc in range(8):
                pa = P.psum()
                for k8 in range(8):
                    P.op('pe', lambda e: e.matmul(pa.t[:, :W], wfc[:, k8, dc * 128:(dc + 1) * 128], PQ[:, k8, :W],
                                                  start=(k8 == 0), stop=(k8 == 7)), r=['wfc', 'PQ'], w=[pa.k])
                pb = P.psum()
                for kc in range(8):
                    P.op('pe', lambda e: e.matmul(pb.t[:, :W], wdl[:, kc, dc * 128:(dc + 1) * 128], odt[:, kc, :W],
                                                  start=(kc == 0), stop=(kc == 7)), r=['mw', 'mt'], w=[pb.k])
                P.op('dve', lambda e: e.tensor_tensor(out=m1[:, :W], in0=pa.t[:, :W], in1=gtt[:, dc, :W], op=ALU.mult),
                     r=[pa.k, 'mt'], w=['m1'])
                P.op('dve', lambda e: e.tensor_tensor(out=m2[:, :W], in0=pb.t[:, :W], in1=gtt[:, 8 + dc, :W],
                                                      op=ALU.mult), r=[pb.k, 'mt'], w=['m2'])
                P.op('dve', lambda e: e.tensor_tensor(out=mg[:, dc, :W], in0=m1[:, :W], in1=m2[:, :W], op=ALU.add),
                     r=['m1', 'm2'], w=['mg'])
            for dc in range(8):
                po = P.psum()
                for kc in range(8):
                    P.op('pe', lambda e: e.matmul(po.t[:, :W], wo[:, kc, dc * 128:(dc + 1) * 128], mg[:, kc, :W],
                                                  start=(kc == 0), stop=(kc == 7)), r=['mw', 'mg'], w=[po.k])
                P.op('dve', lambda e: e.scalar_tensor_tensor(
                    out=xt[:, dc, :W], in0=po.t[:, :W], scalar=modv[:, l, 16 + dc, ws:ws + 1], in1=xt[:, dc, :W],
                    op0=ALU.mult, op1=ALU.add), r=[po.k, 'mod', 'mt'], w=['mt'])
            P.dma('sp', [(xsv[:, :, t0:t0 + W], xt[:, :, :W])], lane='xo', r=['mt'], w=['xs'])
            if os.environ.get('KTRACE') and ti < 2:
                print('merge tile', ti, 'pre-norm2', P.ninst)
            norm_mod(xt, 'mt', W, A2[:, l, :, ws], modv[:, l, 24:32, ws],
                     [(lambda kc: xt[:, kc, :W], 'mt'), (lambda kc: h2b[:, kc, :W], 'h2b')], tl)
            if os.environ.get('KTRACE') and ti < 2:
                print('merge tile', ti, 'pre-h2tk', P.ninst)
            nbk = W // 128
            blk0 = t0 // 128
            for tb in range(nbk):
                for hf in range(2):
                    pt = P.psum_t()
                    for j in range(4):
                        kc = hf * 4 + j
                        P.op('pe', lambda e: e.transpose(pt.t[:, j * 128:(j + 1) * 128],
                                                         h2b[:, kc, tb * 128:(tb + 1) * 128], ident_bf[:]),
                             r=['h2b', CK], w=[pt.k])
                    P.op('act', lambda e: e.copy(out=h2tk[:, tb, hf * 512:(hf + 1) * 512], in_=pt.t[:, 0:512]),
                         r=[pt.k], w=['h2tk'])
            P.dma('sp', [(h2tok_d[:, blk0:blk0 + nbk, :], h2tk[:, :nbk, :])], lane='h2o', r=['h2tk'], w=['h2tok'])
            if os.environ.get('KTRACE') and ti < 2:
                print('merge tile', ti, 'pre-routing', P.ninst)
            for tb in range(W // 128):
                ps = P.psum()
                for kc in range(8):
                    P.op('pe', lambda e: e.matmul(ps.t[:, :36], h2f[:, kc, tb * 128:(tb + 1) * 128], wr[:, kc, :],
                                                  start=(kc == 0), stop=(kc == 7)), r=['mt', 'wr'], w=[ps.k])
                RK = 'rt'

                def dv(fn, r=(), w=(RK,)):
                    P.op('dve', fn, r=list(r) + [RK], w=w)
                P.op('dve', lambda e: e.tensor_tensor(out=lg[:], in0=ps.t[:, :36], in1=brt[:, l, :], op=ALU.add),
                     r=[ps.k, 'par'], w=[RK])
                dv(lambda e: e.reduce_max(out=rs['mx'][:, 0:1], in_=lg[:, 0:4], axis=mybir.AxisListType.X))
                dv(lambda e: e.tensor_scalar(out=rs['ohg'][:], in0=lg[:, 0:4], scalar1=rs['mx'][:, 0:1], scalar2=None,
                                             op0=ALU.is_ge))
                dv(lambda e: e.tensor_scalar(out=rs['ex'][:], in0=lg[:, 0:4], scalar1=rs['mx'][:, 0:1], scalar2=None,
                                             op0=ALU.subtract))
                P.op('act', lambda e: e.activation(out=rs['ex'][:], in_=rs['ex'][:], func=AF.Exp), r=[RK], w=[RK])
                dv(lambda e: e.reduce_sum(out=rs['pg'][:, 0:1], in_=rs['ex'][:], axis=mybir.AxisListType.X))
                dv(lambda e: e.reciprocal(out=rs['pg'][:, 0:1], in_=rs['pg'][:, 0:1]))
                dv(lambda e: e.tensor_scalar(out=rs['ohm'][:], in0=rs['ohg'][:], scalar1=-1.0, scalar2=1.0e9,
                                             op0=ALU.add, op1=ALU.mult))
                dv(lambda e: e.tensor_tensor(out=rt['lem'][:].rearrange("p (g e) -> p g e", e=8),
                                             in0=lg[:, 4:36].rearrange("p (g e) -> p g e", e=8),
                                             in1=rs['ohm'][:].unsqueeze(2).to_broadcast([128, 4, 8]), op=ALU.add))
                dv(lambda e: e.reduce_max(out=rs['m1'][:, 0:1], in_=rt['lem'][:], axis=mybir.AxisListType.X))
                oh1 = rst[:, tb, 0:32]
                oh2 = rst[:, tb, 32:64]
                w1 = rst[:, tb, 64:65]
                w2 = rst[:, tb, 65:66]
                dv(lambda e: e.tensor_scalar(out=oh1, in0=rt['lem'][:], scalar1=rs['m1'][:, 0:1], scalar2=None,
                                             op0=ALU.is_ge), w=(RK, 'rst'))
                dv(lambda e: e.scalar_tensor_tensor(out=rt['tmp'][:], in0=oh1, scalar=-1.0e9, in1=rt['lem'][:],
                                                    op0=ALU.mult, op1=ALU.add), r=['rst'])
                dv(lambda e: e.reduce_max(out=rs['m2'][:, 0:1], in_=rt['tmp'][:], axis=mybir.AxisListType.X))
                dv(lambda e: e.tensor_scalar(out=oh2, in0=rt['tmp'][:], scalar1=rs['m2'][:, 0:1], scalar2=None,
                                             op0=ALU.is_ge), w=(RK, 'rst'))
                dv(lambda e: e.tensor_tensor(out=rs['w1'][:, 0:1], in0=rs['m1'][:, 0:1], in1=rs['m2'][:, 0:1],
                                             op=ALU.subtract))
                P.op('act', lambda e: e.activation(out=rs['w1'][:, 0:1], in_=rs['w1'][:, 0:1], func=AF.Sigmoid),
                     r=[RK], w=[RK])
                dv(lambda e: e.tensor_scalar(out=rs['w2'][:, 0:1], in0=rs['w1'][:, 0:1], scalar1=-1.0, scalar2=1.0,
                                             op0=ALU.mult, op1=ALU.add))
                dv(lambda e: e.tensor_tensor(out=w1, in0=rs['w1'][:, 0:1], in1=rs['pg'][:, 0:1], op=ALU.mult),
                   w=(RK, 'rst'))
                dv(lambda e: e.tensor_tensor(out=w2, in0=rs['w2'][:, 0:1], in1=rs['pg'][:, 0:1], op=ALU.mult),
                   w=(RK, 'rst'))
            P.dma('sp', [(rt_d[:, blk0:blk0 + nbk, :], rst[:, :nbk, :])], lane='wto', r=['rst'], w=['rt_d'])
        P.barrier()
        P.end_scope()

    def phase_moe(l):
        I32 = mybir.dt.int32
        last = (l == L - 1)
        b0 = 2 if last else 0
        nbl = NB - b0
        nblk = 48 if last else NBLK
        xsv = xs.rearrange("(kc p) t -> p kc t", p=128)
        wg_rows = w_gate.rearrange("l e k n -> (l e k) n")
        wu_rows = w_up.rearrange("l e k n -> (l e k) n")
        wd_rows = w_down.rearrange("l e k n -> (l e k) n")
        P.scope()
        RT = P.sb("RT", [128, nbl, 66], F32)
        P.dma('sp', [(RT[:], rt_d[:, b0:NB, :])], lane='hb', w=['RT'])
        IND = P.sb("IND", [128, nbl, 32], F32)
        RANK = P.sb("RANK", [128, nbl, 32], F32)
        carry = P.sb("carry", [128, 32], F32)
        SK = 'moeix'
        P.op('dve', lambda e: e.tensor_tensor(out=IND[:], in0=RT[:, :, 0:32], in1=RT[:, :, 32:64], op=ALU.add),
             r=['RT'], w=[SK])
        P.op('dve', lambda e: e.tensor_copy(out=carry[:], in_=zeros_f[:, 0:32]), r=[CK], w=['carry'])
        for c in range(nbl):
            ps = P.psum()
            P.op('pe', lambda e: e.matmul(ps.t[:, 0:32], strF[:], IND[:, c, :], start=True, stop=True),
                 r=[SK, CK], w=[ps.k])
            P.op('pe', lambda e: e.matmul(ps.t[:, 32:64], ones_f[:], IND[:, c, :], start=True, stop=True),
                 r=[SK, CK], w=[ps.k])
            P.op('dve', lambda e: e.tensor_tensor(out=RANK[:, c, :], in0=ps.t[:, 0:32], in1=carry[:], op=ALU.add),
                 r=[ps.k, 'carry'], w=['RANK'])
            P.op('dve', lambda e: e.tensor_tensor(out=carry[:], in0=ps.t[:, 32:64], in1=carry[:], op=ALU.add),
                 r=[ps.k, 'carry'], w=['carry'])
        cmp9 = P.sb("cmp9", [128, 32, 9], F32)
        thr = P.sb("thr", [128, 9], F32)
        nbe = P.sb("nbe", [128, 32], F32)
        cs = [P.sb("cs%d" % i, [128, 32], F32) for i in range(2)]
        P.op('dve', lambda e: e.tensor_scalar(out=thr[:], in0=iota[:, 0:9], scalar1=512.0, scalar2=None, op0=ALU.mult),
             r=['par'], w=['thr'])
        P.op('dve', lambda e: e.tensor_tensor(out=cmp9[:], in0=carry[:].unsqueeze(2).to_broadcast([128, 32, 9]),
                                              in1=thr[:].unsqueeze(1).to_broadcast([128, 32, 9]), op=ALU.is_gt),
             r=['carry', 'thr'], w=['cmp9'])
        P.op('dve', lambda e: e.reduce_sum(out=nbe[:], in_=cmp9[:], axis=mybir.AxisListType.X), r=['cmp9'], w=['nbe'])
        P.op('dve', lambda e: e.tensor_copy(out=cs[0][:], in_=nbe[:]), r=['nbe'], w=[('cs', 0)])
        ci = 0
        for sh in (1, 2, 4, 8, 16):
            P.op('dve', lambda e: e.tensor_copy(out=cs[1 - ci][:, 0:sh], in_=cs[ci][:, 0:sh]),
                 r=[('cs', ci)], w=[('cs', 1 - ci)])
            P.op('dve', lambda e: e.tensor_tensor(out=cs[1 - ci][:, sh:32], in0=cs[ci][:, sh:32], in1=cs[ci][:, 0:32 - sh],
                                                  op=ALU.add), r=[('cs', ci)], w=[('cs', 1 - ci)])
            ci = 1 - ci
        bend = cs[ci]
        sbase = P.sb("sbase", [128, 32], F32)
        P.op('dve', lambda e: e.tensor_tensor(out=sbase[:], in0=bend[:], in1=nbe[:], op=ALU.subtract),
             r=[('cs', ci), 'nbe'], w=['sbase'])
        P.op('dve', lambda e: e.tensor_scalar(out=sbase[:], in0=sbase[:], scalar1=512.0, scalar2=None, op0=ALU.mult),
             r=['sbase'], w=['sbase'])
        P.op('dve', lambda e: e.tensor_tensor(out=RANK[:], in0=RANK[:],
                                              in1=sbase[:].unsqueeze(1).to_broadcast([128, nbl, 32]), op=ALU.add),
             r=['RANK', 'sbase'], w=['RANK'])
        slf = P.sb("slf", [128, nbl, 2], F32)
        sli = P.sb("sli", [128, nbl, 2], I32)
        for k in range(2):
            P.op('dve', lambda e: e.tensor_tensor(out=IND[:], in0=RANK[:], in1=RT[:, :, k * 32:(k + 1) * 32],
                                                  op=ALU.mult), r=['RANK', 'RT', SK], w=[SK])
            P.op('dve', lambda e: e.reduce_sum(out=slf[:, :, k], in_=IND[:], axis=mybir.AxisListType.X),
                 r=[SK], w=['slf'])
        P.op('dve', lambda e: e.tensor_copy(out=sli[:], in_=slf[:]), r=['slf'], w=['sli'])
        cmpb = P.sb("cmpb", [128, NBLK, 32], F32)
        ebf = P.sb("ebf", [128, NBLK], F32)
        P.op('dve', lambda e: e.tensor_tensor(out=cmpb[:], in0=bend[:].unsqueeze(1).to_broadcast([128, NBLK, 32]),
                                              in1=iota[:, 0:NBLK].unsqueeze(2).to_broadcast([128, NBLK, 32]),
                                              op=ALU.is_le), r=[('cs', ci), 'par'], w=['cmpb'])
        P.op('dve', lambda e: e.reduce_sum(out=ebf[:], in_=cmpb[:], axis=mybir.AxisListType.X), r=['cmpb'], w=['ebf'])
        P.op('dve', lambda e: e.tensor_scalar(out=ebf[:], in0=ebf[:], scalar1=31.0, scalar2=float(l * NE),
                                              op0=ALU.min, op1=ALU.add), r=['ebf'], w=['ebf'])
        wixf = P.sb("wixf", [128, NBLK, 12], F32)
        wix = P.sb("wix", [128, NBLK, 12], I32)
        P.op('dve', lambda e: e.scalar_tensor_tensor(
            out=wixf[:, :, 0:8], in0=ebf[:].unsqueeze(2).to_broadcast([128, NBLK, 8]), scalar=1024.0,
            in1=wbase[:].unsqueeze(1).to_broadcast([128, NBLK, 8]), op0=ALU.mult, op1=ALU.add),
            r=['ebf', 'par'], w=['wixf'])
        P.op('dve', lambda e: e.scalar_tensor_tensor(
            out=wixf[:, :, 8:12], in0=ebf[:].unsqueeze(2).to_broadcast([128, NBLK, 4]), scalar=512.0,
            in1=wbase[:, 0:4].unsqueeze(1).to_broadcast([128, NBLK, 4]), op0=ALU.mult, op1=ALU.add),
            r=['ebf', 'par'], w=['wixf'])
        P.op('dve', lambda e: e.tensor_copy(out=wix[:], in_=wixf[:]), r=['wixf'], w=['wix'])

        if os.environ.get('KTRACE'):
            print('moe ix done', P.ninst)
        P.scope()
        hk = [P.sb("hk%d" % i, [128, 4, D], BF16) for i in range(2)]
        gi = 0
        for c0 in range(0, nbl, 4):
            nn = min(4, nbl - c0)
            b2 = gi % 2
            gi += 1
            P.dma('sp', [(hk[b2][:, :nn, :], h2tok_d[:, b0 + c0:b0 + c0 + nn, :])], lane='hk%d' % b2, w=[('hk', b2)])
            for j in range(nn):
                for k in range(2):
                    P.dma('pool', [lambda h: h.indirect_dma_start(
                        out=xs_d[:, :], out_offset=bass.IndirectOffsetOnAxis(ap=sli[:, c0 + j, k:k + 1], axis=0),
                        in_=hk[b2][:, j, :], in_offset=None)], lane='sc%d' % ((j * 2 + k) % 4),
                        r=[('hk', b2), 'sli'], w=['xs_d'])
        P.barrier()
        P.end_scope()

        if os.environ.get('KTRACE'):
            print('moe dispatch done', P.ninst)
        P.scope()
        wg = [P.sb("wg%d" % i, [128, 8, 512], BF16) for i in range(2)]
        wu = [P.sb("wu%d" % i, [128, 8, 512], BF16) for i in range(2)]
        wd = [P.sb("wd%d" % i, [128, 4, D], BF16) for i in range(2)]
        xsb = [P.sb("xsb%d" % i, [128, 4, D], BF16) for i in range(2)]
        xsT = P.sb("xsT", [128, 8, 512], BF16)
        sg = [P.sb("sg%d" % i, [128, 512], F32) for i in range(2)]
        h1 = P.sb("h1", [128, 4, 512], BF16)
        ysb = [P.sb("ysb%d" % i, [128, 4, D], BF16) for i in range(2)]
        for b in range(nblk):
            if os.environ.get('KTRACE') and b < 2:
                print('moe block', b, P.ninst)
            bi = b % 2
            WK = ('ew', bi)
            prs = []
            for kc in range(8):
                prs.append((lambda kc: lambda h: h.indirect_dma_start(
                    out=wg[bi][:, kc, :], out_offset=None, in_=wg_rows,
                    in_offset=bass.IndirectOffsetOnAxis(ap=wix[:, b, kc:kc + 1], axis=0)))(kc))
                prs.append((lambda kc: lambda h: h.indirect_dma_start(
                    out=wu[bi][:, kc, :], out_offset=None, in_=wu_rows,
                    in_offset=bass.IndirectOffsetOnAxis(ap=wix[:, b, kc:kc + 1], axis=0)))(kc))
            for fc in range(4):
                prs.append((lambda fc: lambda h: h.indirect_dma_start(
                    out=wd[bi][:, fc, :], out_offset=None, in_=wd_rows,
                    in_offset=bass.IndirectOffsetOnAxis(ap=wix[:, b, 8 + fc:9 + fc], axis=0)))(fc))
            P.dma('pool', prs, lane='ew%d' % bi, r=['wix'], w=[WK])
            P.dma('sp', [(xsb[bi][:], xs_d[b * 512:(b + 1) * 512, :].rearrange("(s p) f -> p s f", p=128))],
                  lane='xsb%d' % bi, r=['xs_d'], w=[('xsb', bi)])
            for kc in range(8):
                pt = P.psum_t()
                for sgi in range(4):
                    P.op('pe', lambda e: e.transpose(pt.t[:, sgi * 128:(sgi + 1) * 128],
                                                     xsb[bi][:, sgi, kc * 128:(kc + 1) * 128], ident_bf[:]),
                         r=[('xsb', bi), CK], w=[pt.k])
                if kc % 2 == 0:
                    P.op('act', lambda e: e.copy(out=xsT[:, kc, :], in_=pt.t[:, 0:512]), r=[pt.k], w=['xsT'])
                else:
                    P.op('dve', lambda e: e.tensor_copy(out=xsT[:, kc, :], in_=pt.t[:, 0:512]), r=[pt.k], w=['xsT'])
            for fc in range(4):
                pg = P.psum()
                pu = P.psum()
                for kc in range(8):
                    P.op('pe', lambda e: e.matmul(pg.t[:, :], wg[bi][:, kc, fc * 128:(fc + 1) * 128], xsT[:, kc, :],
                                                  start=(kc == 0), stop=(kc == 7)), r=[WK, 'xsT'], w=[pg.k])
                for kc in range(8):
                    P.op('pe', lambda e: e.matmul(pu.t[:, :], wu[bi][:, kc, fc * 128:(fc + 1) * 128], xsT[:, kc, :],
                                                  start=(kc == 0), stop=(kc == 7)), r=[WK, 'xsT'], w=[pu.k])
                f2 = fc % 2
                P.op('act', lambda e: e.activation(out=sg[f2][:], in_=pg.t[:, :], func=AF.Silu),
                     r=[pg.k], w=[('sg', f2)])
                P.op('dve', lambda e: e.tensor_tensor(out=h1[:, fc, :], in0=sg[f2][:], in1=pu.t[:, :], op=ALU.mult),
                     r=[('sg', f2), pu.k], w=['h1'])
            for sgi in range(4):
                for hf in range(2):
                    py = P.psum()
                    for fc in range(4):
                        P.op('pe', lambda e: e.matmul(py.t[:, :], h1[:, fc, sgi * 128:(sgi + 1) * 128],
                                                      wd[bi][:, fc, hf * 512:(hf + 1) * 512],
                                                      start=(fc == 0), stop=(fc == 3)), r=['h1', WK], w=[py.k])
                    if hf == 0:
                        P.op('act', lambda e: e.copy(out=ysb[bi][:, sgi, 0:512], in_=py.t[:, :]),
                             r=[py.k], w=[('ysb', bi)])
                    else:
                        P.op('dve', lambda e: e.tensor_copy(out=ysb[bi][:, sgi, 512:1024], in_=py.t[:, :]),
                             r=[py.k], w=[('ysb', bi)])
            P.dma('sp', [(ys_d[b * 512:(b + 1) * 512, :].rearrange("(s p) f -> p s f", p=128), ysb[bi][:])],
                  lane='ysb%d' % bi, r=[('ysb', bi)], w=['ys_d'])
        P.barrier()
        P.end_scope()

        if os.environ.get('KTRACE'):
            print('moe blocks done', P.ninst)
        P.scope()
        y1 = [P.sb("y1_%d" % i, [128, 4, D], BF16) for i in range(2)]
        y2 = [P.sb("y2_%d" % i, [128, 4, D], BF16) for i in range(2)]
        yc = P.sb("yc", [128, 4, D], BF16)
        ytmp = P.sb("ytmp", [128, D], F32)
        xt = P.sb("xt", [128, 8, 512], F32)
        tl = None
        if last:
            tl = {'sq': [P.sb("sq%d" % i, [128, 512], BF16) for i in range(2)],
                  'rstd': P.sb("rstd", [128, 512], F32)}
            ot = P.sb("ot", [128, 8, 512], F32)
        gi = 0
        for (t0, W, lat) in TILES:
            if last and not lat:
                continue
            ws = 0 if lat else 1
            nbk = W // 128
            c0 = t0 // 128 - b0
            g2 = gi % 2
            gi += 1
            P.dma('sp', [(xt[:, :, :W], xsv[:, :, t0:t0 + W])], lane='xm', r=['xs'], w=['xm'])
            prs = []
            for j in range(nbk):
                prs.append((lambda j: lambda h: h.indirect_dma_start(
                    out=y1[g2][:, j, :], out_offset=None, in_=ys_d[:, :],
                    in_offset=bass.IndirectOffsetOnAxis(ap=sli[:, c0 + j, 0:1], axis=0)))(j))
                prs.append((lambda j: lambda h: h.indirect_dma_start(
                    out=y2[g2][:, j, :], out_offset=None, in_=ys_d[:, :],
                    in_offset=bass.IndirectOffsetOnAxis(ap=sli[:, c0 + j, 1:2], axis=0)))(j))
            P.dma('pool', prs, lane='yg%d' % g2, r=['sli', 'ys_d'], w=[('yg', g2)])
            for j in range(nbk):
                P.op('dve', lambda e: e.tensor_scalar(out=ytmp[:], in0=y1[g2][:, j, :],
                                                      scalar1=RT[:, c0 + j, 64:65], scalar2=None, op0=ALU.mult),
                     r=[('yg', g2), 'RT'], w=['ytmp'])
                P.op('dve', lambda e: e.scalar_tensor_tensor(out=yc[:, j, :], in0=y2[g2][:, j, :],
                                                             scalar=RT[:, c0 + j, 65:66], in1=ytmp[:],
                                                             op0=ALU.mult, op1=ALU.add),
                     r=[('yg', g2), 'RT', 'ytmp'], w=['yc'])
            for kc in range(8):
                pt = P.psum_t()
                for j in range(nbk):
                    P.op('pe', lambda e: e.transpose(pt.t[:, j * 128:(j + 1) * 128], yc[:, j, kc * 128:(kc + 1) * 128],
                                                     ident_bf[:]), r=['yc', CK], w=[pt.k])
                P.op('dve', lambda e: e.scalar_tensor_tensor(
                    out=xt[:, kc, :W], in0=pt.t[:, :W], scalar=modv[:, l, 40 + kc, ws:ws + 1], in1=xt[:, kc, :W],
                    op0=ALU.mult, op1=ALU.add), r=[pt.k, 'mod', 'xm'], w=['xm'])
            if not last:
                P.dma('sp', [(xsv[:, :, t0:t0 + W], xt[:, :, :W])], lane='xmo', r=['xm'], w=['xs'])
            else:
                ss = P.psum()
                for kc in range(8):
                    sq = tl['sq'][kc % 2]
                    P.op('act', lambda e: e.activation(out=sq[:, :W], in_=xt[:, kc, :W], func=AF.Square),
                         r=['xm'], w=[('sq', kc % 2)])
                    P.op('pe', lambda e: e.matmul(ss.t[:, :W], ones_bf[:], sq[:, :W], start=(kc == 0),
                                                  stop=(kc == 7)), r=[('sq', kc % 2), CK], w=[ss.k])
                rstd = tl['rstd']
                P.op('act', lambda e: e.activation(out=rstd[:, :W], in_=ss.t[:, :W], func=AF.Sqrt, scale=1.0 / D,
                                                   bias=epsT[:, 0:1]), r=[ss.k, CK], w=['rstd'])
                P.op('dve', lambda e: e.reciprocal(out=rstd[:, :W], in_=rstd[:, :W]), r=['rstd'], w=['rstd'])
                for kc in range(8):
                    P.op('dve', lambda e: e.scalar_tensor_tensor(
                        out=ot[:, kc, :W], in0=xt[:, kc, :W], scalar=fnorm[:, kc:kc + 1], in1=rstd[:, :W],
                        op0=ALU.mult, op1=ALU.mult), r=['xm', 'rstd', 'par'], w=['ot'])
                P.dma('sp', [(outT.rearrange("(c p) t -> p c t", p=128)[:, :, t0 - CTX:t0 - CTX + W], ot[:, :, :W])],
                      lane='oo', r=['ot'], w=['outT'])
        P.barrier()
        P.end_scope()
        P.end_scope()

    with nc.named_scope('mod'):
        phase_mod()
    done = False
    for l in range(nl):
        xsrc = xT_in if l == 0 else xs
        for (nm, fn) in (('proj', lambda: phase_proj(l, xsrc)), ('delta', lambda: phase_delta(l)),
                         ('merge', lambda: phase_merge(l, xsrc)), ('moe', lambda: phase_moe(l))):
            with nc.named_scope('%s%d' % (nm, l)):
                fn()
            if stop == (l, nm):
                done = True
                break
        if done:
            break
    P.barrier()
    print("bass program: ninst=%d nwait=%d lanes=%d" % (P.ninst, P.nwait, len(P.lanes)))
    return nc


def _fm(v):
    return np.ascontiguousarray(np.asarray(v).reshape(-1, 128).T)


def prep_shared(inp):
    f32 = np.float32
    sh = {}
    b_mod = np.asarray(inp['b_mod'], f32)
    sh['bmod'] = np.ascontiguousarray(b_mod.reshape(L, 48, 128).transpose(2, 0, 1))
    sh['nmix'] = np.ascontiguousarray(np.asarray(inp['norm_mix'], f32).reshape(L, 8, 128).transpose(2, 0, 1))
    sh['nffn'] = np.ascontiguousarray(np.asarray(inp['norm_ffn'], f32).reshape(L, 8, 128).transpose(2, 0, 1))
    sh['fnorm'] = _fm(np.asarray(inp['final_norm'], f32))
    sh['convw'] = np.ascontiguousarray(np.asarray(inp['conv_w'], f32).reshape(L, 3, 24, 128).transpose(3, 0, 1, 2))
    sh['alog'] = np.ascontiguousarray(np.broadcast_to(np.asarray(inp['a_log'], f32).reshape(1, L, 16), (128, L, 16)))
    sh['dtb'] = np.ascontiguousarray(np.broadcast_to(np.asarray(inp['dt_bias'], f32).reshape(1, L, 16), (128, L, 16)))
    sh['onorm'] = np.ascontiguousarray(np.asarray(inp['out_norm'], f32).T)
    brt = np.concatenate([np.asarray(inp['b_route_group'], f32), np.asarray(inp['b_route_expert'], f32)], axis=1)
    sh['brt'] = np.ascontiguousarray(np.broadcast_to(brt.reshape(1, L, 36), (128, L, 36)))
    sh['w_rt'] = np.ascontiguousarray(np.concatenate([np.asarray(inp['w_route_group'], f32),
                                                      np.asarray(inp['w_route_expert'], f32)], axis=2))
    for k in ('w_mod', 'w_in', 'w_fourier', 'w_delta', 'w_out', 'w_gate', 'w_up', 'w_down'):
        sh[k] = np.ascontiguousarray(np.asarray(inp[k], f32))
    bf = ml_dtypes.bfloat16

    def tab(n, scale):
        k = np.arange(n, dtype=np.int64)
        ang = 2.0 * np.pi * ((k[:, None] * k[None, :]) % n).astype(np.float64) / n
        return (np.cos(ang) * scale).astype(bf), (np.sin(ang) * scale).astype(bf)
    sh['dftc'], sh['dfts'] = tab(SEQ, (SEQ * 128.0) ** -0.5)
    sh['dftc_c'], sh['dfts_c'] = tab(CTX, (CTX * 128.0) ** -0.5)
    c, s = tab(128, 1.0)
    sh['chc'] = c
    sh['chs'] = (-s.astype(np.float32)).astype(bf)
    idx = np.arange(128)
    def bd(b):
        return (idx[:, None] // b == idx[None, :] // b).astype(np.float32)
    bm = np.stack([bd(16), bd(32) - bd(16), bd(64) - bd(32), bd(128) - bd(64)], axis=1)
    sh['bmask'] = np.ascontiguousarray(bm).astype(bf)
    sh['iota'] = np.ascontiguousarray(np.broadcast_to(np.arange(64, dtype=np.float32)[None, :], (128, 64)))
    sh['wbase'] = np.ascontiguousarray((np.arange(8)[None, :] * 128 + np.arange(128)[:, None]).astype(np.float32))
    return sh


def prep_core(inp, b):
    f32 = np.float32
    x = np.asarray(inp['x'][b], f32)
    ctx = np.asarray(inp['ctx'][b], f32)
    xT = np.ascontiguousarray(np.concatenate([ctx, x], axis=0).T)
    cvec = np.stack([_fm(np.asarray(inp['c'][b], f32)), _fm(np.asarray(inp['c_ctx'], f32))], axis=2)
    return {'xT': xT, 'cvec': np.ascontiguousarray(cvec)}


_NC_CACHE = {}


def kernel(**inputs):
    if 'nc' not in _NC_CACHE:
        _NC_CACHE['nc'] = build()
    nc = _NC_CACHE['nc']
    sh = prep_shared(inputs)
    in_maps = []
    for b in range(8):
        m = dict(sh)
        m.update(prep_core(inputs, b))
        in_maps.append(m)
    res = run_bass_kernel_spmd(nc, in_maps, core_ids=list(range(8)))
    out = np.stack([np.ascontiguousarray(r["outT"].T) for r in res.results], axis=0)
    return out.astype(np.float32)
```

```python
import numpy as np
import os
import ml_dtypes
import concourse.bass as bass
import concourse.mybir as mybir
from concourse.bass_utils import run_bass_kernel_spmd

F32 = mybir.dt.float32
BF16 = mybir.dt.bfloat16
AF = mybir.ActivationFunctionType
ALU = mybir.AluOpType

D = 1024
SEQ = 4096
CTX = 256
T = CTX + SEQ
NB = T // 128
L = 4
NE = 32
OFF_A = 3072
OFF_B = 3088
OFF_Z = 3104
OFF_F = 4128
OFF_G = 4640
IN_COLS = 6688
EPS = 1e-6
TILES = [(0, 256, 0)] + [(256 + 512 * i, 512, 1) for i in range(8)]


class PsT:
    def __init__(self, t, k):
        self.t = t
        self.k = k


class Prog:
    ENG = ('pe', 'act', 'dve', 'pool', 'sp')

    def __init__(self, nc, n_lanes=64):
        self.nc = nc
        self.h = {'pe': nc.tensor, 'act': nc.scalar, 'dve': nc.vector, 'pool': nc.gpsimd, 'sp': nc.sync}
        self.sem = {}
        self.val = {}
        for e in self.ENG:
            self.sem[e] = nc.semaphore("s_" + e).__enter__()
            self.val[e] = 0
        self.n_lanes = n_lanes
        for l in range(n_lanes):
            k = ('L', l)
            self.sem[k] = nc.semaphore("s_l%d" % l).__enter__()
            self.val[k] = 0
        self.lanes = {}
        self.seen = {e: {} for e in self.ENG}
        self.clock = {}
        self.lastw = {}
        self.readers = {}
        self.ninst = 0
        self.nwait = 0
        self.stack = [[]]
        self.uid = 0
        self.psum_pool = []
        self.psum_i = 0
        self.cut = int(os.environ['KCUT']) if os.environ.get('KCUT') else None

    def scope(self):
        self.stack.append([])

    def end_scope(self):
        for cm in reversed(self.stack.pop()):
            cm.__exit__(None, None, None)

    def sb(self, name, shape, dt):
        self.uid += 1
        cm = self.nc.sbuf_tensor("%s_%d" % (name, self.uid), list(shape), dt)
        t = cm.__enter__()
        self.stack[-1].append(cm)
        return t

    def init_psum(self):
        self.NPS = 6
        for i in range(self.NPS):
            t = self.nc.psum_tensor("psb%d" % i, [128, 512], F32).__enter__()
            self.psum_pool.append(PsT(t, ('ps', i)))
        self.pst = [self.nc.psum_tensor("pst%d" % i, [128, 1024], BF16).__enter__() for i in range(2)]
        self.pst_i = 0

    def psum(self):
        p = self.psum_pool[self.psum_i % self.NPS]
        self.psum_i += 1
        return p

    def psum_bf(self):
        p = self.psum()
        return PsT(p.t[:, :].bitcast(BF16), p.k)

    def psum_t(self):
        i = self.pst_i % 2
        self.pst_i += 1
        return PsT(self.pst[i][:, 0:512], ('pst', i))

    def lane(self, name):
        if name not in self.lanes:
            assert len(self.lanes) < self.n_lanes, "out of lanes"
            self.lanes[name] = len(self.lanes)
        return self.lanes[name]

    def _deps(self, eng, reads, writes):
        deps = {}

        def add(d, kind):
            if d is None:
                return
            sk, v = d
            if sk == eng:
                if eng == 'pe' or kind != 'raw':
                    return
            if v > deps.get(sk, 0):
                deps[sk] = v
        for k in reads:
            add(self.lastw.get(k), 'raw')
        for k in writes:
            add(self.lastw.get(k), 'waw')
            for r in self.readers.get(k, ()):
                add(r, 'war')
        seen = self.seen[eng]
        return [(sk, v) for sk, v in deps.items() if seen.get(sk, 0) < v]

    def _absorb(self, eng, need):
        seen = self.seen[eng]
        for sk, v in need:
            c = self.clock.get((sk, v))
            if c:
                for k2, v2 in c.items():
                    if seen.get(k2, 0) < v2:
                        seen[k2] = v2
            if seen.get(sk, 0) < v:
                seen[sk] = v

    def _emit_waits(self, eng, need):
        h = self.h[eng]
        for sk, v in need[:-1]:
            h.wait_ge(self.sem[sk], v)
            self.nwait += 1
        return need[-1] if need else None

    def _record(self, tag, reads, writes):
        for k in writes:
            self.lastw[k] = tag
            self.readers[k] = []
        for k in reads:
            self.readers.setdefault(k, []).append(tag)

    def op(self, eng, fn, r=(), w=()):
        if self.cut is not None and self.ninst >= self.cut:
            return None
        need = self._deps(eng, r, w)
        last = self._emit_waits(eng, need)
        ins = fn(self.h[eng])
        if last is not None:
            ins._wait_ge(self.sem[last[0]], last[1])
        self._absorb(eng, need)
        self.val[eng] += 1
        v = self.val[eng]
        ins.then_inc(self.sem[eng], 1)
        snap = dict(self.seen[eng])
        snap[eng] = v
        self.clock[(eng, v)] = snap
        self._record((eng, v), r, w)
        self.ninst += 1
        return ins

    def dma(self, q, pairs, lane, r=(), w=()):
        if self.cut is not None and self.ninst >= self.cut:
            return None
        lk = ('L', self.lane(lane))
        need = self._deps(q, r, w)
        pv = self.val[lk]
        if pv > 0 and self.seen[q].get(lk, 0) < pv:
            need = [(sk, v) for sk, v in need if sk != lk] + [(lk, pv)]
        last = self._emit_waits(q, need)
        h = self.h[q]
        first = True
        for pr in pairs:
            if callable(pr):
                ins = pr(h)
            else:
                ins = h.dma_start(out=pr[0], in_=pr[1])
            if first and last is not None:
                ins._wait_ge(self.sem[last[0]], last[1])
            first = False
            ins.then_inc(self.sem[lk], 16)
            self.val[lk] += 16
            self.ninst += 1
        self._absorb(q, need)
        v = self.val[lk]
        snap = dict(self.seen[q])
        snap[lk] = v
        self.clock[(lk, v)] = snap
        self._record((lk, v), r, w)

    def wait_all(self, eng, keys):
        need = self._deps(eng, keys, ())
        for sk, v in need:
            self.h[eng].wait_ge(self.sem[sk], v)
        self._absorb(eng, need)

    def barrier(self):
        for e in self.ENG:
            need = [(sk, v) for sk, v in self.val.items()
                    if v > 0 and sk != e and self.seen[e].get(sk, 0) < v]
            for sk, v in need:
                self.h[e].wait_ge(self.sem[sk], v)
                self.nwait += 1
            for sk, v in need:
                self.seen[e][sk] = v


def build(nl=L, stop=None, dbg=(), moe_w=True):
    nc = bass.Bass("TRN2", target_bir_lowering=False)
    P = Prog(nc)
    P.init_psum()

    def din(name, shape, dt=F32):
        return nc.dram_tensor(name, list(shape), dt, kind="ExternalInput").ap()

    def dscr(name, shape, dt):
        kind = "ExternalOutput" if name in dbg else "Internal"
        return nc.dram_tensor(name, list(shape), dt, kind=kind).ap()

    xT_in = din("xT", [D, T])
    cvec_d = din("cvec", [128, 8, 2])
    bmod_d = din("bmod", [128, L, 48])
    nmix_d = din("nmix", [128, L, 8])
    nffn_d = din("nffn", [128, L, 8])
    fnorm_d = din("fnorm", [128, 8])
    convw_d = din("convw", [128, L, 3, 24])
    alog_d = din("alog", [128, L, 16])
    dtb_d = din("dtb", [128, L, 16])
    onorm_d = din("onorm", [128, L])
    brt_d = din("brt", [128, L, 36])
    w_mod = din("w_mod", [nl, D, 6 * D])
    w_in = din("w_in", [nl, D, IN_COLS])
    w_fourier = din("w_fourier", [nl, 512, D])
    w_delta = din("w_delta", [nl, D, D])
    w_out = din("w_out", [nl, D, D])
    w_rt = din("w_rt", [nl, D, 36])
    NEd = NE if moe_w else 1
    w_gate = din("w_gate", [nl, NEd, D, 512])
    w_up = din("w_up", [nl, NEd, D, 512])
    w_down = din("w_down", [nl, NEd, 512, D])
    dftc = din("dftc", [SEQ, SEQ], BF16)
    dfts = din("dfts", [SEQ, SEQ], BF16)
    dftc_c = din("dftc_c", [CTX, CTX], BF16)
    dfts_c = din("dfts_c", [CTX, CTX], BF16)
    chc_d = din("chc", [128, 128], BF16)
    chs_d = din("chs", [128, 128], BF16)
    bmask_d = din("bmask", [128, 4, 128], BF16)
    iota_d = din("iota", [128, 64], F32)
    wbase_d = din("wbase", [128, 8], F32)
    outT = nc.dram_tensor("outT", [D, SEQ], F32, kind="ExternalOutput").ap()

    xs = dscr("xs", [D, T], F32)
    qT = dscr("qT", [D, T], BF16)
    kT = dscr("kT", [D, T], BF16)
    k_tok = dscr("k_tok", [8, 128, T], BF16)
    v_tok = dscr("v_tok", [8, 128, T], BF16)
    ab_tok = dscr("ab_tok", [128, NB, 32], F32)
    zT = dscr("zT", [D, T], BF16)
    u_tok = dscr("u_tok", [128, NB, 512], BF16)
    gT = dscr("gT", [2 * D, T], BF16)
    odT = dscr("odT", [D, T], BF16)
    h2tok_d = dscr("h2tok", [128, NB, D], BF16)
    rt_d = dscr("rt", [128, NB, 66], F32)
    NBLK = 49
    xs_d = dscr("xs_slots", [NBLK * 512, D], BF16)
    ys_d = dscr("ys_slots", [NBLK * 512, D], BF16)

    ones_bf = P.sb("ones_bf", [128, 128], BF16)
    ones_f = P.sb("ones_f", [128, 128], F32)
    zeros_f = P.sb("zeros_f", [128, 128], F32)
    ident_bf = P.sb("ident_bf", [128, 128], BF16)
    ident_f = P.sb("ident_f", [128, 128], F32)
    triF = P.sb("triF", [128, 128], F32)
    triB = P.sb("triB", [128, 128], F32)
    negF = P.sb("negF", [128, 128], F32)
    negB = P.sb("negB", [128, 128], F32)
    strF = P.sb("strF", [128, 128], F32)
    strB = P.sb("strB", [128, 128], F32)
    epsT = P.sb("epsT", [128, 1], F32)
    CK = 'const'

    def pool(fn, r=(), w=(CK,)):
        P.op('pool', fn, r=r, w=w)
    pool(lambda e: e.memset(ones_f[:], 1.0))
    pool(lambda e: e.memset(zeros_f[:], 0.0))
    pool(lambda e: e.memset(epsT[:], EPS))
    P.op('dve', lambda e: e.tensor_copy(out=ones_bf[:], in_=ones_f[:]), r=[CK], w=[CK])

    def asel(out, in_, pat, cm, op, fill):
        pool(lambda e: e.affine_select(out=out, in_=in_, pattern=pat, compare_op=op, fill=fill,
                                       base=0, channel_multiplier=cm), r=[CK])
    asel(ident_f[:], ones_f[:], [[1, 128]], -1, ALU.is_equal, 0.0)
    asel(triF[:], ones_f[:], [[1, 128]], -1, ALU.is_ge, 0.0)
    asel(triB[:], ones_f[:], [[-1, 128]], 1, ALU.is_ge, 0.0)
    asel(negF[:], zeros_f[:], [[1, 128]], -1, ALU.is_ge, -1.0e5)
    asel(negB[:], zeros_f[:], [[-1, 128]], 1, ALU.is_ge, -1.0e5)
    asel(strF[:], ones_f[:], [[1, 128]], -1, ALU.is_gt, 0.0)
    asel(strB[:], ones_f[:], [[-1, 128]], 1, ALU.is_gt, 0.0)
    P.op('dve', lambda e: e.tensor_copy(out=ident_bf[:], in_=ident_f[:]), r=[CK], w=[CK])

    bmod = P.sb("bmod", [128, L, 48], F32)
    nmix = P.sb("nmix", [128, L, 8], F32)
    nffn = P.sb("nffn", [128, L, 8], F32)
    fnorm = P.sb("fnorm", [128, 8], F32)
    convw = P.sb("convw", [128, L, 3, 24], F32)
    alog = P.sb("alog", [128, L, 16], F32)
    dtb = P.sb("dtb", [128, L, 16], F32)
    onorm = P.sb("onorm", [128, L], F32)
    brt = P.sb("brt", [128, L, 36], F32)
    chc = P.sb("chc", [128, 128], BF16)
    chs = P.sb("chs", [128, 128], BF16)
    cv = P.sb("cv", [128, 8, 2], F32)
    bmask = P.sb("bmask", [128, 4, 128], BF16)
    iota = P.sb("iota", [128, 64], F32)
    wbase = P.sb("wbase", [128, 8], F32)
    P.dma('sp', [(bmod[:], bmod_d), (nmix[:], nmix_d), (nffn[:], nffn_d), (fnorm[:], fnorm_d),
                 (convw[:], convw_d), (alog[:], alog_d), (dtb[:], dtb_d), (onorm[:], onorm_d),
                 (brt[:], brt_d), (chc[:], chc_d), (chs[:], chs_d), (cv[:], cvec_d), (bmask[:], bmask_d), (iota[:], iota_d), (wbase[:], wbase_d)],
          lane='par', w=['par'])
    modv = P.sb("modv", [128, L, 48, 2], F32)
    A1 = P.sb("A1", [128, L, 8, 2], F32)
    A2 = P.sb("A2", [128, L, 8, 2], F32)
    nea = P.sb("nea", [128, L, 16], F32)
    P.op('act', lambda e: e.activation(out=nea[:], in_=alog[:], func=AF.Exp), r=['par'], w=['nea'])
    P.op('dve', lambda e: e.tensor_scalar(out=nea[:], in0=nea[:], scalar1=-1.0, scalar2=None, op0=ALU.mult),
         r=['nea'], w=['nea'])

    def phase_mod():
        P.scope()
        sv = P.sb("sv", [128, 8, 2], F32)
        P.op('act', lambda e: e.activation(out=sv[:], in_=cv[:], func=AF.Silu), r=['par'], w=['sv'])
        wts = [P.sb("wmt%d" % i, [128, 8, 512], F32) for i in range(2)]
        it = 0
        for l in range(nl):
            wv = w_mod[l].rearrange("(kc p) n -> p kc n", p=128)
            for cb in range(12):
                b = it % 2
                it += 1
                wt = wts[b]
                P.dma('sp', [(wt[:], wv[:, :, cb * 512:(cb + 1) * 512])], lane='wm%d' % b, w=[('wm', b)])
                ps = P.psum()
                for j in range(4):
                    for kc in range(8):
                        P.op('pe', lambda e: e.matmul(ps.t[:, j * 2:j * 2 + 2], wt[:, kc, j * 128:(j + 1) * 128],
                                                      sv[:, kc, :], start=(kc == 0), stop=(kc == 7)),
                             r=[('wm', b), 'sv'], w=[ps.k])
                P.op('dve', lambda e: e.tensor_tensor(
                    out=modv[:, l, cb * 4:cb * 4 + 4, :],
                    in0=ps.t[:, 0:8].rearrange("p (a b) -> p a b", b=2),
                    in1=bmod[:, l, cb * 4:cb * 4 + 4].unsqueeze(2).to_broadcast([128, 4, 2]),
                    op=ALU.add), r=[ps.k, 'par'], w=['mod'])
            for (A, nw, c0) in ((A1, nmix, 8), (A2, nffn, 32)):
                P.op('dve', lambda e: e.tensor_scalar(out=A[:, l], in0=modv[:, l, c0:c0 + 8, :], scalar1=1.0,
                                                      scalar2=None, op0=ALU.add), r=['mod'], w=['mod'])
                P.op('dve', lambda e: e.tensor_tensor(out=A[:, l], in0=A[:, l],
                                                      in1=nw[:, l, :].unsqueeze(2).to_broadcast([128, 8, 2]),
                                                      op=ALU.mult), r=['mod', 'par'], w=['mod'])
        P.barrier()
        P.end_scope()

    def norm_mod(xt, xkey, W, Aap, Bap, outs, tl):
        ss = P.psum()
        for kc in range(8):
            sq = tl['sq'][kc % 2]
            P.op('act', lambda e: e.activation(out=sq[:, :W], in_=xt[:, kc, :W], func=AF.Square),
                 r=[xkey], w=[('sq', kc % 2)])
            P.op('pe', lambda e: e.matmul(ss.t[:, :W], ones_bf[:], sq[:, :W], start=(kc == 0), stop=(kc == 7)),
                 r=[('sq', kc % 2), CK], w=[ss.k])
        rstd = tl['rstd']
        P.op('act', lambda e: e.activation(out=rstd[:, :W], in_=ss.t[:, :W], func=AF.Sqrt, scale=1.0 / D,
                                           bias=epsT[:, 0:1]), r=[ss.k, CK], w=['rstd'])
        P.op('dve', lambda e: e.reciprocal(out=rstd[:, :W], in_=rstd[:, :W]), r=['rstd'], w=['rstd'])
        for kc in range(8):
            tmp = tl['tmp'][kc % 2]
            P.op('dve', lambda e: e.tensor_tensor(out=tmp[:, :W], in0=xt[:, kc, :W], in1=rstd[:, :W], op=ALU.mult),
                 r=[xkey, 'rstd'], w=[('tmp', kc % 2)])
            for (fn, ok) in outs:
                P.op('act', lambda e: e.activation(out=fn(kc), in_=tmp[:, :W], func=AF.Identity,
                                                   scale=Aap[:, kc:kc + 1], bias=Bap[:, kc:kc + 1]),
                     r=[('tmp', kc % 2), 'mod'], w=[ok])

    def phase_proj(l, xsrc):
        P.scope()
        last = (l == L - 1)
        hT = P.sb("hT", [128, 8, T], BF16)
        xt = P.sb("xt", [128, 8, 512], F32)
        tl = {'sq': [P.sb("sq%d" % i, [128, 512], BF16) for i in range(2)],
              'tmp': [P.sb("tmp%d" % i, [128, 512], F32) for i in range(2)],
              'rstd': P.sb("rstd", [128, 512], F32)}
        xv = xsrc.rearrange("(kc p) t -> p kc t", p=128)
        for ti, (t0, W, lat) in enumerate(TILES):
            ws = 0 if lat else 1
            P.dma('sp', [(xt[:, :, :W], xv[:, :, t0:t0 + W])], lane='xt', w=['xt'])
            norm_mod(xt, 'xt', W, A1[:, l, :, ws], modv[:, l, 0:8, ws],
                     [(lambda kc: hT[:, kc, t0:t0 + W], ('hT', ti))], tl)
        wps = [P.sb("wp%d" % i, [128, 8, 1024], BF16) for i in range(2)]
        st8 = [P.sb("st8_%d" % i, [128, 8, 512], BF16) for i in range(2)]
        tokst = [P.sb("tokst%d" % i, [128, 8, 4, 128], BF16) for i in range(2)]
        ust = [P.sb("ust%d" % i, [128, 4, 512], BF16) for i in range(2)]
        abst = P.sb("abst", [128, 4, 32], F32)
        acc = P.sb("acc", [128, 512], F32)
        actf = P.sb("actf", [128, 512], F32)
        actb = P.sb("actb", [128, 512], BF16)
        sqb = P.sb("sqb", [128, 512], BF16)
        rn = P.sb("rn", [128, 512], F32)
        wv = w_in[l].rearrange("(kc p) n -> p kc n", p=128)
        pieces = [('q', 0, 1024), ('k', 1024, 1024), ('v', 2048, 1024), ('ab', OFF_A, 32), ('z', OFF_Z, 1024),
                  ('F', OFF_F, 512), ('Ga', OFF_G, 1024), ('Gb', OFF_G + 1024, 1024)]
        cnt = {'st8': 0, 'tok': 0, 'u': 0}
        for pi, (pn, c0, ncol) in enumerate(pieces):
            b = pi % 2
            wp = wps[b]
            wk = ('wp', b)
            P.dma('pool', [(wp[:, :, :ncol], wv[:, :, c0:c0 + ncol])], lane='wp%d' % b, w=[wk])
            for ti, (t0, W, lat) in enumerate(TILES):
                hk = ('hT', ti)
                nbk = W // 128
                if (not lat) and last and pn in ('z', 'F', 'Ga', 'Gb'):
                    continue
                if pn in ('q', 'k', 'v'):
                    si = cnt['st8'] % 2
                    st = st8[si]
                    sk = ('st8', si)
                    if pn in ('k', 'v'):
                        tki = cnt['tok'] % 2
                        cnt['tok'] += 1
                        tk = tokst[tki]
                        tkk = ('tok', tki)
                    for cc in range(8):
                        gcc = (c0 // 128) + cc
                        ps = P.psum()
                        for kc in range(8):
                            P.op('pe', lambda e: e.matmul(ps.t[:, :W], wp[:, kc, cc * 128:(cc + 1) * 128],
                                                          hT[:, kc, t0:t0 + W], start=(kc == 0), stop=(kc == 7)),
                                 r=[wk, hk], w=[ps.k])
                        rl = 64 if lat else 256
                        pv = ps.t[:, :W].rearrange("p (r c) -> p r c", c=rl)
                        av = acc[:, :W].rearrange("p (r c) -> p r c", c=rl)
                        P.op('dve', lambda e: e.tensor_scalar(out=acc[:, :W], in0=ps.t[:, :W],
                                                              scalar1=convw[:, l, 1, gcc:gcc + 1], scalar2=None,
                                                              op0=ALU.mult), r=[ps.k, 'par'], w=['acc'])
                        P.op('dve', lambda e: e.scalar_tensor_tensor(
                            out=av[:, :, 1:rl], in0=pv[:, :, 0:rl - 1], scalar=convw[:, l, 0, gcc:gcc + 1],
                            in1=av[:, :, 1:rl], op0=ALU.mult, op1=ALU.add), r=[ps.k, 'par', 'acc'], w=['acc'])
                        P.op('dve', lambda e: e.scalar_tensor_tensor(
                            out=av[:, :, 0:rl - 1], in0=pv[:, :, 1:rl], scalar=convw[:, l, 2, gcc:gcc + 1],
                            in1=av[:, :, 0:rl - 1], op0=ALU.mult, op1=ALU.add), r=[ps.k, 'par', 'acc'], w=['acc'])
                        if pn == 'v':
                            P.op('act', lambda e: e.activation(out=actb[:, :W], in_=acc[:, :W], func=AF.Silu),
                                 r=['acc'], w=['actb'])
                            src, srk = actb, 'actb'
                        else:
                            P.op('act', lambda e: e.activation(out=actf[:, :W], in_=acc[:, :W], func=AF.Silu),
                                 r=['acc'], w=['actf'])
                            P.op('act', lambda e: e.activation(out=sqb[:, :W], in_=actf[:, :W], func=AF.Square),
                                 r=['actf'], w=['sqb'])
                            p2 = P.psum()
                            P.op('pe', lambda e: e.matmul(p2.t[:, :W], ones_bf[:], sqb[:, :W], start=True, stop=True),
                                 r=['sqb', CK], w=[p2.k])
                            P.op('act', lambda e: e.activation(out=rn[:, :W], in_=p2.t[:, :W], func=AF.Sqrt,
                                                               bias=epsT[:, 0:1]), r=[p2.k, CK], w=['rn'])
                            P.op('dve', lambda e: e.reciprocal(out=rn[:, :W], in_=rn[:, :W]), r=['rn'], w=['rn'])
                            sc = (128.0 ** -0.5) if pn == 'q' else 1.0
                            P.op('dve', lambda e: e.scalar_tensor_tensor(
                                out=st[:, cc, :W], in0=actf[:, :W], scalar=sc, in1=rn[:, :W],
                                op0=ALU.mult, op1=ALU.mult), r=['actf', 'rn'], w=[sk])
                            src, srk = st[:, cc], sk
                        if pn in ('k', 'v'):
                            pt = P.psum_t()
                            for tb in range(nbk):
                                P.op('pe', lambda e: e.transpose(pt.t[:, tb * 128:(tb + 1) * 128],
                                                                 src[:, tb * 128:(tb + 1) * 128], ident_bf[:]),
                                     r=[srk, CK], w=[pt.k])
                            P.op('act', lambda e: e.copy(
                                out=tk[:, cc, 0:nbk, :],
                                in_=pt.t[:, :nbk * 128].rearrange("p (a b) -> p a b", b=128)),
                                r=[pt.k], w=[tkk])
                    if pn in ('q', 'k'):
                        dst = qT if pn == 'q' else kT
                        P.dma('sp', [(dst.rearrange("(c p) t -> p c t", p=128)[:, :, t0:t0 + W], st[:, :, :W])],
                              lane='st8_%d' % si, r=[sk], w=[pn + 'T'])
                        cnt['st8'] += 1
                    if pn in ('k', 'v'):
                        dst = k_tok if pn == 'k' else v_tok
                        P.dma('sp', [(dst[:, :, t0:t0 + W].rearrange("c p t -> p c t"),
                                      tk[:, :, :nbk, :].rearrange("p c a b -> p c (a b)"))],
                              lane='tok%d' % tki, r=[tkk], w=[pn + '_tok'])
                elif pn == 'ab':
                    ps = P.psum()
                    for tb in range(nbk):
                        for kc in range(8):
                            P.op('pe', lambda e: e.matmul(ps.t[:, tb * 32:(tb + 1) * 32],
                                                          hT[:, kc, t0 + tb * 128:t0 + (tb + 1) * 128],
                                                          wp[:, kc, 0:32], start=(kc == 0), stop=(kc == 7)),
                                 r=[wk, hk], w=[ps.k])
                    P.op('dve', lambda e: e.tensor_copy(out=abst[:, :nbk, :],
                                                        in_=ps.t[:, :nbk * 32].rearrange("p (a b) -> p a b", b=32)),
                         r=[ps.k], w=['abst'])
                    P.dma('sp', [(ab_tok[:, t0 // 128:t0 // 128 + nbk, :], abst[:, :nbk, :])],
                          lane='abst', r=['abst'], w=['ab_tok'])
                elif pn in ('z', 'Ga', 'Gb'):
                    si = cnt['st8'] % 2
                    cnt['st8'] += 1
                    st = st8[si]
                    sk = ('st8', si)
                    fnc = AF.Silu if pn == 'z' else AF.Sigmoid
                    for cc in range(8):
                        ps = P.psum()
                        for kc in range(8):
                            P.op('pe', lambda e: e.matmul(ps.t[:, :W], wp[:, kc, cc * 128:(cc + 1) * 128],
                                                          hT[:, kc, t0:t0 + W], start=(kc == 0), stop=(kc == 7)),
                                 r=[wk, hk], w=[ps.k])
                        P.op('act', lambda e: e.activation(out=st[:, cc, :W], in_=ps.t[:, :W], func=fnc),
                             r=[ps.k], w=[sk])
                    if pn == 'z':
                        dv = zT.rearrange("(c p) t -> p c t", p=128)
                    else:
                        o8 = 0 if pn == 'Ga' else 8
                        dv = gT.rearrange("(c p) t -> p c t", p=128)[:, o8:o8 + 8, :]
                    P.dma('sp', [(dv[:, :, t0:t0 + W], st[:, :, :W])], lane='st8_%d' % si, r=[sk], w=[pn])
                elif pn == 'F':
                    ui = cnt['u'] % 2
                    cnt['u'] += 1
                    us = ust[ui]
                    uk = ('ust', ui)
                    for tb in range(nbk):
                        ps = P.psum()
                        for kc in range(8):
                            P.op('pe', lambda e: e.matmul(ps.t[:, :512], hT[:, kc, t0 + tb * 128:t0 + (tb + 1) * 128],
                                                          wp[:, kc, 0:512], start=(kc == 0), stop=(kc == 7)),
                                 r=[wk, hk], w=[ps.k])
                        P.op('act', lambda e: e.copy(out=us[:, tb, :], in_=ps.t[:, :512]), r=[ps.k], w=[uk])
                    P.dma('sp', [(u_tok[:, t0 // 128:t0 // 128 + nbk, :], us[:, :nbk, :])],
                          lane='ust%d' % ui, r=[uk], w=['u_tok'])
        P.barrier()
        P.end_scope()

    def phase_delta(l):
        P.scope()
        last = (l == L - 1)
        ab = P.sb("ab", [128, NB, 32], F32)
        P.dma('sp', [(ab[:], ab_tok)], lane='ab', w=['ab'])
        g = P.sb("g", [128, NB, 16], F32)
        beta = P.sb("beta", [128, NB, 16], F32)
        nbeta = P.sb("nbeta", [128, NB, 16], F32)
        gcum = P.sb("gcum", [128, NB, 16], F32)
        ngam = P.sb("ngam", [128, NB, 16], F32)
        tail = P.sb("tail", [128, NB, 16], F32)
        egl = P.sb("egl", [128, NB, 16], F32)
        GK = 'gates'
        P.op('dve', lambda e: e.tensor_tensor(out=g[:], in0=ab[:, :, 0:16],
                                              in1=dtb[:, l, :].unsqueeze(1).to_broadcast([128, NB, 16]), op=ALU.add),
             r=['ab', 'par'], w=[GK])
        P.op('act', lambda e: e.activation(out=g[:], in_=g[:], func=AF.Exp), r=[GK], w=[GK])
        P.op('act', lambda e: e.activation(out=g[:], in_=g[:], func=AF.Ln, bias=ones_f[:, 0:1]), r=[GK, CK], w=[GK])
        P.op('dve', lambda e: e.tensor_tensor(out=g[:], in0=g[:],
                                              in1=nea[:, l, :].unsqueeze(1).to_broadcast([128, NB, 16]), op=ALU.mult),
             r=[GK, 'nea'], w=[GK])
        P.op('act', lambda e: e.activation(out=beta[:], in_=ab[:, :, 16:32], func=AF.Sigmoid), r=['ab'], w=[GK])
        P.op('dve', lambda e: e.tensor_scalar(out=nbeta[:], in0=beta[:], scalar1=-1.0, scalar2=None, op0=ALU.mult),
             r=[GK], w=[GK])
        for d in range(2):
            tri = triF if d == 0 else triB
            ps = P.psum()
            P.op('pe', lambda e: e.matmul(ps.t[:, :NB * 8].rearrange("p (a b) -> p a b", b=8), tri[:],
                                          g[:, :, d * 8:(d + 1) * 8], start=True, stop=True), r=[GK, CK], w=[ps.k])
            P.op('dve', lambda e: e.tensor_copy(out=gcum[:, :, d * 8:(d + 1) * 8],
                                                in_=ps.t[:, :NB * 8].rearrange("p (a b) -> p a b", b=8)),
                 r=[ps.k], w=[GK])
            ps2 = P.psum()
            P.op('pe', lambda e: e.matmul(ps2.t[:, :NB * 8].rearrange("p (a b) -> p a b", b=8), ones_f[:],
                                          g[:, :, d * 8:(d + 1) * 8], start=True, stop=True), r=[GK, CK], w=[ps2.k])
            P.op('act', lambda e: e.activation(out=egl[:, :, d * 8:(d + 1) * 8],
                                               in_=ps2.t[:, :NB * 8].rearrange("p (a b) -> p a b", b=8), func=AF.Exp),
                 r=[ps2.k], w=[GK])
            P.op('dve', lambda e: e.tensor_tensor(out=tail[:, :, d * 8:(d + 1) * 8],
                                                  in0=ps2.t[:, :NB * 8].rearrange("p (a b) -> p a b", b=8),
                                                  in1=gcum[:, :, d * 8:(d + 1) * 8], op=ALU.subtract),
                 r=[ps2.k, GK], w=[GK])
        P.op('act', lambda e: e.activation(out=tail[:], in_=tail[:], func=AF.Exp), r=[GK], w=[GK])
        P.op('act', lambda e: e.activation(out=ngam[:], in_=gcum[:], func=AF.Exp), r=[GK], w=[GK])
        P.op('dve', lambda e: e.tensor_scalar(out=ngam[:], in0=ngam[:], scalar1=-1.0, scalar2=None, op0=ALU.mult),
             r=[GK], w=[GK])

        DSTOP = os.environ.get('KDSTOP', '')
        if DSTOP == 'gates':
            P.barrier()
            P.end_scope()
            return
        qTh = P.sb("qTh", [128, T], BF16)
        kTh = P.sb("kTh", [128, T], BF16)
        ktk = P.sb("ktk", [128, NB, 128], BF16)
        vtk = P.sb("vtk", [128, NB, 128], BF16)
        oacc = P.sb("oacc", [128, T], F32)
        Tt = P.sb("Tt", [128, NB, 2, 128], BF16)
        AT = P.sb("AT", [128, NB, 2, 128], BF16)
        qdec = P.sb("qdec", [128, NB, 2, 128], BF16)
        ktail = P.sb("ktail", [128, NB, 2, 128], BF16)
        GP = 4
        Ef = [P.sb("Ef%d" % i, [128, 2, 128], F32) for i in range(GP)]
        Es = [P.sb("Es%d" % i, [128, 2, 128], F32) for i in range(GP)]
        Ge = [P.sb("Ge%d" % i, [128, 2, 128], F32) for i in range(GP)]
        ABt = [[P.sb("ABt%d_%d" % (g_, i), [128, 2, 2, 128], BF16) for i in range(2)] for g_ in range(GP)]
        Ttw = [[P.sb("Ttw%d_%d" % (g_, i), [128, 2, 128], BF16) for i in range(2)] for g_ in range(GP)]
        Xf = [P.sb("Xf%d" % i, [128, 2, 128], BF16) for i in range(GP)]
        XfT = [P.sb("XfT%d" % i, [128, 2, 128], BF16) for i in range(GP)]
        UT = [P.sb("UT%d" % i, [128, 3, 2, 128], BF16) for i in range(GP)]
        Hh = [P.sb("Hh%d" % i, [128, 2, 128], BF16) for i in range(GP)]
        Wn = [P.sb("Wn%d" % i, [128, 2, 128], BF16) for i in range(GP)]
        Sf = [P.sb("Sf%d" % i, [128, 128], F32) for i in range(2)]
        Sb = [P.sb("Sb%d" % i, [128, 128], BF16) for i in range(2)]
        Rt = [P.sb("Rt%d" % i, [128, 128], BF16) for i in range(2)]
        vn = [P.sb("vn%d" % i, [128, 128], BF16) for i in range(2)]
        sqo = [P.sb("sqo%d" % i, [128, 512], BF16) for i in range(2)]
        rno = P.sb("rno", [128, 512], F32)
        odf = P.sb("odf", [128, 512], F32)
        odst = [P.sb("odst%d" % i, [128, 512], BF16) for i in range(2)]
        tris = (triF, triB)
        negs = (negF, negB)
        strs = (strF, strB)
        uidx = 0
        for h in range(8):
            hs = slice(h * 128, (h + 1) * 128)
            HK = ('hd', 0)
            P.dma('sp', [(qTh[:], qT[hs, :]), (kTh[:], kT[hs, :]),
                         (ktk[:].rearrange("p b f -> p (b f)"), k_tok[h]),
                         (vtk[:].rearrange("p b f -> p (b f)"), v_tok[h])], lane='hd', w=[HK])
            OK_ = [('oacc', n) for n in range(NB)]
            P.op('pool', lambda e: e.memset(oacc[:], 0.0), w=OK_)
            def pre(n, g_):
                bs = slice(n * 128, (n + 1) * 128)
                kE, kS, kG, kX, kXT, kU, kH, kW = (('Ef', g_), ('Es', g_), ('Ge', g_), ('Xf', g_), ('XfT', g_),
                                                   ('UT', g_), ('Hh', g_), ('Wn', g_))
                AB = ABt[g_]
                TW = Ttw[g_]
                bank = P.psum_pool[g_]
                bankbf = PsT(bank.t[:, :].bitcast(BF16), bank.k)
                pk = bank
                P.op('pe', lambda e: e.matmul(pk.t[:, 0:128], kTh[:, bs], kTh[:, bs], start=True, stop=True),
                     r=[HK], w=[pk.k])
                P.op('pe', lambda e: e.matmul(pk.t[:, 128:256], kTh[:, bs], qTh[:, bs], start=True, stop=True),
                     r=[HK], w=[pk.k])
                for d in range(2):
                    col = d * 8 + h
                    P.op('pe', lambda e: e.matmul(pk.t[:, 256 + d * 128:384 + d * 128],
                                                  g[:, n, col:col + 1].to_broadcast([128, 128]), tris[d][:],
                                                  start=True, stop=True), r=[GK, CK], w=[pk.k])
                yield
                for d in range(2):
                    col = d * 8 + h
                    P.op('dve', lambda e: e.scalar_tensor_tensor(
                        out=Ef[g_][:, d, :], in0=pk.t[:, 256 + d * 128:384 + d * 128], scalar=gcum[:, n, col:col + 1],
                        in1=negs[d][:], op0=ALU.subtract, op1=ALU.add), r=[pk.k, GK, CK], w=[kE])
                yield
                P.op('act', lambda e: e.activation(out=Ef[g_][:], in_=Ef[g_][:], func=AF.Exp), r=[kE], w=[kE])
                P.op('act', lambda e: e.activation(out=Ge[g_][:],
                                                   in_=pk.t[:, 256:512].rearrange("p (a b) -> p a b", b=128),
                                                   func=AF.Exp), r=[pk.k], w=[kG])
                yield
                for d in range(2):
                    P.op('dve', lambda e: e.tensor_tensor(out=Es[g_][:, d, :], in0=Ef[g_][:, d, :], in1=strs[d][:],
                                                          op=ALU.mult), r=[kE, CK], w=[kS])
                for d in range(2):
                    col = d * 8 + h
                    P.op('dve', lambda e: e.tensor_tensor(out=AT[:, n, d, :], in0=pk.t[:, 128:256], in1=Ef[g_][:, d, :],
                                                          op=ALU.mult), r=[pk.k, kE], w=[('pre', n)])
                    P.op('dve', lambda e: e.scalar_tensor_tensor(
                        out=Xf[g_][:, d, :], in0=pk.t[:, 0:128], scalar=nbeta[:, n, col:col + 1],
                        in1=Es[g_][:, d, :], op0=ALU.mult, op1=ALU.mult), r=[pk.k, GK, kS], w=[kX])
                    P.op('dve', lambda e: e.tensor_tensor(out=qdec[:, n, d, :], in0=qTh[:, bs], in1=Ge[g_][:, d, :],
                                                          op=ALU.mult), r=[HK, kG], w=[('pre', n)])
                    P.op('act', lambda e: e.activation(out=ktail[:, n, d, :], in_=ktk[:, n, :], func=AF.Copy,
                                                       scale=tail[:, n, col:col + 1]), r=[HK, GK], w=[('pre', n)])
                P.op('dve', lambda e: e.tensor_tensor(out=AB[0][:, :, 0, :], in0=Xf[g_][:],
                                                      in1=bmask[:, 0:1, :].to_broadcast([128, 2, 128]), op=ALU.mult),
                     r=[kX, 'par'], w=[('AB', g_, 0)])
                yield
                pt = bankbf
                for d in range(2):
                    P.op('pe', lambda e: e.transpose(pt.t[:, d * 128:(d + 1) * 128], AB[0][:, d, 0, :], ident_bf[:]),
                         r=[('AB', g_, 0), CK], w=[pt.k])
                    P.op('pe', lambda e: e.transpose(pt.t[:, 256 + d * 128:256 + (d + 1) * 128], Xf[g_][:, d, :],
                                                     ident_bf[:]), r=[kX, CK], w=[pt.k])
                yield
                P.op('act', lambda e: e.copy(out=AB[0][:, :, 1, :],
                                             in_=pt.t[:, 0:256].rearrange("p (a b) -> p a b", b=128)),
                     r=[pt.k], w=[('AB', g_, 0)])
                P.op('act', lambda e: e.copy(out=XfT[g_][:], in_=pt.t[:, 256:512].rearrange("p (a b) -> p a b", b=128)),
                     r=[pt.k], w=[kXT])
                P.op('dve', lambda e: e.tensor_tensor(out=TW[0][:], in0=AB[0][:, :, 0, :],
                                                      in1=ident_bf[:].unsqueeze(1).to_broadcast([128, 2, 128]),
                                                      op=ALU.add), r=[('AB', g_, 0), CK], w=[('Ttw', g_, 0)])
                for b3 in range(3):
                    P.op('dve', lambda e: e.tensor_tensor(out=UT[g_][:, b3], in0=XfT[g_][:],
                                                          in1=bmask[:, 1 + b3:2 + b3, :].to_broadcast([128, 2, 128]),
                                                          op=ALU.mult), r=[kXT, 'par'], w=[kU])
                yield
                gi_ = 0
                for lev in range(1, 4):
                    cur = lev % 2
                    prv = 1 - cur
                    pab = bank
                    for d in range(2):
                        if lev < 3:
                            P.op('pe', lambda e: e.matmul(pab.t[:, d * 256:d * 256 + 128], AB[prv][:, d, 1, :],
                                                          AB[prv][:, d, 0, :], start=True, stop=True),
                                 r=[('AB', g_, prv)], w=[pab.k])
                        P.op('pe', lambda e: e.matmul(pab.t[:, d * 256 + 128:d * 256 + 256], AB[prv][:, d, 0, :],
                                                      AB[prv][:, d, 1, :], start=True, stop=True),
                             r=[('AB', g_, prv)], w=[pab.k])
                    yield
                    if lev < 3:
                        P.op('act', lambda e: e.copy(out=AB[cur][:],
                                                     in_=pab.t[:, :].rearrange("p (a b c) -> p a b c", a=2, b=2)),
                             r=[pab.k], w=[('AB', g_, cur)])
                    else:
                        P.op('act', lambda e: e.copy(
                            out=AB[cur][:, :, 1, :],
                            in_=pab.t[:, :].rearrange("p (a b c) -> p a b c", a=2, b=2)[:, :, 1, :]),
                            r=[pab.k], w=[('AB', g_, cur)])
                    yield
                    ptt = bank
                    for d in range(2):
                        P.op('pe', lambda e: e.matmul(ptt.t[:, d * 128:(d + 1) * 128], AB[cur][:, d, 1, :],
                                                      TW[gi_][:, d, :], start=True, stop=True),
                             r=[('AB', g_, cur), ('Ttw', g_, gi_)], w=[ptt.k])
                    yield
                    P.op('dve', lambda e: e.tensor_tensor(
                        out=TW[1 - gi_][:], in0=ptt.t[:, 0:256].rearrange("p (a b) -> p a b", b=128),
                        in1=TW[gi_][:], op=ALU.add), r=[ptt.k, ('Ttw', g_, gi_)], w=[('Ttw', g_, 1 - gi_)])
                    gi_ = 1 - gi_
                    yield
                for b3 in range(3):
                    G = TW[gi_]
                    pw2 = bank
                    pt2 = PsT(pw2.t[:, :].bitcast(BF16), pw2.k)
                    for d in range(2):
                        P.op('pe', lambda e: e.transpose(pt2.t[:, d * 128:(d + 1) * 128], G[:, d, :], ident_bf[:]),
                             r=[('Ttw', g_, gi_), CK], w=[pt2.k])
                    for d in range(2):
                        P.op('pe', lambda e: e.matmul(pw2.t[:, 256 + d * 128:256 + (d + 1) * 128], UT[g_][:, b3, d, :], G[:, d, :],
                                                      start=True, stop=True), r=[kU, ('Ttw', g_, gi_)], w=[pw2.k])
                    yield
                    P.op('act', lambda e: e.copy(out=Hh[g_][:], in_=pt2.t[:, 0:256].rearrange("p (a b) -> p a b", b=128)),
                         r=[pt2.k], w=[kH])
                    P.op('act', lambda e: e.copy(out=Wn[g_][:], in_=pw2.t[:, 256:512].rearrange("p (a b) -> p a b", b=128)),
                         r=[pw2.k], w=[kW])
                    yield
                    pg2 = bank
                    for d in range(2):
                        P.op('pe', lambda e: e.matmul(pg2.t[:, d * 128:(d + 1) * 128], Hh[g_][:, d, :], Wn[g_][:, d, :],
                                                      start=True, stop=True), r=[kH, kW], w=[pg2.k])
                    yield
                    if b3 < 2:
                        P.op('dve', lambda e: e.tensor_tensor(
                            out=TW[1 - gi_][:], in0=pg2.t[:, 0:256].rearrange("p (a b) -> p a b", b=128),
                            in1=G[:], op=ALU.add), r=[pg2.k, ('Ttw', g_, gi_)], w=[('Ttw', g_, 1 - gi_)])
                    else:
                        P.op('dve', lambda e: e.tensor_tensor(
                            out=Tt[:, n, :, :], in0=pg2.t[:, 0:256].rearrange("p (a b) -> p a b", b=128),
                            in1=G[:], op=ALU.add), r=[pg2.k, ('Ttw', g_, gi_)], w=[('pre', n)])
                    gi_ = 1 - gi_
                    yield

            orders = [list(range(NB)), [1, 0] + list(range(NB - 1, 1, -1))]
            done_pre = set()

            def chain(d):
                col = d * 8 + h
                pc = P.psum_pool[4 + d]
                P.op('pool', lambda e: e.memset(Sf[d][:], 0.0), w=[('S', d)])
                P.op('pool', lambda e: e.memset(Sb[d][:], 0.0), w=[('Sb', d)])
                for n in orders[d]:
                    while n not in done_pre:
                        yield
                    bs = slice(n * 128, (n + 1) * 128)
                    need_o = not (last and n < 2)
                    P.op('pe', lambda e: e.matmul(pc.t[:, 0:128], kTh[:, bs], Sb[d][:], start=True, stop=True),
                         r=[HK, ('Sb', d)], w=[pc.k])
                    yield
                    P.op('dve', lambda e: e.scalar_tensor_tensor(
                        out=Rt[d][:], in0=pc.t[:, 0:128], scalar=ngam[:, n, col:col + 1], in1=vtk[:, n, :],
                        op0=ALU.mult, op1=ALU.add), r=[pc.k, GK, HK], w=[('R', d)])
                    yield
                    P.op('pe', lambda e: e.matmul(pc.t[:, 128:256], Tt[:, n, d, :], Rt[d][:], start=True, stop=True),
                         r=[('pre', n), ('R', d)], w=[pc.k])
                    yield
                    P.op('act', lambda e: e.activation(out=vn[d][:], in_=pc.t[:, 128:256], func=AF.Copy,
                                                       scale=beta[:, n, col:col + 1]), r=[pc.k, GK], w=[('vn', d)])
                    yield
                    if need_o:
                        P.op('pe', lambda e: e.matmul(pc.t[:, 256:384], Sb[d][:], qdec[:, n, d, :],
                                                      start=True, stop=False), r=[('Sb', d), ('pre', n)], w=[pc.k])
                        P.op('pe', lambda e: e.matmul(pc.t[:, 256:384], vn[d][:], AT[:, n, d, :],
                                                      start=False, stop=True), r=[('vn', d), ('pre', n)], w=[pc.k])
                    P.op('pe', lambda e: e.matmul(pc.t[:, 384:512], ktail[:, n, d, :], vn[d][:], start=True, stop=True),
                         r=[('pre', n), ('vn', d)], w=[pc.k])
                    yield
                    if need_o:
                        P.op('dve', lambda e: e.tensor_tensor(out=oacc[:, bs], in0=pc.t[:, 256:384], in1=oacc[:, bs],
                                                              op=ALU.add), r=[pc.k, ('oacc', n)], w=[('oacc', n)])
                    P.op('dve', lambda e: e.scalar_tensor_tensor(
                        out=Sf[d][:], in0=Sf[d][:], scalar=egl[:, n, col:col + 1], in1=pc.t[:, 384:512],
                        op0=ALU.mult, op1=ALU.add), r=[pc.k, GK, ('S', d)], w=[('S', d)])
                    yield
                    P.op('act', lambda e: e.copy(out=Sb[d][:], in_=Sf[d][:]), r=[('S', d)], w=[('Sb', d)])
                    yield

            pend = []
            seen_ = set()
            fi_ = bi_ = 0
            while len(pend) < NB:
                while fi_ < NB and orders[0][fi_] in seen_:
                    fi_ += 1
                if fi_ < NB:
                    pend.append(orders[0][fi_])
                    seen_.add(orders[0][fi_])
                while bi_ < NB and orders[1][bi_] in seen_:
                    bi_ += 1
                if bi_ < NB:
                    pend.append(orders[1][bi_])
                    seen_.add(orders[1][bi_])
            if DSTOP == 'pre1':
                pend = pend[:1]
            slots = {}
            chains = [] if DSTOP.startswith('pre') else [chain(0), chain(1)]
            while pend or slots or chains:
                for g_ in range(GP):
                    if g_ not in slots and pend:
                        n_ = pend.pop(0)
                        slots[g_] = (pre(n_, g_), n_)
                for g_ in list(slots.keys()):
                    gen, n_ = slots[g_]
                    try:
                        next(gen)
                    except StopIteration:
                        done_pre.add(n_)
                        del slots[g_]
                for cg in list(chains):
                    try:
                        next(cg)
                    except StopIteration:
                        chains.remove(cg)
            if DSTOP.startswith('pre'):
                P.barrier()
                P.end_scope()
                return
            if DSTOP == 'chain':
                P.barrier()
                P.end_scope()
                return
            zTh = ktail[:].rearrange("p n d f -> p (n d f)")
            PREK = [('pre', n) for n in range(NB)]
            P.dma('sp', [(zTh[:, 0:T], zT[hs, :])], lane='zld', w=PREK)
            for ti, (t0, W, lat) in enumerate(TILES):
                if last and not lat:
                    continue
                i2 = ti % 2
                P.op('act', lambda e: e.activation(out=sqo[i2][:, :W], in_=oacc[:, t0:t0 + W], func=AF.Square),
                     r=OK_, w=[('sqo', i2)])
                ps = P.psum()
                P.op('pe', lambda e: e.matmul(ps.t[:, :W], ones_bf[:], sqo[i2][:, :W], start=True, stop=True),
                     r=[('sqo', i2), CK], w=[ps.k])
                P.op('act', lambda e: e.activation(out=rno[:, :W], in_=ps.t[:, :W], func=AF.Sqrt, scale=1.0 / 128,
                                                   bias=epsT[:, 0:1]), r=[ps.k, CK], w=['rno'])
                P.op('dve', lambda e: e.reciprocal(out=rno[:, :W], in_=rno[:, :W]), r=['rno'], w=['rno'])
                P.op('dve', lambda e: e.scalar_tensor_tensor(
                    out=odf[:, :W], in0=oacc[:, t0:t0 + W], scalar=onorm[:, l:l + 1], in1=rno[:, :W],
                    op0=ALU.mult, op1=ALU.mult), r=OK_ + ['rno', 'par'], w=['odf'])
                P.op('dve', lambda e: e.tensor_tensor(out=odst[i2][:, :W], in0=odf[:, :W], in1=zTh[:, t0:t0 + W],
                                                      op=ALU.mult), r=['odf'] + PREK, w=[('odst', i2)])
                P.dma('sp', [(odT[hs, t0:t0 + W], odst[i2][:, :W])], lane='odst%d' % i2, r=[('odst', i2)], w=['odT'])
        P.barrier()
        P.end_scope()

    def phase_merge(l, xsrc):
        P.scope()
        last = (l == L - 1)
        wfc = P.sb("wfc", [128, 8, D], BF16)
        wdl = P.sb("wdl", [128, 8, D], BF16)
        wo = P.sb("wo", [128, 8, D], BF16)
        wr = P.sb("wr", [128, 8, 36], F32)
        P.scope()
        wfr = P.sb("wfr", [128, 4, D], BF16)
        P.dma('pool', [(wfr[:], w_fourier[l].rearrange("(g p) n -> p g n", p=128)),
                       (wdl[:], w_delta[l].rearrange("(kc p) n -> p kc n", p=128)),
                       (wo[:], w_out[l].rearrange("(kc p) n -> p kc n", p=128))], lane='mw', w=['mw'])
        P.dma('sp', [(wr[:], w_rt[l].rearrange("(kc p) n -> p kc n", p=128))], lane='wr', w=['wr'])
        for gi in range(4):
            for (j, mat) in ((0, chc), (1, chs)):
                for hf in range(2):
                    ps = P.psum()
                    P.op('pe', lambda e: e.matmul(ps.t[:, :512], mat[:], wfr[:, gi, hf * 512:(hf + 1) * 512],
                                                  start=True, stop=True), r=['mw', 'par'], w=[ps.k])
                    P.op('act', lambda e: e.copy(out=wfc[:, gi * 2 + j, hf * 512:(hf + 1) * 512], in_=ps.t[:, :512]),
                         r=[ps.k], w=['wfc'])
        P.barrier()
        P.end_scope()
        u_l = P.sb("u_l", [128, 32, 512], BF16)
        u_c = P.sb("u_c", [128, 2, 512], BF16)
        P.dma('sp', [(u_l[:], u_tok[:, 2:NB, :])], lane='ul', w=['u_l'])
        if not last:
            P.dma('sp', [(u_c[:], u_tok[:, 0:2, :])], lane='uc', w=['u_c'])
        NDB = 3
        dbuf = {'c': [P.sb("dcb%d" % i, [128, 4, 512], BF16) for i in range(NDB)],
                's': [P.sb("dsb%d" % i, [128, 4, 512], BF16) for i in range(NDB)]}
        dctr = 0
        PQ = P.sb("PQ", [128, 8, 512], BF16)
        odt = P.sb("odt", [128, 8, 512], BF16)
        gtt = P.sb("gtt", [128, 16, 512], BF16)
        mg = P.sb("mg", [128, 8, 512], BF16)
        m1 = P.sb("m1", [128, 512], F32)
        m2 = P.sb("m2", [128, 512], F32)
        xt = P.sb("xt", [128, 8, 512], F32)
        h2f = xt
        h2b = P.sb("h2b", [128, 8, 512], BF16)
        tl = {'sq': [P.sb("sq%d" % i, [128, 512], BF16) for i in range(2)],
              'tmp': [P.sb("tmp%d" % i, [128, 512], F32) for i in range(2)],
              'rstd': P.sb("rstd", [128, 512], F32)}
        lg = P.sb("lg", [128, 36], F32)
        rt = {k: P.sb("rt_" + k, [128, 32], F32) for k in ('lem', 'tmp')}
        rs = {k: P.sb("rs_" + k, [128, 4], F32) for k in ('mx', 'ohg', 'ex', 'pg', 'm1', 'm2', 'w1', 'w2', 'ohm')}
        rst = P.sb("rst", [128, 4, 66], F32)
        h2tk = P.sb("h2tk", [128, 4, D], BF16)
        xv = xsrc.rearrange("(kc p) t -> p kc t", p=128)
        xsv = xs.rearrange("(kc p) t -> p kc t", p=128)
        for ti, (t0, W, lat) in enumerate(TILES):
            if last and not lat:
                continue
            ws = 0 if lat else 1
            ntc = 32 if lat else 2
            uu = u_l if lat else u_c
            ukey = 'u_l' if lat else 'u_c'
            f0 = t0 - CTX if lat else 0
            mats = {'c': dftc if lat else dftc_c, 's': dfts if lat else dfts_c}
            sw = 4 if lat else 2
            nsub = ntc // sw
            P.dma('sp', [(odt[:, :, :W], odT.rearrange("(c p) t -> p c t", p=128)[:, :, t0:t0 + W]),
                         (gtt[:, :, :W], gT.rearrange("(c p) t -> p c t", p=128)[:, :, t0:t0 + W]),
                         (xt[:, :, :W], xv[:, :, t0:t0 + W])], lane='mt', w=['mt'])
            for gh in range(2):
                accs = [[P.psum(), P.psum()] for _ in range(2)]
                for sb_ in range(nsub):
                    bi = dctr % NDB
                    dctr += 1
                    for cs in ('c', 's'):
                        P.dma('sp', [(dbuf[cs][bi][:, :sw, :W],
                                      mats[cs][sb_ * sw * 128:(sb_ + 1) * sw * 128, f0:f0 + W].rearrange(
                                          "(b p) f -> p b f", p=128))],
                              lane='d%sb%d' % (cs, bi), w=[('d' + cs, bi)])
                    for t4 in range(sw):
                        tc = sb_ * sw + t4
                        for gl in range(2):
                            gi = gh * 2 + gl
                            for j, cs in enumerate(('c', 's')):
                                P.op('pe', lambda e: e.matmul(accs[gl][j].t[:, :W], uu[:, tc, gi * 128:(gi + 1) * 128],
                                                              dbuf[cs][bi][:, t4, :W], start=(tc == 0),
                                                              stop=(tc == ntc - 1)),
                                     r=[ukey, ('d' + cs, bi)], w=[accs[gl][j].k])
                for gl in range(2):
                    gi = gh * 2 + gl
                    for j in range(2):
                        P.op('act', lambda e: e.copy(out=PQ[:, gi * 2 + j, :W], in_=accs[gl][j].t[:, :W]),
                             r=[accs[gl][j].k], w=['PQ'])
            for dc in range(8):
                pa = P.psum()
                for k8 in range(8):
                    P.op('pe', lambda e: e.matmul(pa.t[:, :W], wfc[:, k8, dc * 128:(dc + 1) * 128], PQ[:, k8, :W],
                                                  start=(k8 == 0), stop=(k8 == 7)), r=['wfc', 'PQ'], w=[pa.k])
                pb = P.psum()
                for kc in range(8):
                    P.op('pe', lambda e: e.matmul(pb.t[:, :W], wdl[:, kc, dc * 128:(dc + 1) * 128], odt[:, kc, :W],
                                                  start=(kc == 0), stop=(kc == 7)), r=['mw', 'mt'], w=[pb.k])
                P.op('dve', lambda e: e.tensor_tensor(out=m1[:, :W], in0=pa.t[:, :W], in1=gtt[:, dc, :W], op=ALU.mult),
                     r=[pa.k, 'mt'], w=['m1'])
                P.op('dve', lambda e: e.tensor_tensor(out=m2[:, :W], in0=pb.t[:, :W], in1=gtt[:, 8 + dc, :W],
                                                      op=ALU.mult), r=[pb.k, 'mt'], w=['m2'])
                P.op('dve', lambda e: e.tensor_tensor(out=mg[:, dc, :W], in0=m1[:, :W], in1=m2[:, :W], op=ALU.add),
                     r=['m1', 'm2'], w=['mg'])
            for dc in range(8):
                po = P.psum()
                for kc in range(8):
                    P.op('pe', lambda e: e.matmul(po.t[:, :W], wo[:, kc, dc * 128:(dc + 1) * 128], mg[:, kc, :W],
                                                  start=(kc == 0), stop=(kc == 7)), r=['mw', 'mg'], w=[po.k])
                P.op('dve', lambda e: e.scalar_tensor_tensor(
                    out=xt[:, dc, :W], in0=po.t[:, :W], scalar=modv[:, l, 16 + dc, ws:ws + 1], in1=xt[:, dc, :W],
                    op0=ALU.mult, op1=ALU.add), r=[po.k, 'mod', 'mt'], w=['mt'])
            P.dma('sp', [(xsv[:, :, t0:t0 + W], xt[:, :, :W])], lane='xo', r=['mt'], w=['xs'])
            if os.environ.get('KTRACE') and ti < 2:
                print('merge tile', ti, 'pre-norm2', P.ninst)
            norm_mod(xt, 'mt', W, A2[:, l, :, ws], modv[:, l, 24:32, ws],
                     [(lambda kc: xt[:, kc, :W], 'mt'), (lambda kc: h2b[:, kc, :W], 'h2b')], tl)
            if os.environ.get('KTRACE') and ti < 2:
                print('merge tile', ti, 'pre-h2tk', P.ninst)
            nbk = W // 128
            blk0 = t0 // 128
            for tb in range(nbk):
                for hf in range(2):
                    pt = P.psum_t()
                    for j in range(4):
                        kc = hf * 4 + j
                        P.op('pe', lambda e: e.transpose(pt.t[:, j * 128:(j + 1) * 128],
                                                         h2b[:, kc, tb * 128:(tb + 1) * 128], ident_bf[:]),
                             r=['h2b', CK], w=[pt.k])
                    P.op('act', lambda e: e.copy(out=h2tk[:, tb, hf * 512:(hf + 1) * 512], in_=pt.t[:, 0:512]),
                         r=[pt.k], w=['h2tk'])
            P.dma('sp', [(h2tok_d[:, blk0:blk0 + nbk, :], h2tk[:, :nbk, :])], lane='h2o', r=['h2tk'], w=['h2tok'])
            if os.environ.get('KTRACE') and ti < 2:
                print('merge tile', ti, 'pre-routing', P.ninst)
            for tb in range(W // 128):
                ps = P.psum()
                for kc in range(8):
                    P.op('pe', lambda e: e.matmul(ps.t[:, :36], h2f[:, kc, tb * 128:(tb + 1) * 128], wr[:, kc, :],
                                                  start=(kc == 0), stop=(kc == 7)), r=['mt', 'wr'], w=[ps.k])
                RK = 'rt'

                def dv(fn, r=(), w=(RK,)):
                    P.op('dve', fn, r=list(r) + [RK], w=w)
                P.op('dve', lambda e: e.tensor_tensor(out=lg[:], in0=ps.t[:, :36], in1=brt[:, l, :], op=ALU.add),
                     r=[ps.k, 'par'], w=[RK])
                dv(lambda e: e.reduce_max(out=rs['mx'][:, 0:1], in_=lg[:, 0:4], axis=mybir.AxisListType.X))
                dv(lambda e: e.tensor_scalar(out=rs['ohg'][:], in0=lg[:, 0:4], scalar1=rs['mx'][:, 0:1], scalar2=None,
                                             op0=ALU.is_ge))
                dv(lambda e: e.tensor_scalar(out=rs['ex'][:], in0=lg[:, 0:4], scalar1=rs['mx'][:, 0:1], scalar2=None,
                                             op0=ALU.subtract))
                P.op('act', lambda e: e.activation(out=rs['ex'][:], in_=rs['ex'][:], func=AF.Exp), r=[RK], w=[RK])
                dv(lambda e: e.reduce_sum(out=rs['pg'][:, 0:1], in_=rs['ex'][:], axis=mybir.AxisListType.X))
                dv(lambda e: e.reciprocal(out=rs['pg'][:, 0:1], in_=rs['pg'][:, 0:1]))
                dv(lambda e: e.tensor_scalar(out=rs['ohm'][:], in0=rs['ohg'][:], scalar1=-1.0, scalar2=1.0e9,
                                             op0=ALU.add, op1=ALU.mult))
                dv(lambda e: e.tensor_tensor(out=rt['lem'][:].rearrange("p (g e) -> p g e", e=8),
                                             in0=lg[:, 4:36].rearrange("p (g e) -> p g e", e=8),
                                             in1=rs['ohm'][:].unsqueeze(2).to_broadcast([128, 4, 8]), op=ALU.add))
                dv(lambda e: e.reduce_max(out=rs['m1'][:, 0:1], in_=rt['lem'][:], axis=mybir.AxisListType.X))
                oh1 = rst[:, tb, 0:32]
                oh2 = rst[:, tb, 32:64]
                w1 = rst[:, tb, 64:65]
                w2 = rst[:, tb, 65:66]
                dv(lambda e: e.tensor_scalar(out=oh1, in0=rt['lem'][:], scalar1=rs['m1'][:, 0:1], scalar2=None,
                                             op0=ALU.is_ge), w=(RK, 'rst'))
                dv(lambda e: e.scalar_tensor_tensor(out=rt['tmp'][:], in0=oh1, scalar=-1.0e9, in1=rt['lem'][:],
                                                    op0=ALU.mult, op1=ALU.add), r=['rst'])
                dv(lambda e: e.reduce_max(out=rs['m2'][:, 0:1], in_=rt['tmp'][:], axis=mybir.AxisListType.X))
                dv(lambda e: e.tensor_scalar(out=oh2, in0=rt['tmp'][:], scalar1=rs['m2'][:, 0:1], scalar2=None,
                                             op0=ALU.is_ge), w=(RK, 'rst'))
                dv(lambda e: e.tensor_tensor(out=rs['w1'][:, 0:1], in0=rs['m1'][:, 0:1], in1=rs['m2'][:, 0:1],
                                             op=ALU.subtract))
                P.op('act', lambda e: e.activation(out=rs['w1'][:, 0:1], in_=rs['w1'][:, 0:1], func=AF.Sigmoid),
                     r=[RK], w=[RK])
                dv(lambda e: e.tensor_scalar(out=rs['w2'][:, 0:1], in0=rs['w1'][:, 0:1], scalar1=-1.0, scalar2=1.0,
                                             op0=ALU.mult, op1=ALU.add))
                dv(lambda e: e.tensor_tensor(out=w1, in0=rs['w1'][:, 0:1], in1=rs['pg'][:, 0:1], op=ALU.mult),
                   w=(RK, 'rst'))
                dv(lambda e: e.tensor_tensor(out=w2, in0=rs['w2'][:, 0:1], in1=rs['pg'][:, 0:1], op=ALU.mult),
                   w=(RK, 'rst'))
            P.dma('sp', [(rt_d[:, blk0:blk0 + nbk, :], rst[:, :nbk, :])], lane='wto', r=['rst'], w=['rt_d'])
        P.barrier()
        P.end_scope()

    def phase_moe(l):
        I32 = mybir.dt.int32
        last = (l == L - 1)
        b0 = 2 if last else 0
        nbl = NB - b0
        nblk = 48 if last else NBLK
        xsv = xs.rearrange("(kc p) t -> p kc t", p=128)
        wg_rows = w_gate.rearrange("l e k n -> (l e k) n")
        wu_rows = w_up.rearrange("l e k n -> (l e k) n")
        wd_rows = w_down.rearrange("l e k n -> (l e k) n")
        P.scope()
        RT = P.sb("RT", [128, nbl, 66], F32)
        P.dma('sp', [(RT[:], rt_d[:, b0:NB, :])], lane='hb', w=['RT'])
        IND = P.sb("IND", [128, nbl, 32], F32)
        RANK = P.sb("RANK", [128, nbl, 32], F32)
        carry = P.sb("carry", [128, 32], F32)
        SK = 'moeix'
        P.op('dve', lambda e: e.tensor_tensor(out=IND[:], in0=RT[:, :, 0:32], in1=RT[:, :, 32:64], op=ALU.add),
             r=['RT'], w=[SK])
        P.op('dve', lambda e: e.tensor_copy(out=carry[:], in_=zeros_f[:, 0:32]), r=[CK], w=['carry'])
        for c in range(nbl):
            ps = P.psum()
            P.op('pe', lambda e: e.matmul(ps.t[:, 0:32], strF[:], IND[:, c, :], start=True, stop=True),
                 r=[SK, CK], w=[ps.k])
            P.op('pe', lambda e: e.matmul(ps.t[:, 32:64], ones_f[:], IND[:, c, :], start=True, stop=True),
                 r=[SK, CK], w=[ps.k])
            P.op('dve', lambda e: e.tensor_tensor(out=RANK[:, c, :], in0=ps.t[:, 0:32], in1=carry[:], op=ALU.add),
                 r=[ps.k, 'carry'], w=['RANK'])
            P.op('dve', lambda e: e.tensor_tensor(out=carry[:], in0=ps.t[:, 32:64], in1=carry[:], op=ALU.add),
                 r=[ps.k, 'carry'], w=['carry'])
        cmp9 = P.sb("cmp9", [128, 32, 9], F32)
        thr = P.sb("thr", [128, 9], F32)
        nbe = P.sb("nbe", [128, 32], F32)
        cs = [P.sb("cs%d" % i, [128, 32], F32) for i in range(2)]
        P.op('dve', lambda e: e.tensor_scalar(out=thr[:], in0=iota[:, 0:9], scalar1=512.0, scalar2=None, op0=ALU.mult),
             r=['par'], w=['thr'])
        P.op('dve', lambda e: e.tensor_tensor(out=cmp9[:], in0=carry[:].unsqueeze(2).to_broadcast([128, 32, 9]),
                                              in1=thr[:].unsqueeze(1).to_broadcast([128, 32, 9]), op=ALU.is_gt),
             r=['carry', 'thr'], w=['cmp9'])
        P.op('dve', lambda e: e.reduce_sum(out=nbe[:], in_=cmp9[:], axis=mybir.AxisListType.X), r=['cmp9'], w=['nbe'])
        P.op('dve', lambda e: e.tensor_copy(out=cs[0][:], in_=nbe[:]), r=['nbe'], w=[('cs', 0)])
        ci = 0
        for sh in (1, 2, 4, 8, 16):
            P.op('dve', lambda e: e.tensor_copy(out=cs[1 - ci][:, 0:sh], in_=cs[ci][:, 0:sh]),
                 r=[('cs', ci)], w=[('cs', 1 - ci)])
            P.op('dve', lambda e: e.tensor_tensor(out=cs[1 - ci][:, sh:32], in0=cs[ci][:, sh:32], in1=cs[ci][:, 0:32 - sh],
                                                  op=ALU.add), r=[('cs', ci)], w=[('cs', 1 - ci)])
            ci = 1 - ci
        bend = cs[ci]
        sbase = P.sb("sbase", [128, 32], F32)
        P.op('dve', lambda e: e.tensor_tensor(out=sbase[:], in0=bend[:], in1=nbe[:], op=ALU.subtract),
             r=[('cs', ci), 'nbe'], w=['sbase'])
        P.op('dve', lambda e: e.tensor_scalar(out=sbase[:], in0=sbase[:], scalar1=512.0, scalar2=None, op0=ALU.mult),
             r=['sbase'], w=['sbase'])
        P.op('dve', lambda e: e.tensor_tensor(out=RANK[:], in0=RANK[:],
                                              in1=sbase[:].unsqueeze(1).to_broadcast([128, nbl, 32]), op=ALU.add),
             r=['RANK', 'sbase'], w=['RANK'])
        slf = P.sb("slf", [128, nbl, 2], F32)
        sli = P.sb("sli", [128, nbl, 2], I32)
        for k in range(2):
            P.op('dve', lambda e: e.tensor_tensor(out=IND[:], in0=RANK[:], in1=RT[:, :, k * 32:(k + 1) * 32],
                                                  op=ALU.mult), r=['RANK', 'RT', SK], w=[SK])
            P.op('dve', lambda e: e.reduce_sum(out=slf[:, :, k], in_=IND[:], axis=mybir.AxisListType.X),
                 r=[SK], w=['slf'])
        P.op('dve', lambda e: e.tensor_copy(out=sli[:], in_=slf[:]), r=['slf'], w=['sli'])
        cmpb = P.sb("cmpb", [128, NBLK, 32], F32)
        ebf = P.sb("ebf", [128, NBLK], F32)
        P.op('dve', lambda e: e.tensor_tensor(out=cmpb[:], in0=bend[:].unsqueeze(1).to_broadcast([128, NBLK, 32]),
                                              in1=iota[:, 0:NBLK].unsqueeze(2).to_broadcast([128, NBLK, 32]),
                                              op=ALU.is_le), r=[('cs', ci), 'par'], w=['cmpb'])
        P.op('dve', lambda e: e.reduce_sum(out=ebf[:], in_=cmpb[:], axis=mybir.AxisListType.X), r=['cmpb'], w=['ebf'])
        P.op('dve', lambda e: e.tensor_scalar(out=ebf[:], in0=ebf[:], scalar1=31.0, scalar2=float(l * NE),
                                              op0=ALU.min, op1=ALU.add), r=['ebf'], w=['ebf'])
        wixf = P.sb("wixf", [128, NBLK, 12], F32)
        wix = P.sb("wix", [128, NBLK, 12], I32)
        P.op('dve', lambda e: e.scalar_tensor_tensor(
            out=wixf[:, :, 0:8], in0=ebf[:].unsqueeze(2).to_broadcast([128, NBLK, 8]), scalar=1024.0,
            in1=wbase[:].unsqueeze(1).to_broadcast([128, NBLK, 8]), op0=ALU.mult, op1=ALU.add),
            r=['ebf', 'par'], w=['wixf'])
        P.op('dve', lambda e: e.scalar_tensor_tensor(
            out=wixf[:, :, 8:12], in0=ebf[:].unsqueeze(2).to_broadcast([128, NBLK, 4]), scalar=512.0,
            in1=wbase[:, 0:4].unsqueeze(1).to_broadcast([128, NBLK, 4]), op0=ALU.mult, op1=ALU.add),
            r=['ebf', 'par'], w=['wixf'])
        P.op('dve', lambda e: e.tensor_copy(out=wix[:], in_=wixf[:]), r=['wixf'], w=['wix'])

        if os.environ.get('KTRACE'):
            print('moe ix done', P.ninst)
        P.scope()
        hk = [P.sb("hk%d" % i, [128, 4, D], BF16) for i in range(2)]
        gi = 0
        for c0 in range(0, nbl, 4):
            nn = min(4, nbl - c0)
            b2 = gi % 2
            gi += 1
            P.dma('sp', [(hk[b2][:, :nn, :], h2tok_d[:, b0 + c0:b0 + c0 + nn, :])], lane='hk%d' % b2, w=[('hk', b2)])
            for j in range(nn):
                for k in range(2):
                    P.dma('pool', [lambda h: h.indirect_dma_start(
                        out=xs_d[:, :], out_offset=bass.IndirectOffsetOnAxis(ap=sli[:, c0 + j, k:k + 1], axis=0),
                        in_=hk[b2][:, j, :], in_offset=None)], lane='sc%d' % ((j * 2 + k) % 4),
                        r=[('hk', b2), 'sli'], w=['xs_d'])
        P.barrier()
        P.end_scope()

        if os.environ.get('KTRACE'):
            print('moe dispatch done', P.ninst)
        P.scope()
        wg = [P.sb("wg%d" % i, [128, 8, 512], BF16) for i in range(2)]
        wu = [P.sb("wu%d" % i, [128, 8, 512], BF16) for i in range(2)]
        wd = [P.sb("wd%d" % i, [128, 4, D], BF16) for i in range(2)]
        xsb = [P.sb("xsb%d" % i, [128, 4, D], BF16) for i in range(2)]
        xsT = P.sb("xsT", [128, 8, 512], BF16)
        sg = [P.sb("sg%d" % i, [128, 512], F32) for i in range(2)]
        h1 = P.sb("h1", [128, 4, 512], BF16)
        ysb = [P.sb("ysb%d" % i, [128, 4, D], BF16) for i in range(2)]
        for b in range(nblk):
            if os.environ.get('KTRACE') and b < 2:
                print('moe block', b, P.ninst)
            bi = b % 2
            WK = ('ew', bi)
            prs = []
            for kc in range(8):
                prs.append((lambda kc: lambda h: h.indirect_dma_start(
                    out=wg[bi][:, kc, :], out_offset=None, in_=wg_rows,
                    in_offset=bass.IndirectOffsetOnAxis(ap=wix[:, b, kc:kc + 1], axis=0)))(kc))
                prs.append((lambda kc: lambda h: h.indirect_dma_start(
                    out=wu[bi][:, kc, :], out_offset=None, in_=wu_rows,
                    in_offset=bass.IndirectOffsetOnAxis(ap=wix[:, b, kc:kc + 1], axis=0)))(kc))
            for fc in range(4):
                prs.append((lambda fc: lambda h: h.indirect_dma_start(
                    out=wd[bi][:, fc, :], out_offset=None, in_=wd_rows,
                    in_offset=bass.IndirectOffsetOnAxis(ap=wix[:, b, 8 + fc:9 + fc], axis=0)))(fc))
            P.dma('pool', prs, lane='ew%d' % bi, r=['wix'], w=[WK])
            P.dma('sp', [(xsb[bi][:], xs_d[b * 512:(b + 1) * 512, :].rearrange("(s p) f -> p s f", p=128))],
                  lane='xsb%d' % bi, r=['xs_d'], w=[('xsb', bi)])
            for kc in range(8):
                pt = P.psum_t()
                for sgi in range(4):
                    P.op('pe', lambda e: e.transpose(pt.t[:, sgi * 128:(sgi + 1) * 128],
                                                     xsb[bi][:, sgi, kc * 128:(kc + 1) * 128], ident_bf[:]),
                         r=[('xsb', bi), CK], w=[pt.k])
                if kc % 2 == 0:
                    P.op('act', lambda e: e.copy(out=xsT[:, kc, :], in_=pt.t[:, 0:512]), r=[pt.k], w=['xsT'])
                else:
                    P.op('dve', lambda e: e.tensor_copy(out=xsT[:, kc, :], in_=pt.t[:, 0:512]), r=[pt.k], w=['xsT'])
            for fc in range(4):
                pg = P.psum()
                pu = P.psum()
                for kc in range(8):
                    P.op('pe', lambda e: e.matmul(pg.t[:, :], wg[bi][:, kc, fc * 128:(fc + 1) * 128], xsT[:, kc, :],
                                                  start=(kc == 0), stop=(kc == 7)), r=[WK, 'xsT'], w=[pg.k])
                for kc in range(8):
                    P.op('pe', lambda e: e.matmul(pu.t[:, :], wu[bi][:, kc, fc * 128:(fc + 1) * 128], xsT[:, kc, :],
                                                  start=(kc == 0), stop=(kc == 7)), r=[WK, 'xsT'], w=[pu.k])
                f2 = fc % 2
                P.op('act', lambda e: e.activation(out=sg[f2][:], in_=pg.t[:, :], func=AF.Silu),
                     r=[pg.k], w=[('sg', f2)])
                P.op('dve', lambda e: e.tensor_tensor(out=h1[:, fc, :], in0=sg[f2][:], in1=pu.t[:, :], op=ALU.mult),
                     r=[('sg', f2), pu.k], w=['h1'])
            for sgi in range(4):
                for hf in range(2):
                    py = P.psum()
                    for fc in range(4):
                        P.op('pe', lambda e: e.matmul(py.t[:, :], h1[:, fc, sgi * 128:(sgi + 1) * 128],
                                                      wd[bi][:, fc, hf * 512:(hf + 1) * 512],
                                                      start=(fc == 0), stop=(fc == 3)), r=['h1', WK], w=[py.k])
                    if hf == 0:
                        P.op('act', lambda e: e.copy(out=ysb[bi][:, sgi, 0:512], in_=py.t[:, :]),
                             r=[py.k], w=[('ysb', bi)])
                    else:
                        P.op('dve', lambda e: e.tensor_copy(out=ysb[bi][:, sgi, 512:1024], in_=py.t[:, :]),
                             r=[py.k], w=[('ysb', bi)])
            P.dma('sp', [(ys_d[b * 512:(b + 1) * 512, :].rearrange("(s p) f -> p s f", p=128), ysb[bi][:])],
                  lane='ysb%d' % bi, r=[('ysb', bi)], w=['ys_d'])
        P.barrier()
        P.end_scope()

        if os.environ.get('KTRACE'):
            print('moe blocks done', P.ninst)
        P.scope()
        y1 = [P.sb("y1_%d" % i, [128, 4, D], BF16) for i in range(2)]
        y2 = [P.sb("y2_%d" % i, [128, 4, D], BF16) for i in range(2)]
        yc = P.sb("yc", [128, 4, D], BF16)
        ytmp = P.sb("ytmp", [128, D], F32)
        xt = P.sb("xt", [128, 8, 512], F32)
        tl = None
        if last:
            tl = {'sq': [P.sb("sq%d" % i, [128, 512], BF16) for i in range(2)],
                  'rstd': P.sb("rstd", [128, 512], F32)}
            ot = P.sb("ot", [128, 8, 512], F32)
        gi = 0
        for (t0, W, lat) in TILES:
            if last and not lat:
                continue
            ws = 0 if lat else 1
            nbk = W // 128
            c0 = t0 // 128 - b0
            g2 = gi % 2
            gi += 1
            P.dma('sp', [(xt[:, :, :W], xsv[:, :, t0:t0 + W])], lane='xm', r=['xs'], w=['xm'])
            prs = []
            for j in range(nbk):
                prs.append((lambda j: lambda h: h.indirect_dma_start(
                    out=y1[g2][:, j, :], out_offset=None, in_=ys_d[:, :],
                    in_offset=bass.IndirectOffsetOnAxis(ap=sli[:, c0 + j, 0:1], axis=0)))(j))
                prs.append((lambda j: lambda h: h.indirect_dma_start(
                    out=y2[g2][:, j, :], out_offset=None, in_=ys_d[:, :],
                    in_offset=bass.IndirectOffsetOnAxis(ap=sli[:, c0 + j, 1:2], axis=0)))(j))
            P.dma('pool', prs, lane='yg%d' % g2, r=['sli', 'ys_d'], w=[('yg', g2)])
            for j in range(nbk):
                P.op('dve', lambda e: e.tensor_scalar(out=ytmp[:], in0=y1[g2][:, j, :],
                                                      scalar1=RT[:, c0 + j, 64:65], scalar2=None, op0=ALU.mult),
                     r=[('yg', g2), 'RT'], w=['ytmp'])
                P.op('dve', lambda e: e.scalar_tensor_tensor(out=yc[:, j, :], in0=y2[g2][:, j, :],
                                                             scalar=RT[:, c0 + j, 65:66], in1=ytmp[:],
                                                             op0=ALU.mult, op1=ALU.add),
                     r=[('yg', g2), 'RT', 'ytmp'], w=['yc'])
            for kc in range(8):
                pt = P.psum_t()
                for j in range(nbk):
                    P.op('pe', lambda e: e.transpose(pt.t[:, j * 128:(j + 1) * 128], yc[:, j, kc * 128:(kc + 1) * 128],
                                                     ident_bf[:]), r=['yc', CK], w=[pt.k])
                P.op('dve', lambda e: e.scalar_tensor_tensor(
                    out=xt[:, kc, :W], in0=pt.t[:, :W], scalar=modv[:, l, 40 + kc, ws:ws + 1], in1=xt[:, kc, :W],
                    op0=ALU.mult, op1=ALU.add), r=[pt.k, 'mod', 'xm'], w=['xm'])
            if not last:
                P.dma('sp', [(xsv[:, :, t0:t0 + W], xt[:, :, :W])], lane='xmo', r=['xm'], w=['xs'])
            else:
                ss = P.psum()
                for kc in range(8):
                    sq = tl['sq'][kc % 2]
                    P.op('act', lambda e: e.activation(out=sq[:, :W], in_=xt[:, kc, :W], func=AF.Square),
                         r=['xm'], w=[('sq', kc % 2)])
                    P.op('pe', lambda e: e.matmul(ss.t[:, :W], ones_bf[:], sq[:, :W], start=(kc == 0),
                                                  stop=(kc == 7)), r=[('sq', kc % 2), CK], w=[ss.k])
                rstd = tl['rstd']
                P.op('act', lambda e: e.activation(out=rstd[:, :W], in_=ss.t[:, :W], func=AF.Sqrt, scale=1.0 / D,
                                                   bias=epsT[:, 0:1]), r=[ss.k, CK], w=['rstd'])
                P.op('dve', lambda e: e.reciprocal(out=rstd[:, :W], in_=rstd[:, :W]), r=['rstd'], w=['rstd'])
                for kc in range(8):
                    P.op('dve', lambda e: e.scalar_tensor_tensor(
                        out=ot[:, kc, :W], in0=xt[:, kc, :W], scalar=fnorm[:, kc:kc + 1], in1=rstd[:, :W],
                        op0=ALU.mult, op1=ALU.mult), r=['xm', 'rstd', 'par'], w=['ot'])
                P.dma('sp', [(outT.rearrange("(c p) t -> p c t", p=128)[:, :, t0 - CTX:t0 - CTX + W], ot[:, :, :W])],
                      lane='oo', r=['ot'], w=['outT'])
        P.barrier()
        P.end_scope()
        P.end_scope()

    with nc.named_scope('mod'):
        phase_mod()
    done = False
    for l in range(nl):
        xsrc = xT_in if l == 0 else xs
        for (nm, fn) in (('proj', lambda: phase_proj(l, xsrc)), ('delta', lambda: phase_delta(l)),
                         ('merge', lambda: phase_merge(l, xsrc)), ('moe', lambda: phase_moe(l))):
            with nc.named_scope('%s%d' % (nm, l)):
                fn()
            if stop == (l, nm):
                done = True
                break
        if done:
            break
    P.barrier()
    print("bass program: ninst=%d nwait=%d lanes=%d" % (P.ninst, P.nwait, len(P.lanes)))
    return nc


def _fm(v):
    return np.ascontiguousarray(np.asarray(v).reshape(-1, 128).T)


def prep_shared(inp):
    f32 = np.float32
    sh = {}
    b_mod = np.asarray(inp['b_mod'], f32)
    sh['bmod'] = np.ascontiguousarray(b_mod.reshape(L, 48, 128).transpose(2, 0, 1))
    sh['nmix'] = np.ascontiguousarray(np.asarray(inp['norm_mix'], f32).reshape(L, 8, 128).transpose(2, 0, 1))
    sh['nffn'] = np.ascontiguousarray(np.asarray(inp['norm_ffn'], f32).reshape(L, 8, 128).transpose(2, 0, 1))
    sh['fnorm'] = _fm(np.asarray(inp['final_norm'], f32))
    sh['convw'] = np.ascontiguousarray(np.asarray(inp['conv_w'], f32).reshape(L, 3, 24, 128).transpose(3, 0, 1, 2))
    sh['alog'] = np.ascontiguousarray(np.broadcast_to(np.asarray(inp['a_log'], f32).reshape(1, L, 16), (128, L, 16)))
    sh['dtb'] = np.ascontiguousarray(np.broadcast_to(np.asarray(inp['dt_bias'], f32).reshape(1, L, 16), (128, L, 16)))
    sh['onorm'] = np.ascontiguousarray(np.asarray(inp['out_norm'], f32).T)
    brt = np.concatenate([np.asarray(inp['b_route_group'], f32), np.asarray(inp['b_route_expert'], f32)], axis=1)
    sh['brt'] = np.ascontiguousarray(np.broadcast_to(brt.reshape(1, L, 36), (128, L, 36)))
    sh['w_rt'] = np.ascontiguousarray(np.concatenate([np.asarray(inp['w_route_group'], f32),
                                                      np.asarray(inp['w_route_expert'], f32)], axis=2))
    for k in ('w_mod', 'w_in', 'w_fourier', 'w_delta', 'w_out', 'w_gate', 'w_up', 'w_down'):
        sh[k] = np.ascontiguousarray(np.asarray(inp[k], f32))
    bf = ml_dtypes.bfloat16

    def tab(n, scale):
        k = np.arange(n, dtype=np.int64)
        ang = 2.0 * np.pi * ((k[:, None] * k[None, :]) % n).astype(np.float64) / n
        return (np.cos(ang) * scale).astype(bf), (np.sin(ang) * scale).astype(bf)
    sh['dftc'], sh['dfts'] = tab(SEQ, (SEQ * 128.0) ** -0.5)
    sh['dftc_c'], sh['dfts_c'] = tab(CTX, (CTX * 128.0) ** -0.5)
    c, s = tab(128, 1.0)
    sh['chc'] = c
    sh['chs'] = (-s.astype(np.float32)).astype(bf)
    idx = np.arange(128)
    def bd(b):
        return (idx[:, None] // b == idx[None, :] // b).astype(np.float32)
    bm = np.stack([bd(16), bd(32) - bd(16), bd(64) - bd(32), bd(128) - bd(64)], axis=1)
    sh['bmask'] = np.ascontiguousarray(bm).astype(bf)
    sh['iota'] = np.ascontiguousarray(np.broadcast_to(np.arange(64, dtype=np.float32)[None, :], (128, 64)))
    sh['wbase'] = np.ascontiguousarray((np.arange(8)[None, :] * 128 + np.arange(128)[:, None]).astype(np.float32))
    return sh


def prep_core(inp, b):
    f32 = np.float32
    x = np.asarray(inp['x'][b], f32)
    ctx = np.asarray(inp['ctx'][b], f32)
    xT = np.ascontiguousarray(np.concatenate([ctx, x], axis=0).T)
    cvec = np.stack([_fm(np.asarray(inp['c'][b], f32)), _fm(np.asarray(inp['c_ctx'], f32))], axis=2)
    return {'xT': xT, 'cvec': np.ascontiguousarray(cvec)}


_NC_CACHE = {}


def kernel(**inputs):
    if 'nc' not in _NC_CACHE:
        _NC_CACHE['nc'] = build()
    nc = _NC_CACHE['nc']
    sh = prep_shared(inputs)
    in_maps = []
    for b in range(8):
        m = dict(sh)
        m.update(prep_core(inputs, b))
        in_maps.append(m)
    res = run_bass_kernel_spmd(nc, in_maps, core_ids=list(range(8)))
    out = np.stack([np.ascontiguousarray(r["outT"].T) for r in res.results], axis=0)
    return out.astype(np.float32)
```

```python
import numpy as np
import os
import ml_dtypes
import concourse.bass as bass
import concourse.mybir as mybir
from concourse.bass_utils import run_bass_kernel_spmd

F32 = mybir.dt.float32
BF16 = mybir.dt.bfloat16
AF = mybir.ActivationFunctionType
ALU = mybir.AluOpType

D = 1024
SEQ = 4096
CTX = 256
T = CTX + SEQ
NB = T // 128
L = 4
NE = 32
OFF_A = 3072
OFF_B = 3088
OFF_Z = 3104
OFF_F = 4128
OFF_G = 4640
IN_COLS = 6688
EPS = 1e-6
TILES = [(0, 256, 0)] + [(256 + 512 * i, 512, 1) for i in range(8)]


class PsT:
    def __init__(self, t, k):
        self.t = t
        self.k = k


class Prog:
    ENG = ('pe', 'act', 'dve', 'pool', 'sp')

    def __init__(self, nc, n_lanes=64):
        self.nc = nc
        self.h = {'pe': nc.tensor, 'act': nc.scalar, 'dve': nc.vector, 'pool': nc.gpsimd, 'sp': nc.sync}
        self.sem = {}
        self.val = {}
        for e in self.ENG:
            self.sem[e] = nc.semaphore("s_" + e).__enter__()
            self.val[e] = 0
        self.n_lanes = n_lanes
        for l in range(n_lanes):
            k = ('L', l)
            self.sem[k] = nc.semaphore("s_l%d" % l).__enter__()
            self.val[k] = 0
        self.lanes = {}
        self.seen = {e: {} for e in self.ENG}
        self.clock = {}
        self.lastw = {}
        self.readers = {}
        self.ninst = 0
        self.nwait = 0
        self.stack = [[]]
        self.uid = 0
        self.psum_pool = []
        self.psum_i = 0
        self.cut = int(os.environ['KCUT']) if os.environ.get('KCUT') else None

    def scope(self):
        self.stack.append([])

    def end_scope(self):
        for cm in reversed(self.stack.pop()):
            cm.__exit__(None, None, None)

    def sb(self, name, shape, dt):
        self.uid += 1
        cm = self.nc.sbuf_tensor("%s_%d" % (name, self.uid), list(shape), dt)
        t = cm.__enter__()
        self.stack[-1].append(cm)
        return t

    def init_psum(self):
        self.NPS = 6
        for i in range(self.NPS):
            t = self.nc.psum_tensor("psb%d" % i, [128, 512], F32).__enter__()
            self.psum_pool.append(PsT(t, ('ps', i)))
        self.pst = [self.nc.psum_tensor("pst%d" % i, [128, 1024], BF16).__enter__() for i in range(2)]
        self.pst_i = 0

    def psum(self):
        p = self.psum_pool[self.psum_i % self.NPS]
        self.psum_i += 1
        return p

    def psum_bf(self):
        p = self.psum()
        return PsT(p.t[:, :].bitcast(BF16), p.k)

    def psum_t(self):
        i = self.pst_i % 2
        self.pst_i += 1
        return PsT(self.pst[i][:, 0:512], ('pst', i))

    def lane(self, name):
        if name not in self.lanes:
            assert len(self.lanes) < self.n_lanes, "out of lanes"
            self.lanes[name] = len(self.lanes)
        return self.lanes[name]

    def _deps(self, eng, reads, writes):
        deps = {}

        def add(d, kind):
            if d is None:
                return
            sk, v = d
            if sk == eng:
                if eng == 'pe' or kind != 'raw':
                    return
            if v > deps.get(sk, 0):
                deps[sk] = v
        for k in reads:
            add(self.lastw.get(k), 'raw')
        for k in writes:
            add(self.lastw.get(k), 'waw')
            for r in self.readers.get(k, ()):
                add(r, 'war')
        seen = self.seen[eng]
        return [(sk, v) for sk, v in deps.items() if seen.get(sk, 0) < v]

    def _absorb(self, eng, need):
        seen = self.seen[eng]
        for sk, v in need:
            c = self.clock.get((sk, v))
            if c:
                for k2, v2 in c.items():
                    if seen.get(k2, 0) < v2:
                        seen[k2] = v2
            if seen.get(sk, 0) < v:
                seen[sk] = v

    def _emit_waits(self, eng, need):
        h = self.h[eng]
        for sk, v in need[:-1]:
            h.wait_ge(self.sem[sk], v)
            self.nwait += 1
        return need[-1] if need else None

    def _record(self, tag, reads, writes):
        for k in writes:
            self.lastw[k] = tag
            self.readers[k] = []
        for k in reads:
            self.readers.setdefault(k, []).append(tag)

    def op(self, eng, fn, r=(), w=()):
        if self.cut is not None and self.ninst >= self.cut:
            return None
        need = self._deps(eng, r, w)
        last = self._emit_waits(eng, need)
        ins = fn(self.h[eng])
        if last is not None:
            ins._wait_ge(self.sem[last[0]], last[1])
        self._absorb(eng, need)
        self.val[eng] += 1
        v = self.val[eng]
        ins.then_inc(self.sem[eng], 1)
        snap = dict(self.seen[eng])
        snap[eng] = v
        self.clock[(eng, v)] = snap
        self._record((eng, v), r, w)
        self.ninst += 1
        return ins

    def dma(self, q, pairs, lane, r=(), w=()):
        if self.cut is not None and self.ninst >= self.cut:
            return None
        lk = ('L', self.lane(lane))
        need = self._deps(q, r, w)
        pv = self.val[lk]
        if pv > 0 and self.seen[q].get(lk, 0) < pv:
            need = [(sk, v) for sk, v in need if sk != lk] + [(lk, pv)]
        last = self._emit_waits(q, need)
        h = self.h[q]
        first = True
        for pr in pairs:
            if callable(pr):
                ins = pr(h)
            else:
                ins = h.dma_start(out=pr[0], in_=pr[1])
            if first and last is not None:
                ins._wait_ge(self.sem[last[0]], last[1])
            first = False
            ins.then_inc(self.sem[lk], 16)
            self.val[lk] += 16
            self.ninst += 1
        self._absorb(q, need)
        v = self.val[lk]
        snap = dict(self.seen[q])
        snap[lk] = v
        self.clock[(lk, v)] = snap
        self._record((lk, v), r, w)

    def wait_all(self, eng, keys):
        need = self._deps(eng, keys, ())
        for sk, v in need:
            self.h[eng].wait_ge(self.sem[sk], v)
        self._absorb(eng, need)

    def barrier(self):
        for e in self.ENG:
            need = [(sk, v) for sk, v in self.val.items()
                    if v > 0 and sk != e and self.seen[e].get(sk, 0) < v]
            for sk, v in need:
                self.h[e].wait_ge(self.sem[sk], v)
                self.nwait += 1
            for sk, v in need:
                self.seen[e][sk] = v


def build(nl=L, stop=None, dbg=(), moe_w=True):
    nc = bass.Bass("TRN2", target_bir_lowering=False)
    P = Prog(nc)
    P.init_psum()

    def din(name, shape, dt=F32):
        return nc.dram_tensor(name, list(shape), dt, kind="ExternalInput").ap()

    def dscr(name, shape, dt):
        kind = "ExternalOutput" if name in dbg else "Internal"
        return nc.dram_tensor(name, list(shape), dt, kind=kind).ap()

    xT_in = din("xT", [D, T])
    cvec_d = din("cvec", [128, 8, 2])
    bmod_d = din("bmod", [128, L, 48])
    nmix_d = din("nmix", [128, L, 8])
    nffn_d = din("nffn", [128, L, 8])
    fnorm_d = din("fnorm", [128, 8])
    convw_d = din("convw", [128, L, 3, 24])
    alog_d = din("alog", [128, L, 16])
    dtb_d = din("dtb", [128, L, 16])
    onorm_d = din("onorm", [128, L])
    brt_d = din("brt", [128, L, 36])
    w_mod = din("w_mod", [nl, D, 6 * D])
    w_in = din("w_in", [nl, D, IN_COLS])
    w_fourier = din("w_fourier", [nl, 512, D])
    w_delta = din("w_delta", [nl, D, D])
    w_out = din("w_out", [nl, D, D])
    w_rt = din("w_rt", [nl, D, 36])
    NEd = NE if moe_w else 1
    w_gate = din("w_gate", [nl, NEd, D, 512])
    w_up = din("w_up", [nl, NEd, D, 512])
    w_down = din("w_down", [nl, NEd, 512, D])
    dftc = din("dftc", [SEQ, SEQ], BF16)
    dfts = din("dfts", [SEQ, SEQ], BF16)
    dftc_c = din("dftc_c", [CTX, CTX], BF16)
    dfts_c = din("dfts_c", [CTX, CTX], BF16)
    chc_d = din("chc", [128, 128], BF16)
    chs_d = din("chs", [128, 128], BF16)
    bmask_d = din("bmask", [128, 4, 128], BF16)
    iota_d = din("iota", [128, 64], F32)
    wbase_d = din("wbase", [128, 8], F32)
    outT = nc.dram_tensor("outT", [D, SEQ], F32, kind="ExternalOutput").ap()

    xs = dscr("xs", [D, T], F32)
    qT = dscr("qT", [D, T], BF16)
    kT = dscr("kT", [D, T], BF16)
    k_tok = dscr("k_tok", [8, 128, T], BF16)
    v_tok = dscr("v_tok", [8, 128, T], BF16)
    ab_tok = dscr("ab_tok", [128, NB, 32], F32)
    zT = dscr("zT", [D, T], BF16)
    u_tok = dscr("u_tok", [128, NB, 512], BF16)
    gT = dscr("gT", [2 * D, T], BF16)
    odT = dscr("odT", [D, T], BF16)
    h2tok_d = dscr("h2tok", [128, NB, D], BF16)
    rt_d = dscr("rt", [128, NB, 66], F32)
    NBLK = 49
    xs_d = dscr("xs_slots", [NBLK * 512, D], BF16)
    ys_d = dscr("ys_slots", [NBLK * 512, D], BF16)

    ones_bf = P.sb("ones_bf", [128, 128], BF16)
    ones_f = P.sb("ones_f", [128, 128], F32)
    zeros_f = P.sb("zeros_f", [128, 128], F32)
    ident_bf = P.sb("ident_bf", [128, 128], BF16)
    ident_f = P.sb("ident_f", [128, 128], F32)
    triF = P.sb("triF", [128, 128], F32)
    triB = P.sb("triB", [128, 128], F32)
    negF = P.sb("negF", [128, 128], F32)
    negB = P.sb("negB", [128, 128], F32)
    strF = P.sb("strF", [128, 128], F32)
    strB = P.sb("strB", [128, 128], F32)
    epsT = P.sb("epsT", [128, 1], F32)
    CK = 'const'

    def pool(fn, r=(), w=(CK,)):
        P.op('pool', fn, r=r, w=w)
    pool(lambda e: e.memset(ones_f[:], 1.0))
    pool(lambda e: e.memset(zeros_f[:], 0.0))
    pool(lambda e: e.memset(epsT[:], EPS))
    P.op('dve', lambda e: e.tensor_copy(out=ones_bf[:], in_=ones_f[:]), r=[CK], w=[CK])

    def asel(out, in_, pat, cm, op, fill):
        pool(lambda e: e.affine_select(out=out, in_=in_, pattern=pat, compare_op=op, fill=fill,
                                       base=0, channel_multiplier=cm), r=[CK])
    asel(ident_f[:], ones_f[:], [[1, 128]], -1, ALU.is_equal, 0.0)
    asel(triF[:], ones_f[:], [[1, 128]], -1, ALU.is_ge, 0.0)
    asel(triB[:], ones_f[:], [[-1, 128]], 1, ALU.is_ge, 0.0)
    asel(negF[:], zeros_f[:], [[1, 128]], -1, ALU.is_ge, -1.0e5)
    asel(negB[:], zeros_f[:], [[-1, 128]], 1, ALU.is_ge, -1.0e5)
    asel(strF[:], ones_f[:], [[1, 128]], -1, ALU.is_gt, 0.0)
    asel(strB[:], ones_f[:], [[-1, 128]], 1, ALU.is_gt, 0.0)
    P.op('dve', lambda e: e.tensor_copy(out=ident_bf[:], in_=ident_f[:]), r=[CK], w=[CK])

    bmod = P.sb("bmod", [128, L, 48], F32)
    nmix = P.sb("nmix", [128, L, 8], F32)
    nffn = P.sb("nffn", [128, L, 8], F32)
    fnorm = P.sb("fnorm", [128, 8], F32)
    convw = P.sb("convw", [128, L, 3, 24], F32)
    alog = P.sb("alog", [128, L, 16], F32)
    dtb = P.sb("dtb", [128, L, 16], F32)
    onorm = P.sb("onorm", [128, L], F32)
    brt = P.sb("brt", [128, L, 36], F32)
    chc = P.sb("chc", [128, 128], BF16)
    chs = P.sb("chs", [128, 128], BF16)
    cv = P.sb("cv", [128, 8, 2], F32)
    bmask = P.sb("bmask", [128, 4, 128], BF16)
    iota = P.sb("iota", [128, 64], F32)
    wbase = P.sb("wbase", [128, 8], F32)
    P.dma('sp', [(bmod[:], bmod_d), (nmix[:], nmix_d), (nffn[:], nffn_d), (fnorm[:], fnorm_d),
                 (convw[:], convw_d), (alog[:], alog_d), (dtb[:], dtb_d), (onorm[:], onorm_d),
                 (brt[:], brt_d), (chc[:], chc_d), (chs[:], chs_d), (cv[:], cvec_d), (bmask[:], bmask_d), (iota[:], iota_d), (wbase[:], wbase_d)],
          lane='par', w=['par'])
    modv = P.sb("modv", [128, L, 48, 2], F32)
    A1 = P.sb("A1", [128, L, 8, 2], F32)
    A2 = P.sb("A2", [128, L, 8, 2], F32)
    nea = P.sb("nea", [128, L, 16], F32)
    P.op('act', lambda e: e.activation(out=nea[:], in_=alog[:], func=AF.Exp), r=['par'], w=['nea'])
    P.op('dve', lambda e: e.tensor_scalar(out=nea[:], in0=nea[:], scalar1=-1.0, scalar2=None, op0=ALU.mult),
         r=['nea'], w=['nea'])

    def phase_mod():
        P.scope()
        sv = P.sb("sv", [128, 8, 2], F32)
        P.op('act', lambda e: e.activation(out=sv[:], in_=cv[:], func=AF.Silu), r=['par'], w=['sv'])
        wts = [P.sb("wmt%d" % i, [128, 8, 512], F32) for i in range(2)]
        it = 0
        for l in range(nl):
            wv = w_mod[l].rearrange("(kc p) n -> p kc n", p=128)
            for cb in range(12):
                b = it % 2
                it += 1
                wt = wts[b]
                P.dma('sp', [(wt[:], wv[:, :, cb * 512:(cb + 1) * 512])], lane='wm%d' % b, w=[('wm', b)])
                ps = P.psum()
                for j in range(4):
                    for kc in range(8):
                        P.op('pe', lambda e: e.matmul(ps.t[:, j * 2:j * 2 + 2], wt[:, kc, j * 128:(j + 1) * 128],
                                                      sv[:, kc, :], start=(kc == 0), stop=(kc == 7)),
                             r=[('wm', b), 'sv'], w=[ps.k])
                P.op('dve', lambda e: e.tensor_tensor(
                    out=modv[:, l, cb * 4:cb * 4 + 4, :],
                    in0=ps.t[:, 0:8].rearrange("p (a b) -> p a b", b=2),
                    in1=bmod[:, l, cb * 4:cb * 4 + 4].unsqueeze(2).to_broadcast([128, 4, 2]),
                    op=ALU.add), r=[ps.k, 'par'], w=['mod'])
            for (A, nw, c0) in ((A1, nmix, 8), (A2, nffn, 32)):
                P.op('dve', lambda e: e.tensor_scalar(out=A[:, l], in0=modv[:, l, c0:c0 + 8, :], scalar1=1.0,
                                                      scalar2=None, op0=ALU.add), r=['mod'], w=['mod'])
                P.op('dve', lambda e: e.tensor_tensor(out=A[:, l], in0=A[:, l],
                                                      in1=nw[:, l, :].unsqueeze(2).to_broadcast([128, 8, 2]),
                                                      op=ALU.mult), r=['mod', 'par'], w=['mod'])
        P.barrier()
        P.end_scope()

    def norm_mod(xt, xkey, W, Aap, Bap, outs, tl):
        ss = P.psum()
        for kc in range(8):
            sq = tl['sq'][kc % 2]
            P.op('act', lambda e: e.activation(out=sq[:, :W], in_=xt[:, kc, :W], func=AF.Square),
                 r=[xkey], w=[('sq', kc % 2)])
            P.op('pe', lambda e: e.matmul(ss.t[:, :W], ones_bf[:], sq[:, :W], start=(kc == 0), stop=(kc == 7)),
                 r=[('sq', kc % 2), CK], w=[ss.k])
        rstd = tl['rstd']
        P.op('act', lambda e: e.activation(out=rstd[:, :W], in_=ss.t[:, :W], func=AF.Sqrt, scale=1.0 / D,
                                           bias=epsT[:, 0:1]), r=[ss.k, CK], w=['rstd'])
        P.op('dve', lambda e: e.reciprocal(out=rstd[:, :W], in_=rstd[:, :W]), r=['rstd'], w=['rstd'])
        for kc in range(8):
            tmp = tl['tmp'][kc % 2]
            P.op('dve', lambda e: e.tensor_tensor(out=tmp[:, :W], in0=xt[:, kc, :W], in1=rstd[:, :W], op=ALU.mult),
                 r=[xkey, 'rstd'], w=[('tmp', kc % 2)])
            for (fn, ok) in outs:
                P.op('act', lambda e: e.activation(out=fn(kc), in_=tmp[:, :W], func=AF.Identity,
                                                   scale=Aap[:, kc:kc + 1], bias=Bap[:, kc:kc + 1]),
                     r=[('tmp', kc % 2), 'mod'], w=[ok])

    def phase_proj(l, xsrc):
        P.scope()
        last = (l == L - 1)
        hT = P.sb("hT", [128, 8, T], BF16)
        P.scope()
        xts = [P.sb("xt%d" % i, [128, 8, 512], F32) for i in range(2)]
        tl = {'sq': [P.sb("sq%d" % i, [128, 512], BF16) for i in range(2)],
              'tmp': [P.sb("tmp%d" % i, [128, 512], F32) for i in range(2)],
              'rstd': P.sb("rstd", [128, 512], F32)}
        xv = xsrc.rearrange("(kc p) t -> p kc t", p=128)
        for ti, (t0, W, lat) in enumerate(TILES):
            ws = 0 if lat else 1
            xt = xts[ti % 2]
            P.dma('sp', [(xt[:, :, :W], xv[:, :, t0:t0 + W])], lane='xt%d' % (ti % 2), w=[('xt', ti % 2)])
            norm_mod(xt, ('xt', ti % 2), W, A1[:, l, :, ws], modv[:, l, 0:8, ws],
                     [(lambda kc: hT[:, kc, t0:t0 + W], ('hT', ti))], tl)
        P.barrier()
        P.end_scope()
        wps = [P.sb("wp%d" % i, [128, 8, 1024], BF16) for i in range(2)]
        st8 = [P.sb("st8_%d" % i, [128, 8, 512], BF16) for i in range(2)]
        tokst = [P.sb("tokst%d" % i, [128, 8, 4, 128], BF16) for i in range(2)]
        ust = [P.sb("ust%d" % i, [128, 4, 512], BF16) for i in range(2)]
        abst = P.sb("abst", [128, 4, 32], F32)
        NSL = 6
        accS = [P.sb("acc%d" % i, [128, 512], F32) for i in range(NSL)]
        actfS = [P.sb("actf%d" % i, [128, 512], F32) for i in range(NSL)]
        sqbS = [P.sb("sqb%d" % i, [128, 512], BF16) for i in range(NSL)]
        actbS = sqbS
        rnS = [P.sb("rn%d" % i, [128, 512], F32) for i in range(NSL)]
        wv = w_in[l].rearrange("(kc p) n -> p kc n", p=128)
        pieces = [('q', 0, 1024), ('k', 1024, 1024), ('v', 2048, 1024), ('ab', OFF_A, 32), ('z', OFF_Z, 1024),
                  ('F', OFF_F, 512), ('Ga', OFF_G, 1024), ('Gb', OFF_G + 1024, 1024)]
        cnt = {'st8': 0, 'tok': 0, 'u': 0}
        for pi, (pn, c0, ncol) in enumerate(pieces):
            if os.environ.get('KSCOPE'):
                if pi > 0:
                    nc.leave_named_scope(*_sc, False)
                _sc = ('pj_' + pn, nc.enter_named_scope('pj_' + pn, False)[0])
            b = pi % 2
            wp = wps[b]
            wk = ('wp', b)
            P.dma('pool', [(wp[:, :, :ncol], wv[:, :, c0:c0 + ncol])], lane='wp%d' % b, w=[wk])
            for ti, (t0, W, lat) in enumerate(TILES):
                hk = ('hT', ti)
                nbk = W // 128
                if (not lat) and last and pn in ('z', 'F', 'Ga', 'Gb'):
                    continue
                if pn in ('q', 'k', 'v'):
                    si = cnt['st8'] % 2
                    st = st8[si]
                    sk = ('st8', si)
                    if pn in ('k', 'v'):
                        tki = cnt['tok'] % 2
                        cnt['tok'] += 1
                        tk = tokst[tki]
                        tkk = ('tok', tki)
                    def job(cc, sl):
                        gcc = (c0 // 128) + cc
                        ps = P.psum_pool[sl]
                        acc, actf, actb, sqb, rn = accS[sl], actfS[sl], actbS[sl], sqbS[sl], rnS[sl]
                        ka, kf, kb, kq, kr = ('acc', sl), ('actf', sl), ('sqb', sl), ('sqb', sl), ('rn', sl)
                        for kc in range(8):
                            P.op('pe', lambda e: e.matmul(ps.t[:, :W], wp[:, kc, cc * 128:(cc + 1) * 128],
                                                          hT[:, kc, t0:t0 + W], start=(kc == 0), stop=(kc == 7)),
                                 r=[wk, hk], w=[ps.k])
                        yield
                        rl = 64 if lat else 256
                        pv = ps.t[:, :W].rearrange("p (r c) -> p r c", c=rl)
                        av = acc[:, :W].rearrange("p (r c) -> p r c", c=rl)
                        P.op('dve', lambda e: e.tensor_scalar(out=acc[:, :W], in0=ps.t[:, :W],
                                                              scalar1=convw[:, l, 1, gcc:gcc + 1], scalar2=None,
                                                              op0=ALU.mult), r=[ps.k, 'par'], w=[ka])
                        P.op('dve', lambda e: e.scalar_tensor_tensor(
                            out=av[:, :, 1:rl], in0=pv[:, :, 0:rl - 1], scalar=convw[:, l, 0, gcc:gcc + 1],
                            in1=av[:, :, 1:rl], op0=ALU.mult, op1=ALU.add), r=[ps.k, 'par', ka], w=[ka])
                        P.op('dve', lambda e: e.scalar_tensor_tensor(
                            out=av[:, :, 0:rl - 1], in0=pv[:, :, 1:rl], scalar=convw[:, l, 2, gcc:gcc + 1],
                            in1=av[:, :, 0:rl - 1], op0=ALU.mult, op1=ALU.add), r=[ps.k, 'par', ka], w=[ka])
                        yield
                        if pn == 'v':
                            P.op('act', lambda e: e.activation(out=actb[:, :W], in_=acc[:, :W], func=AF.Silu),
                                 r=[ka], w=[kb])
                            src, srk = actb, kb
                            yield
                        else:
                            P.op('act', lambda e: e.activation(out=actf[:, :W], in_=acc[:, :W], func=AF.Silu),
                                 r=[ka], w=[kf])
                            yield
                            P.op('act', lambda e: e.activation(out=sqb[:, :W], in_=actf[:, :W], func=AF.Square),
                                 r=[kf], w=[kq])
                            yield
                            P.op('pe', lambda e: e.matmul(ps.t[:, :W], ones_bf[:], sqb[:, :W], start=True, stop=True),
                                 r=[kq, CK], w=[ps.k])
                            yield
                            P.op('act', lambda e: e.activation(out=rn[:, :W], in_=ps.t[:, :W], func=AF.Sqrt,
                                                               bias=epsT[:, 0:1]), r=[ps.k, CK], w=[kr])
                            yield
                            P.op('dve', lambda e: e.reciprocal(out=rn[:, :W], in_=rn[:, :W]), r=[kr], w=[kr])
                            sc = (128.0 ** -0.5) if pn == 'q' else 1.0
                            P.op('dve', lambda e: e.scalar_tensor_tensor(
                                out=st[:, cc, :W], in0=actf[:, :W], scalar=sc, in1=rn[:, :W],
                                op0=ALU.mult, op1=ALU.mult), r=[kf, kr], w=[sk])
                            src, srk = st[:, cc], sk
                            yield
                        if pn in ('k', 'v'):
                            ptv = ps.t[:, :].bitcast(BF16)
                            for tb in range(nbk):
                                P.op('pe', lambda e: e.transpose(ptv[:, tb * 128:(tb + 1) * 128],
                                                                 src[:, tb * 128:(tb + 1) * 128], ident_bf[:]),
                                     r=[srk, CK], w=[ps.k])
                            yield
                            P.op('act', lambda e: e.copy(
                                out=tk[:, cc, 0:nbk, :],
                                in_=ptv[:, :nbk * 128].rearrange("p (a b) -> p a b", b=128)),
                                r=[ps.k], w=[tkk])
                            yield

                    pendj = list(range(8))
                    slotsj = {}
                    while pendj or slotsj:
                        for sl in range(NSL):
                            if sl not in slotsj and pendj:
                                slotsj[sl] = job(pendj.pop(0), sl)
                        for sl in list(slotsj.keys()):
                            try:
                                next(slotsj[sl])
                            except StopIteration:
                                del slotsj[sl]
                    if pn in ('q', 'k'):
                        dst = qT if pn == 'q' else kT
                        P.dma('sp', [(dst.rearrange("(c p) t -> p c t", p=128)[:, :, t0:t0 + W], st[:, :, :W])],
                              lane='st8_%d' % si, r=[sk], w=[pn + 'T'])
                        cnt['st8'] += 1
                    if pn in ('k', 'v'):
                        dst = k_tok if pn == 'k' else v_tok
                        P.dma('sp', [(dst[:, :, t0:t0 + W].rearrange("c p t -> p c t"),
                                      tk[:, :, :nbk, :].rearrange("p c a b -> p c (a b)"))],
                              lane='tok%d' % tki, r=[tkk], w=[pn + '_tok'])
                elif pn == 'ab':
                    ps = P.psum()
                    for tb in range(nbk):
                        for kc in range(8):
                            P.op('pe', lambda e: e.matmul(ps.t[:, tb * 32:(tb + 1) * 32],
                                                          hT[:, kc, t0 + tb * 128:t0 + (tb + 1) * 128],
                                                          wp[:, kc, 0:32], start=(kc == 0), stop=(kc == 7)),
                                 r=[wk, hk], w=[ps.k])
                    P.op('dve', lambda e: e.tensor_copy(out=abst[:, :nbk, :],
                                                        in_=ps.t[:, :nbk * 32].rearrange("p (a b) -> p a b", b=32)),
                         r=[ps.k], w=['abst'])
                    P.dma('sp', [(ab_tok[:, t0 // 128:t0 // 128 + nbk, :], abst[:, :nbk, :])],
                          lane='abst', r=['abst'], w=['ab_tok'])
                elif pn in ('z', 'Ga', 'Gb'):
                    si = cnt['st8'] % 2
                    cnt['st8'] += 1
                    st = st8[si]
                    sk = ('st8', si)
                    fnc = AF.Silu if pn == 'z' else AF.Sigmoid
                    for cc in range(8):
                        ps = P.psum()
                        for kc in range(8):
                            P.op('pe', lambda e: e.matmul(ps.t[:, :W], wp[:, kc, cc * 128:(cc + 1) * 128],
                                                          hT[:, kc, t0:t0 + W], start=(kc == 0), stop=(kc == 7)),
                                 r=[wk, hk], w=[ps.k])
                        P.op('act', lambda e: e.activation(out=st[:, cc, :W], in_=ps.t[:, :W], func=fnc),
                             r=[ps.k], w=[sk])
                    if pn == 'z':
                        dv = zT.rearrange("(c p) t -> p c t", p=128)
                    else:
                        o8 = 0 if pn == 'Ga' else 8
                        dv = gT.rearrange("(c p) t -> p c t", p=128)[:, o8:o8 + 8, :]
                    P.dma('sp', [(dv[:, :, t0:t0 + W], st[:, :, :W])], lane='st8_%d' % si, r=[sk], w=[pn])
                elif pn == 'F':
                    ui = cnt['u'] % 2
                    cnt['u'] += 1
                    us = ust[ui]
                    uk = ('ust', ui)
                    for tb in range(nbk):
                        ps = P.psum()
                        for kc in range(8):
                            P.op('pe', lambda e: e.matmul(ps.t[:, :512], hT[:, kc, t0 + tb * 128:t0 + (tb + 1) * 128],
                                                          wp[:, kc, 0:512], start=(kc == 0), stop=(kc == 7)),
                                 r=[wk, hk], w=[ps.k])
                        P.op('act', lambda e: e.copy(out=us[:, tb, :], in_=ps.t[:, :512]), r=[ps.k], w=[uk])
                    P.dma('sp', [(u_tok[:, t0 // 128:t0 // 128 + nbk, :], us[:, :nbk, :])],
                          lane='ust%d' % ui, r=[uk], w=['u_tok'])
        if os.environ.get('KSCOPE'):
            nc.leave_named_scope(*_sc, False)
        P.barrier()
        P.end_scope()

    def phase_delta(l):
        P.scope()
        last = (l == L - 1)
        ab = P.sb("ab", [128, NB, 32], F32)
        P.dma('sp', [(ab[:], ab_tok)], lane='ab', w=['ab'])
        g = P.sb("g", [128, NB, 16], F32)
        beta = P.sb("beta", [128, NB, 16], F32)
        nbeta = P.sb("nbeta", [128, NB, 16], F32)
        gcum = P.sb("gcum", [128, NB, 16], F32)
        ngam = P.sb("ngam", [128, NB, 16], F32)
        tail = P.sb("tail", [128, NB, 16], F32)
        egl = P.sb("egl", [128, NB, 16], F32)
        GK = 'gates'
        P.op('dve', lambda e: e.tensor_tensor(out=g[:], in0=ab[:, :, 0:16],
                                              in1=dtb[:, l, :].unsqueeze(1).to_broadcast([128, NB, 16]), op=ALU.add),
             r=['ab', 'par'], w=[GK])
        P.op('act', lambda e: e.activation(out=g[:], in_=g[:], func=AF.Exp), r=[GK], w=[GK])
        P.op('act', lambda e: e.activation(out=g[:], in_=g[:], func=AF.Ln, bias=ones_f[:, 0:1]), r=[GK, CK], w=[GK])
        P.op('dve', lambda e: e.tensor_tensor(out=g[:], in0=g[:],
                                              in1=nea[:, l, :].unsqueeze(1).to_broadcast([128, NB, 16]), op=ALU.mult),
             r=[GK, 'nea'], w=[GK])
        P.op('act', lambda e: e.activation(out=beta[:], in_=ab[:, :, 16:32], func=AF.Sigmoid), r=['ab'], w=[GK])
        P.op('dve', lambda e: e.tensor_scalar(out=nbeta[:], in0=beta[:], scalar1=-1.0, scalar2=None, op0=ALU.mult),
             r=[GK], w=[GK])
        for d in range(2):
            tri = triF if d == 0 else triB
            ps = P.psum()
            P.op('pe', lambda e: e.matmul(ps.t[:, :NB * 8].rearrange("p (a b) -> p a b", b=8), tri[:],
                                          g[:, :, d * 8:(d + 1) * 8], start=True, stop=True), r=[GK, CK], w=[ps.k])
            P.op('dve', lambda e: e.tensor_copy(out=gcum[:, :, d * 8:(d + 1) * 8],
                                                in_=ps.t[:, :NB * 8].rearrange("p (a b) -> p a b", b=8)),
                 r=[ps.k], w=[GK])
            ps2 = P.psum()
            P.op('pe', lambda e: e.matmul(ps2.t[:, :NB * 8].rearrange("p (a b) -> p a b", b=8), ones_f[:],
                                          g[:, :, d * 8:(d + 1) * 8], start=True, stop=True), r=[GK, CK], w=[ps2.k])
            P.op('act', lambda e: e.activation(out=egl[:, :, d * 8:(d + 1) * 8],
                                               in_=ps2.t[:, :NB * 8].rearrange("p (a b) -> p a b", b=8), func=AF.Exp),
                 r=[ps2.k], w=[GK])
            P.op('dve', lambda e: e.tensor_tensor(out=tail[:, :, d * 8:(d + 1) * 8],
                                                  in0=ps2.t[:, :NB * 8].rearrange("p (a b) -> p a b", b=8),
                                                  in1=gcum[:, :, d * 8:(d + 1) * 8], op=ALU.subtract),
                 r=[ps2.k, GK], w=[GK])
        P.op('act', lambda e: e.activation(out=tail[:], in_=tail[:], func=AF.Exp), r=[GK], w=[GK])
        P.op('act', lambda e: e.activation(out=ngam[:], in_=gcum[:], func=AF.Exp), r=[GK], w=[GK])
        P.op('dve', lambda e: e.tensor_scalar(out=ngam[:], in0=ngam[:], scalar1=-1.0, scalar2=None, op0=ALU.mult),
             r=[GK], w=[GK])

        DSTOP = os.environ.get('KDSTOP', '')
        if DSTOP == 'gates':
            P.barrier()
            P.end_scope()
            return
        qTh = P.sb("qTh", [128, T], BF16)
        kTh = P.sb("kTh", [128, T], BF16)
        ktk = P.sb("ktk", [128, NB, 128], BF16)
        vtk = P.sb("vtk", [128, NB, 128], BF16)
        oacc = P.sb("oacc", [128, T], F32)
        Tt = P.sb("Tt", [128, NB, 2, 128], BF16)
        AT = P.sb("AT", [128, NB, 2, 128], BF16)
        qdec = P.sb("qdec", [128, NB, 2, 128], BF16)
        ktail = P.sb("ktail", [128, NB, 2, 128], BF16)
        GP = 4
        Ef = [P.sb("Ef%d" % i, [128, 2, 128], F32) for i in range(GP)]
        Es = [P.sb("Es%d" % i, [128, 2, 128], F32) for i in range(GP)]
        Ge = [P.sb("Ge%d" % i, [128, 2, 128], F32) for i in range(GP)]
        ABt = [[P.sb("ABt%d_%d" % (g_, i), [128, 2, 2, 128], BF16) for i in range(2)] for g_ in range(GP)]
        Ttw = [[P.sb("Ttw%d_%d" % (g_, i), [128, 2, 128], BF16) for i in range(2)] for g_ in range(GP)]
        Xf = [P.sb("Xf%d" % i, [128, 2, 128], BF16) for i in range(GP)]
        XfT = [P.sb("XfT%d" % i, [128, 2, 128], BF16) for i in range(GP)]
        UT = [P.sb("UT%d" % i, [128, 3, 2, 128], BF16) for i in range(GP)]
        Hh = [P.sb("Hh%d" % i, [128, 2, 128], BF16) for i in range(GP)]
        Wn = [P.sb("Wn%d" % i, [128, 2, 128], BF16) for i in range(GP)]
        Sf = [P.sb("Sf%d" % i, [128, 128], F32) for i in range(2)]
        Sb = [P.sb("Sb%d" % i, [128, 128], BF16) for i in range(2)]
        Rt = [P.sb("Rt%d" % i, [128, 128], BF16) for i in range(2)]
        vn = [P.sb("vn%d" % i, [128, 128], BF16) for i in range(2)]
        sqo = [P.sb("sqo%d" % i, [128, 512], BF16) for i in range(2)]
        rno = P.sb("rno", [128, 512], F32)
        odf = P.sb("odf", [128, 512], F32)
        odst = [P.sb("odst%d" % i, [128, 512], BF16) for i in range(2)]
        tris = (triF, triB)
        negs = (negF, negB)
        strs = (strF, strB)
        uidx = 0
        for h in range(8):
            hs = slice(h * 128, (h + 1) * 128)
            HK = ('hd', 0)
            P.dma('sp', [(qTh[:], qT[hs, :]), (kTh[:], kT[hs, :]),
                         (ktk[:].rearrange("p b f -> p (b f)"), k_tok[h]),
                         (vtk[:].rearrange("p b f -> p (b f)"), v_tok[h])], lane='hd', w=[HK])
            OK_ = [('oacc', n) for n in range(NB)]
            P.op('pool', lambda e: e.memset(oacc[:], 0.0), w=OK_)
            def pre(n, g_):
                bs = slice(n * 128, (n + 1) * 128)
                kE, kS, kG, kX, kXT, kU, kH, kW = (('Ef', g_), ('Es', g_), ('Ge', g_), ('Xf', g_), ('XfT', g_),
                                                   ('UT', g_), ('Hh', g_), ('Wn', g_))
                AB = ABt[g_]
                TW = Ttw[g_]
                bank = P.psum_pool[g_]
                bankbf = PsT(bank.t[:, :].bitcast(BF16), bank.k)
                pk = bank
                P.op('pe', lambda e: e.matmul(pk.t[:, 0:128], kTh[:, bs], kTh[:, bs], start=True, stop=True),
                     r=[HK], w=[pk.k])
                P.op('pe', lambda e: e.matmul(pk.t[:, 128:256], kTh[:, bs], qTh[:, bs], start=True, stop=True),
                     r=[HK], w=[pk.k])
                for d in range(2):
                    col = d * 8 + h
                    P.op('pe', lambda e: e.matmul(pk.t[:, 256 + d * 128:384 + d * 128],
                                                  g[:, n, col:col + 1].to_broadcast([128, 128]), tris[d][:],
                                                  start=True, stop=True), r=[GK, CK], w=[pk.k])
                yield
                for d in range(2):
                    col = d * 8 + h
                    P.op('dve', lambda e: e.scalar_tensor_tensor(
                        out=Ef[g_][:, d, :], in0=pk.t[:, 256 + d * 128:384 + d * 128], scalar=gcum[:, n, col:col + 1],
                        in1=negs[d][:], op0=ALU.subtract, op1=ALU.add), r=[pk.k, GK, CK], w=[kE])
                yield
                P.op('act', lambda e: e.activation(out=Ef[g_][:], in_=Ef[g_][:], func=AF.Exp), r=[kE], w=[kE])
                P.op('act', lambda e: e.activation(out=Ge[g_][:],
                                                   in_=pk.t[:, 256:512].rearrange("p (a b) -> p a b", b=128),
                                                   func=AF.Exp), r=[pk.k], w=[kG])
                yield
                for d in range(2):
                    P.op('dve', lambda e: e.tensor_tensor(out=Es[g_][:, d, :], in0=Ef[g_][:, d, :], in1=strs[d][:],
                                                          op=ALU.mult), r=[kE, CK], w=[kS])
                for d in range(2):
                    col = d * 8 + h
                    P.op('dve', lambda e: e.tensor_tensor(out=AT[:, n, d, :], in0=pk.t[:, 128:256], in1=Ef[g_][:, d, :],
                                                          op=ALU.mult), r=[pk.k, kE], w=[('pre', n)])
                    P.op('dve', lambda e: e.scalar_tensor_tensor(
                        out=Xf[g_][:, d, :], in0=pk.t[:, 0:128], scalar=nbeta[:, n, col:col + 1],
                        in1=Es[g_][:, d, :], op0=ALU.mult, op1=ALU.mult), r=[pk.k, GK, kS], w=[kX])
                    P.op('dve', lambda e: e.tensor_tensor(out=qdec[:, n, d, :], in0=qTh[:, bs], in1=Ge[g_][:, d, :],
                                                          op=ALU.mult), r=[HK, kG], w=[('pre', n)])
                    P.op('act', lambda e: e.activation(out=ktail[:, n, d, :], in_=ktk[:, n, :], func=AF.Copy,
                                                       scale=tail[:, n, col:col + 1]), r=[HK, GK], w=[('pre', n)])
                P.op('dve', lambda e: e.tensor_tensor(out=AB[0][:, :, 0, :], in0=Xf[g_][:],
                                                      in1=bmask[:, 0:1, :].to_broadcast([128, 2, 128]), op=ALU.mult),
                     r=[kX, 'par'], w=[('AB', g_, 0)])
                yield
                pt = bankbf
                for d in range(2):
                    P.op('pe', lambda e: e.transpose(pt.t[:, d * 128:(d + 1) * 128], AB[0][:, d, 0, :], ident_bf[:]),
                         r=[('AB', g_, 0), CK], w=[pt.k])
                    P.op('pe', lambda e: e.transpose(pt.t[:, 256 + d * 128:256 + (d + 1) * 128], Xf[g_][:, d, :],
                                                     ident_bf[:]), r=[kX, CK], w=[pt.k])
                yield
                P.op('act', lambda e: e.copy(out=AB[0][:, :, 1, :],
                                             in_=pt.t[:, 0:256].rearrange("p (a b) -> p a b", b=128)),
                     r=[pt.k], w=[('AB', g_, 0)])
                P.op('act', lambda e: e.copy(out=XfT[g_][:], in_=pt.t[:, 256:512].rearrange("p (a b) -> p a b", b=128)),
                     r=[pt.k], w=[kXT])
                P.op('dve', lambda e: e.tensor_tensor(out=TW[0][:], in0=AB[0][:, :, 0, :],
                                                      in1=ident_bf[:].unsqueeze(1).to_broadcast([128, 2, 128]),
                                                      op=ALU.add), r=[('AB', g_, 0), CK], w=[('Ttw', g_, 0)])
                for b3 in range(3):
                    P.op('dve', lambda e: e.tensor_tensor(out=UT[g_][:, b3], in0=XfT[g_][:],
                                                          in1=bmask[:, 1 + b3:2 + b3, :].to_broadcast([128, 2, 128]),
                                                          op=ALU.mult), r=[kXT, 'par'], w=[kU])
                yield
                gi_ = 0
                for lev in range(1, 4):
                    cur = lev % 2
                    prv = 1 - cur
                    pab = bank
                    for d in range(2):
                        if lev < 3:
                            P.op('pe', lambda e: e.matmul(pab.t[:, d * 256:d * 256 + 128], AB[prv][:, d, 1, :],
                                                          AB[prv][:, d, 0, :], start=True, stop=True),
                                 r=[('AB', g_, prv)], w=[pab.k])
                        P.op('pe', lambda e: e.matmul(pab.t[:, d * 256 + 128:d * 256 + 256], AB[prv][:, d, 0, :],
                                                      AB[prv][:, d, 1, :], start=True, stop=True),
                             r=[('AB', g_, prv)], w=[pab.k])
                    yield
                    if lev < 3:
                        P.op('act', lambda e: e.copy(out=AB[cur][:],
                                                     in_=pab.t[:, :].rearrange("p (a b c) -> p a b c", a=2, b=2)),
                             r=[pab.k], w=[('AB', g_, cur)])
                    else:
                        P.op('act', lambda e: e.copy(
                            out=AB[cur][:, :, 1, :],
                            in_=pab.t[:, :].rearrange("p (a b c) -> p a b c", a=2, b=2)[:, :, 1, :]),
                            r=[pab.k], w=[('AB', g_, cur)])
                    yield
                    ptt = bank
                    for d in range(2):
                        P.op('pe', lambda e: e.matmul(ptt.t[:, d * 128:(d + 1) * 128], AB[cur][:, d, 1, :],
                                                      TW[gi_][:, d, :], start=True, stop=True),
                             r=[('AB', g_, cur), ('Ttw', g_, gi_)], w=[ptt.k])
                    yield
                    P.op('dve', lambda e: e.tensor_tensor(
                        out=TW[1 - gi_][:], in0=ptt.t[:, 0:256].rearrange("p (a b) -> p a b", b=128),
                        in1=TW[gi_][:], op=ALU.add), r=[ptt.k, ('Ttw', g_, gi_)], w=[('Ttw', g_, 1 - gi_)])
                    gi_ = 1 - gi_
                    yield
                for b3 in range(3):
                    G = TW[gi_]
                    pw2 = bank
                    pt2 = PsT(pw2.t[:, :].bitcast(BF16), pw2.k)
                    for d in range(2):
                        P.op('pe', lambda e: e.transpose(pt2.t[:, d * 128:(d + 1) * 128], G[:, d, :], ident_bf[:]),
                             r=[('Ttw', g_, gi_), CK], w=[pt2.k])
                    for d in range(2):
                        P.op('pe', lambda e: e.matmul(pw2.t[:, 256 + d * 128:256 + (d + 1) * 128], UT[g_][:, b3, d, :], G[:, d, :],
                                                      start=True, stop=True), r=[kU, ('Ttw', g_, gi_)], w=[pw2.k])
                    yield
                    P.op('act', lambda e: e.copy(out=Hh[g_][:], in_=pt2.t[:, 0:256].rearrange("p (a b) -> p a b", b=128)),
                         r=[pt2.k], w=[kH])
                    P.op('act', lambda e: e.copy(out=Wn[g_][:], in_=pw2.t[:, 256:512].rearrange("p (a b) -> p a b", b=128)),
                         r=[pw2.k], w=[kW])
                    yield
                    pg2 = bank
                    for d in range(2):
                        P.op('pe', lambda e: e.matmul(pg2.t[:, d * 128:(d + 1) * 128], Hh[g_][:, d, :], Wn[g_][:, d, :],
                                                      start=True, stop=True), r=[kH, kW], w=[pg2.k])
                    yield
                    if b3 < 2:
                        P.op('dve', lambda e: e.tensor_tensor(
                            out=TW[1 - gi_][:], in0=pg2.t[:, 0:256].rearrange("p (a b) -> p a b", b=128),
                            in1=G[:], op=ALU.add), r=[pg2.k, ('Ttw', g_, gi_)], w=[('Ttw', g_, 1 - gi_)])
                    else:
                        P.op('dve', lambda e: e.tensor_tensor(
                            out=Tt[:, n, :, :], in0=pg2.t[:, 0:256].rearrange("p (a b) -> p a b", b=128),
                            in1=G[:], op=ALU.add), r=[pg2.k, ('Ttw', g_, gi_)], w=[('pre', n)])
                    gi_ = 1 - gi_
                    yield

            orders = [list(range(NB)), [1, 0] + list(range(NB - 1, 1, -1))]
            done_pre = set()

            def chain(d):
                col = d * 8 + h
                pc = P.psum_pool[4 + d]
                P.op('pool', lambda e: e.memset(Sf[d][:], 0.0), w=[('S', d)])
                P.op('pool', lambda e: e.memset(Sb[d][:], 0.0), w=[('Sb', d)])
                for n in orders[d]:
                    while n not in done_pre:
                        yield
                    bs = slice(n * 128, (n + 1) * 128)
                    need_o = not (last and n < 2)
                    P.op('pe', lambda e: e.matmul(pc.t[:, 0:128], kTh[:, bs], Sb[d][:], start=True, stop=True),
                         r=[HK, ('Sb', d)], w=[pc.k])
                    yield
                    P.op('dve', lambda e: e.scalar_tensor_tensor(
                        out=Rt[d][:], in0=pc.t[:, 0:128], scalar=ngam[:, n, col:col + 1], in1=vtk[:, n, :],
                        op0=ALU.mult, op1=ALU.add), r=[pc.k, GK, HK], w=[('R', d)])
                    yield
                    P.op('pe', lambda e: e.matmul(pc.t[:, 128:256], Tt[:, n, d, :], Rt[d][:], start=True, stop=True),
                         r=[('pre', n), ('R', d)], w=[pc.k])
                    yield
                    P.op('act', lambda e: e.activation(out=vn[d][:], in_=pc.t[:, 128:256], func=AF.Copy,
                                                       scale=beta[:, n, col:col + 1]), r=[pc.k, GK], w=[('vn', d)])
                    yield
                    if need_o:
                        P.op('pe', lambda e: e.matmul(pc.t[:, 256:384], Sb[d][:], qdec[:, n, d, :],
                                                      start=True, stop=False), r=[('Sb', d), ('pre', n)], w=[pc.k])
                        P.op('pe', lambda e: e.matmul(pc.t[:, 256:384], vn[d][:], AT[:, n, d, :],
                                                      start=False, stop=True), r=[('vn', d), ('pre', n)], w=[pc.k])
                    P.op('pe', lambda e: e.matmul(pc.t[:, 384:512], ktail[:, n, d, :], vn[d][:], start=True, stop=True),
                         r=[('pre', n), ('vn', d)], w=[pc.k])
                    yield
                    if need_o:
                        P.op('dve', lambda e: e.tensor_tensor(out=oacc[:, bs], in0=pc.t[:, 256:384], in1=oacc[:, bs],
                                                              op=ALU.add), r=[pc.k, ('oacc', n)], w=[('oacc', n)])
                    P.op('dve', lambda e: e.scalar_tensor_tensor(
                        out=Sf[d][:], in0=Sf[d][:], scalar=egl[:, n, col:col + 1], in1=pc.t[:, 384:512],
                        op0=ALU.mult, op1=ALU.add), r=[pc.k, GK, ('S', d)], w=[('S', d)])
                    yield
                    P.op('act', lambda e: e.copy(out=Sb[d][:], in_=Sf[d][:]), r=[('S', d)], w=[('Sb', d)])
                    yield

            pend = []
            seen_ = set()
            fi_ = bi_ = 0
            while len(pend) < NB:
                while fi_ < NB and orders[0][fi_] in seen_:
                    fi_ += 1
                if fi_ < NB:
                    pend.append(orders[0][fi_])
                    seen_.add(orders[0][fi_])
                while bi_ < NB and orders[1][bi_] in seen_:
                    bi_ += 1
                if bi_ < NB:
                    pend.append(orders[1][bi_])
                    seen_.add(orders[1][bi_])
            if DSTOP == 'pre1':
                pend = pend[:1]
            slots = {}
            chains = [] if DSTOP.startswith('pre') else [chain(0), chain(1)]
            while pend or slots or chains:
                for g_ in range(GP):
                    if g_ not in slots and pend:
                        n_ = pend.pop(0)
                        slots[g_] = (pre(n_, g_), n_)
                for g_ in list(slots.keys()):
                    gen, n_ = slots[g_]
                    try:
                        next(gen)
                    except StopIteration:
                        done_pre.add(n_)
                        del slots[g_]
                for cg in list(chains):
                    try:
                        next(cg)
                    except StopIteration:
                        chains.remove(cg)
            if DSTOP.startswith('pre'):
                P.barrier()
                P.end_scope()
                return
            if DSTOP == 'chain':
                P.barrier()
                P.end_scope()
                return
            zTh = ktail[:].rearrange("p n d f -> p (n d f)")
            PREK = [('pre', n) for n in range(NB)]
            P.dma('sp', [(zTh[:, 0:T], zT[hs, :])], lane='zld', w=PREK)
            for ti, (t0, W, lat) in enumerate(TILES):
                if last and not lat:
                    continue
                i2 = ti % 2
                P.op('act', lambda e: e.activation(out=sqo[i2][:, :W], in_=oacc[:, t0:t0 + W], func=AF.Square),
                     r=OK_, w=[('sqo', i2)])
                ps = P.psum()
                P.op('pe', lambda e: e.matmul(ps.t[:, :W], ones_bf[:], sqo[i2][:, :W], start=True, stop=True),
                     r=[('sqo', i2), CK], w=[ps.k])
                P.op('act', lambda e: e.activation(out=rno[:, :W], in_=ps.t[:, :W], func=AF.Sqrt, scale=1.0 / 128,
                                                   bias=epsT[:, 0:1]), r=[ps.k, CK], w=['rno'])
                P.op('dve', lambda e: e.reciprocal(out=rno[:, :W], in_=rno[:, :W]), r=['rno'], w=['rno'])
                P.op('dve', lambda e: e.scalar_tensor_tensor(
                    out=odf[:, :W], in0=oacc[:, t0:t0 + W], scalar=onorm[:, l:l + 1], in1=rno[:, :W],
                    op0=ALU.mult, op1=ALU.mult), r=OK_ + ['rno', 'par'], w=['odf'])
                P.op('dve', lambda e: e.tensor_tensor(out=odst[i2][:, :W], in0=odf[:, :W], in1=zTh[:, t0:t0 + W],
                                                      op=ALU.mult), r=['odf'] + PREK, w=[('odst', i2)])
                P.dma('sp', [(odT[hs, t0:t0 + W], odst[i2][:, :W])], lane='odst%d' % i2, r=[('odst', i2)], w=['odT'])
        P.barrier()
        P.end_scope()

    def phase_merge(l, xsrc):
        P.scope()
        last = (l == L - 1)
        wfc = P.sb("wfc", [128, 8, D], BF16)
        wdl = P.sb("wdl", [128, 8, D], BF16)
        wo = P.sb("wo", [128, 8, D], BF16)
        wr = P.sb("wr", [128, 8, 36], F32)
        P.scope()
        wfr = P.sb("wfr", [128, 4, D], BF16)
        P.dma('pool', [(wfr[:], w_fourier[l].rearrange("(g p) n -> p g n", p=128)),
                       (wdl[:], w_delta[l].rearrange("(kc p) n -> p kc n", p=128)),
                       (wo[:], w_out[l].rearrange("(kc p) n -> p kc n", p=128))], lane='mw', w=['mw'])
        P.dma('sp', [(wr[:], w_rt[l].rearrange("(kc p) n -> p kc n", p=128))], lane='wr', w=['wr'])
        for gi in range(4):
            for (j, mat) in ((0, chc), (1, chs)):
                for hf in range(2):
                    ps = P.psum()
                    P.op('pe', lambda e: e.matmul(ps.t[:, :512], mat[:], wfr[:, gi, hf * 512:(hf + 1) * 512],
                                                  start=True, stop=True), r=['mw', 'par'], w=[ps.k])
                    P.op('act', lambda e: e.copy(out=wfc[:, gi * 2 + j, hf * 512:(hf + 1) * 512], in_=ps.t[:, :512]),
                         r=[ps.k], w=['wfc'])
        P.barrier()
        P.end_scope()
        u_l = P.sb("u_l", [128, 32, 512], BF16)
        u_c = P.sb("u_c", [128, 2, 512], BF16)
        P.dma('sp', [(u_l[:], u_tok[:, 2:NB, :])], lane='ul', w=['u_l'])
        if not last:
            P.dma('sp', [(u_c[:], u_tok[:, 0:2, :])], lane='uc', w=['u_c'])
        NDB = 3
        dbuf = {'c': [P.sb("dcb%d" % i, [128, 4, 512], BF16) for i in range(NDB)],
                's': [P.sb("dsb%d" % i, [128, 4, 512], BF16) for i in range(NDB)]}
        dctr = 0
        PQ = P.sb("PQ", [128, 8, 512], BF16)
        odt = P.sb("odt", [128, 8, 512], BF16)
        gtt = P.sb("gtt", [128, 16, 512], BF16)
        mg = P.sb("mg", [128, 8, 512], BF16)
        m1 = P.sb("m1", [128, 512], F32)
        m2 = P.sb("m2", [128, 512], F32)
        xt = P.sb("xt", [128, 8, 512], F32)
        h2f = xt
        h2b = P.sb("h2b", [128, 8, 512], BF16)
        tl = {'sq': [P.sb("sq%d" % i, [128, 512], BF16) for i in range(2)],
              'tmp': [P.sb("tmp%d" % i, [128, 512], F32) for i in range(2)],
              'rstd': P.sb("rstd", [128, 512], F32)}
        lg = P.sb("lg", [128, 36], F32)
        rt = {k: P.sb("rt_" + k, [128, 32], F32) for k in ('lem', 'tmp')}
        rs = {k: P.sb("rs_" + k, [128, 4], F32) for k in ('mx', 'ohg', 'ex', 'pg', 'm1', 'm2', 'w1', 'w2', 'ohm')}
        rst = P.sb("rst", [128, 4, 66], F32)
        h2tk = P.sb("h2tk", [128, 4, D], BF16)
        xv = xsrc.rearrange("(kc p) t -> p kc t", p=128)
        xsv = xs.rearrange("(kc p) t -> p kc t", p=128)
        for ti, (t0, W, lat) in enumerate(TILES):
            if last and not lat:
                continue
            ws = 0 if lat else 1
            ntc = 32 if lat else 2
            uu = u_l if lat else u_c
            ukey = 'u_l' if lat else 'u_c'
            f0 = t0 - CTX if lat else 0
            mats = {'c': dftc if lat else dftc_c, 's': dfts if lat else dfts_c}
            sw = 4 if lat else 2
            nsub = ntc // sw
            P.dma('sp', [(odt[:, :, :W], odT.rearrange("(c p) t -> p c t", p=128)[:, :, t0:t0 + W]),
                         (gtt[:, :, :W], gT.rearrange("(c p) t -> p c t", p=128)[:, :, t0:t0 + W]),
                         (xt[:, :, :W], xv[:, :, t0:t0 + W])], lane='mt', w=['mt'])
            for gh in range(2):
                accs = [[P.psum(), P.psum()] for _ in range(2)]
                for sb_ in range(nsub):
                    bi = dctr % NDB
                    dctr += 1
                    for cs in ('c', 's'):
                        P.dma('sp', [(dbuf[cs][bi][:, :sw, :W],
                                      mats[cs][sb_ * sw * 128:(sb_ + 1) * sw * 128, f0:f0 + W].rearrange(
                                          "(b p) f -> p b f", p=128))],
                              lane='d%sb%d' % (cs, bi), w=[('d' + cs, bi)])
                    for t4 in range(sw):
                        tc = sb_ * sw + t4
                        for gl in range(2):
                            gi = gh * 2 + gl
                            for j, cs in enumerate(('c', 's')):
                                P.op('pe', lambda e: e.matmul(accs[gl][j].t[:, :W], uu[:, tc, gi * 128:(gi + 1) * 128],
                                                              dbuf[cs][bi][:, t4, :W], start=(tc == 0),
                                                              stop=(tc == ntc - 1)),
                                     r=[ukey, ('d' + cs, bi)], w=[accs[gl][j].k])
                for gl in range(2):
                    gi = gh * 2 + gl
                    for j in range(2):
                        P.op('act', lambda e: e.copy(out=PQ[:, gi * 2 + j, :W], in_=accs[gl][j].t[:, :W]),
                             r=[accs[gl][j].k], w=['PQ'])
            for dc in range(8):
                pa = P.psum()
                for k8 in range(8):
                    P.op('pe', lambda e: e.matmul(pa.t[:, :W], wfc[:, k8, dc * 128:(dc + 1) * 128], PQ[:, k8, :W],
                                                  start=(k8 == 0), stop=(k8 == 7)), r=['wfc', 'PQ'], w=[pa.k])
                pb = P.psum()
                for kc in range(8):
                    P.op('pe', lambda e: e.matmul(pb.t[:, :W], wdl[:, kc, dc * 128:(dc + 1) * 128], odt[:, kc, :W],
                                                  start=(kc == 0), stop=(kc == 7)), r=['mw', 'mt'], w=[pb.k])
                P.op('dve', lambda e: e.tensor_tensor(out=m1[:, :W], in0=pa.t[:, :W], in1=gtt[:, dc, :W], op=ALU.mult),
                     r=[pa.k, 'mt'], w=['m1'])
                P.op('dve', lambda e: e.tensor_tensor(out=m2[:, :W], in0=pb.t[:, :W], in1=gtt[:, 8 + dc, :W],
                                                      op=ALU.mult), r=[pb.k, 'mt'], w=['m2'])
                P.op('dve', lambda e: e.tensor_tensor(out=mg[:, dc, :W], in0=m1[:, :W], in1=m2[:, :W], op=ALU.add),
                     r=['m1', 'm2'], w=['mg'])
            for dc in range(8):
                po = P.psum()
                for kc in range(8):
                    P.op('pe', lambda e: e.matmul(po.t[:, :W], wo[:, kc, dc * 128:(dc + 1) * 128], mg[:, kc, :W],
                                                  start=(kc == 0), stop=(kc == 7)), r=['mw', 'mg'], w=[po.k])
                P.op('dve', lambda e: e.scalar_tensor_tensor(
                    out=xt[:, dc, :W], in0=po.t[:, :W], scalar=modv[:, l, 16 + dc, ws:ws + 1], in1=xt[:, dc, :W],
                    op0=ALU.mult, op1=ALU.add), r=[po.k, 'mod', 'mt'], w=['mt'])
            P.dma('sp', [(xsv[:, :, t0:t0 + W], xt[:, :, :W])], lane='xo', r=['mt'], w=['xs'])
            if os.environ.get('KTRACE') and ti < 2:
                print('merge tile', ti, 'pre-norm2', P.ninst)
            norm_mod(xt, 'mt', W, A2[:, l, :, ws], modv[:, l, 24:32, ws],
                     [(lambda kc: xt[:, kc, :W], 'mt'), (lambda kc: h2b[:, kc, :W], 'h2b')], tl)
            if os.environ.get('KTRACE') and ti < 2:
                print('merge tile', ti, 'pre-h2tk', P.ninst)
            nbk = W // 128
            blk0 = t0 // 128
            for tb in range(nbk):
                for hf in range(2):
                    pt = P.psum_t()
                    for j in range(4):
                        kc = hf * 4 + j
                        P.op('pe', lambda e: e.transpose(pt.t[:, j * 128:(j + 1) * 128],
                                                         h2b[:, kc, tb * 128:(tb + 1) * 128], ident_bf[:]),
                             r=['h2b', CK], w=[pt.k])
                    P.op('act', lambda e: e.copy(out=h2tk[:, tb, hf * 512:(hf + 1) * 512], in_=pt.t[:, 0:512]),
                         r=[pt.k], w=['h2tk'])
            P.dma('sp', [(h2tok_d[:, blk0:blk0 + nbk, :], h2tk[:, :nbk, :])], lane='h2o', r=['h2tk'], w=['h2tok'])
            if os.environ.get('KTRACE') and ti < 2:
                print('merge tile', ti, 'pre-routing', P.ninst)
            for tb in range(W // 128):
                ps = P.psum()
                for kc in range(8):
                    P.op('pe', lambda e: e.matmul(ps.t[:, :36], h2f[:, kc, tb * 128:(tb + 1) * 128], wr[:, kc, :],
                                                  start=(kc == 0), stop=(kc == 7)), r=['mt', 'wr'], w=[ps.k])
                RK = 'rt'

                def dv(fn, r=(), w=(RK,)):
                    P.op('dve', fn, r=list(r) + [RK], w=w)
                P.op('dve', lambda e: e.tensor_tensor(out=lg[:], in0=ps.t[:, :36], in1=brt[:, l, :], op=ALU.add),
                     r=[ps.k, 'par'], w=[RK])
                dv(lambda e: e.reduce_max(out=rs['mx'][:, 0:1], in_=lg[:, 0:4], axis=mybir.AxisListType.X))
                dv(lambda e: e.tensor_scalar(out=rs['ohg'][:], in0=lg[:, 0:4], scalar1=rs['mx'][:, 0:1], scalar2=None,
                                             op0=ALU.is_ge))
                dv(lambda e: e.tensor_scalar(out=rs['ex'][:], in0=lg[:, 0:4], scalar1=rs['mx'][:, 0:1], scalar2=None,
                                             op0=ALU.subtract))
                P.op('act', lambda e: e.activation(out=rs['ex'][:], in_=rs['ex'][:], func=AF.Exp), r=[RK], w=[RK])
                dv(lambda e: e.reduce_sum(out=rs['pg'][:, 0:1], in_=rs['ex'][:], axis=mybir.AxisListType.X))
                dv(lambda e: e.reciprocal(out=rs['pg'][:, 0:1], in_=rs['pg'][:, 0:1]))
                dv(lambda e: e.tensor_scalar(out=rs['ohm'][:], in0=rs['ohg'][:], scalar1=-1.0, scalar2=1.0e9,
                                             op0=ALU.add, op1=ALU.mult))
                dv(lambda e: e.tensor_tensor(out=rt['lem'][:].rearrange("p (g e) -> p g e", e=8),
                                             in0=lg[:, 4:36].rearrange("p (g e) -> p g e", e=8),
                                             in1=rs['ohm'][:].unsqueeze(2).to_broadcast([128, 4, 8]), op=ALU.add))
                dv(lambda e: e.reduce_max(out=rs['m1'][:, 0:1], in_=rt['lem'][:], axis=mybir.AxisListType.X))
                oh1 = rst[:, tb, 0:32]
                oh2 = rst[:, tb, 32:64]
                w1 = rst[:, tb, 64:65]
                w2 = rst[:, tb, 65:66]
                dv(lambda e: e.tensor_scalar(out=oh1, in0=rt['lem'][:], scalar1=rs['m1'][:, 0:1], scalar2=None,
                                             op0=ALU.is_ge), w=(RK, 'rst'))
                dv(lambda e: e.scalar_tensor_tensor(out=rt['tmp'][:], in0=oh1, scalar=-1.0e9, in1=rt['lem'][:],
                                                    op0=ALU.mult, op1=ALU.add), r=['rst'])
                dv(lambda e: e.reduce_max(out=rs['m2'][:, 0:1], in_=rt['tmp'][:], axis=mybir.AxisListType.X))
                dv(lambda e: e.tensor_scalar(out=oh2, in0=rt['tmp'][:], scalar1=rs['m2'][:, 0:1], scalar2=None,
                                             op0=ALU.is_ge), w=(RK, 'rst'))
                dv(lambda e: e.tensor_tensor(out=rs['w1'][:, 0:1], in0=rs['m1'][:, 0:1], in1=rs['m2'][:, 0:1],
                                             op=ALU.subtract))
                P.op('act', lambda e: e.activation(out=rs['w1'][:, 0:1], in_=rs['w1'][:, 0:1], func=AF.Sigmoid),
                     r=[RK], w=[RK])
                dv(lambda e: e.tensor_scalar(out=rs['w2'][:, 0:1], in0=rs['w1'][:, 0:1], scalar1=-1.0, scalar2=1.0,
                                             op0=ALU.mult, op1=ALU.add))
                dv(lambda e: e.tensor_tensor(out=w1, in0=rs['w1'][:, 0:1], in1=rs['pg'][:, 0:1], op=ALU.mult),
                   w=(RK, 'rst'))
                dv(lambda e: e.tensor_tensor(out=w2, in0=rs['w2'][:, 0:1], in1=rs['pg'][:, 0:1], op=ALU.mult),
                   w=(RK, 'rst'))
            P.dma('sp', [(rt_d[:, blk0:blk0 + nbk, :], rst[:, :nbk, :])], lane='wto', r=['rst'], w=['rt_d'])
        P.barrier()
        P.end_scope()

    def phase_moe(l):
        I32 = mybir.dt.int32
        last = (l == L - 1)
        b0 = 2 if last else 0
        nbl = NB - b0
        nblk = 48 if last else NBLK
        xsv = xs.rearrange("(kc p) t -> p kc t", p=128)
        wg_rows = w_gate.rearrange("l e k n -> (l e k) n")
        wu_rows = w_up.rearrange("l e k n -> (l e k) n")
        wd_rows = w_down.rearrange("l e k n -> (l e k) n")
        P.scope()
        RT = P.sb("RT", [128, nbl, 66], F32)
        P.dma('sp', [(RT[:], rt_d[:, b0:NB, :])], lane='hb', w=['RT'])
        IND = P.sb("IND", [128, nbl, 32], F32)
        RANK = P.sb("RANK", [128, nbl, 32], F32)
        carry = P.sb("carry", [128, 32], F32)
        SK = 'moeix'
        P.op('dve', lambda e: e.tensor_tensor(out=IND[:], in0=RT[:, :, 0:32], in1=RT[:, :, 32:64], op=ALU.add),
             r=['RT'], w=[SK])
        P.op('dve', lambda e: e.tensor_copy(out=carry[:], in_=zeros_f[:, 0:32]), r=[CK], w=['carry'])
        for c in range(nbl):
            ps = P.psum()
            P.op('pe', lambda e: e.matmul(ps.t[:, 0:32], strF[:], IND[:, c, :], start=True, stop=True),
                 r=[SK, CK], w=[ps.k])
            P.op('pe', lambda e: e.matmul(ps.t[:, 32:64], ones_f[:], IND[:, c, :], start=True, stop=True),
                 r=[SK, CK], w=[ps.k])
            P.op('dve', lambda e: e.tensor_tensor(out=RANK[:, c, :], in0=ps.t[:, 0:32], in1=carry[:], op=ALU.add),
                 r=[ps.k, 'carry'], w=['RANK'])
            P.op('dve', lambda e: e.tensor_tensor(out=carry[:], in0=ps.t[:, 32:64], in1=carry[:], op=ALU.add),
                 r=[ps.k, 'carry'], w=['carry'])
        cmp9 = P.sb("cmp9", [128, 32, 9], F32)
        thr = P.sb("thr", [128, 9], F32)
        nbe = P.sb("nbe", [128, 32], F32)
        cs = [P.sb("cs%d" % i, [128, 32], F32) for i in range(2)]
        P.op('dve', lambda e: e.tensor_scalar(out=thr[:], in0=iota[:, 0:9], scalar1=512.0, scalar2=None, op0=ALU.mult),
             r=['par'], w=['thr'])
        P.op('dve', lambda e: e.tensor_tensor(out=cmp9[:], in0=carry[:].unsqueeze(2).to_broadcast([128, 32, 9]),
                                              in1=thr[:].unsqueeze(1).to_broadcast([128, 32, 9]), op=ALU.is_gt),
             r=['carry', 'thr'], w=['cmp9'])
        P.op('dve', lambda e: e.reduce_sum(out=nbe[:], in_=cmp9[:], axis=mybir.AxisListType.X), r=['cmp9'], w=['nbe'])
        P.op('dve', lambda e: e.tensor_copy(out=cs[0][:], in_=nbe[:]), r=['nbe'], w=[('cs', 0)])
        ci = 0
        for sh in (1, 2, 4, 8, 16):
            P.op('dve', lambda e: e.tensor_copy(out=cs[1 - ci][:, 0:sh], in_=cs[ci][:, 0:sh]),
                 r=[('cs', ci)], w=[('cs', 1 - ci)])
            P.op('dve', lambda e: e.tensor_tensor(out=cs[1 - ci][:, sh:32], in0=cs[ci][:, sh:32], in1=cs[ci][:, 0:32 - sh],
                                                  op=ALU.add), r=[('cs', ci)], w=[('cs', 1 - ci)])
            ci = 1 - ci
        bend = cs[ci]
        sbase = P.sb("sbase", [128, 32], F32)
        P.op('dve', lambda e: e.tensor_tensor(out=sbase[:], in0=bend[:], in1=nbe[:], op=ALU.subtract),
             r=[('cs', ci), 'nbe'], w=['sbase'])
        P.op('dve', lambda e: e.tensor_scalar(out=sbase[:], in0=sbase[:], scalar1=512.0, scalar2=None, op0=ALU.mult),
             r=['sbase'], w=['sbase'])
        P.op('dve', lambda e: e.tensor_tensor(out=RANK[:], in0=RANK[:],
                                              in1=sbase[:].unsqueeze(1).to_broadcast([128, nbl, 32]), op=ALU.add),
             r=['RANK', 'sbase'], w=['RANK'])
        slf = P.sb("slf", [128, nbl, 2], F32)
        sli = P.sb("sli", [128, nbl, 2], I32)
        for k in range(2):
            P.op('dve', lambda e: e.tensor_tensor(out=IND[:], in0=RANK[:], in1=RT[:, :, k * 32:(k + 1) * 32],
                                                  op=ALU.mult), r=['RANK', 'RT', SK], w=[SK])
            P.op('dve', lambda e: e.reduce_sum(out=slf[:, :, k], in_=IND[:], axis=mybir.AxisListType.X),
                 r=[SK], w=['slf'])
        P.op('dve', lambda e: e.tensor_copy(out=sli[:], in_=slf[:]), r=['slf'], w=['sli'])
        cmpb = P.sb("cmpb", [128, NBLK, 32], F32)
        ebf = P.sb("ebf", [128, NBLK], F32)
        P.op('dve', lambda e: e.tensor_tensor(out=cmpb[:], in0=bend[:].unsqueeze(1).to_broadcast([128, NBLK, 32]),
                                              in1=iota[:, 0:NBLK].unsqueeze(2).to_broadcast([128, NBLK, 32]),
                                              op=ALU.is_le), r=[('cs', ci), 'par'], w=['cmpb'])
        P.op('dve', lambda e: e.reduce_sum(out=ebf[:], in_=cmpb[:], axis=mybir.AxisListType.X), r=['cmpb'], w=['ebf'])
        P.op('dve', lambda e: e.tensor_scalar(out=ebf[:], in0=ebf[:], scalar1=31.0, scalar2=float(l * NE),
                                              op0=ALU.min, op1=ALU.add), r=['ebf'], w=['ebf'])
        wixf = P.sb("wixf", [128, NBLK, 12], F32)
        wix = P.sb("wix", [128, NBLK, 12], I32)
        P.op('dve', lambda e: e.scalar_tensor_tensor(
            out=wixf[:, :, 0:8], in0=ebf[:].unsqueeze(2).to_broadcast([128, NBLK, 8]), scalar=1024.0,
            in1=wbase[:].unsqueeze(1).to_broadcast([128, NBLK, 8]), op0=ALU.mult, op1=ALU.add),
            r=['ebf', 'par'], w=['wixf'])
        P.op('dve', lambda e: e.scalar_tensor_tensor(
            out=wixf[:, :, 8:12], in0=ebf[:].unsqueeze(2).to_broadcast([128, NBLK, 4]), scalar=512.0,
            in1=wbase[:, 0:4].unsqueeze(1).to_broadcast([128, NBLK, 4]), op0=ALU.mult, op1=ALU.add),
            r=['ebf', 'par'], w=['wixf'])
        P.op('dve', lambda e: e.tensor_copy(out=wix[:], in_=wixf[:]), r=['wixf'], w=['wix'])

        if os.environ.get('KTRACE'):
            print('moe ix done', P.ninst)
        P.scope()
        hk = [P.sb("hk%d" % i, [128, 4, D], BF16) for i in range(2)]
        gi = 0
        for c0 in range(0, nbl, 4):
            nn = min(4, nbl - c0)
            b2 = gi % 2
            gi += 1
            P.dma('sp', [(hk[b2][:, :nn, :], h2tok_d[:, b0 + c0:b0 + c0 + nn, :])], lane='hk%d' % b2, w=[('hk', b2)])
            for j in range(nn):
                for k in range(2):
                    P.dma('pool', [lambda h: h.indirect_dma_start(
                        out=xs_d[:, :], out_offset=bass.IndirectOffsetOnAxis(ap=sli[:, c0 + j, k:k + 1], axis=0),
                        in_=hk[b2][:, j, :], in_offset=None)], lane='sc%d' % ((j * 2 + k) % 4),
                        r=[('hk', b2), 'sli'], w=['xs_d'])
        P.barrier()
        P.end_scope()

        if os.environ.get('KTRACE'):
            print('moe dispatch done', P.ninst)
        P.scope()
        wg = [P.sb("wg%d" % i, [128, 8, 512], BF16) for i in range(2)]
        wu = [P.sb("wu%d" % i, [128, 8, 512], BF16) for i in range(2)]
        wd = [P.sb("wd%d" % i, [128, 4, D], BF16) for i in range(2)]
        xsb = [P.sb("xsb%d" % i, [128, 4, D], BF16) for i in range(2)]
        xsT = P.sb("xsT", [128, 8, 512], BF16)
        sg = [P.sb("sg%d" % i, [128, 512], F32) for i in range(2)]
        h1 = P.sb("h1", [128, 4, 512], BF16)
        ysb = [P.sb("ysb%d" % i, [128, 4, D], BF16) for i in range(2)]
        for b in range(nblk):
            if os.environ.get('KTRACE') and b < 2:
                print('moe block', b, P.ninst)
            bi = b % 2
            WK = ('ew', bi)
            prs = []
            for kc in range(8):
                prs.append((lambda kc: lambda h: h.indirect_dma_start(
                    out=wg[bi][:, kc, :], out_offset=None, in_=wg_rows,
                    in_offset=bass.IndirectOffsetOnAxis(ap=wix[:, b, kc:kc + 1], axis=0)))(kc))
                prs.append((lambda kc: lambda h: h.indirect_dma_start(
                    out=wu[bi][:, kc, :], out_offset=None, in_=wu_rows,
                    in_offset=bass.IndirectOffsetOnAxis(ap=wix[:, b, kc:kc + 1], axis=0)))(kc))
            for fc in range(4):
                prs.append((lambda fc: lambda h: h.indirect_dma_start(
                    out=wd[bi][:, fc, :], out_offset=None, in_=wd_rows,
                    in_offset=bass.IndirectOffsetOnAxis(ap=wix[:, b, 8 + fc:9 + fc], axis=0)))(fc))
            P.dma('pool', prs, lane='ew%d' % bi, r=['wix'], w=[WK])
            P.dma('sp', [(xsb[bi][:], xs_d[b * 512:(b + 1) * 512, :].rearrange("(s p) f -> p s f", p=128))],
                  lane='xsb%d' % bi, r=['xs_d'], w=[('xsb', bi)])
            for kc in range(8):
                pt = P.psum_t()
                for sgi in range(4):
                    P.op('pe', lambda e: e.transpose(pt.t[:, sgi * 128:(sgi + 1) * 128],
                                                     xsb[bi][:, sgi, kc * 128:(kc + 1) * 128], ident_bf[:]),
                         r=[('xsb', bi), CK], w=[pt.k])
                if kc % 2 == 0:
                    P.op('act', lambda e: e.copy(out=xsT[:, kc, :], in_=pt.t[:, 0:512]), r=[pt.k], w=['xsT'])
                else:
                    P.op('dve', lambda e: e.tensor_copy(out=xsT[:, kc, :], in_=pt.t[:, 0:512]), r=[pt.k], w=['xsT'])
            for fc in range(4):
                pg = P.psum()
                pu = P.psum()
                for kc in range(8):
                    P.op('pe', lambda e: e.matmul(pg.t[:, :], wg[bi][:, kc, fc * 128:(fc + 1) * 128], xsT[:, kc, :],
                                                  start=(kc == 0), stop=(kc == 7)), r=[WK, 'xsT'], w=[pg.k])
                for kc in range(8):
                    P.op('pe', lambda e: e.matmul(pu.t[:, :], wu[bi][:, kc, fc * 128:(fc + 1) * 128], xsT[:, kc, :],
                                                  start=(kc == 0), stop=(kc == 7)), r=[WK, 'xsT'], w=[pu.k])
                f2 = fc % 2
                P.op('act', lambda e: e.activation(out=sg[f2][:], in_=pg.t[:, :], func=AF.Silu),
                     r=[pg.k], w=[('sg', f2)])
                P.op('dve', lambda e: e.tensor_tensor(out=h1[:, fc, :], in0=sg[f2][:], in1=pu.t[:, :], op=ALU.mult),
                     r=[('sg', f2), pu.k], w=['h1'])
            for sgi in range(4):
                for hf in range(2):
                    py = P.psum()
                    for fc in range(4):
                        P.op('pe', lambda e: e.matmul(py.t[:, :], h1[:, fc, sgi * 128:(sgi + 1) * 128],
                                                      wd[bi][:, fc, hf * 512:(hf + 1) * 512],
                                                      start=(fc == 0), stop=(fc == 3)), r=['h1', WK], w=[py.k])
                    if hf == 0:
                        P.op('act', lambda e: e.copy(out=ysb[bi][:, sgi, 0:512], in_=py.t[:, :]),
                             r=[py.k], w=[('ysb', bi)])
                    else:
                        P.op('dve', lambda e: e.tensor_copy(out=ysb[bi][:, sgi, 512:1024], in_=py.t[:, :]),
                             r=[py.k], w=[('ysb', bi)])
            P.dma('sp', [(ys_d[b * 512:(b + 1) * 512, :].rearrange("(s p) f -> p s f", p=128), ysb[bi][:])],
                  lane='ysb%d' % bi, r=[('ysb', bi)], w=['ys_d'])
        P.barrier()
        P.end_scope()

        if os.environ.get('KTRACE'):
            print('moe blocks done', P.ninst)
        P.scope()
        y1 = [P.sb("y1_%d" % i, [128, 4, D], BF16) for i in range(2)]
        y2 = [P.sb("y2_%d" % i, [128, 4, D], BF16) for i in range(2)]
        yc = P.sb("yc", [128, 4, D], BF16)
        ytmp = P.sb("ytmp", [128, D], F32)
        xt = P.sb("xt", [128, 8, 512], F32)
        tl = None
        if last:
            tl = {'sq': [P.sb("sq%d" % i, [128, 512], BF16) for i in range(2)],
                  'rstd': P.sb("rstd", [128, 512], F32)}
            ot = P.sb("ot", [128, 8, 512], F32)
        gi = 0
        for (t0, W, lat) in TILES:
            if last and not lat:
                continue
            ws = 0 if lat else 1
            nbk = W // 128
            c0 = t0 // 128 - b0
            g2 = gi % 2
            gi += 1
            P.dma('sp', [(xt[:, :, :W], xsv[:, :, t0:t0 + W])], lane='xm', r=['xs'], w=['xm'])
            prs = []
            for j in range(nbk):
                prs.append((lambda j: lambda h: h.indirect_dma_start(
                    out=y1[g2][:, j, :], out_offset=None, in_=ys_d[:, :],
                    in_offset=bass.IndirectOffsetOnAxis(ap=sli[:, c0 + j, 0:1], axis=0)))(j))
                prs.append((lambda j: lambda h: h.indirect_dma_start(
                    out=y2[g2][:, j, :], out_offset=None, in_=ys_d[:, :],
                    in_offset=bass.IndirectOffsetOnAxis(ap=sli[:, c0 + j, 1:2], axis=0)))(j))
            P.dma('pool', prs, lane='yg%d' % g2, r=['sli', 'ys_d'], w=[('yg', g2)])
            for j in range(nbk):
                P.op('dve', lambda e: e.tensor_scalar(out=ytmp[:], in0=y1[g2][:, j, :],
                                                      scalar1=RT[:, c0 + j, 64:65], scalar2=None, op0=ALU.mult),
                     r=[('yg', g2), 'RT'], w=['ytmp'])
                P.op('dve', lambda e: e.scalar_tensor_tensor(out=yc[:, j, :], in0=y2[g2][:, j, :],
                                                             scalar=RT[:, c0 + j, 65:66], in1=ytmp[:],
                                                             op0=ALU.mult, op1=ALU.add),
                     r=[('yg', g2), 'RT', 'ytmp'], w=['yc'])
            for kc in range(8):
                pt = P.psum_t()
                for j in range(nbk):
                    P.op('pe', lambda e: e.transpose(pt.t[:, j * 128:(j + 1) * 128], yc[:, j, kc * 128:(kc + 1) * 128],
                                                     ident_bf[:]), r=['yc', CK], w=[pt.k])
                P.op('dve', lambda e: e.scalar_tensor_tensor(
                    out=xt[:, kc, :W], in0=pt.t[:, :W], scalar=modv[:, l, 40 + kc, ws:ws + 1], in1=xt[:, kc, :W],
                    op0=ALU.mult, op1=ALU.add), r=[pt.k, 'mod', 'xm'], w=['xm'])
            if not last:
                P.dma('sp', [(xsv[:, :, t0:t0 + W], xt[:, :, :W])], lane='xmo', r=['xm'], w=['xs'])
            else:
                ss = P.psum()
                for kc in range(8):
                    sq = tl['sq'][kc % 2]
                    P.op('act', lambda e: e.activation(out=sq[:, :W], in_=xt[:, kc, :W], func=AF.Square),
                         r=['xm'], w=[('sq', kc % 2)])
                    P.op('pe', lambda e: e.matmul(ss.t[:, :W], ones_bf[:], sq[:, :W], start=(kc == 0),
                                                  stop=(kc == 7)), r=[('sq', kc % 2), CK], w=[ss.k])
                rstd = tl['rstd']
                P.op('act', lambda e: e.activation(out=rstd[:, :W], in_=ss.t[:, :W], func=AF.Sqrt, scale=1.0 / D,
                                                   bias=epsT[:, 0:1]), r=[ss.k, CK], w=['rstd'])
                P.op('dve', lambda e: e.reciprocal(out=rstd[:, :W], in_=rstd[:, :W]), r=['rstd'], w=['rstd'])
                for kc in range(8):
                    P.op('dve', lambda e: e.scalar_tensor_tensor(
                        out=ot[:, kc, :W], in0=xt[:, kc, :W], scalar=fnorm[:, kc:kc + 1], in1=rstd[:, :W],
                        op0=ALU.mult, op1=ALU.mult), r=['xm', 'rstd', 'par'], w=['ot'])
                P.dma('sp', [(outT.rearrange("(c p) t -> p c t", p=128)[:, :, t0 - CTX:t0 - CTX + W], ot[:, :, :W])],
                      lane='oo', r=['ot'], w=['outT'])
        P.barrier()
        P.end_scope()
        P.end_scope()

    with nc.named_scope('mod'):
        phase_mod()
    done = False
    for l in range(nl):
        xsrc = xT_in if l == 0 else xs
        for (nm, fn) in (('proj', lambda: phase_proj(l, xsrc)), ('delta', lambda: phase_delta(l)),
                         ('merge', lambda: phase_merge(l, xsrc)), ('moe', lambda: phase_moe(l))):
            if os.environ.get('KSCOPE') and nm == 'proj':
                fn()
            else:
                with nc.named_scope('%s%d' % (nm, l)):
                    fn()
            if stop == (l, nm):
                done = True
                break
        if done:
            break
    P.barrier()
    print("bass program: ninst=%d nwait=%d lanes=%d" % (P.ninst, P.nwait, len(P.lanes)))
    return nc


def _fm(v):
    return np.ascontiguousarray(np.asarray(v).reshape(-1, 128).T)


def prep_shared(inp):
    f32 = np.float32
    sh = {}
    b_mod = np.asarray(inp['b_mod'], f32)
    sh['bmod'] = np.ascontiguousarray(b_mod.reshape(L, 48, 128).transpose(2, 0, 1))
    sh['nmix'] = np.ascontiguousarray(np.asarray(inp['norm_mix'], f32).reshape(L, 8, 128).transpose(2, 0, 1))
    sh['nffn'] = np.ascontiguousarray(np.asarray(inp['norm_ffn'], f32).reshape(L, 8, 128).transpose(2, 0, 1))
    sh['fnorm'] = _fm(np.asarray(inp['final_norm'], f32))
    sh['convw'] = np.ascontiguousarray(np.asarray(inp['conv_w'], f32).reshape(L, 3, 24, 128).transpose(3, 0, 1, 2))
    sh['alog'] = np.ascontiguousarray(np.broadcast_to(np.asarray(inp['a_log'], f32).reshape(1, L, 16), (128, L, 16)))
    sh['dtb'] = np.ascontiguousarray(np.broadcast_to(np.asarray(inp['dt_bias'], f32).reshape(1, L, 16), (128, L, 16)))
    sh['onorm'] = np.ascontiguousarray(np.asarray(inp['out_norm'], f32).T)
    brt = np.concatenate([np.asarray(inp['b_route_group'], f32), np.asarray(inp['b_route_expert'], f32)], axis=1)
    sh['brt'] = np.ascontiguousarray(np.broadcast_to(brt.reshape(1, L, 36), (128, L, 36)))
    sh['w_rt'] = np.ascontiguousarray(np.concatenate([np.asarray(inp['w_route_group'], f32),
                                                      np.asarray(inp['w_route_expert'], f32)], axis=2))
    for k in ('w_mod', 'w_in', 'w_fourier', 'w_delta', 'w_out', 'w_gate', 'w_up', 'w_down'):
        sh[k] = np.ascontiguousarray(np.asarray(inp[k], f32))
    bf = ml_dtypes.bfloat16

    def tab(n, scale):
        k = np.arange(n, dtype=np.int64)
        ang = 2.0 * np.pi * ((k[:, None] * k[None, :]) % n).astype(np.float64) / n
        return (np.cos(ang) * scale).astype(bf), (np.sin(ang) * scale).astype(bf)
    sh['dftc'], sh['dfts'] = tab(SEQ, (SEQ * 128.0) ** -0.5)
    sh['dftc_c'], sh['dfts_c'] = tab(CTX, (CTX * 128.0) ** -0.5)
    c, s = tab(128, 1.0)
    sh['chc'] = c
    sh['chs'] = (-s.astype(np.float32)).astype(bf)
    idx = np.arange(128)
    def bd(b):
        return (idx[:, None] // b == idx[None, :] // b).astype(np.float32)
    bm = np.stack([bd(16), bd(32) - bd(16), bd(64) - bd(32), bd(128) - bd(64)], axis=1)
    sh['bmask'] = np.ascontiguousarray(bm).astype(bf)
    sh['iota'] = np.ascontiguousarray(np.broadcast_to(np.arange(64, dtype=np.float32)[None, :], (128, 64)))
    sh['wbase'] = np.ascontiguousarray((np.arange(8)[None, :] * 128 + np.arange(128)[:, None]).astype(np.float32))
    return sh


def prep_core(inp, b):
    f32 = np.float32
    x = np.asarray(inp['x'][b], f32)
    ctx = np.asarray(inp['ctx'][b], f32)
    xT = np.ascontiguousarray(np.concatenate([ctx, x], axis=0).T)
    cvec = np.stack([_fm(np.asarray(inp['c'][b], f32)), _fm(np.asarray(inp['c_ctx'], f32))], axis=2)
    return {'xT': xT, 'cvec': np.ascontiguousarray(cvec)}


_NC_CACHE = {}


def kernel(**inputs):
    if 'nc' not in _NC_CACHE:
        _NC_CACHE['nc'] = build()
    nc = _NC_CACHE['nc']
    sh = prep_shared(inputs)
    in_maps = []
    for b in range(8):
        m = dict(sh)
        m.update(prep_core(inputs, b))
        in_maps.append(m)
    res = run_bass_kernel_spmd(nc, in_maps, core_ids=list(range(8)))
    out = np.stack([np.ascontiguousarray(r["outT"].T) for r in res.results], axis=0)
    return out.astype(np.float32)
```

```python
import numpy as np
import os
import ml_dtypes
import concourse.bass as bass
import concourse.mybir as mybir
from concourse.bass_utils import run_bass_kernel_spmd

F32 = mybir.dt.float32
BF16 = mybir.dt.bfloat16
AF = mybir.ActivationFunctionType
ALU = mybir.AluOpType

D = 1024
SEQ = 4096
CTX = 256
T = CTX + SEQ
NB = T // 128
L = 4
NE = 32
OFF_A = 3072
OFF_B = 3088
OFF_Z = 3104
OFF_F = 4128
OFF_G = 4640
IN_COLS = 6688
EPS = 1e-6
TILES = [(0, 256, 0)] + [(256 + 512 * i, 512, 1) for i in range(8)]


class PsT:
    def __init__(self, t, k):
        self.t = t
        self.k = k


class Prog:
    ENG = ('pe', 'act', 'dve', 'pool', 'sp')

    def __init__(self, nc, n_lanes=64):
        self.nc = nc
        self.h = {'pe': nc.tensor, 'act': nc.scalar, 'dve': nc.vector, 'pool': nc.gpsimd, 'sp': nc.sync}
        self.sem = {}
        self.val = {}
        for e in self.ENG:
            self.sem[e] = nc.semaphore("s_" + e).__enter__()
            self.val[e] = 0
        self.n_lanes = n_lanes
        for l in range(n_lanes):
            k = ('L', l)
            self.sem[k] = nc.semaphore("s_l%d" % l).__enter__()
            self.val[k] = 0
        self.lanes = {}
        self.seen = {e: {} for e in self.ENG}
        self.clock = {}
        self.lastw = {}
        self.readers = {}
        self.ninst = 0
        self.nwait = 0
        self.stack = [[]]
        self.uid = 0
        self.psum_pool = []
        self.psum_i = 0
        self.cut = int(os.environ['KCUT']) if os.environ.get('KCUT') else None

    def scope(self):
        self.stack.append([])

    def end_scope(self):
        for cm in reversed(self.stack.pop()):
            cm.__exit__(None, None, None)

    def sb(self, name, shape, dt):
        self.uid += 1
        cm = self.nc.sbuf_tensor("%s_%d" % (name, self.uid), list(shape), dt)
        t = cm.__enter__()
        self.stack[-1].append(cm)
        return t

    def init_psum(self):
        self.NPS = 6
        for i in range(self.NPS):
            t = self.nc.psum_tensor("psb%d" % i, [128, 512], F32).__enter__()
            self.psum_pool.append(PsT(t, ('ps', i)))
        self.pst = [self.nc.psum_tensor("pst%d" % i, [128, 1024], BF16).__enter__() for i in range(2)]
        self.pst_i = 0

    def psum(self):
        p = self.psum_pool[self.psum_i % self.NPS]
        self.psum_i += 1
        return p

    def psum_bf(self):
        p = self.psum()
        return PsT(p.t[:, :].bitcast(BF16), p.k)

    def psum_t(self):
        i = self.pst_i % 2
        self.pst_i += 1
        return PsT(self.pst[i][:, 0:512], ('pst', i))

    def lane(self, name):
        if name not in self.lanes:
            assert len(self.lanes) < self.n_lanes, "out of lanes"
            self.lanes[name] = len(self.lanes)
        return self.lanes[name]

    def _deps(self, eng, reads, writes):
        deps = {}

        def add(d, kind):
            if d is None:
                return
            sk, v = d
            if sk == eng:
                if eng == 'pe' or kind != 'raw':
                    return
            if v > deps.get(sk, 0):
                deps[sk] = v
        for k in reads:
            add(self.lastw.get(k), 'raw')
        for k in writes:
            add(self.lastw.get(k), 'waw')
            for r in self.readers.get(k, ()):
                add(r, 'war')
        seen = self.seen[eng]
        return [(sk, v) for sk, v in deps.items() if seen.get(sk, 0) < v]

    def _absorb(self, eng, need):
        seen = self.seen[eng]
        for sk, v in need:
            c = self.clock.get((sk, v))
            if c:
                for k2, v2 in c.items():
                    if seen.get(k2, 0) < v2:
                        seen[k2] = v2
            if seen.get(sk, 0) < v:
                seen[sk] = v

    def _emit_waits(self, eng, need):
        h = self.h[eng]
        for sk, v in need[:-1]:
            h.wait_ge(self.sem[sk], v)
            self.nwait += 1
        return need[-1] if need else None

    def _record(self, tag, reads, writes):
        for k in writes:
            self.lastw[k] = tag
            self.readers[k] = []
        for k in reads:
            self.readers.setdefault(k, []).append(tag)

    def op(self, eng, fn, r=(), w=()):
        if self.cut is not None and self.ninst >= self.cut:
            return None
        need = self._deps(eng, r, w)
        last = self._emit_waits(eng, need)
        ins = fn(self.h[eng])
        if last is not None:
            ins._wait_ge(self.sem[last[0]], last[1])
        self._absorb(eng, need)
        self.val[eng] += 1
        v = self.val[eng]
        ins.then_inc(self.sem[eng], 1)
        snap = dict(self.seen[eng])
        snap[eng] = v
        self.clock[(eng, v)] = snap
        self._record((eng, v), r, w)
        self.ninst += 1
        return ins

    def dma(self, q, pairs, lane, r=(), w=()):
        if self.cut is not None and self.ninst >= self.cut:
            return None
        lk = ('L', self.lane(lane))
        need = self._deps(q, r, w)
        pv = self.val[lk]
        if pv > 0 and self.seen[q].get(lk, 0) < pv:
            need = [(sk, v) for sk, v in need if sk != lk] + [(lk, pv)]
        last = self._emit_waits(q, need)
        h = self.h[q]
        first = True
        for pr in pairs:
            if callable(pr):
                ins = pr(h)
            else:
                ins = h.dma_start(out=pr[0], in_=pr[1])
            if first and last is not None:
                ins._wait_ge(self.sem[last[0]], last[1])
            first = False
            ins.then_inc(self.sem[lk], 16)
            self.val[lk] += 16
            self.ninst += 1
        self._absorb(q, need)
        v = self.val[lk]
        snap = dict(self.seen[q])
        snap[lk] = v
        self.clock[(lk, v)] = snap
        self._record((lk, v), r, w)

    def wait_all(self, eng, keys):
        need = self._deps(eng, keys, ())
        for sk, v in need:
            self.h[eng].wait_ge(self.sem[sk], v)
        self._absorb(eng, need)

    def barrier(self):
        for e in self.ENG:
            need = [(sk, v) for sk, v in self.val.items()
                    if v > 0 and sk != e and self.seen[e].get(sk, 0) < v]
            for sk, v in need:
                self.h[e].wait_ge(self.sem[sk], v)
                self.nwait += 1
            for sk, v in need:
                self.seen[e][sk] = v


def build(nl=L, stop=None, dbg=(), moe_w=True):
    nc = bass.Bass("TRN2", target_bir_lowering=False)
    P = Prog(nc)
    P.init_psum()

    def din(name, shape, dt=F32):
        return nc.dram_tensor(name, list(shape), dt, kind="ExternalInput").ap()

    def dscr(name, shape, dt):
        kind = "ExternalOutput" if name in dbg else "Internal"
        return nc.dram_tensor(name, list(shape), dt, kind=kind).ap()

    xT_in = din("xT", [D, T])
    cvec_d = din("cvec", [128, 8, 2])
    bmod_d = din("bmod", [128, L, 48])
    nmix_d = din("nmix", [128, L, 8])
    nffn_d = din("nffn", [128, L, 8])
    fnorm_d = din("fnorm", [128, 8])
    convw_d = din("convw", [128, L, 3, 24])
    alog_d = din("alog", [128, L, 16])
    dtb_d = din("dtb", [128, L, 16])
    onorm_d = din("onorm", [128, L])
    brt_d = din("brt", [128, L, 36])
    w_mod = din("w_mod", [nl, D, 6 * D])
    w_in = din("w_in", [nl, D, IN_COLS])
    w_fourier = din("w_fourier", [nl, 512, D])
    w_delta = din("w_delta", [nl, D, D])
    w_out = din("w_out", [nl, D, D])
    w_rt = din("w_rt", [nl, D, 36])
    NEd = NE if moe_w else 1
    w_gate = din("w_gate", [nl, NEd, D, 512])
    w_up = din("w_up", [nl, NEd, D, 512])
    w_down = din("w_down", [nl, NEd, 512, D])
    dftc = din("dftc", [SEQ, SEQ], BF16)
    dfts = din("dfts", [SEQ, SEQ], BF16)
    dftc_c = din("dftc_c", [CTX, CTX], BF16)
    dfts_c = din("dfts_c", [CTX, CTX], BF16)
    chc_d = din("chc", [128, 128], BF16)
    chs_d = din("chs", [128, 128], BF16)
    bmask_d = din("bmask", [128, 4, 128], BF16)
    iota_d = din("iota", [128, 64], F32)
    wbase_d = din("wbase", [128, 8], F32)
    outT = nc.dram_tensor("outT", [D, SEQ], F32, kind="ExternalOutput").ap()

    xs = dscr("xs", [D, T], F32)
    qT = dscr("qT", [D, T], BF16)
    kT = dscr("kT", [D, T], BF16)
    k_tok = dscr("k_tok", [8, 128, T], BF16)
    v_tok = dscr("v_tok", [8, 128, T], BF16)
    ab_tok = dscr("ab_tok", [128, NB, 32], F32)
    zT = dscr("zT", [D, T], BF16)
    u_tok = dscr("u_tok", [128, NB, 512], BF16)
    gT = dscr("gT", [2 * D, T], BF16)
    odT = dscr("odT", [D, T], BF16)
    h2tok_d = dscr("h2tok", [128, NB, D], BF16)
    rt_d = dscr("rt", [128, NB, 66], F32)
    NBLK = 49
    xs_d = dscr("xs_slots", [NBLK * 512, D], BF16)
    ys_d = dscr("ys_slots", [NBLK * 512, D], BF16)

    ones_bf = P.sb("ones_bf", [128, 128], BF16)
    ones_f = P.sb("ones_f", [128, 128], F32)
    zeros_f = P.sb("zeros_f", [128, 128], F32)
    ident_bf = P.sb("ident_bf", [128, 128], BF16)
    ident_f = P.sb("ident_f", [128, 128], F32)
    triF = P.sb("triF", [128, 128], F32)
    triB = P.sb("triB", [128, 128], F32)
    negF = P.sb("negF", [128, 128], F32)
    negB = P.sb("negB", [128, 128], F32)
    strF = P.sb("strF", [128, 128], F32)
    strB = P.sb("strB", [128, 128], F32)
    epsT = P.sb("epsT", [128, 1], F32)
    CK = 'const'

    def pool(fn, r=(), w=(CK,)):
        P.op('pool', fn, r=r, w=w)
    pool(lambda e: e.memset(ones_f[:], 1.0))
    pool(lambda e: e.memset(zeros_f[:], 0.0))
    pool(lambda e: e.memset(epsT[:], EPS))
    P.op('dve', lambda e: e.tensor_copy(out=ones_bf[:], in_=ones_f[:]), r=[CK], w=[CK])

    def asel(out, in_, pat, cm, op, fill):
        pool(lambda e: e.affine_select(out=out, in_=in_, pattern=pat, compare_op=op, fill=fill,
                                       base=0, channel_multiplier=cm), r=[CK])
    asel(ident_f[:], ones_f[:], [[1, 128]], -1, ALU.is_equal, 0.0)
    asel(triF[:], ones_f[:], [[1, 128]], -1, ALU.is_ge, 0.0)
    asel(triB[:], ones_f[:], [[-1, 128]], 1, ALU.is_ge, 0.0)
    asel(negF[:], zeros_f[:], [[1, 128]], -1, ALU.is_ge, -1.0e5)
    asel(negB[:], zeros_f[:], [[-1, 128]], 1, ALU.is_ge, -1.0e5)
    asel(strF[:], ones_f[:], [[1, 128]], -1, ALU.is_gt, 0.0)
    asel(strB[:], ones_f[:], [[-1, 128]], 1, ALU.is_gt, 0.0)
    P.op('dve', lambda e: e.tensor_copy(out=ident_bf[:], in_=ident_f[:]), r=[CK], w=[CK])

    bmod = P.sb("bmod", [128, L, 48], F32)
    nmix = P.sb("nmix", [128, L, 8], F32)
    nffn = P.sb("nffn", [128, L, 8], F32)
    fnorm = P.sb("fnorm", [128, 8], F32)
    convw = P.sb("convw", [128, L, 3, 24], F32)
    alog = P.sb("alog", [128, L, 16], F32)
    dtb = P.sb("dtb", [128, L, 16], F32)
    onorm = P.sb("onorm", [128, L], F32)
    brt = P.sb("brt", [128, L, 36], F32)
    chc = P.sb("chc", [128, 128], BF16)
    chs = P.sb("chs", [128, 128], BF16)
    cv = P.sb("cv", [128, 8, 2], F32)
    bmask = P.sb("bmask", [128, 4, 128], BF16)
    iota = P.sb("iota", [128, 64], F32)
    wbase = P.sb("wbase", [128, 8], F32)
    P.dma('sp', [(bmod[:], bmod_d), (nmix[:], nmix_d), (nffn[:], nffn_d), (fnorm[:], fnorm_d),
                 (convw[:], convw_d), (alog[:], alog_d), (dtb[:], dtb_d), (onorm[:], onorm_d),
                 (brt[:], brt_d), (chc[:], chc_d), (chs[:], chs_d), (cv[:], cvec_d), (bmask[:], bmask_d), (iota[:], iota_d), (wbase[:], wbase_d)],
          lane='par', w=['par'])
    modv = P.sb("modv", [128, L, 48, 2], F32)
    A1 = P.sb("A1", [128, L, 8, 2], F32)
    A2 = P.sb("A2", [128, L, 8, 2], F32)
    nea = P.sb("nea", [128, L, 16], F32)
    P.op('act', lambda e: e.activation(out=nea[:], in_=alog[:], func=AF.Exp), r=['par'], w=['nea'])
    P.op('dve', lambda e: e.tensor_scalar(out=nea[:], in0=nea[:], scalar1=-1.0, scalar2=None, op0=ALU.mult),
         r=['nea'], w=['nea'])

    def phase_mod():
        P.scope()
        sv = P.sb("sv", [128, 8, 2], F32)
        P.op('act', lambda e: e.activation(out=sv[:], in_=cv[:], func=AF.Silu), r=['par'], w=['sv'])
        wts = [P.sb("wmt%d" % i, [128, 8, 512], F32) for i in range(2)]
        it = 0
        for l in range(nl):
            wv = w_mod[l].rearrange("(kc p) n -> p kc n", p=128)
            for cb in range(12):
                b = it % 2
                it += 1
                wt = wts[b]
                P.dma('sp', [(wt[:], wv[:, :, cb * 512:(cb + 1) * 512])], lane='wm%d' % b, w=[('wm', b)])
                ps = P.psum()
                for j in range(4):
                    for kc in range(8):
                        P.op('pe', lambda e: e.matmul(ps.t[:, j * 2:j * 2 + 2], wt[:, kc, j * 128:(j + 1) * 128],
                                                      sv[:, kc, :], start=(kc == 0), stop=(kc == 7)),
                             r=[('wm', b), 'sv'], w=[ps.k])
                P.op('dve', lambda e: e.tensor_tensor(
                    out=modv[:, l, cb * 4:cb * 4 + 4, :],
                    in0=ps.t[:, 0:8].rearrange("p (a b) -> p a b", b=2),
                    in1=bmod[:, l, cb * 4:cb * 4 + 4].unsqueeze(2).to_broadcast([128, 4, 2]),
                    op=ALU.add), r=[ps.k, 'par'], w=['mod'])
            for (A, nw, c0) in ((A1, nmix, 8), (A2, nffn, 32)):
                P.op('dve', lambda e: e.tensor_scalar(out=A[:, l], in0=modv[:, l, c0:c0 + 8, :], scalar1=1.0,
                                                      scalar2=None, op0=ALU.add), r=['mod'], w=['mod'])
                P.op('dve', lambda e: e.tensor_tensor(out=A[:, l], in0=A[:, l],
                                                      in1=nw[:, l, :].unsqueeze(2).to_broadcast([128, 8, 2]),
                                                      op=ALU.mult), r=['mod', 'par'], w=['mod'])
        P.barrier()
        P.end_scope()

    def norm_mod(xt, xkey, W, Aap, Bap, outs, tl):
        ss = P.psum()
        for kc in range(8):
            sq = tl['sq'][kc % 2]
            P.op('act', lambda e: e.activation(out=sq[:, :W], in_=xt[:, kc, :W], func=AF.Square),
                 r=[xkey], w=[('sq', kc % 2)])
            P.op('pe', lambda e: e.matmul(ss.t[:, :W], ones_bf[:], sq[:, :W], start=(kc == 0), stop=(kc == 7)),
                 r=[('sq', kc % 2), CK], w=[ss.k])
        rstd = tl['rstd']
        P.op('act', lambda e: e.activation(out=rstd[:, :W], in_=ss.t[:, :W], func=AF.Sqrt, scale=1.0 / D,
                                           bias=epsT[:, 0:1]), r=[ss.k, CK], w=['rstd'])
        P.op('dve', lambda e: e.reciprocal(out=rstd[:, :W], in_=rstd[:, :W]), r=['rstd'], w=['rstd'])
        for kc in range(8):
            tmp = tl['tmp'][kc % 2]
            P.op('dve', lambda e: e.tensor_tensor(out=tmp[:, :W], in0=xt[:, kc, :W], in1=rstd[:, :W], op=ALU.mult),
                 r=[xkey, 'rstd'], w=[('tmp', kc % 2)])
            for (fn, ok) in outs:
                P.op('act', lambda e: e.activation(out=fn(kc), in_=tmp[:, :W], func=AF.Identity,
                                                   scale=Aap[:, kc:kc + 1], bias=Bap[:, kc:kc + 1]),
                     r=[('tmp', kc % 2), 'mod'], w=[ok])

    def phase_proj(l, xsrc):
        P.scope()
        last = (l == L - 1)
        hT = P.sb("hT", [128, 8, T], BF16)
        P.scope()
        xts = [P.sb("xt%d" % i, [128, 8, 512], F32) for i in range(2)]
        tl = {'sq': [P.sb("sq%d" % i, [128, 512], BF16) for i in range(2)],
              'tmp': [P.sb("tmp%d" % i, [128, 512], F32) for i in range(2)],
              'rstd': P.sb("rstd", [128, 512], F32)}
        xv = xsrc.rearrange("(kc p) t -> p kc t", p=128)
        for ti, (t0, W, lat) in enumerate(TILES):
            ws = 0 if lat else 1
            xt = xts[ti % 2]
            P.dma('sp', [(xt[:, :, :W], xv[:, :, t0:t0 + W])], lane='xt%d' % (ti % 2), w=[('xt', ti % 2)])
            norm_mod(xt, ('xt', ti % 2), W, A1[:, l, :, ws], modv[:, l, 0:8, ws],
                     [(lambda kc: hT[:, kc, t0:t0 + W], ('hT', ti))], tl)
        P.barrier()
        P.end_scope()
        wps = [P.sb("wp%d" % i, [128, 8, 1024], BF16) for i in range(2)]
        st8 = [P.sb("st8_%d" % i, [128, 8, 512], BF16) for i in range(2)]
        tokst = [P.sb("tokst%d" % i, [128, 8, 4, 128], BF16) for i in range(2)]
        ust = [P.sb("ust%d" % i, [128, 4, 512], BF16) for i in range(2)]
        abst = P.sb("abst", [128, 4, 32], F32)
        NSL = 6
        accS = [P.sb("acc%d" % i, [128, 512], F32) for i in range(NSL)]
        actfS = [P.sb("actf%d" % i, [128, 512], F32) for i in range(NSL)]
        sqbS = [P.sb("sqb%d" % i, [128, 512], BF16) for i in range(NSL)]
        actbS = sqbS
        rnS = [P.sb("rn%d" % i, [128, 512], F32) for i in range(NSL)]
        wv = w_in[l].rearrange("(kc p) n -> p kc n", p=128)
        pieces = [('q', 0, 1024), ('k', 1024, 1024), ('v', 2048, 1024), ('ab', OFF_A, 32), ('z', OFF_Z, 1024),
                  ('F', OFF_F, 512), ('Ga', OFF_G, 1024), ('Gb', OFF_G + 1024, 1024)]
        cnt = {'st8': 0, 'tok': 0, 'u': 0}
        for pi, (pn, c0, ncol) in enumerate(pieces):
            if os.environ.get('KSCOPE'):
                if pi > 0:
                    nc.leave_named_scope(*_sc, False)
                _sc = ('pj_' + pn, nc.enter_named_scope('pj_' + pn, False)[0])
            b = pi % 2
            wp = wps[b]
            wk = ('wp', b)
            P.dma('pool', [(wp[:, :, :ncol], wv[:, :, c0:c0 + ncol])], lane='wp%d' % b, w=[wk])
            for ti, (t0, W, lat) in enumerate(TILES):
                hk = ('hT', ti)
                nbk = W // 128
                if (not lat) and last and pn in ('z', 'F', 'Ga', 'Gb'):
                    continue
                if pn in ('q', 'k', 'v'):
                    si = cnt['st8'] % 2
                    st = st8[si]
                    sk = ('st8', si)
                    if pn in ('k', 'v'):
                        tki = cnt['tok'] % 2
                        cnt['tok'] += 1
                        tk = tokst[tki]
                        tkk = ('tok', tki)
                    def job(cc, sl):
                        gcc = (c0 // 128) + cc
                        ps = P.psum_pool[sl]
                        acc, actf, actb, sqb, rn = accS[sl], actfS[sl], actbS[sl], sqbS[sl], rnS[sl]
                        ka, kf, kb, kq, kr = ('acc', sl), ('actf', sl), ('sqb', sl), ('sqb', sl), ('rn', sl)
                        for kc in range(8):
                            P.op('pe', lambda e: e.matmul(ps.t[:, :W], wp[:, kc, cc * 128:(cc + 1) * 128],
                                                          hT[:, kc, t0:t0 + W], start=(kc == 0), stop=(kc == 7)),
                                 r=[wk, hk], w=[ps.k])
                        yield
                        rl = 64 if lat else 256
                        pv = ps.t[:, :W].rearrange("p (r c) -> p r c", c=rl)
                        av = acc[:, :W].rearrange("p (r c) -> p r c", c=rl)
                        P.op('dve', lambda e: e.tensor_scalar(out=acc[:, :W], in0=ps.t[:, :W],
                                                              scalar1=convw[:, l, 1, gcc:gcc + 1], scalar2=None,
                                                              op0=ALU.mult), r=[ps.k, 'par'], w=[ka])
                        P.op('dve', lambda e: e.scalar_tensor_tensor(
                            out=av[:, :, 1:rl], in0=pv[:, :, 0:rl - 1], scalar=convw[:, l, 0, gcc:gcc + 1],
                            in1=av[:, :, 1:rl], op0=ALU.mult, op1=ALU.add), r=[ps.k, 'par', ka], w=[ka])
                        P.op('dve', lambda e: e.scalar_tensor_tensor(
                            out=av[:, :, 0:rl - 1], in0=pv[:, :, 1:rl], scalar=convw[:, l, 2, gcc:gcc + 1],
                            in1=av[:, :, 0:rl - 1], op0=ALU.mult, op1=ALU.add), r=[ps.k, 'par', ka], w=[ka])
                        yield
                        if pn == 'v':
                            P.op('act', lambda e: e.activation(out=actb[:, :W], in_=acc[:, :W], func=AF.Silu),
                                 r=[ka], w=[kb])
                            src, srk = actb, kb
                            yield
                        else:
                            P.op('act', lambda e: e.activation(out=actf[:, :W], in_=acc[:, :W], func=AF.Silu),
                                 r=[ka], w=[kf])
                            yield
                            P.op('act', lambda e: e.activation(out=sqb[:, :W], in_=actf[:, :W], func=AF.Square),
                                 r=[kf], w=[kq])
                            yield
                            P.op('pe', lambda e: e.matmul(ps.t[:, :W], ones_bf[:], sqb[:, :W], start=True, stop=True),
                                 r=[kq, CK], w=[ps.k])
                            yield
                            P.op('act', lambda e: e.activation(out=rn[:, :W], in_=ps.t[:, :W], func=AF.Sqrt,
                                                               bias=epsT[:, 0:1]), r=[ps.k, CK], w=[kr])
                            yield
                            P.op('dve', lambda e: e.reciprocal(out=rn[:, :W], in_=rn[:, :W]), r=[kr], w=[kr])
                            sc = (128.0 ** -0.5) if pn == 'q' else 1.0
                            P.op('dve', lambda e: e.scalar_tensor_tensor(
                                out=st[:, cc, :W], in0=actf[:, :W], scalar=sc, in1=rn[:, :W],
                                op0=ALU.mult, op1=ALU.mult), r=[kf, kr], w=[sk])
                            src, srk = st[:, cc], sk
                            yield
                        if pn in ('k', 'v'):
                            ptv = ps.t[:, :].bitcast(BF16)
                            for tb in range(nbk):
                                P.op('pe', lambda e: e.transpose(ptv[:, tb * 128:(tb + 1) * 128],
                                                                 src[:, tb * 128:(tb + 1) * 128], ident_bf[:]),
                                     r=[srk, CK], w=[ps.k])
                            yield
                            P.op('act', lambda e: e.copy(
                                out=tk[:, cc, 0:nbk, :],
                                in_=ptv[:, :nbk * 128].rearrange("p (a b) -> p a b", b=128)),
                                r=[ps.k], w=[tkk])
                            yield

                    pendj = list(range(8))
                    slotsj = {}
                    while pendj or slotsj:
                        for sl in range(NSL):
                            if sl not in slotsj and pendj:
                                slotsj[sl] = job(pendj.pop(0), sl)
                        for sl in list(slotsj.keys()):
                            try:
                                next(slotsj[sl])
                            except StopIteration:
                                del slotsj[sl]
                    if pn in ('q', 'k'):
                        dst = qT if pn == 'q' else kT
                        P.dma('sp', [(dst.rearrange("(c p) t -> p c t", p=128)[:, :, t0:t0 + W], st[:, :, :W])],
                              lane='st8_%d' % si, r=[sk], w=[pn + 'T'])
                        cnt['st8'] += 1
                    if pn in ('k', 'v'):
                        dst = k_tok if pn == 'k' else v_tok
                        P.dma('sp', [(dst[:, :, t0:t0 + W].rearrange("c p t -> p c t"),
                                      tk[:, :, :nbk, :].rearrange("p c a b -> p c (a b)"))],
                              lane='tok%d' % tki, r=[tkk], w=[pn + '_tok'])
                elif pn == 'ab':
                    ps = P.psum()
                    for tb in range(nbk):
                        for kc in range(8):
                            P.op('pe', lambda e: e.matmul(ps.t[:, tb * 32:(tb + 1) * 32],
                                                          hT[:, kc, t0 + tb * 128:t0 + (tb + 1) * 128],
                                                          wp[:, kc, 0:32], start=(kc == 0), stop=(kc == 7)),
                                 r=[wk, hk], w=[ps.k])
                    P.op('dve', lambda e: e.tensor_copy(out=abst[:, :nbk, :],
                                                        in_=ps.t[:, :nbk * 32].rearrange("p (a b) -> p a b", b=32)),
                         r=[ps.k], w=['abst'])
                    P.dma('sp', [(ab_tok[:, t0 // 128:t0 // 128 + nbk, :], abst[:, :nbk, :])],
                          lane='abst', r=['abst'], w=['ab_tok'])
                elif pn in ('z', 'Ga', 'Gb'):
                    si = cnt['st8'] % 2
                    cnt['st8'] += 1
                    st = st8[si]
                    sk = ('st8', si)
                    fnc = AF.Silu if pn == 'z' else AF.Sigmoid
                    for cc in range(8):
                        ps = P.psum()
                        for kc in range(8):
                            P.op('pe', lambda e: e.matmul(ps.t[:, :W], wp[:, kc, cc * 128:(cc + 1) * 128],
                                                          hT[:, kc, t0:t0 + W], start=(kc == 0), stop=(kc == 7)),
                                 r=[wk, hk], w=[ps.k])
                        P.op('act', lambda e: e.activation(out=st[:, cc, :W], in_=ps.t[:, :W], func=fnc),
                             r=[ps.k], w=[sk])
                    if pn == 'z':
                        dv = zT.rearrange("(c p) t -> p c t", p=128)
                    else:
                        o8 = 0 if pn == 'Ga' else 8
                        dv = gT.rearrange("(c p) t -> p c t", p=128)[:, o8:o8 + 8, :]
                    P.dma('sp', [(dv[:, :, t0:t0 + W], st[:, :, :W])], lane='st8_%d' % si, r=[sk], w=[pn])
                elif pn == 'F':
                    ui = cnt['u'] % 2
                    cnt['u'] += 1
                    us = ust[ui]
                    uk = ('ust', ui)
                    for tb in range(nbk):
                        ps = P.psum()
                        for kc in range(8):
                            P.op('pe', lambda e: e.matmul(ps.t[:, :512], hT[:, kc, t0 + tb * 128:t0 + (tb + 1) * 128],
                                                          wp[:, kc, 0:512], start=(kc == 0), stop=(kc == 7)),
                                 r=[wk, hk], w=[ps.k])
                        P.op('act', lambda e: e.copy(out=us[:, tb, :], in_=ps.t[:, :512]), r=[ps.k], w=[uk])
                    P.dma('sp', [(u_tok[:, t0 // 128:t0 // 128 + nbk, :], us[:, :nbk, :])],
                          lane='ust%d' % ui, r=[uk], w=['u_tok'])
        if os.environ.get('KSCOPE'):
            nc.leave_named_scope(*_sc, False)
        P.barrier()
        P.end_scope()

    def phase_delta(l):
        P.scope()
        last = (l == L - 1)
        ab = P.sb("ab", [128, NB, 32], F32)
        P.dma('sp', [(ab[:], ab_tok)], lane='ab', w=['ab'])
        g = P.sb("g", [128, NB, 16], F32)
        beta = P.sb("beta", [128, NB, 16], F32)
        nbeta = P.sb("nbeta", [128, NB, 16], F32)
        gcum = P.sb("gcum", [128, NB, 16], F32)
        ngam = P.sb("ngam", [128, NB, 16], F32)
        tail = P.sb("tail", [128, NB, 16], F32)
        egl = P.sb("egl", [128, NB, 16], F32)
        GK = 'gates'
        P.op('dve', lambda e: e.tensor_tensor(out=g[:], in0=ab[:, :, 0:16],
                                              in1=dtb[:, l, :].unsqueeze(1).to_broadcast([128, NB, 16]), op=ALU.add),
             r=['ab', 'par'], w=[GK])
        P.op('act', lambda e: e.activation(out=g[:], in_=g[:], func=AF.Exp), r=[GK], w=[GK])
        P.op('act', lambda e: e.activation(out=g[:], in_=g[:], func=AF.Ln, bias=ones_f[:, 0:1]), r=[GK, CK], w=[GK])
        P.op('dve', lambda e: e.tensor_tensor(out=g[:], in0=g[:],
                                              in1=nea[:, l, :].unsqueeze(1).to_broadcast([128, NB, 16]), op=ALU.mult),
             r=[GK, 'nea'], w=[GK])
        P.op('act', lambda e: e.activation(out=beta[:], in_=ab[:, :, 16:32], func=AF.Sigmoid), r=['ab'], w=[GK])
        P.op('dve', lambda e: e.tensor_scalar(out=nbeta[:], in0=beta[:], scalar1=-1.0, scalar2=None, op0=ALU.mult),
             r=[GK], w=[GK])
        for d in range(2):
            tri = triF if d == 0 else triB
            ps = P.psum()
            P.op('pe', lambda e: e.matmul(ps.t[:, :NB * 8].rearrange("p (a b) -> p a b", b=8), tri[:],
                                          g[:, :, d * 8:(d + 1) * 8], start=True, stop=True), r=[GK, CK], w=[ps.k])
            P.op('dve', lambda e: e.tensor_copy(out=gcum[:, :, d * 8:(d + 1) * 8],
                                                in_=ps.t[:, :NB * 8].rearrange("p (a b) -> p a b", b=8)),
                 r=[ps.k], w=[GK])
            ps2 = P.psum()
            P.op('pe', lambda e: e.matmul(ps2.t[:, :NB * 8].rearrange("p (a b) -> p a b", b=8), ones_f[:],
                                          g[:, :, d * 8:(d + 1) * 8], start=True, stop=True), r=[GK, CK], w=[ps2.k])
            P.op('act', lambda e: e.activation(out=egl[:, :, d * 8:(d + 1) * 8],
                                               in_=ps2.t[:, :NB * 8].rearrange("p (a b) -> p a b", b=8), func=AF.Exp),
                 r=[ps2.k], w=[GK])
            P.op('dve', lambda e: e.tensor_tensor(out=tail[:, :, d * 8:(d + 1) * 8],
                                                  in0=ps2.t[:, :NB * 8].rearrange("p (a b) -> p a b", b=8),
                                                  in1=gcum[:, :, d * 8:(d + 1) * 8], op=ALU.subtract),
                 r=[ps2.k, GK], w=[GK])
        P.op('act', lambda e: e.activation(out=tail[:], in_=tail[:], func=AF.Exp), r=[GK], w=[GK])
        P.op('act', lambda e: e.activation(out=ngam[:], in_=gcum[:], func=AF.Exp), r=[GK], w=[GK])
        P.op('dve', lambda e: e.tensor_scalar(out=ngam[:], in0=ngam[:], scalar1=-1.0, scalar2=None, op0=ALU.mult),
             r=[GK], w=[GK])

        DSTOP = os.environ.get('KDSTOP', '')
        if DSTOP == 'gates':
            P.barrier()
            P.end_scope()
            return
        qTh = P.sb("qTh", [128, T], BF16)
        kTh = P.sb("kTh", [128, T], BF16)
        ktk = P.sb("ktk", [128, NB, 128], BF16)
        vtk = P.sb("vtk", [128, NB, 128], BF16)
        oacc = P.sb("oacc", [128, T], F32)
        Tt = P.sb("Tt", [128, NB, 2, 128], BF16)
        AT = P.sb("AT", [128, NB, 2, 128], BF16)
        qdec = P.sb("qdec", [128, NB, 2, 128], BF16)
        ktail = P.sb("ktail", [128, NB, 2, 128], BF16)
        GP = 4
        Ef = [P.sb("Ef%d" % i, [128, 2, 128], F32) for i in range(GP)]
        Es = [P.sb("Es%d" % i, [128, 2, 128], F32) for i in range(GP)]
        Ge = [P.sb("Ge%d" % i, [128, 2, 128], F32) for i in range(GP)]
        ABt = [[P.sb("ABt%d_%d" % (g_, i), [128, 2, 2, 128], BF16) for i in range(2)] for g_ in range(GP)]
        Ttw = [[P.sb("Ttw%d_%d" % (g_, i), [128, 2, 128], BF16) for i in range(2)] for g_ in range(GP)]
        Xf = [P.sb("Xf%d" % i, [128, 2, 128], BF16) for i in range(GP)]
        XfT = [P.sb("XfT%d" % i, [128, 2, 128], BF16) for i in range(GP)]
        UT = [P.sb("UT%d" % i, [128, 3, 2, 128], BF16) for i in range(GP)]
        Hh = [P.sb("Hh%d" % i, [128, 2, 128], BF16) for i in range(GP)]
        Wn = [P.sb("Wn%d" % i, [128, 2, 128], BF16) for i in range(GP)]
        Sf = [P.sb("Sf%d" % i, [128, 128], F32) for i in range(2)]
        Sb = [P.sb("Sb%d" % i, [128, 128], BF16) for i in range(2)]
        Rt = [P.sb("Rt%d" % i, [128, 128], BF16) for i in range(2)]
        vn = [P.sb("vn%d" % i, [128, 128], BF16) for i in range(2)]
        sqo = [P.sb("sqo%d" % i, [128, 512], BF16) for i in range(2)]
        rno = P.sb("rno", [128, 512], F32)
        odf = P.sb("odf", [128, 512], F32)
        odst = [P.sb("odst%d" % i, [128, 512], BF16) for i in range(2)]
        tris = (triF, triB)
        negs = (negF, negB)
        strs = (strF, strB)
        uidx = 0
        for h in range(8):
            hs = slice(h * 128, (h + 1) * 128)
            HK = ('hd', 0)
            P.dma('sp', [(qTh[:], qT[hs, :]), (kTh[:], kT[hs, :]),
                         (ktk[:].rearrange("p b f -> p (b f)"), k_tok[h]),
                         (vtk[:].rearrange("p b f -> p (b f)"), v_tok[h])], lane='hd', w=[HK])
            OK_ = [('oacc', n) for n in range(NB)]
            P.op('pool', lambda e: e.memset(oacc[:], 0.0), w=OK_)
            def pre(n, g_):
                bs = slice(n * 128, (n + 1) * 128)
                kE, kS, kG, kX, kXT, kU, kH, kW = (('Ef', g_), ('Es', g_), ('Ge', g_), ('Xf', g_), ('XfT', g_),
                                                   ('UT', g_), ('Hh', g_), ('Wn', g_))
                AB = ABt[g_]
                TW = Ttw[g_]
                bank = P.psum_pool[g_]
                bankbf = PsT(bank.t[:, :].bitcast(BF16), bank.k)
                pk = bank
                P.op('pe', lambda e: e.matmul(pk.t[:, 0:128], kTh[:, bs], kTh[:, bs], start=True, stop=True),
                     r=[HK], w=[pk.k])
                P.op('pe', lambda e: e.matmul(pk.t[:, 128:256], kTh[:, bs], qTh[:, bs], start=True, stop=True),
                     r=[HK], w=[pk.k])
                for d in range(2):
                    col = d * 8 + h
                    P.op('pe', lambda e: e.matmul(pk.t[:, 256 + d * 128:384 + d * 128],
                                                  g[:, n, col:col + 1].to_broadcast([128, 128]), tris[d][:],
                                                  start=True, stop=True), r=[GK, CK], w=[pk.k])
                yield
                for d in range(2):
                    col = d * 8 + h
                    P.op('dve', lambda e: e.scalar_tensor_tensor(
                        out=Ef[g_][:, d, :], in0=pk.t[:, 256 + d * 128:384 + d * 128], scalar=gcum[:, n, col:col + 1],
                        in1=negs[d][:], op0=ALU.subtract, op1=ALU.add), r=[pk.k, GK, CK], w=[kE])
                yield
                P.op('act', lambda e: e.activation(out=Ef[g_][:], in_=Ef[g_][:], func=AF.Exp), r=[kE], w=[kE])
                P.op('act', lambda e: e.activation(out=Ge[g_][:],
                                                   in_=pk.t[:, 256:512].rearrange("p (a b) -> p a b", b=128),
                                                   func=AF.Exp), r=[pk.k], w=[kG])
                yield
                for d in range(2):
                    P.op('dve', lambda e: e.tensor_tensor(out=Es[g_][:, d, :], in0=Ef[g_][:, d, :], in1=strs[d][:],
                                                          op=ALU.mult), r=[kE, CK], w=[kS])
                for d in range(2):
                    col = d * 8 + h
                    P.op('dve', lambda e: e.tensor_tensor(out=AT[:, n, d, :], in0=pk.t[:, 128:256], in1=Ef[g_][:, d, :],
                                                          op=ALU.mult), r=[pk.k, kE], w=[('pre', n)])
                    P.op('dve', lambda e: e.scalar_tensor_tensor(
                        out=Xf[g_][:, d, :], in0=pk.t[:, 0:128], scalar=nbeta[:, n, col:col + 1],
                        in1=Es[g_][:, d, :], op0=ALU.mult, op1=ALU.mult), r=[pk.k, GK, kS], w=[kX])
                    P.op('dve', lambda e: e.tensor_tensor(out=qdec[:, n, d, :], in0=qTh[:, bs], in1=Ge[g_][:, d, :],
                                                          op=ALU.mult), r=[HK, kG], w=[('pre', n)])
                    P.op('act', lambda e: e.activation(out=ktail[:, n, d, :], in_=ktk[:, n, :], func=AF.Copy,
                                                       scale=tail[:, n, col:col + 1]), r=[HK, GK], w=[('pre', n)])
                P.op('dve', lambda e: e.tensor_tensor(out=AB[0][:, :, 0, :], in0=Xf[g_][:],
                                                      in1=bmask[:, 0:1, :].to_broadcast([128, 2, 128]), op=ALU.mult),
                     r=[kX, 'par'], w=[('AB', g_, 0)])
                yield
                pt = bankbf
                for d in range(2):
                    P.op('pe', lambda e: e.transpose(pt.t[:, d * 128:(d + 1) * 128], AB[0][:, d, 0, :], ident_bf[:]),
                         r=[('AB', g_, 0), CK], w=[pt.k])
                    P.op('pe', lambda e: e.transpose(pt.t[:, 256 + d * 128:256 + (d + 1) * 128], Xf[g_][:, d, :],
                                                     ident_bf[:]), r=[kX, CK], w=[pt.k])
                yield
                P.op('act', lambda e: e.copy(out=AB[0][:, :, 1, :],
                                             in_=pt.t[:, 0:256].rearrange("p (a b) -> p a b", b=128)),
                     r=[pt.k], w=[('AB', g_, 0)])
                P.op('act', lambda e: e.copy(out=XfT[g_][:], in_=pt.t[:, 256:512].rearrange("p (a b) -> p a b", b=128)),
                     r=[pt.k], w=[kXT])
                P.op('dve', lambda e: e.tensor_tensor(out=TW[0][:], in0=AB[0][:, :, 0, :],
                                                      in1=ident_bf[:].unsqueeze(1).to_broadcast([128, 2, 128]),
                                                      op=ALU.add), r=[('AB', g_, 0), CK], w=[('Ttw', g_, 0)])
                for b3 in range(3):
                    P.op('dve', lambda e: e.tensor_tensor(out=UT[g_][:, b3], in0=XfT[g_][:],
                                                          in1=bmask[:, 1 + b3:2 + b3, :].to_broadcast([128, 2, 128]),
                                                          op=ALU.mult), r=[kXT, 'par'], w=[kU])
                yield
                gi_ = 0
                for lev in range(1, 4):
                    cur = lev % 2
                    prv = 1 - cur
                    pab = bank
                    for d in range(2):
                        if lev < 3:
                            P.op('pe', lambda e: e.matmul(pab.t[:, d * 256:d * 256 + 128], AB[prv][:, d, 1, :],
                                                          AB[prv][:, d, 0, :], start=True, stop=True),
                                 r=[('AB', g_, prv)], w=[pab.k])
                        P.op('pe', lambda e: e.matmul(pab.t[:, d * 256 + 128:d * 256 + 256], AB[prv][:, d, 0, :],
                                                      AB[prv][:, d, 1, :], start=True, stop=True),
                             r=[('AB', g_, prv)], w=[pab.k])
                    yield
                    if lev < 3:
                        P.op('act', lambda e: e.copy(out=AB[cur][:],
                                                     in_=pab.t[:, :].rearrange("p (a b c) -> p a b c", a=2, b=2)),
                             r=[pab.k], w=[('AB', g_, cur)])
                    else:
                        P.op('act', lambda e: e.copy(
                            out=AB[cur][:, :, 1, :],
                            in_=pab.t[:, :].rearrange("p (a b c) -> p a b c", a=2, b=2)[:, :, 1, :]),
                            r=[pab.k], w=[('AB', g_, cur)])
                    yield
                    ptt = bank
                    for d in range(2):
                        P.op('pe', lambda e: e.matmul(ptt.t[:, d * 128:(d + 1) * 128], AB[cur][:, d, 1, :],
                                                      TW[gi_][:, d, :], start=True, stop=True),
                             r=[('AB', g_, cur), ('Ttw', g_, gi_)], w=[ptt.k])
                    yield
                    P.op('dve', lambda e: e.tensor_tensor(
                        out=TW[1 - gi_][:], in0=ptt.t[:, 0:256].rearrange("p (a b) -> p a b", b=128),
                        in1=TW[gi_][:], op=ALU.add), r=[ptt.k, ('Ttw', g_, gi_)], w=[('Ttw', g_, 1 - gi_)])
                    gi_ = 1 - gi_
                    yield
                for b3 in range(3):
                    G = TW[gi_]
                    pw2 = bank
                    pt2 = PsT(pw2.t[:, :].bitcast(BF16), pw2.k)
                    for d in range(2):
                        P.op('pe', lambda e: e.transpose(pt2.t[:, d * 128:(d + 1) * 128], G[:, d, :], ident_bf[:]),
                             r=[('Ttw', g_, gi_), CK], w=[pt2.k])
                    for d in range(2):
                        P.op('pe', lambda e: e.matmul(pw2.t[:, 256 + d * 128:256 + (d + 1) * 128], UT[g_][:, b3, d, :], G[:, d, :],
                                                      start=True, stop=True), r=[kU, ('Ttw', g_, gi_)], w=[pw2.k])
                    yield
                    P.op('act', lambda e: e.copy(out=Hh[g_][:], in_=pt2.t[:, 0:256].rearrange("p (a b) -> p a b", b=128)),
                         r=[pt2.k], w=[kH])
                    P.op('act', lambda e: e.copy(out=Wn[g_][:], in_=pw2.t[:, 256:512].rearrange("p (a b) -> p a b", b=128)),
                         r=[pw2.k], w=[kW])
                    yield
                    pg2 = bank
                    for d in range(2):
                        P.op('pe', lambda e: e.matmul(pg2.t[:, d * 128:(d + 1) * 128], Hh[g_][:, d, :], Wn[g_][:, d, :],
                                                      start=True, stop=True), r=[kH, kW], w=[pg2.k])
                    yield
                    if b3 < 2:
                        P.op('dve', lambda e: e.tensor_tensor(
                            out=TW[1 - gi_][:], in0=pg2.t[:, 0:256].rearrange("p (a b) -> p a b", b=128),
                            in1=G[:], op=ALU.add), r=[pg2.k, ('Ttw', g_, gi_)], w=[('Ttw', g_, 1 - gi_)])
                    else:
                        P.op('dve', lambda e: e.tensor_tensor(
                            out=Tt[:, n, :, :], in0=pg2.t[:, 0:256].rearrange("p (a b) -> p a b", b=128),
                            in1=G[:], op=ALU.add), r=[pg2.k, ('Ttw', g_, gi_)], w=[('pre', n)])
                    gi_ = 1 - gi_
                    yield

            orders = [list(range(NB)), [1, 0] + list(range(NB - 1, 1, -1))]
            done_pre = set()

            def chain(d):
                col = d * 8 + h
                pc = P.psum_pool[4 + d]
                P.op('pool', lambda e: e.memset(Sf[d][:], 0.0), w=[('S', d)])
                P.op('pool', lambda e: e.memset(Sb[d][:], 0.0), w=[('Sb', d)])
                for n in orders[d]:
                    while n not in done_pre:
                        yield
                    bs = slice(n * 128, (n + 1) * 128)
                    need_o = not (last and n < 2)
                    P.op('pe', lambda e: e.matmul(pc.t[:, 0:128], kTh[:, bs], Sb[d][:], start=True, stop=True),
                         r=[HK, ('Sb', d)], w=[pc.k])
                    yield
                    P.op('dve', lambda e: e.scalar_tensor_tensor(
                        out=Rt[d][:], in0=pc.t[:, 0:128], scalar=ngam[:, n, col:col + 1], in1=vtk[:, n, :],
                        op0=ALU.mult, op1=ALU.add), r=[pc.k, GK, HK], w=[('R', d)])
                    yield
                    P.op('pe', lambda e: e.matmul(pc.t[:, 128:256], Tt[:, n, d, :], Rt[d][:], start=True, stop=True),
                         r=[('pre', n), ('R', d)], w=[pc.k])
                    yield
                    P.op('act', lambda e: e.activation(out=vn[d][:], in_=pc.t[:, 128:256], func=AF.Copy,
                                                       scale=beta[:, n, col:col + 1]), r=[pc.k, GK], w=[('vn', d)])
                    yield
                    if need_o:
                        P.op('pe', lambda e: e.matmul(pc.t[:, 256:384], Sb[d][:], qdec[:, n, d, :],
                                                      start=True, stop=False), r=[('Sb', d), ('pre', n)], w=[pc.k])
                        P.op('pe', lambda e: e.matmul(pc.t[:, 256:384], vn[d][:], AT[:, n, d, :],
                                                      start=False, stop=True), r=[('vn', d), ('pre', n)], w=[pc.k])
                    P.op('pe', lambda e: e.matmul(pc.t[:, 384:512], ktail[:, n, d, :], vn[d][:], start=True, stop=True),
                         r=[('pre', n), ('vn', d)], w=[pc.k])
                    yield
                    if need_o:
                        P.op('dve', lambda e: e.tensor_tensor(out=oacc[:, bs], in0=pc.t[:, 256:384], in1=oacc[:, bs],
                                                              op=ALU.add), r=[pc.k, ('oacc', n)], w=[('oacc', n)])
                    P.op('dve', lambda e: e.scalar_tensor_tensor(
                        out=Sf[d][:], in0=Sf[d][:], scalar=egl[:, n, col:col + 1], in1=pc.t[:, 384:512],
                        op0=ALU.mult, op1=ALU.add), r=[pc.k, GK, ('S', d)], w=[('S', d)])
                    yield
                    P.op('act', lambda e: e.copy(out=Sb[d][:], in_=Sf[d][:]), r=[('S', d)], w=[('Sb', d)])
                    yield

            pend = []
            seen_ = set()
            fi_ = bi_ = 0
            while len(pend) < NB:
                while fi_ < NB and orders[0][fi_] in seen_:
                    fi_ += 1
                if fi_ < NB:
                    pend.append(orders[0][fi_])
                    seen_.add(orders[0][fi_])
                while bi_ < NB and orders[1][bi_] in seen_:
                    bi_ += 1
                if bi_ < NB:
                    pend.append(orders[1][bi_])
                    seen_.add(orders[1][bi_])
            if DSTOP == 'pre1':
                pend = pend[:1]
            slots = {}
            chains = [] if DSTOP.startswith('pre') else [chain(0), chain(1)]
            while pend or slots or chains:
                for g_ in range(GP):
                    if g_ not in slots and pend:
                        n_ = pend.pop(0)
                        slots[g_] = (pre(n_, g_), n_)
                for g_ in list(slots.keys()):
                    gen, n_ = slots[g_]
                    try:
                        next(gen)
                    except StopIteration:
                        done_pre.add(n_)
                        del slots[g_]
                for cg in list(chains):
                    try:
                        next(cg)
                    except StopIteration:
                        chains.remove(cg)
            if DSTOP.startswith('pre'):
                P.barrier()
                P.end_scope()
                return
            if DSTOP == 'chain':
                P.barrier()
                P.end_scope()
                return
            zTh = ktail[:].rearrange("p n d f -> p (n d f)")
            PREK = [('pre', n) for n in range(NB)]
            P.dma('sp', [(zTh[:, 0:T], zT[hs, :])], lane='zld', w=PREK)
            for ti, (t0, W, lat) in enumerate(TILES):
                if last and not lat:
                    continue
                i2 = ti % 2
                P.op('act', lambda e: e.activation(out=sqo[i2][:, :W], in_=oacc[:, t0:t0 + W], func=AF.Square),
                     r=OK_, w=[('sqo', i2)])
                ps = P.psum()
                P.op('pe', lambda e: e.matmul(ps.t[:, :W], ones_bf[:], sqo[i2][:, :W], start=True, stop=True),
                     r=[('sqo', i2), CK], w=[ps.k])
                P.op('act', lambda e: e.activation(out=rno[:, :W], in_=ps.t[:, :W], func=AF.Sqrt, scale=1.0 / 128,
                                                   bias=epsT[:, 0:1]), r=[ps.k, CK], w=['rno'])
                P.op('dve', lambda e: e.reciprocal(out=rno[:, :W], in_=rno[:, :W]), r=['rno'], w=['rno'])
                P.op('dve', lambda e: e.scalar_tensor_tensor(
                    out=odf[:, :W], in0=oacc[:, t0:t0 + W], scalar=onorm[:, l:l + 1], in1=rno[:, :W],
                    op0=ALU.mult, op1=ALU.mult), r=OK_ + ['rno', 'par'], w=['odf'])
                P.op('dve', lambda e: e.tensor_tensor(out=odst[i2][:, :W], in0=odf[:, :W], in1=zTh[:, t0:t0 + W],
                                                      op=ALU.mult), r=['odf'] + PREK, w=[('odst', i2)])
                P.dma('pool', [(odT[hs, t0:t0 + W], odst[i2][:, :W])], lane='odst%d' % i2, r=[('odst', i2)], w=['odT'])
        P.barrier()
        P.end_scope()

    def phase_merge(l, xsrc):
        P.scope()
        last = (l == L - 1)
        wfc = P.sb("wfc", [128, 8, D], BF16)
        wdl = P.sb("wdl", [128, 8, D], BF16)
        wo = P.sb("wo", [128, 8, D], BF16)
        wr = P.sb("wr", [128, 8, 36], F32)
        P.scope()
        wfr = P.sb("wfr", [128, 4, D], BF16)
        P.dma('pool', [(wfr[:], w_fourier[l].rearrange("(g p) n -> p g n", p=128)),
                       (wdl[:], w_delta[l].rearrange("(kc p) n -> p kc n", p=128)),
                       (wo[:], w_out[l].rearrange("(kc p) n -> p kc n", p=128))], lane='mw', w=['mw'])
        P.dma('sp', [(wr[:], w_rt[l].rearrange("(kc p) n -> p kc n", p=128))], lane='wr', w=['wr'])
        for gi in range(4):
            for (j, mat) in ((0, chc), (1, chs)):
                for hf in range(2):
                    ps = P.psum()
                    P.op('pe', lambda e: e.matmul(ps.t[:, :512], mat[:], wfr[:, gi, hf * 512:(hf + 1) * 512],
                                                  start=True, stop=True), r=['mw', 'par'], w=[ps.k])
                    P.op('act', lambda e: e.copy(out=wfc[:, gi * 2 + j, hf * 512:(hf + 1) * 512], in_=ps.t[:, :512]),
                         r=[ps.k], w=['wfc'])
        P.barrier()
        P.end_scope()
        u_l = P.sb("u_l", [128, 32, 512], BF16)
        u_c = P.sb("u_c", [128, 2, 512], BF16)
        P.dma('sp', [(u_l[:], u_tok[:, 2:NB, :])], lane='ul', w=['u_l'])
        if not last:
            P.dma('sp', [(u_c[:], u_tok[:, 0:2, :])], lane='uc', w=['u_c'])
        NDB = 3
        dbuf = {'c': [P.sb("dcb%d" % i, [128, 4, 512], BF16) for i in range(NDB)],
                's': [P.sb("dsb%d" % i, [128, 4, 512], BF16) for i in range(NDB)]}
        dctr = 0
        PQ = P.sb("PQ", [128, 8, 512], BF16)
        odt = P.sb("odt", [128, 8, 512], BF16)
        gtt = P.sb("gtt", [128, 16, 512], BF16)
        mg = P.sb("mg", [128, 8, 512], BF16)
        m1 = P.sb("m1", [128, 512], F32)
        m2 = P.sb("m2", [128, 512], F32)
        xt = P.sb("xt", [128, 8, 512], F32)
        h2f = xt
        h2b = P.sb("h2b", [128, 8, 512], BF16)
        tl = {'sq': [P.sb("sq%d" % i, [128, 512], BF16) for i in range(2)],
              'tmp': [P.sb("tmp%d" % i, [128, 512], F32) for i in range(2)],
              'rstd': P.sb("rstd", [128, 512], F32)}
        lg = P.sb("lg", [128, 36], F32)
        rt = {k: P.sb("rt_" + k, [128, 32], F32) for k in ('lem', 'tmp')}
        rs = {k: P.sb("rs_" + k, [128, 4], F32) for k in ('mx', 'ohg', 'ex', 'pg', 'm1', 'm2', 'w1', 'w2', 'ohm')}
        rst = P.sb("rst", [128, 4, 66], F32)
        h2tk = P.sb("h2tk", [128, 4, D], BF16)
        xv = xsrc.rearrange("(kc p) t -> p kc t", p=128)
        xsv = xs.rearrange("(kc p) t -> p kc t", p=128)
        for ti, (t0, W, lat) in enumerate(TILES):
            if last and not lat:
                continue
            ws = 0 if lat else 1
            ntc = 32 if lat else 2
            uu = u_l if lat else u_c
            ukey = 'u_l' if lat else 'u_c'
            f0 = t0 - CTX if lat else 0
            mats = {'c': dftc if lat else dftc_c, 's': dfts if lat else dfts_c}
            sw = 4 if lat else 2
            nsub = ntc // sw
            P.dma('sp', [(odt[:, :, :W], odT.rearrange("(c p) t -> p c t", p=128)[:, :, t0:t0 + W]),
                         (gtt[:, :, :W], gT.rearrange("(c p) t -> p c t", p=128)[:, :, t0:t0 + W]),
                         (xt[:, :, :W], xv[:, :, t0:t0 + W])], lane='mt', w=['mt'])
            for gh in range(2):
                accs = [[P.psum(), P.psum()] for _ in range(2)]
                for sb_ in range(nsub):
                    bi = dctr % NDB
                    dctr += 1
                    for cs in ('c', 's'):
                        P.dma('sp', [(dbuf[cs][bi][:, :sw, :W],
                                      mats[cs][sb_ * sw * 128:(sb_ + 1) * sw * 128, f0:f0 + W].rearrange(
                                          "(b p) f -> p b f", p=128))],
                              lane='d%sb%d' % (cs, bi), w=[('d' + cs, bi)])
                    for t4 in range(sw):
                        tc = sb_ * sw + t4
                        for gl in range(2):
                            gi = gh * 2 + gl
                            for j, cs in enumerate(('c', 's')):
                                P.op('pe', lambda e: e.matmul(accs[gl][j].t[:, :W], uu[:, tc, gi * 128:(gi + 1) * 128],
                                                              dbuf[cs][bi][:, t4, :W], start=(tc == 0),
                                                              stop=(tc == ntc - 1)),
                                     r=[ukey, ('d' + cs, bi)], w=[accs[gl][j].k])
                for gl in range(2):
                    gi = gh * 2 + gl
                    for j in range(2):
                        P.op('act', lambda e: e.copy(out=PQ[:, gi * 2 + j, :W], in_=accs[gl][j].t[:, :W]),
                             r=[accs[gl][j].k], w=['PQ'])
            for dc in range(8):
                pa = P.psum()
                for k8 in range(8):
                    P.op('pe', lambda e: e.matmul(pa.t[:, :W], wfc[:, k8, dc * 128:(dc + 1) * 128], PQ[:, k8, :W],
                                                  start=(k8 == 0), stop=(k8 == 7)), r=['wfc', 'PQ'], w=[pa.k])
                pb = P.psum()
                for kc in range(8):
                    P.op('pe', lambda e: e.matmul(pb.t[:, :W], wdl[:, kc, dc * 128:(dc + 1) * 128], odt[:, kc, :W],
                                                  start=(kc == 0), stop=(kc == 7)), r=['mw', 'mt'], w=[pb.k])
                P.op('dve', lambda e: e.tensor_tensor(out=m1[:, :W], in0=pa.t[:, :W], in1=gtt[:, dc, :W], op=ALU.mult),
                     r=[pa.k, 'mt'], w=['m1'])
                P.op('dve', lambda e: e.tensor_tensor(out=m2[:, :W], in0=pb.t[:, :W], in1=gtt[:, 8 + dc, :W],
                                                      op=ALU.mult), r=[pb.k, 'mt'], w=['m2'])
                P.op('dve', lambda e: e.tensor_tensor(out=mg[:, dc, :W], in0=m1[:, :W], in1=m2[:, :W], op=ALU.add),
                     r=['m1', 'm2'], w=['mg'])
            for dc in range(8):
                po = P.psum()
                for kc in range(8):
                    P.op('pe', lambda e: e.matmul(po.t[:, :W], wo[:, kc, dc * 128:(dc + 1) * 128], mg[:, kc, :W],
                                                  start=(kc == 0), stop=(kc == 7)), r=['mw', 'mg'], w=[po.k])
                P.op('dve', lambda e: e.scalar_tensor_tensor(
                    out=xt[:, dc, :W], in0=po.t[:, :W], scalar=modv[:, l, 16 + dc, ws:ws + 1], in1=xt[:, dc, :W],
                    op0=ALU.mult, op1=ALU.add), r=[po.k, 'mod', 'mt'], w=['mt'])
            P.dma('pool', [(xsv[:, :, t0:t0 + W], xt[:, :, :W])], lane='xo', r=['mt'], w=['xs'])
            if os.environ.get('KTRACE') and ti < 2:
                print('merge tile', ti, 'pre-norm2', P.ninst)
            norm_mod(xt, 'mt', W, A2[:, l, :, ws], modv[:, l, 24:32, ws],
                     [(lambda kc: xt[:, kc, :W], 'mt'), (lambda kc: h2b[:, kc, :W], 'h2b')], tl)
            if os.environ.get('KTRACE') and ti < 2:
                print('merge tile', ti, 'pre-h2tk', P.ninst)
            nbk = W // 128
            blk0 = t0 // 128
            for tb in range(nbk):
                for hf in range(2):
                    pt = P.psum_t()
                    for j in range(4):
                        kc = hf * 4 + j
                        P.op('pe', lambda e: e.transpose(pt.t[:, j * 128:(j + 1) * 128],
                                                         h2b[:, kc, tb * 128:(tb + 1) * 128], ident_bf[:]),
                             r=['h2b', CK], w=[pt.k])
                    P.op('act', lambda e: e.copy(out=h2tk[:, tb, hf * 512:(hf + 1) * 512], in_=pt.t[:, 0:512]),
                         r=[pt.k], w=['h2tk'])
            P.dma('pool', [(h2tok_d[:, blk0:blk0 + nbk, :], h2tk[:, :nbk, :])], lane='h2o', r=['h2tk'], w=['h2tok'])
            if os.environ.get('KTRACE') and ti < 2:
                print('merge tile', ti, 'pre-routing', P.ninst)
            for tb in range(W // 128):
                ps = P.psum()
                for kc in range(8):
                    P.op('pe', lambda e: e.matmul(ps.t[:, :36], h2f[:, kc, tb * 128:(tb + 1) * 128], wr[:, kc, :],
                                                  start=(kc == 0), stop=(kc == 7)), r=['mt', 'wr'], w=[ps.k])
                RK = 'rt'

                def dv(fn, r=(), w=(RK,)):
                    P.op('dve', fn, r=list(r) + [RK], w=w)
                P.op('dve', lambda e: e.tensor_tensor(out=lg[:], in0=ps.t[:, :36], in1=brt[:, l, :], op=ALU.add),
                     r=[ps.k, 'par'], w=[RK])
                dv(lambda e: e.reduce_max(out=rs['mx'][:, 0:1], in_=lg[:, 0:4], axis=mybir.AxisListType.X))
                dv(lambda e: e.tensor_scalar(out=rs['ohg'][:], in0=lg[:, 0:4], scalar1=rs['mx'][:, 0:1], scalar2=None,
                                             op0=ALU.is_ge))
                dv(lambda e: e.tensor_scalar(out=rs['ex'][:], in0=lg[:, 0:4], scalar1=rs['mx'][:, 0:1], scalar2=None,
                                             op0=ALU.subtract))
                P.op('act', lambda e: e.activation(out=rs['ex'][:], in_=rs['ex'][:], func=AF.Exp), r=[RK], w=[RK])
                dv(lambda e: e.reduce_sum(out=rs['pg'][:, 0:1], in_=rs['ex'][:], axis=mybir.AxisListType.X))
                dv(lambda e: e.reciprocal(out=rs['pg'][:, 0:1], in_=rs['pg'][:, 0:1]))
                dv(lambda e: e.tensor_scalar(out=rs['ohm'][:], in0=rs['ohg'][:], scalar1=-1.0, scalar2=1.0e9,
                                             op0=ALU.add, op1=ALU.mult))
                dv(lambda e: e.tensor_tensor(out=rt['lem'][:].rearrange("p (g e) -> p g e", e=8),
                                             in0=lg[:, 4:36].rearrange("p (g e) -> p g e", e=8),
                                             in1=rs['ohm'][:].unsqueeze(2).to_broadcast([128, 4, 8]), op=ALU.add))
                dv(lambda e: e.reduce_max(out=rs['m1'][:, 0:1], in_=rt['lem'][:], axis=mybir.AxisListType.X))
                oh1 = rst[:, tb, 0:32]
                oh2 = rst[:, tb, 32:64]
                w1 = rst[:, tb, 64:65]
                w2 = rst[:, tb, 65:66]
                dv(lambda e: e.tensor_scalar(out=oh1, in0=rt['lem'][:], scalar1=rs['m1'][:, 0:1], scalar2=None,
                                             op0=ALU.is_ge), w=(RK, 'rst'))
                dv(lambda e: e.scalar_tensor_tensor(out=rt['tmp'][:], in0=oh1, scalar=-1.0e9, in1=rt['lem'][:],
                                                    op0=ALU.mult, op1=ALU.add), r=['rst'])
                dv(lambda e: e.reduce_max(out=rs['m2'][:, 0:1], in_=rt['tmp'][:], axis=mybir.AxisListType.X))
                dv(lambda e: e.tensor_scalar(out=oh2, in0=rt['tmp'][:], scalar1=rs['m2'][:, 0:1], scalar2=None,
                                             op0=ALU.is_ge), w=(RK, 'rst'))
                dv(lambda e: e.tensor_tensor(out=rs['w1'][:, 0:1], in0=rs['m1'][:, 0:1], in1=rs['m2'][:, 0:1],
                                             op=ALU.subtract))
                P.op('act', lambda e: e.activation(out=rs['w1'][:, 0:1], in_=rs['w1'][:, 0:1], func=AF.Sigmoid),
                     r=[RK], w=[RK])
                dv(lambda e: e.tensor_scalar(out=rs['w2'][:, 0:1], in0=rs['w1'][:, 0:1], scalar1=-1.0, scalar2=1.0,
                                             op0=ALU.mult, op1=ALU.add))
                dv(lambda e: e.tensor_tensor(out=w1, in0=rs['w1'][:, 0:1], in1=rs['pg'][:, 0:1], op=ALU.mult),
                   w=(RK, 'rst'))
                dv(lambda e: e.tensor_tensor(out=w2, in0=rs['w2'][:, 0:1], in1=rs['pg'][:, 0:1], op=ALU.mult),
                   w=(RK, 'rst'))
            P.dma('pool', [(rt_d[:, blk0:blk0 + nbk, :], rst[:, :nbk, :])], lane='wto', r=['rst'], w=['rt_d'])
        P.barrier()
        P.end_scope()

    def phase_moe(l):
        I32 = mybir.dt.int32
        last = (l == L - 1)
        b0 = 2 if last else 0
        nbl = NB - b0
        nblk = 48 if last else NBLK
        xsv = xs.rearrange("(kc p) t -> p kc t", p=128)
        wg_rows = w_gate.rearrange("l e k n -> (l e k) n")
        wu_rows = w_up.rearrange("l e k n -> (l e k) n")
        wd_rows = w_down.rearrange("l e k n -> (l e k) n")
        P.scope()
        RT = P.sb("RT", [128, nbl, 66], F32)
        P.dma('sp', [(RT[:], rt_d[:, b0:NB, :])], lane='hb', w=['RT'])
        IND = P.sb("IND", [128, nbl, 32], F32)
        RANK = P.sb("RANK", [128, nbl, 32], F32)
        carry = P.sb("carry", [128, 32], F32)
        SK = 'moeix'
        P.op('dve', lambda e: e.tensor_tensor(out=IND[:], in0=RT[:, :, 0:32], in1=RT[:, :, 32:64], op=ALU.add),
             r=['RT'], w=[SK])
        P.op('dve', lambda e: e.tensor_copy(out=carry[:], in_=zeros_f[:, 0:32]), r=[CK], w=['carry'])
        for c in range(nbl):
            ps = P.psum()
            P.op('pe', lambda e: e.matmul(ps.t[:, 0:32], strF[:], IND[:, c, :], start=True, stop=True),
                 r=[SK, CK], w=[ps.k])
            P.op('pe', lambda e: e.matmul(ps.t[:, 32:64], ones_f[:], IND[:, c, :], start=True, stop=True),
                 r=[SK, CK], w=[ps.k])
            P.op('dve', lambda e: e.tensor_tensor(out=RANK[:, c, :], in0=ps.t[:, 0:32], in1=carry[:], op=ALU.add),
                 r=[ps.k, 'carry'], w=['RANK'])
            P.op('dve', lambda e: e.tensor_tensor(out=carry[:], in0=ps.t[:, 32:64], in1=carry[:], op=ALU.add),
                 r=[ps.k, 'carry'], w=['carry'])
        cmp9 = P.sb("cmp9", [128, 32, 9], F32)
        thr = P.sb("thr", [128, 9], F32)
        nbe = P.sb("nbe", [128, 32], F32)
        cs = [P.sb("cs%d" % i, [128, 32], F32) for i in range(2)]
        P.op('dve', lambda e: e.tensor_scalar(out=thr[:], in0=iota[:, 0:9], scalar1=512.0, scalar2=None, op0=ALU.mult),
             r=['par'], w=['thr'])
        P.op('dve', lambda e: e.tensor_tensor(out=cmp9[:], in0=carry[:].unsqueeze(2).to_broadcast([128, 32, 9]),
                                              in1=thr[:].unsqueeze(1).to_broadcast([128, 32, 9]), op=ALU.is_gt),
             r=['carry', 'thr'], w=['cmp9'])
        P.op('dve', lambda e: e.reduce_sum(out=nbe[:], in_=cmp9[:], axis=mybir.AxisListType.X), r=['cmp9'], w=['nbe'])
        P.op('dve', lambda e: e.tensor_copy(out=cs[0][:], in_=nbe[:]), r=['nbe'], w=[('cs', 0)])
        ci = 0
        for sh in (1, 2, 4, 8, 16):
            P.op('dve', lambda e: e.tensor_copy(out=cs[1 - ci][:, 0:sh], in_=cs[ci][:, 0:sh]),
                 r=[('cs', ci)], w=[('cs', 1 - ci)])
            P.op('dve', lambda e: e.tensor_tensor(out=cs[1 - ci][:, sh:32], in0=cs[ci][:, sh:32], in1=cs[ci][:, 0:32 - sh],
                                                  op=ALU.add), r=[('cs', ci)], w=[('cs', 1 - ci)])
            ci = 1 - ci
        bend = cs[ci]
        sbase = P.sb("sbase", [128, 32], F32)
        P.op('dve', lambda e: e.tensor_tensor(out=sbase[:], in0=bend[:], in1=nbe[:], op=ALU.subtract),
             r=[('cs', ci), 'nbe'], w=['sbase'])
        P.op('dve', lambda e: e.tensor_scalar(out=sbase[:], in0=sbase[:], scalar1=512.0, scalar2=None, op0=ALU.mult),
             r=['sbase'], w=['sbase'])
        P.op('dve', lambda e: e.tensor_tensor(out=RANK[:], in0=RANK[:],
                                              in1=sbase[:].unsqueeze(1).to_broadcast([128, nbl, 32]), op=ALU.add),
             r=['RANK', 'sbase'], w=['RANK'])
        slf = P.sb("slf", [128, nbl, 2], F32)
        sli = P.sb("sli", [128, nbl, 2], I32)
        for k in range(2):
            P.op('dve', lambda e: e.tensor_tensor(out=IND[:], in0=RANK[:], in1=RT[:, :, k * 32:(k + 1) * 32],
                                                  op=ALU.mult), r=['RANK', 'RT', SK], w=[SK])
            P.op('dve', lambda e: e.reduce_sum(out=slf[:, :, k], in_=IND[:], axis=mybir.AxisListType.X),
                 r=[SK], w=['slf'])
        P.op('dve', lambda e: e.tensor_copy(out=sli[:], in_=slf[:]), r=['slf'], w=['sli'])
        cmpb = P.sb("cmpb", [128, NBLK, 32], F32)
        ebf = P.sb("ebf", [128, NBLK], F32)
        P.op('dve', lambda e: e.tensor_tensor(out=cmpb[:], in0=bend[:].unsqueeze(1).to_broadcast([128, NBLK, 32]),
                                              in1=iota[:, 0:NBLK].unsqueeze(2).to_broadcast([128, NBLK, 32]),
                                              op=ALU.is_le), r=[('cs', ci), 'par'], w=['cmpb'])
        P.op('dve', lambda e: e.reduce_sum(out=ebf[:], in_=cmpb[:], axis=mybir.AxisListType.X), r=['cmpb'], w=['ebf'])
        P.op('dve', lambda e: e.tensor_scalar(out=ebf[:], in0=ebf[:], scalar1=31.0, scalar2=float(l * NE),
                                              op0=ALU.min, op1=ALU.add), r=['ebf'], w=['ebf'])
        wixf = P.sb("wixf", [128, NBLK, 12], F32)
        wix = P.sb("wix", [128, NBLK, 12], I32)
        P.op('dve', lambda e: e.scalar_tensor_tensor(
            out=wixf[:, :, 0:8], in0=ebf[:].unsqueeze(2).to_broadcast([128, NBLK, 8]), scalar=1024.0,
            in1=wbase[:].unsqueeze(1).to_broadcast([128, NBLK, 8]), op0=ALU.mult, op1=ALU.add),
            r=['ebf', 'par'], w=['wixf'])
        P.op('dve', lambda e: e.scalar_tensor_tensor(
            out=wixf[:, :, 8:12], in0=ebf[:].unsqueeze(2).to_broadcast([128, NBLK, 4]), scalar=512.0,
            in1=wbase[:, 0:4].unsqueeze(1).to_broadcast([128, NBLK, 4]), op0=ALU.mult, op1=ALU.add),
            r=['ebf', 'par'], w=['wixf'])
        P.op('dve', lambda e: e.tensor_copy(out=wix[:], in_=wixf[:]), r=['wixf'], w=['wix'])

        if os.environ.get('KTRACE'):
            print('moe ix done', P.ninst)
        P.scope()
        hk = [P.sb("hk%d" % i, [128, 4, D], BF16) for i in range(2)]
        gi = 0
        for c0 in range(0, nbl, 4):
            nn = min(4, nbl - c0)
            b2 = gi % 2
            gi += 1
            P.dma('sp', [(hk[b2][:, :nn, :], h2tok_d[:, b0 + c0:b0 + c0 + nn, :])], lane='hk%d' % b2, w=[('hk', b2)])
            for j in range(nn):
                for k in range(2):
                    P.dma('pool', [lambda h: h.indirect_dma_start(
                        out=xs_d[:, :], out_offset=bass.IndirectOffsetOnAxis(ap=sli[:, c0 + j, k:k + 1], axis=0),
                        in_=hk[b2][:, j, :], in_offset=None)], lane='sc%d' % ((j * 2 + k) % 4),
                        r=[('hk', b2), 'sli'], w=['xs_d'])
        P.barrier()
        P.end_scope()

        if os.environ.get('KTRACE'):
            print('moe dispatch done', P.ninst)
        P.scope()
        wg = [P.sb("wg%d" % i, [128, 8, 512], BF16) for i in range(2)]
        wu = [P.sb("wu%d" % i, [128, 8, 512], BF16) for i in range(2)]
        wd = [P.sb("wd%d" % i, [128, 4, D], BF16) for i in range(2)]
        xsb = [P.sb("xsb%d" % i, [128, 4, D], BF16) for i in range(2)]
        xsT = P.sb("xsT", [128, 8, 512], BF16)
        sg = [P.sb("sg%d" % i, [128, 512], F32) for i in range(2)]
        h1 = P.sb("h1", [128, 4, 512], BF16)
        ysb = [P.sb("ysb%d" % i, [128, 4, D], BF16) for i in range(2)]
        for b in range(nblk):
            if os.environ.get('KTRACE') and b < 2:
                print('moe block', b, P.ninst)
            bi = b % 2
            WK = ('ew', bi)
            prs = []
            for kc in range(8):
                prs.append((lambda kc: lambda h: h.indirect_dma_start(
                    out=wg[bi][:, kc, :], out_offset=None, in_=wg_rows,
                    in_offset=bass.IndirectOffsetOnAxis(ap=wix[:, b, kc:kc + 1], axis=0)))(kc))
                prs.append((lambda kc: lambda h: h.indirect_dma_start(
                    out=wu[bi][:, kc, :], out_offset=None, in_=wu_rows,
                    in_offset=bass.IndirectOffsetOnAxis(ap=wix[:, b, kc:kc + 1], axis=0)))(kc))
            for fc in range(4):
                prs.append((lambda fc: lambda h: h.indirect_dma_start(
                    out=wd[bi][:, fc, :], out_offset=None, in_=wd_rows,
                    in_offset=bass.IndirectOffsetOnAxis(ap=wix[:, b, 8 + fc:9 + fc], axis=0)))(fc))
            P.dma('pool', prs, lane='ew%d' % bi, r=['wix'], w=[WK])
            if b == 0:
                P.dma('sp', [(xsb[0][:], xs_d[0:512, :].rearrange("(s p) f -> p s f", p=128))],
                      lane='xsb0', r=['xs_d'], w=[('xsb', 0)])
            if b + 1 < nblk:
                P.dma('sp', [(xsb[1 - bi][:],
                              xs_d[(b + 1) * 512:(b + 2) * 512, :].rearrange("(s p) f -> p s f", p=128))],
                      lane='xsb%d' % (1 - bi), r=['xs_d'], w=[('xsb', 1 - bi)])
            for kc in range(8):
                pt = P.psum_t()
                for sgi in range(4):
                    P.op('pe', lambda e: e.transpose(pt.t[:, sgi * 128:(sgi + 1) * 128],
                                                     xsb[bi][:, sgi, kc * 128:(kc + 1) * 128], ident_bf[:]),
                         r=[('xsb', bi), CK], w=[pt.k])
                if kc % 2 == 0:
                    P.op('act', lambda e: e.copy(out=xsT[:, kc, :], in_=pt.t[:, 0:512]), r=[pt.k], w=['xsT'])
                else:
                    P.op('dve', lambda e: e.tensor_copy(out=xsT[:, kc, :], in_=pt.t[:, 0:512]), r=[pt.k], w=['xsT'])
            for fc in range(4):
                pg = P.psum()
                pu = P.psum()
                for kc in range(8):
                    P.op('pe', lambda e: e.matmul(pg.t[:, :], wg[bi][:, kc, fc * 128:(fc + 1) * 128], xsT[:, kc, :],
                                                  start=(kc == 0), stop=(kc == 7)), r=[WK, 'xsT'], w=[pg.k])
                for kc in range(8):
                    P.op('pe', lambda e: e.matmul(pu.t[:, :], wu[bi][:, kc, fc * 128:(fc + 1) * 128], xsT[:, kc, :],
                                                  start=(kc == 0), stop=(kc == 7)), r=[WK, 'xsT'], w=[pu.k])
                f2 = fc % 2
                P.op('act', lambda e: e.activation(out=sg[f2][:], in_=pg.t[:, :], func=AF.Silu),
                     r=[pg.k], w=[('sg', f2)])
                P.op('dve', lambda e: e.tensor_tensor(out=h1[:, fc, :], in0=sg[f2][:], in1=pu.t[:, :], op=ALU.mult),
                     r=[('sg', f2), pu.k], w=['h1'])
            for sgi in range(4):
                for hf in range(2):
                    py = P.psum()
                    for fc in range(4):
                        P.op('pe', lambda e: e.matmul(py.t[:, :], h1[:, fc, sgi * 128:(sgi + 1) * 128],
                                                      wd[bi][:, fc, hf * 512:(hf + 1) * 512],
                                                      start=(fc == 0), stop=(fc == 3)), r=['h1', WK], w=[py.k])
                    if hf == 0:
                        P.op('act', lambda e: e.copy(out=ysb[bi][:, sgi, 0:512], in_=py.t[:, :]),
                             r=[py.k], w=[('ysb', bi)])
                    else:
                        P.op('dve', lambda e: e.tensor_copy(out=ysb[bi][:, sgi, 512:1024], in_=py.t[:, :]),
                             r=[py.k], w=[('ysb', bi)])
            P.dma('sp', [(ys_d[b * 512:(b + 1) * 512, :].rearrange("(s p) f -> p s f", p=128), ysb[bi][:])],
                  lane='ysb%d' % bi, r=[('ysb', bi)], w=['ys_d'])
        P.barrier()
        P.end_scope()

        if os.environ.get('KTRACE'):
            print('moe blocks done', P.ninst)
        P.scope()
        y1 = [P.sb("y1_%d" % i, [128, 4, D], BF16) for i in range(2)]
        y2 = [P.sb("y2_%d" % i, [128, 4, D], BF16) for i in range(2)]
        yc = P.sb("yc", [128, 4, D], BF16)
        ytmp = P.sb("ytmp", [128, D], F32)
        xt = P.sb("xt", [128, 8, 512], F32)
        tl = None
        if last:
            tl = {'sq': [P.sb("sq%d" % i, [128, 512], BF16) for i in range(2)],
                  'rstd': P.sb("rstd", [128, 512], F32)}
            ot = P.sb("ot", [128, 8, 512], F32)
        gi = 0
        for (t0, W, lat) in TILES:
            if last and not lat:
                continue
            ws = 0 if lat else 1
            nbk = W // 128
            c0 = t0 // 128 - b0
            g2 = gi % 2
            gi += 1
            P.dma('sp', [(xt[:, :, :W], xsv[:, :, t0:t0 + W])], lane='xm', r=['xs'], w=['xm'])
            prs = []
            for j in range(nbk):
                prs.append((lambda j: lambda h: h.indirect_dma_start(
                    out=y1[g2][:, j, :], out_offset=None, in_=ys_d[:, :],
                    in_offset=bass.IndirectOffsetOnAxis(ap=sli[:, c0 + j, 0:1], axis=0)))(j))
                prs.append((lambda j: lambda h: h.indirect_dma_start(
                    out=y2[g2][:, j, :], out_offset=None, in_=ys_d[:, :],
                    in_offset=bass.IndirectOffsetOnAxis(ap=sli[:, c0 + j, 1:2], axis=0)))(j))
            P.dma('pool', prs, lane='yg%d' % g2, r=['sli', 'ys_d'], w=[('yg', g2)])
            for j in range(nbk):
                P.op('dve', lambda e: e.tensor_scalar(out=ytmp[:], in0=y1[g2][:, j, :],
                                                      scalar1=RT[:, c0 + j, 64:65], scalar2=None, op0=ALU.mult),
                     r=[('yg', g2), 'RT'], w=['ytmp'])
                P.op('dve', lambda e: e.scalar_tensor_tensor(out=yc[:, j, :], in0=y2[g2][:, j, :],
                                                             scalar=RT[:, c0 + j, 65:66], in1=ytmp[:],
                                                             op0=ALU.mult, op1=ALU.add),
                     r=[('yg', g2), 'RT', 'ytmp'], w=['yc'])
            for kc in range(8):
                pt = P.psum_t()
                for j in range(nbk):
                    P.op('pe', lambda e: e.transpose(pt.t[:, j * 128:(j + 1) * 128], yc[:, j, kc * 128:(kc + 1) * 128],
                                                     ident_bf[:]), r=['yc', CK], w=[pt.k])
                P.op('dve', lambda e: e.scalar_tensor_tensor(
                    out=xt[:, kc, :W], in0=pt.t[:, :W], scalar=modv[:, l, 40 + kc, ws:ws + 1], in1=xt[:, kc, :W],
                    op0=ALU.mult, op1=ALU.add), r=[pt.k, 'mod', 'xm'], w=['xm'])
            if not last:
                P.dma('sp', [(xsv[:, :, t0:t0 + W], xt[:, :, :W])], lane='xmo', r=['xm'], w=['xs'])
            else:
                ss = P.psum()
                for kc in range(8):
                    sq = tl['sq'][kc % 2]
                    P.op('act', lambda e: e.activation(out=sq[:, :W], in_=xt[:, kc, :W], func=AF.Square),
                         r=['xm'], w=[('sq', kc % 2)])
                    P.op('pe', lambda e: e.matmul(ss.t[:, :W], ones_bf[:], sq[:, :W], start=(kc == 0),
                                                  stop=(kc == 7)), r=[('sq', kc % 2), CK], w=[ss.k])
                rstd = tl['rstd']
                P.op('act', lambda e: e.activation(out=rstd[:, :W], in_=ss.t[:, :W], func=AF.Sqrt, scale=1.0 / D,
                                                   bias=epsT[:, 0:1]), r=[ss.k, CK], w=['rstd'])
                P.op('dve', lambda e: e.reciprocal(out=rstd[:, :W], in_=rstd[:, :W]), r=['rstd'], w=['rstd'])
                for kc in range(8):
                    P.op('dve', lambda e: e.scalar_tensor_tensor(
                        out=ot[:, kc, :W], in0=xt[:, kc, :W], scalar=fnorm[:, kc:kc + 1], in1=rstd[:, :W],
                        op0=ALU.mult, op1=ALU.mult), r=['xm', 'rstd', 'par'], w=['ot'])
                P.dma('sp', [(outT.rearrange("(c p) t -> p c t", p=128)[:, :, t0 - CTX:t0 - CTX + W], ot[:, :, :W])],
                      lane='oo', r=['ot'], w=['outT'])
        P.barrier()
        P.end_scope()
        P.end_scope()

    with nc.named_scope('mod'):
        phase_mod()
    done = False
    for l in range(nl):
        xsrc = xT_in if l == 0 else xs
        for (nm, fn) in (('proj', lambda: phase_proj(l, xsrc)), ('delta', lambda: phase_delta(l)),
                         ('merge', lambda: phase_merge(l, xsrc)), ('moe', lambda: phase_moe(l))):
            if os.environ.get('KSCOPE') and nm == 'proj':
                fn()
            else:
                with nc.named_scope('%s%d' % (nm, l)):
                    fn()
            if stop == (l, nm):
                done = True
                break
        if done:
            break
    P.barrier()
    print("bass program: ninst=%d nwait=%d lanes=%d" % (P.ninst, P.nwait, len(P.lanes)))
    return nc


def _fm(v):
    return np.ascontiguousarray(np.asarray(v).reshape(-1, 128).T)


def prep_shared(inp):
    f32 = np.float32
    sh = {}
    b_mod = np.asarray(inp['b_mod'], f32)
    sh['bmod'] = np.ascontiguousarray(b_mod.reshape(L, 48, 128).transpose(2, 0, 1))
    sh['nmix'] = np.ascontiguousarray(np.asarray(inp['norm_mix'], f32).reshape(L, 8, 128).transpose(2, 0, 1))
    sh['nffn'] = np.ascontiguousarray(np.asarray(inp['norm_ffn'], f32).reshape(L, 8, 128).transpose(2, 0, 1))
    sh['fnorm'] = _fm(np.asarray(inp['final_norm'], f32))
    sh['convw'] = np.ascontiguousarray(np.asarray(inp['conv_w'], f32).reshape(L, 3, 24, 128).transpose(3, 0, 1, 2))
    sh['alog'] = np.ascontiguousarray(np.broadcast_to(np.asarray(inp['a_log'], f32).reshape(1, L, 16), (128, L, 16)))
    sh['dtb'] = np.ascontiguousarray(np.broadcast_to(np.asarray(inp['dt_bias'], f32).reshape(1, L, 16), (128, L, 16)))
    sh['onorm'] = np.ascontiguousarray(np.asarray(inp['out_norm'], f32).T)
    brt = np.concatenate([np.asarray(inp['b_route_group'], f32), np.asarray(inp['b_route_expert'], f32)], axis=1)
    sh['brt'] = np.ascontiguousarray(np.broadcast_to(brt.reshape(1, L, 36), (128, L, 36)))
    sh['w_rt'] = np.ascontiguousarray(np.concatenate([np.asarray(inp['w_route_group'], f32),
                                                      np.asarray(inp['w_route_expert'], f32)], axis=2))
    for k in ('w_mod', 'w_in', 'w_fourier', 'w_delta', 'w_out', 'w_gate', 'w_up', 'w_down'):
        sh[k] = np.ascontiguousarray(np.asarray(inp[k], f32))
    bf = ml_dtypes.bfloat16

    def tab(n, scale):
        k = np.arange(n, dtype=np.int64)
        ang = 2.0 * np.pi * ((k[:, None] * k[None, :]) % n).astype(np.float64) / n
        return (np.cos(ang) * scale).astype(bf), (np.sin(ang) * scale).astype(bf)
    sh['dftc'], sh['dfts'] = tab(SEQ, (SEQ * 128.0) ** -0.5)
    sh['dftc_c'], sh['dfts_c'] = tab(CTX, (CTX * 128.0) ** -0.5)
    c, s = tab(128, 1.0)
    sh['chc'] = c
    sh['chs'] = (-s.astype(np.float32)).astype(bf)
    idx = np.arange(128)
    def bd(b):
        return (idx[:, None] // b == idx[None, :] // b).astype(np.float32)
    bm = np.stack([bd(16), bd(32) - bd(16), bd(64) - bd(32), bd(128) - bd(64)], axis=1)
    sh['bmask'] = np.ascontiguousarray(bm).astype(bf)
    sh['iota'] = np.ascontiguousarray(np.broadcast_to(np.arange(64, dtype=np.float32)[None, :], (128, 64)))
    sh['wbase'] = np.ascontiguousarray((np.arange(8)[None, :] * 128 + np.arange(128)[:, None]).astype(np.float32))
    return sh


def prep_core(inp, b):
    f32 = np.float32
    x = np.asarray(inp['x'][b], f32)
    ctx = np.asarray(inp['ctx'][b], f32)
    xT = np.ascontiguousarray(np.concatenate([ctx, x], axis=0).T)
    cvec = np.stack([_fm(np.asarray(inp['c'][b], f32)), _fm(np.asarray(inp['c_ctx'], f32))], axis=2)
    return {'xT': xT, 'cvec': np.ascontiguousarray(cvec)}


_NC_CACHE = {}


def kernel(**inputs):
    if 'nc' not in _NC_CACHE:
        _NC_CACHE['nc'] = build()
    nc = _NC_CACHE['nc']
    sh = prep_shared(inputs)
    in_maps = []
    for b in range(8):
        m = dict(sh)
        m.update(prep_core(inputs, b))
        in_maps.append(m)
    res = run_bass_kernel_spmd(nc, in_maps, core_ids=list(range(8)))
    out = np.stack([np.ascontiguousarray(r["outT"].T) for r in res.results], axis=0)
    return out.astype(np.float32)
```
